# Optimizing a Trainium2 kernel written in Bass

```python
import jax
import jax.numpy as jnp
from jax import lax
import numpy as np

D_MODEL = 1024
BATCH = 8
SEQ = 4096
DEPTH = 2

GRID_W = 64
CTX_LEN = 256
BLOCK_Q = 128
ROPE_THETA = 10000.0
EPS = 1e-6
NEG_INF = -1e30

N_BRANCH = 4
BRANCH_W = D_MODEL // N_BRANCH
HEAD_DIM = 64

A_HEADS = BRANCH_W // HEAD_DIM
A_Q_RANK = D_MODEL // 4
A_KV_RANK = D_MODEL // 8
A_NOPE = 64
A_ROPE = 32
A_V = BRANCH_W // A_HEADS

B_HEADS = BRANCH_W // HEAD_DIM
B_KV_HEADS = 2
WINDOW = 128

C_HEADS = BRANCH_W // HEAD_DIM
NA_KH = 8
NA_KW = 16
NA_QROWS = BLOCK_Q // GRID_W

D_HEADS = BRANCH_W // HEAD_DIM
D_KV_HEADS = 2

A_COLS = A_Q_RANK + A_KV_RANK + A_ROPE
B_COLS = (B_HEADS + 2 * B_KV_HEADS) * HEAD_DIM
C_COLS = 3 * C_HEADS * HEAD_DIM
D_COLS = (D_HEADS + 2 * D_KV_HEADS) * HEAD_DIM
IN_COLS = A_COLS + B_COLS + C_COLS + D_COLS

N_GROUPS = 4
EXPERTS_PER_GROUP = 8
N_EXPERTS = N_GROUPS * EXPERTS_PER_GROUP
TOP_K_IN_GROUP = 2
EXPERT_FF = D_MODEL // 4
MOE_BLOCK = 128

kernel_name = 'hybrid_gated_mixers_hier_moe_dit'


def rmsnorm(x, g):
    xf = x.astype(jnp.float32)
    y = xf * lax.rsqrt(jnp.mean(xf * xf, axis=-1, keepdims=True) + EPS)
    return (y * g.astype(jnp.float32)).astype(x.dtype)


def modulate(x, g, shift, scale):
    return rmsnorm(x, g) * (1 + scale) + shift


def rope_tables(n_tok, rot_dim):
    t = jnp.arange(n_tok, dtype=jnp.int32)
    rows = (t // GRID_W).astype(jnp.float32)
    cols = (t % GRID_W).astype(jnp.float32)
    half = rot_dim // 2
    inv = ROPE_THETA ** (-jnp.arange(0, half, 2, dtype=jnp.float32) / half)
    ang = jnp.concatenate([rows[:, None] * inv, cols[:, None] * inv], axis=-1)
    return jnp.cos(ang), jnp.sin(ang)


def _rot_half(x, cos, sin):
    x1, x2 = jnp.split(x, 2, axis=-1)
    return jnp.concatenate([x1 * cos - x2 * sin, x2 * cos + x1 * sin], axis=-1)


def apply_axial_rope(x, cos, sin):
    xr, xc = jnp.split(x, 2, axis=-1)
    cr, cc = jnp.split(cos.astype(x.dtype), 2, axis=-1)
    sr, sc = jnp.split(sin.astype(x.dtype), 2, axis=-1)
    return jnp.concatenate([_rot_half(xr, cr[:, None], sr[:, None]),
                            _rot_half(xc, cc[:, None], sc[:, None])], axis=-1)


def attend(q, k, v, scale, bias=None, mask=None, sink=None):
    s = jnp.einsum('bqhgd,blhd->bhgql', q, k, preferred_element_type=jnp.float32) * scale
    if bias is not None:
        s = s + bias.astype(jnp.float32)
    if mask is not None:
        s = jnp.where(mask, s, NEG_INF)
    if sink is None:
        p = jax.nn.softmax(s, axis=-1)
    else:
        sk = sink.astype(jnp.float32)[None, :, :, None, None]
        m = jnp.maximum(jnp.max(s, axis=-1, keepdims=True), sk)
        e = jnp.exp(s - m)
        p = e / (jnp.sum(e, axis=-1, keepdims=True) + jnp.exp(sk - m))
    return jnp.einsum('bhgql,blhd->bqhgd', p.astype(v.dtype), v)


def sweep_blocks(fn, q):
    b, s = q.shape[:2]
    nb = s // BLOCK_Q
    qb = jnp.moveaxis(q.reshape((b, nb, BLOCK_Q) + q.shape[2:]), 1, 0)
    out = lax.map(lambda args: fn(args[0], args[1]), (qb, jnp.arange(nb, dtype=jnp.int32)))
    out = jnp.moveaxis(out, 0, 1)
    return out.reshape((b, s) + out.shape[3:])


def mla_project(pa, g_q_a, w_q_b, g_kv_a, w_kv_b, rope, with_q):
    b, s, _ = pa.shape
    cq, ckv, kr = jnp.split(pa, [A_Q_RANK, A_Q_RANK + A_KV_RANK], axis=-1)
    kv = (rmsnorm(ckv, g_kv_a) @ w_kv_b).reshape(b, s, A_HEADS, A_NOPE + A_V)
    k_nope, v = jnp.split(kv, [A_NOPE], axis=-1)
    kr = kr[:, :, None, :]
    if rope is not None:
        kr = apply_axial_rope(kr, *rope)
    k = jnp.concatenate([k_nope, jnp.broadcast_to(kr, (b, s, A_HEADS, A_ROPE))], axis=-1)
    q = None
    if with_q:
        q = (rmsnorm(cq, g_q_a) @ w_q_b).reshape(b, s, A_HEADS, A_NOPE + A_ROPE)
        if rope is not None:
            q = jnp.concatenate([q[..., :A_NOPE], apply_axial_rope(q[..., A_NOPE:], *rope)], axis=-1)
        q = q[:, :, :, None, :]
    return q, k, v


def branch_mla(pa, pa_c, rope, g_q_a, w_q_b, g_kv_a, w_kv_b, with_ctx):
    b, s, _ = pa.shape
    n_ctx = pa_c.shape[1]
    scale = (A_NOPE + A_ROPE) ** -0.5
    q, k, v = mla_project(pa, g_q_a, w_q_b, g_kv_a, w_kv_b, rope, True)
    qc, kc, vc = mla_project(pa_c, g_q_a, w_q_b, g_kv_a, w_kv_b, None, with_ctx)
    k_all = jnp.concatenate([k, kc], axis=1)
    v_all = jnp.concatenate([v, vc], axis=1)
    o = sweep_blocks(lambda qb, i: attend(qb, k_all, v_all, scale), q).reshape(b, s, BRANCH_W)
    oc = attend(qc, kc, vc, scale).reshape(b, n_ctx, BRANCH_W) if with_ctx else None
    return o, oc


def branch_window(pb, pb_c, rope, sink, with_ctx):
    b, s, _ = pb.shape
    n_ctx = pb_c.shape[1]
    grp = B_HEADS // B_KV_HEADS
    scale = HEAD_DIM ** -0.5
    cuts = [B_HEADS * HEAD_DIM, (B_HEADS + B_KV_HEADS) * HEAD_DIM]
    q, k, v = jnp.split(pb, cuts, axis=-1)
    q = apply_axial_rope(q.reshape(b, s, B_HEADS, HEAD_DIM), *rope).reshape(b, s, B_KV_HEADS, grp, HEAD_DIM)
    k = apply_axial_rope(k.reshape(b, s, B_KV_HEADS, HEAD_DIM), *rope)
    v = v.reshape(b, s, B_KV_HEADS, HEAD_DIM)
    qc, kc, vc = jnp.split(pb_c, cuts, axis=-1)
    kc = kc.reshape(b, n_ctx, B_KV_HEADS, HEAD_DIM)
    vc = vc.reshape(b, n_ctx, B_KV_HEADS, HEAD_DIM)
    sink_g = sink.reshape(B_KV_HEADS, grp)
    pad = ((0, 0), (WINDOW, WINDOW), (0, 0), (0, 0))
    kp = jnp.pad(k, pad)
    vp = jnp.pad(v, pad)
    band = BLOCK_Q + 2 * WINDOW
    qi = jnp.arange(BLOCK_Q, dtype=jnp.int32)[:, None]
    kj = jnp.arange(band, dtype=jnp.int32)[None, :]
    in_window = jnp.abs(qi + WINDOW - kj) <= WINDOW
    ctx_ok = jnp.ones((BLOCK_Q, n_ctx), dtype=bool)

    def block(qb, i):
        start = i * BLOCK_Q
        kb = lax.dynamic_slice_in_dim(kp, start, band, axis=1)
        vb = lax.dynamic_slice_in_dim(vp, start, band, axis=1)
        kpos = start - WINDOW + kj
        mask = jnp.concatenate([in_window & (kpos >= 0) & (kpos < s), ctx_ok], axis=-1)
        return attend(qb, jnp.concatenate([kb, kc], axis=1), jnp.concatenate([vb, vc], axis=1),
                      scale, mask=mask, sink=sink_g)

    o = sweep_blocks(block, q).reshape(b, s, BRANCH_W)
    oc = None
    if with_ctx:
        qc = qc.reshape(b, n_ctx, B_KV_HEADS, grp, HEAD_DIM)
        oc = attend(qc, kc, vc, scale, sink=sink_g).reshape(b, n_ctx, BRANCH_W)
    return o, oc


def branch_neighbourhood(pn, pn_c, rows, rpb, with_ctx):
    b, s, _ = pn.shape
    n_ctx = pn_c.shape[1]
    scale = HEAD_DIM ** -0.5
    q, k, v = jnp.split(pn, 3, axis=-1)
    q = q.reshape(b, s, C_HEADS, 1, HEAD_DIM)
    kg = k.reshape(b, rows, GRID_W, C_HEADS, HEAD_DIM)
    vg = v.reshape(b, rows, GRID_W, C_HEADS, HEAD_DIM)
    qc, kc, vc = jnp.split(pn_c, 3, axis=-1)
    kc = kc.reshape(b, n_ctx, C_HEADS, HEAD_DIM)
    vc = vc.reshape(b, n_ctx, C_HEADS, HEAD_DIM)
    kh = min(NA_KH, rows)
    nbr = min(kh + NA_QROWS - 1, rows)
    a = jnp.arange(BLOCK_Q, dtype=jnp.int32)
    j = jnp.arange(nbr * GRID_W, dtype=jnp.int32)
    q_col = a % GRID_W
    k_col = j % GRID_W
    c_start = jnp.clip(q_col - NA_KW // 2, 0, GRID_W - NA_KW)
    col_ok = (k_col[None] >= c_start[:, None]) & (k_col[None] < c_start[:, None] + NA_KW)
    dc = jnp.clip(k_col[None] - q_col[:, None], -(NA_KW - 1), NA_KW - 1) + NA_KW - 1
    ctx_ok = jnp.ones((BLOCK_Q, n_ctx), dtype=bool)
    ctx_bias = jnp.zeros((C_HEADS, BLOCK_Q, n_ctx), dtype=rpb.dtype)

    def block(qb, i):
        r0 = i * NA_QROWS
        s0 = jnp.clip(r0 - kh // 2, 0, rows - kh)
        band_start = jnp.clip(s0, 0, rows - nbr)
        kb = lax.dynamic_slice_in_dim(kg, band_start, nbr, axis=1).reshape(b, nbr * GRID_W, C_HEADS, HEAD_DIM)
        vb = lax.dynamic_slice_in_dim(vg, band_start, nbr, axis=1).reshape(b, nbr * GRID_W, C_HEADS, HEAD_DIM)
        q_row = r0 + a // GRID_W
        k_row = band_start + j // GRID_W
        r_start = jnp.clip(q_row - kh // 2, 0, rows - kh)
        row_ok = (k_row[None] >= r_start[:, None]) & (k_row[None] < r_start[:, None] + kh)
        dr = jnp.clip(k_row[None] - q_row[:, None], -(NA_KH - 1), NA_KH - 1) + NA_KH - 1
        bias = jnp.concatenate([rpb[:, dr, dc], ctx_bias], axis=-1)[None, :, None]
        mask = jnp.concatenate([row_ok & col_ok, ctx_ok], axis=-1)
        return attend(qb, jnp.concatenate([kb, kc], axis=1), jnp.concatenate([vb, vc], axis=1),
                      scale, bias=bias, mask=mask)

    o = sweep_blocks(block, q).reshape(b, s, BRANCH_W)
    oc = None
    if with_ctx:
        qc = qc.reshape(b, n_ctx, C_HEADS, 1, HEAD_DIM)
        oc = attend(qc, kc, vc, scale).reshape(b, n_ctx, BRANCH_W)
    return o, oc


def branch_qknorm(pd, pd_c, rope, g_qn, g_kn, with_ctx):
    b, s, _ = pd.shape
    n_ctx = pd_c.shape[1]
    grp = D_HEADS // D_KV_HEADS
    scale = HEAD_DIM ** -0.5
    cuts = [D_HEADS * HEAD_DIM, (D_HEADS + D_KV_HEADS) * HEAD_DIM]
    q, k, v = jnp.split(pd, cuts, axis=-1)
    q = apply_axial_rope(rmsnorm(q.reshape(b, s, D_HEADS, HEAD_DIM), g_qn), *rope)
    q = q.reshape(b, s, D_KV_HEADS, grp, HEAD_DIM)
    k = apply_axial_rope(rmsnorm(k.reshape(b, s, D_KV_HEADS, HEAD_DIM), g_kn), *rope)
    v = v.reshape(b, s, D_KV_HEADS, HEAD_DIM)
    qc, kc, vc = jnp.split(pd_c, cuts, axis=-1)
    kc = rmsnorm(kc.reshape(b, n_ctx, D_KV_HEADS, HEAD_DIM), g_kn)
    vc = vc.reshape(b, n_ctx, D_KV_HEADS, HEAD_DIM)
    k_all = jnp.concatenate([k, kc], axis=1)
    v_all = jnp.concatenate([v, vc], axis=1)
    o = sweep_blocks(lambda qb, i: attend(qb, k_all, v_all, scale), q).reshape(b, s, BRANCH_W)
    oc = None
    if with_ctx:
        qc = rmsnorm(qc.reshape(b, n_ctx, D_HEADS, HEAD_DIM), g_qn).reshape(b, n_ctx, D_KV_HEADS, grp, HEAD_DIM)
        oc = attend(qc, kc, vc, scale).reshape(b, n_ctx, BRANCH_W)
    return o, oc


def merge_branches(h, outs, w_gate, b_gate, w_branch, w_out):
    y = None
    for n in range(N_BRANCH):
        t = jax.nn.sigmoid(h @ w_gate[n] + b_gate[n]) * (outs[n] @ w_branch[n])
        y = t if y is None else y + t
    return y @ w_out


def token_mixer(h, hc, rows, rope_a, rope_h, w_in, g_q_a, w_q_b, g_kv_a, w_kv_b,
                sink, rpb, g_qn, g_kn, w_gate, b_gate, w_branch, w_out, with_ctx):
    cuts = [A_COLS, A_COLS + B_COLS, A_COLS + B_COLS + C_COLS]
    pa, pb, pn, pd = jnp.split(h @ w_in, cuts, axis=-1)
    pa_c, pb_c, pn_c, pd_c = jnp.split(hc @ w_in, cuts, axis=-1)
    oa, oa_c = branch_mla(pa, pa_c, rope_a, g_q_a, w_q_b, g_kv_a, w_kv_b, with_ctx)
    ob, ob_c = branch_window(pb, pb_c, rope_h, sink, with_ctx)
    on, on_c = branch_neighbourhood(pn, pn_c, rows, rpb, with_ctx)
    od, od_c = branch_qknorm(pd, pd_c, rope_h, g_qn, g_kn, with_ctx)
    y = merge_branches(h, (oa, ob, on, od), w_gate, b_gate, w_branch, w_out)
    yc = None
    if with_ctx:
        yc = merge_branches(hc, (oa_c, ob_c, on_c, od_c), w_gate, b_gate, w_branch, w_out)
    return y, yc


def hier_moe(h, w_group, b_group, w_router, b_router, w_ff1, w_ff3, w_ff2):
    shape = h.shape
    x = h.reshape(-1, shape[-1])
    n_tok = x.shape[0]
    g_logit = (x @ w_group + b_group).astype(jnp.float32)
    g_prob = jax.nn.softmax(g_logit, axis=-1)
    g_sel = jnp.argmax(g_logit, axis=-1).astype(jnp.int32)
    g_w = jnp.take_along_axis(g_prob, g_sel[:, None], axis=-1)
    e_logit = (x @ w_router + b_router).astype(jnp.float32).reshape(n_tok, N_GROUPS, EXPERTS_PER_GROUP)
    e_logit = jnp.take_along_axis(e_logit, g_sel[:, None, None], axis=1)[:, 0]
    top_v, top_i = lax.top_k(e_logit, TOP_K_IN_GROUP)
    wts = (g_w * jax.nn.softmax(top_v, axis=-1)).reshape(-1)
    eid = (g_sel[:, None] * EXPERTS_PER_GROUP + top_i).reshape(-1).astype(jnp.int32)
    tok = jnp.repeat(jnp.arange(n_tok, dtype=jnp.int32), TOP_K_IN_GROUP)
    n_assign = n_tok * TOP_K_IN_GROUP
    order = jnp.argsort(eid)
    e_s, tok_s, w_s = eid[order], tok[order], wts[order]
    counts = jnp.zeros((N_EXPERTS,), jnp.int32).at[eid].add(1)
    padded = (counts + MOE_BLOCK - 1) // MOE_BLOCK * MOE_BLOCK
    starts = jnp.cumsum(counts) - counts
    p_ends = jnp.cumsum(padded)
    p_starts = p_ends - padded
    dest = p_starts[e_s] + jnp.arange(n_assign, dtype=jnp.int32) - starts[e_s]
    n_blocks = (n_assign + MOE_BLOCK - 1) // MOE_BLOCK + N_EXPERTS
    n_slots = n_blocks * MOE_BLOCK
    slot_tok = jnp.zeros((n_slots,), jnp.int32).at[dest].set(tok_s)
    slot_w = jnp.zeros((n_slots,), jnp.float32).at[dest].set(w_s)
    blk_start = jnp.arange(n_blocks, dtype=jnp.int32) * MOE_BLOCK
    blk_e = jnp.minimum(jnp.searchsorted(p_ends, blk_start, side='right'), N_EXPERTS - 1)
    xs = x[slot_tok].reshape(n_blocks, MOE_BLOCK, shape[-1])

    def expert_block(args):
        xb, e = args
        return (jax.nn.silu(xb @ w_ff1[e]) * (xb @ w_ff3[e])) @ w_ff2[e]

    ys = lax.map(expert_block, (xs, blk_e)).reshape(n_slots, shape[-1])
    out = jnp.zeros_like(x).at[slot_tok].add(ys * slot_w[:, None].astype(ys.dtype))
    return out.reshape(shape)


def setup_inputs(seed: int = 0) -> dict:
    key = jax.random.key(seed)
    keys = iter(jax.random.split(key, 40))
    L, D = DEPTH, D_MODEL

    def normal(shape, std=1.0):
        return std * jax.random.normal(next(keys), shape, jnp.float32)

    def dense(shape, fan_in, gain=1.0):
        return normal(shape, gain * fan_in ** -0.5)

    def norm_gain(shape):
        return 1.0 + normal(shape, 0.01)

    return {
        'x': normal((BATCH, SEQ, D)),
        'c': normal((BATCH, D)),
        'ctx': normal((BATCH, CTX_LEN, D)),
        'c_ctx': normal((D,)),
        'w_mod': dense((L, D, 6 * D), D, 0.5),
        'b_mod': normal((L, 6 * D), 0.01),
        'g_norm_mix': norm_gain((L, D)),
        'w_in': dense((L, D, IN_COLS), D),
        'g_q_a': norm_gain((L, A_Q_RANK)),
        'w_q_b': dense((L, A_Q_RANK, A_HEADS * (A_NOPE + A_ROPE)), A_Q_RANK),
        'g_kv_a': norm_gain((L, A_KV_RANK)),
        'w_kv_b': dense((L, A_KV_RANK, A_HEADS * (A_NOPE + A_V)), A_KV_RANK),
        'sink_b': normal((L, B_HEADS), 0.5),
        'rpb_c': normal((L, C_HEADS, 2 * NA_KH - 1, 2 * NA_KW - 1), 0.1),
        'g_q_d': norm_gain((L, HEAD_DIM)),
        'g_k_d': norm_gain((L, HEAD_DIM)),
        'w_gate': dense((L, N_BRANCH, D, D), D),
        'b_gate': normal((L, N_BRANCH, D), 0.01),
        'w_branch': dense((L, N_BRANCH, BRANCH_W, D), BRANCH_W),
        'w_out': dense((L, D, D), D),
        'g_norm_ffn': norm_gain((L, D)),
        'w_group': dense((L, D, N_GROUPS), D),
        'b_group': normal((L, N_GROUPS), 0.01),
        'w_router': dense((L, D, N_EXPERTS), D),
        'b_router': normal((L, N_EXPERTS), 0.01),
        'w_ff1': dense((L, N_EXPERTS, D, EXPERT_FF), D),
        'w_ff3': dense((L, N_EXPERTS, D, EXPERT_FF), D),
        'w_ff2': dense((L, N_EXPERTS, EXPERT_FF, D), EXPERT_FF),
        'g_final': norm_gain((D,)),
    }


def reference(x, c, ctx, c_ctx, w_mod, b_mod, g_norm_mix, w_in, g_q_a, w_q_b, g_kv_a, w_kv_b,
              sink_b, rpb_c, g_q_d, g_k_d, w_gate, b_gate, w_branch, w_out, g_norm_ffn,
              w_group, b_group, w_router, b_router, w_ff1, w_ff3, w_ff2, g_final):
    s = x.shape[1]
    n_ctx = ctx.shape[1]
    rows = s // GRID_W
    rope_a = rope_tables(s, A_ROPE)
    rope_h = rope_tables(s, HEAD_DIM)
    silu_c = jax.nn.silu(c)
    silu_cc = jax.nn.silu(c_ctx)
    for l in range(DEPTH):
        with_ctx = l < DEPTH - 1
        mod = (silu_c @ w_mod[l] + b_mod[l])[:, None, :]
        mod_c = silu_cc @ w_mod[l] + b_mod[l]
        sh1, sc1, gt1, sh2, sc2, gt2 = jnp.split(mod, 6, axis=-1)
        csh1, csc1, cgt1, csh2, csc2, cgt2 = jnp.split(mod_c, 6, axis=-1)
        h = modulate(x, g_norm_mix[l], sh1, sc1)
        hc = modulate(ctx, g_norm_mix[l], csh1, csc1)
        y, yc = token_mixer(h, hc, rows, rope_a, rope_h, w_in[l], g_q_a[l], w_q_b[l], g_kv_a[l], w_kv_b[l],
                            sink_b[l], rpb_c[l], g_q_d[l], g_k_d[l], w_gate[l], b_gate[l], w_branch[l],
                            w_out[l], with_ctx)
        x = x + gt1 * y
        h2 = modulate(x, g_norm_ffn[l], sh2, sc2)
        if with_ctx:
            ctx = ctx + cgt1 * yc
            h2c = modulate(ctx, g_norm_ffn[l], csh2, csc2)
            f = hier_moe(jnp.concatenate([h2c, h2], axis=1), w_group[l], b_group[l], w_router[l],
                         b_router[l], w_ff1[l], w_ff3[l], w_ff2[l])
            ctx = ctx + cgt2 * f[:, :n_ctx]
            x = x + gt2 * f[:, n_ctx:]
        else:
            x = x + gt2 * hier_moe(h2, w_group[l], b_group[l], w_router[l], b_router[l],
                                   w_ff1[l], w_ff3[l], w_ff2[l])
    return rmsnorm(x, g_final)
```

```python
import contextlib
import numpy as np
import concourse.bass as bass
import concourse.mybir as mybir
from concourse.bass_utils import run_bass_kernel_spmd

F32 = mybir.dt.float32
BF16 = mybir.dt.bfloat16
AF = mybir.ActivationFunctionType
ALU = mybir.AluOpType
AX = mybir.AxisListType
I32 = mybir.dt.int32
NBLK_MAX = 100

SAME_ENGINE_SYNC = True
NDMA_SEMS = 10
import os
MAXOPS = int(os.environ.get('KMAXOPS', '100000000'))
D = 1024
EPS = 1e-6
NEG = -30000.0
IN_COLS = 2208
NE = 32


class Op:
    __slots__ = ("eng", "fn", "deps", "flag", "semv", "sem", "is_dma")

    def __init__(self, eng, fn, is_dma):
        self.eng = eng
        self.fn = fn
        self.deps = []
        self.flag = False
        self.semv = 0
        self.sem = None
        self.is_dma = is_dma


class Sched:
    ENGS = ("pe", "act", "dve", "pool", "sp")

    def __init__(self):
        self.ops = {e: [] for e in self.ENGS}
        self.last_w = {}
        self.readers = {}
        self.pending_barrier = {e: None for e in self.ENGS}
        self.out_ops = []

    def add(self, eng, fn, reads=(), writes=(), dma=False):
        op = Op(eng, fn, dma)
        self.nadd = getattr(self, "nadd", 0) + 1
        if self.nadd > MAXOPS:
            return op
        deps = {}
        raw = set()
        for k in reads:
            w = self.last_w.get(k)
            if w is not None:
                deps[id(w)] = w
                raw.add(id(w))
        for k in writes:
            w = self.last_w.get(k)
            if w is not None:
                deps[id(w)] = w
            for r in self.readers.get(k, ()):
                deps[id(r)] = r
        for k in reads:
            self.readers.setdefault(k, []).append(op)
        for k in writes:
            self.last_w[k] = op
            self.readers[k] = []
        pb = self.pending_barrier[eng]
        if pb is not None:
            for d in pb:
                deps[id(d)] = d
            self.pending_barrier[eng] = None
        for d in deps.values():
            if d is op:
                continue
            if (not d.is_dma) and d.eng == eng and (not dma) and (eng == "pe" or not SAME_ENGINE_SYNC):
                continue
            d.flag = True
            op.deps.append(d)
        self.ops[eng].append(op)
        return op

    def barrier(self):
        lst = []
        for e in self.ENGS:
            ops = self.ops[e]
            if not ops:
                continue
            nd = 0
            got_c = False
            for op in reversed(ops):
                if op.is_dma:
                    if nd < NDMA_SEMS:
                        lst.append(op)
                        nd += 1
                elif not got_c:
                    lst.append(op)
                    got_c = True
                if nd >= NDMA_SEMS and got_c:
                    break
        for e in self.ENGS:
            prev = self.pending_barrier[e]
            self.pending_barrier[e] = lst if prev is None else (prev + lst)
        self.last_w = {}
        self.readers = {}

    def emit(self, nc):
        final_ops = self.out_ops
        for o in final_ops:
            o.flag = True
        with contextlib.ExitStack() as es:
            csem = {}
            for e in ("pe", "act", "dve", "pool"):
                csem[e] = es.enter_context(nc.semaphore("s_" + e))
            dsem = {}
            for e in ("act", "pool", "sp"):
                dsem[e] = [es.enter_context(nc.semaphore("d_%s%d" % (e, i))) for i in range(NDMA_SEMS)]
            for e in self.ENGS:
                cnt = 0
                dcnt = 0
                dvals = [0] * NDMA_SEMS
                for op in self.ops[e]:
                    if op.is_dma:
                        slot = dcnt % NDMA_SEMS
                        dcnt += 1
                        dvals[slot] += 16
                        op.sem = dsem[e][slot]
                        op.semv = dvals[slot]
                    elif op.flag:
                        cnt += 1
                        op.sem = csem[e]
                        op.semv = cnt
            block = es.enter_context(nc.Block())

            def run_engine(e, eng):
                waited = {}
                prev_dma = [None] * NDMA_SEMS
                dcnt = 0
                for op in self.ops[e]:
                    for d in op.deps:
                        key = id(d.sem)
                        if waited.get(key, 0) >= d.semv:
                            continue
                        eng.wait_ge(d.sem, d.semv)
                        waited[key] = d.semv
                    if op.is_dma:
                        slot = dcnt % NDMA_SEMS
                        dcnt += 1
                        p = prev_dma[slot]
                        if p is not None:
                            key = id(p.sem)
                            if waited.get(key, 0) < p.semv:
                                eng.wait_ge(p.sem, p.semv)
                                waited[key] = p.semv
                        prev_dma[slot] = op
                        ins = op.fn(eng)
                        ins.then_inc(op.sem, 16)
                    else:
                        ins = op.fn(eng)
                        if op.flag:
                            ins.then_inc(op.sem, 1)
                if e == "sp":
                    for o in final_ops:
                        key = id(o.sem)
                        if waited.get(key, 0) < o.semv:
                            eng.wait_ge(o.sem, o.semv)
                            waited[key] = o.semv

            @block.tensor
            def _(eng):
                run_engine("pe", eng)

            @block.scalar
            def _(eng):
                run_engine("act", eng)

            @block.vector
            def _(eng):
                run_engine("dve", eng)

            @block.gpsimd
            def _(eng):
                run_engine("pool", eng)

            @block.sync
            def _(eng):
                run_engine("sp", eng)


class Arena:
    def __init__(self, t, nbytes):
        self.t = t
        self.cap = nbytes
        self.off = 0
        self.peak = 0

    def alloc(self, free_shape, dt):
        n = 1
        for s in free_shape:
            n *= s
        esz = 2 if dt == BF16 else 4
        off = (self.off + 63) // 64 * 64
        nb = n * esz
        assert off + nb <= self.cap, ("arena overflow", off + nb, self.cap)
        self.off = off + nb
        self.peak = max(self.peak, self.off)
        v = self.t[:, off // 2:(off + nb) // 2]
        if dt != BF16:
            v = v.bitcast(dt)
        if len(free_shape) > 1:
            names = ["a%d" % i for i in range(len(free_shape))]
            kw = {names[i]: free_shape[i] for i in range(len(free_shape))}
            v = v.rearrange("p (" + " ".join(names) + ") -> p " + " ".join(names), **kw)
        return v

    def mark(self):
        return self.off

    def release(self, m):
        self.off = m


class Cfg:
    def __init__(self, SEQ=4096, CTX=256, DEPTH=2):
        self.SEQ = SEQ
        self.CTX = CTX
        self.DEPTH = DEPTH
        self.T = SEQ + CTX
        self.NT = self.T // 128
        self.NTL = SEQ // 128
        self.NTC = CTX // 128
        self.ROWS = SEQ // 64
        self.NG = SEQ // 512


def rope_tab(cfg):
    t = np.arange(cfg.SEQ)
    rows = (t // 64).astype(np.float32)
    cols = (t % 64).astype(np.float32)
    out = np.zeros((cfg.T, 192), np.float32)
    out[cfg.SEQ:, 0:64] = 1.0
    out[cfg.SEQ:, 128:160] = 1.0
    off = 0
    for R in (64, 32):
        half = R // 2
        inv = (np.float32(10000.0) ** (-np.arange(0, half, 2, dtype=np.float32) / np.float32(half))).astype(np.float32)
        ar = rows[:, None] * inv
        ac = cols[:, None] * inv
        cr, sr, cc, sc = np.cos(ar), np.sin(ar), np.cos(ac), np.sin(ac)
        C = np.concatenate([cr, cr, cc, cc], axis=1)
        S_ = np.concatenate([-sr, sr, -sc, sc], axis=1)
        out[:cfg.SEQ, off:off + R] = C
        out[:cfg.SEQ, off + R:off + 2 * R] = S_
        off += 2 * R
    return out.astype(np.float32)


def mask_b():
    k = np.arange(128)[:, None]
    q = np.arange(512)[None, :]
    out = np.zeros((6, 128, 512), np.float32)
    for r in range(6):
        rel = r - 1
        ok = np.abs(q - 128 * rel - k) <= 128
        out[r] = np.where(ok, 0.0, NEG)
    return out


def c_tables(cfg):
    rows = cfg.ROWS
    kh = min(8, rows)
    k = np.arange(128)[:, None]
    q = np.arange(512)[None, :]
    k_col = k % 64
    q_col = q % 64
    c_start = np.clip(q_col - 8, 0, 64 - 16)
    col_ok = (k_col >= c_start) & (k_col < c_start + 16)
    DC = (np.clip(k_col - q_col, -15, 15) + 15).astype(np.int64) + np.zeros((128, 512), np.int64)
    DR = np.zeros((8, 128, 512), np.int64)
    for r in range(8):
        rel = r - 2
        d = 2 * rel + k // 64 - q // 64
        DR[r] = np.clip(d, -7, 7) + 7
    masks = []
    case_of_g = []
    valid = []
    for g in range(cfg.NG):
        m = np.full((8, 128, 512), NEG, np.float32)
        v = []
        for r in range(8):
            kt = 4 * g + r - 2
            if kt < 0 or kt >= cfg.NTL:
                continue
            k_row = 2 * kt + k // 64
            q_row = 8 * g + q // 64
            r_start = np.clip(q_row - kh // 2, 0, rows - kh)
            row_ok = (k_row >= r_start) & (k_row < r_start + kh)
            ok = row_ok & col_ok
            if ok.any():
                v.append(r)
            m[r] = np.where(ok, 0.0, NEG)
        found = None
        for ci, mm in enumerate(masks):
            if np.array_equal(mm, m):
                found = ci
                break
        if found is None:
            masks.append(m)
            found = len(masks) - 1
        case_of_g.append(found)
        valid.append(v)
    return np.stack(masks).astype(np.float32), case_of_g, valid, DR, DC


class Prog:
    def __init__(self, cfg):
        self.cfg = cfg
        self.S = Sched()
        self.nc = bass.Bass("TRN2", target_bir_lowering=False)
        self.maskc_np, self.case_of_g, self.valid_c, self.DR, self.DC = c_tables(cfg)
        self.ncase = self.maskc_np.shape[0]

    def MM(self, out, lhsT, rhs, start, stop, r, w):
        return self.S.add("pe", lambda e: e.matmul(out, lhsT=lhsT, rhs=rhs, start=start, stop=stop), r, w)

    def TR(self, out, in_, ident, r, w):
        return self.S.add("pe", lambda e: e.transpose(out=out, in_=in_, identity=ident), r, w)

    def ACT(self, out, in_, func, r, w, scale=1.0, bias=0.0, accum=None):
        if accum is None:
            return self.S.add("act", lambda e: e.activation(out=out, in_=in_, func=func, bias=bias, scale=scale), r, w)
        return self.S.add("act", lambda e: e.activation(out=out, in_=in_, func=func, bias=bias, scale=scale,
                                                        accum_out=accum), r, w)

    def TT(self, eng, out, in0, in1, op, r, w):
        return self.S.add(eng, lambda e: e.tensor_tensor(out=out, in0=in0, in1=in1, op=op), r, w)

    def TS(self, eng, out, in0, s1, s2, op0, op1, r, w):
        if s2 is None:
            return self.S.add(eng, lambda e: e.tensor_scalar(out=out, in0=in0, scalar1=s1, scalar2=None, op0=op0), r, w)
        return self.S.add(eng, lambda e: e.tensor_scalar(out=out, in0=in0, scalar1=s1, scalar2=s2, op0=op0, op1=op1), r, w)

    def STT(self, eng, out, in0, scalar, in1, op0, op1, r, w):
        return self.S.add(eng, lambda e: e.scalar_tensor_tensor(out=out, in0=in0, scalar=scalar, in1=in1,
                                                                 op0=op0, op1=op1), r, w)

    def CP(self, eng, out, in_, r, w):
        if eng == "act":
            return self.S.add("act", lambda e: e.copy(out=out, in_=in_), r, w)
        return self.S.add(eng, lambda e: e.tensor_copy(out=out, in_=in_), r, w)

    def RED(self, out, in_, op, r, w):
        return self.S.add("dve", lambda e: e.tensor_reduce(out=out, in_=in_, axis=AX.X, op=op), r, w)

    def RCP(self, out, in_, r, w):
        return self.S.add("dve", lambda e: e.reciprocal(out=out, in_=in_), r, w)

    def DMA(self, q, out, in_, r, w):
        return self.S.add(q, lambda e: e.dma_start(out=out, in_=in_), r, w, dma=True)

    def MEMSET(self, eng, ap, val, w):
        return self.S.add(eng, lambda e: e.memset(ap, val), (), w)

    def build(self):
        cfg = self.cfg
        nc = self.nc
        L = cfg.DEPTH
        T, NT, NTL = cfg.T, cfg.NT, cfg.NTL

        def din(name, shape):
            return nc.dram_tensor(name, list(shape), F32, kind="ExternalInput").ap()

        self.I = I = {}
        I["x"] = din("x", [cfg.SEQ, D])
        I["c"] = din("c", [1, D])
        I["ctx"] = din("ctx", [cfg.CTX, D])
        I["c_ctx"] = din("c_ctx", [1, D])
        I["w_mod"] = din("w_mod", [L, D, 6 * D])
        I["b_mod"] = din("b_mod", [L, 6 * D])
        I["g_norm_mix"] = din("g_norm_mix", [L, D])
        I["w_in"] = din("w_in", [L, D, IN_COLS])
        I["g_q_a"] = din("g_q_a", [L, 256])
        I["w_q_b"] = din("w_q_b", [L, 256, 384])
        I["g_kv_a"] = din("g_kv_a", [L, 128])
        I["w_kv_b"] = din("w_kv_b", [L, 128, 512])
        I["sink_b"] = din("sink_b", [L, 4])
        I["biasg"] = din("biasg", [L, 4, 8, 128, 512])
        I["g_q_d"] = din("g_q_d", [L, 64])
        I["g_k_d"] = din("g_k_d", [L, 64])
        I["w_gate"] = din("w_gate", [L, 4, D, D])
        I["b_gate"] = din("b_gate", [L, 4 * D])
        I["w_branch"] = din("w_branch", [L, 4, 256, D])
        I["w_out"] = din("w_out", [L, D, D])
        I["g_norm_ffn"] = din("g_norm_ffn", [L, D])
        I["w_group"] = din("w_group", [L, D, 4])
        I["b_group"] = din("b_group", [L, 4])
        I["w_router"] = din("w_router", [L, D, 32])
        I["b_router"] = din("b_router", [L, 32])
        I["w13r"] = din("w13r", [L, NE * 128, 4096])
        I["w2r"] = din("w2r", [L, NE * 128, 2048])
        I["iota32"] = din("iota32", [1, 32])
        I["ltri"] = din("ltri", [128, 128])
        I["pidx"] = din("pidx", [128, 1])
        I["thr"] = din("thr", [1, NBLK_MAX])
        I["g_final"] = din("g_final", [1, D])
        I["ident"] = din("ident", [128, 128])
        I["ropetab"] = din("ropetab", [T, 192])
        I["maskb"] = din("maskb", [6, 128, 512])
        I["maskc"] = din("maskc", [self.ncase, 8, 128, 512])
        self.out = nc.dram_tensor("out", [cfg.SEQ, D], F32, kind="ExternalOutput").ap()

        def dscr(name, shape, dt):
            return nc.dram_tensor(name, list(shape), dt, kind="Internal").ap()

        self.xcur = dscr("xcur", [T, D], F32)
        self.QT = [dscr("QT_A", [96, 4, T], BF16)] + [dscr("QT_%d" % n, [128, 2, T], BF16) for n in (1, 2, 3)]
        self.KT = [dscr("KT_A", [96, 4, T], BF16)] + [dscr("KT_%d" % n, [128, 2, T], BF16) for n in (1, 2, 3)]
        self.VA = [dscr("VA_%d" % n, [128, NT, 4, 128], BF16) for n in range(4)]
        self.OT = dscr("OT", [8, 128, T], BF16)
        self.H2 = dscr("H2", [T, D], BF16)
        self.XS = dscr("XS", [NBLK_MAX * 128, D], BF16)
        self.YS = dscr("YS", [NBLK_MAX * 128, D], F32)

        with contextlib.ExitStack() as es:
            ARENA_BYTES = 207 * 1024
            at = es.enter_context(nc.sbuf_tensor("arena", [128, ARENA_BYTES // 2], BF16))
            self.A = Arena(at, ARENA_BYTES)
            self.banks = [es.enter_context(nc.psum_tensor("pb%d" % i, [128, 1024], F32)) for i in range(4)]
            self.setup()
            for l in range(L):
                self.layer(l)
            self.S.emit(nc)
        return nc

    def bank(self, i):
        return self.banks[i // 2][:, (i % 2) * 512:(i % 2 + 1) * 512]

    def bank_bf(self, i):
        return self.bank(i).bitcast(BF16)

    def setup(self):
        A, I = self.A, self.I
        self.ident_f = A.alloc([128], F32)
        self.ident_b = A.alloc([128], BF16)
        self.ones_f = A.alloc([128], F32)
        self.ones_b = A.alloc([128], BF16)
        self.DMA("sp", self.ident_f, I["ident"], (), ["ident_f"])
        self.DMA("pool", self.ident_b, I["ident"], (), ["ident_b"])
        self.MEMSET("dve", self.ones_f, 1.0, ["ones_f"])
        self.MEMSET("dve", self.ones_b, 1.0, ["ones_b"])
        self.mod = [A.alloc([6 * D], F32), A.alloc([6 * D], F32)]
        self.rt = A.alloc([self.cfg.NT, 8], F32)
        self.base = A.alloc([32], F32)
        self.iota = A.alloc([32], F32)
        self.ltb = A.alloc([128], BF16)
        self.onesbb = A.alloc([128], BF16)
        self.pidx = A.alloc([1], F32)
        self.thr = A.alloc([NBLK_MAX], F32)
        self.slot_i = A.alloc([self.cfg.NT, 2], I32)
        self.widx = A.alloc([NBLK_MAX], I32)
        self.DMA("sp", self.iota, I["iota32"].partition_broadcast(128), (), ["iota"])
        self.DMA("pool", self.ltb, I["ltri"], (), ["ltb"])
        self.MEMSET("dve", self.onesbb, 1.0, ["onesbb"])
        self.DMA("sp", self.pidx, I["pidx"], (), ["pidx"])
        self.DMA("sp", self.thr, I["thr"].partition_broadcast(128), (), ["thr"])
        self.pers_mark = A.mark()

    def layer(self, l):
        import os
        cfg = self.cfg
        stop = int(os.environ.get("KSTOP", "99"))
        self.with_ctx = l < cfg.DEPTH - 1
        phases = [self.phase_mod, self.phase_p1, self.phase_p2, self.phase_p3, self.phase_p4]
        for pi, ph in enumerate(phases):
            if l * 5 + pi > stop:
                return
            self.S.barrier()
            self.A.release(self.pers_mark)
            ph(l)

    def phase_mod(self, l):
        A, I = self.A, self.I
        cb = [A.alloc([D], F32), A.alloc([D], F32)]
        lh = [A.alloc([8, 128], F32), A.alloc([8, 128], F32)]
        bmod = A.alloc([6 * D], F32)
        gm = A.alloc([D], F32)
        gf = A.alloc([D], F32)
        wm = [A.alloc([8, 512], F32), A.alloc([8, 512], F32)]
        self.DMA("sp", cb[0], I["c"].partition_broadcast(128), (), ["cb0"])
        self.DMA("sp", cb[1], I["c_ctx"].partition_broadcast(128), (), ["cb1"])
        self.DMA("sp", bmod[0:1, :], I["b_mod"][l:l + 1, :], (), ["bmod"])
        self.DMA("sp", gm, I["g_norm_mix"][l:l + 1, :].partition_broadcast(128), (), ["gm"])
        self.DMA("sp", gf, I["g_norm_ffn"][l:l + 1, :].partition_broadcast(128), (), ["gf"])
        for w in range(2):
            self.ACT(cb[w], cb[w], AF.Silu, ["cb%d" % w], ["cb%d" % w])
            for k in range(8):
                bk = self.banks[w][:, k * 128:(k + 1) * 128]
                self.TR(bk, cb[w][:, k * 128:(k + 1) * 128], self.ident_f, ["cb%d" % w, "ident_f"], ["pbk%d" % w])
            self.CP("act", lh[w], self.banks[w][:, :].rearrange("p (k m) -> p k m", k=8), ["pbk%d" % w], ["lh%d" % w])
        for j in range(12):
            wmj = wm[j % 2]
            wk = "wm%d" % (j % 2)
            self.DMA("sp", wmj, I["w_mod"][l, :, j * 512:(j + 1) * 512].rearrange("(k p) n -> p k n", p=128), (), [wk])
            for w in range(2):
                bi = 4 + (j * 2 + w) % 4
                bk = self.bank(bi)
                bkk = "bank%d" % bi
                for k in range(8):
                    self.MM(bk, lh[w][:, k, :], wmj[:, k, :], k == 0, False, ["lh%d" % w, wk], [bkk])
                self.MM(bk, self.ones_f[0:1, :], bmod[0:1, j * 512:(j + 1) * 512], False, True, ["ones_f", "bmod"], [bkk])
                self.CP("dve" if w == 0 else "act", self.mod[w][:, j * 512:(j + 1) * 512], bk, [bkk], ["mod%d" % w])
        for w in range(2):
            m = self.mod[w]
            self.STT("dve", m[:, D:2 * D], m[:, D:2 * D], 1.0, gm, ALU.add, ALU.mult, ["mod%d" % w, "gm"], ["mod%d" % w])
            self.STT("dve", m[:, 4 * D:5 * D], m[:, 4 * D:5 * D], 1.0, gf, ALU.add, ALU.mult, ["mod%d" % w, "gf"], ["mod%d" % w])

    def x_src(self, l, t):
        cfg = self.cfg
        if l == 0:
            if t < cfg.NTL:
                return self.I["x"][t * 128:(t + 1) * 128, :]
            return self.I["ctx"][(t - cfg.NTL) * 128:(t - cfg.NTL + 1) * 128, :]
        return self.xcur[t * 128:(t + 1) * 128, :]

    def rstd_of(self, src, n, junk, ssq, rstd, rk, tag, jk="junk"):
        self.ACT(junk, src, AF.Square, rk, [jk, "ssq" + tag], accum=ssq)
        self.ACT(ssq, ssq, AF.Sqrt, ["ssq" + tag], ["ssq" + tag], scale=1.0 / n, bias=EPS)
        self.RCP(rstd, ssq, ["ssq" + tag], ["rstd" + tag])

    def norm_mod(self, xt, xk, w, goff, soff, junk, tmp, out, outk, ssq, rstd, tag, jk="junk", tk="tmp"):
        self.rstd_of(xt, D, junk, ssq, rstd, [xk], tag, jk=jk)
        m = self.mod[w]
        self.STT("dve", tmp, xt, rstd, m[:, goff:goff + D], ALU.mult, ALU.mult, [xk, "rstd" + tag, "mod%d" % w], [tk])
        self.TT("dve", out, tmp, m[:, soff:soff + D], ALU.add, [tk, "mod%d" % w], [outk])

    def transpose8(self, src, srck, bank_bf, bankk, dst, dstk, nblk=8, ident=None, identk="ident_b"):
        ident = self.ident_b if ident is None else ident
        srcks = [srck] if isinstance(srck, str) else list(srck)
        for k in range(nblk):
            self.TR(bank_bf[:, k * 128:(k + 1) * 128], src[:, k * 128:(k + 1) * 128], ident, srcks + [identk], [bankk])
        self.CP("act", dst, bank_bf[:, 0:nblk * 128].rearrange("p (k m) -> p k m", k=nblk), [bankk], [dstk])

    def rope(self, src, srck, H, R, tab, tabk, coff, soff, t1, t2, outs, tag):
        w = R // 4
        C = tab[:, coff:coff + R]
        Sg = tab[:, soff:soff + R].rearrange("p (a b w) -> p a b w", a=2, b=2)
        t1v = t1[:, 0:H * R].rearrange("p (h r) -> p h r", h=H)
        t2v = t2[:, 0:H * R].rearrange("p (h r) -> p h r", h=H)
        self.TT("dve", t1v, src, C.unsqueeze(1).to_broadcast([128, H, R]), ALU.mult, [srck, tabk], ["t1" + tag])
        s5 = src.rearrange("p h (a b w) -> p h a b w", a=2, b=2)
        t5 = t2v.rearrange("p h (a b w) -> p h a b w", a=2, b=2)
        self.TT("dve", t5[:, :, :, 0, :], s5[:, :, :, 1, :], Sg[:, :, 0, :].unsqueeze(1).to_broadcast([128, H, 2, w]),
                ALU.mult, [srck, tabk], ["t2a" + tag])
        self.TT("dve", t5[:, :, :, 1, :], s5[:, :, :, 0, :], Sg[:, :, 1, :].unsqueeze(1).to_broadcast([128, H, 2, w]),
                ALU.mult, [srck, tabk], ["t2b" + tag])
        for (dst, h0, h1, dk) in outs:
            self.TT("dve", dst, t1v[:, h0:h1, :], t2v[:, h0:h1, :], ALU.add, ["t1" + tag, "t2a" + tag, "t2b" + tag], [dk])

    def phase_p1(self, l):
        cfg, A, I = self.cfg, self.A, self.I
        NT, NTL = cfg.NT, cfg.NTL
        win = A.alloc([8, IN_COLS], BF16)
        for k in range(8):
            self.DMA("pool", win[:, k, :], I["w_in"][l, k * 128:(k + 1) * 128, :], (), ["win"])
        wqb = A.alloc([2, 384], BF16)
        self.DMA("pool", wqb, I["w_q_b"][l].rearrange("(k p) n -> p k n", p=128), (), ["wqb"])
        wkvb = A.alloc([512], BF16)
        self.DMA("pool", wkvb, I["w_kv_b"][l], (), ["wkvb"])
        gqa = A.alloc([256], F32)
        gkv = A.alloc([128], F32)
        gd = A.alloc([6, 64], F32)
        self.DMA("sp", gqa, I["g_q_a"][l:l + 1, :].partition_broadcast(128), (), ["gqa"])
        self.DMA("sp", gkv, I["g_kv_a"][l:l + 1, :].partition_broadcast(128), (), ["gkv"])
        for h in range(6):
            src = I["g_q_d"] if h < 4 else I["g_k_d"]
            self.DMA("sp", gd[:, h, :], src[l:l + 1, :].partition_broadcast(128), (), ["gd"])
        xt2 = [A.alloc([D], F32) for _ in range(2)]
        tab2 = [A.alloc([192], F32) for _ in range(2)]
        junk = A.alloc([D], F32)
        tmp = A.alloc([D], F32)
        hb = A.alloc([D], BF16)
        hT = A.alloc([8, 128], BF16)
        ssq = A.alloc([8], F32)
        rstd = A.alloc([8], F32)
        ssqD = A.alloc([8], F32)
        krs = A.alloc([32], F32)
        qf = A.alloc([384], F32)
        kvf = A.alloc([512], F32)
        bdf = A.alloc([512], F32)
        bdfD = A.alloc([512], F32)
        c2f = A.alloc([256], F32)
        rstdD = A.alloc([8], F32)
        cqkv = A.alloc([384], BF16)
        cT = A.alloc([3, 128], BF16)
        qa = A.alloc([4, 96], BF16)
        ka = A.alloc([4, 96], BF16)
        krr = A.alloc([1, 32], F32)
        t1 = A.alloc([512], F32)
        t2 = A.alloc([512], F32)
        sq = A.alloc([384], F32)
        qn = A.alloc([6, 64], F32)
        qkb = A.alloc([8, 64], BF16)
        qkc = A.alloc([512], BF16)
        stA = [A.alloc([8, 128], BF16) for _ in range(2)]
        stP = [A.alloc([4, 128], BF16) for _ in range(2)]
        va = [[A.alloc([4, 128], BF16) for _ in range(2)] for _ in range(4)]
        for n in range(4):
            for b in range(2):
                self.MEMSET("pool", va[n][b], 1.0, ["va%d_%d" % (n, b)])
        T0, T1 = self.bank_bf(0), self.bank_bf(1)
        pA, pBD, pC1, pC2, pQ, pKV = (self.bank(i) for i in (2, 3, 4, 5, 6, 7))

        def load(t):
            b = t % 2
            self.DMA("sp", xt2[b], self.x_src(l, t), (), ["xt%d" % b])
            self.DMA("sp", tab2[b], I["ropetab"][t * 128:(t + 1) * 128, :], (), ["tab%d" % b])

        import os
        KCUT = int(os.environ.get("KCUT", "99"))
        load(0)
        for t in range(NT):
            if t + 1 < NT:
                load(t + 1)
            b = t % 2
            xt, xk, tab, tabk = xt2[b], "xt%d" % b, tab2[b], "tab%d" % b
            w = 0 if t < NTL else 1
            tok = slice(t * 128, (t + 1) * 128)
            self.norm_mod(xt, xk, w, D, 0, junk, tmp, hb, "hb", ssq[:, 0:1], rstd[:, 0:1], "n1")
            self.transpose8(hb, "hb", T0, "T0", hT, "hT")
            if KCUT == 1:
                return
            def proj(dst, dk, c0, c1):
                for k in range(8):
                    self.MM(dst[:, 0:c1 - c0], hT[:, k, :], win[:, k, c0:c1], k == 0, k == 7, ["hT", "win"], [dk])
            proj(pA, "pA", 0, 416)
            proj(pBD, "pBD", 416, 928)
            self.CP("act", bdf, pBD, ["pBD"], ["bdf"])
            proj(pC1, "pC1", 928, 1440)
            proj(pC2, "pC2", 1440, 1696)
            proj(pBD, "pBD", 1696, 2208)
            if KCUT == 2:
                return
            self.ACT(junk[:, 0:256], pA[:, 0:256], AF.Square, ["pA"], ["junk", "ssqA0"], accum=ssq[:, 1:2])
            self.ACT(junk[:, 256:384], pA[:, 256:384], AF.Square, ["pA"], ["junk", "ssqA1"], accum=ssq[:, 2:3])
            self.ACT(ssq[:, 1:2], ssq[:, 1:2], AF.Sqrt, ["ssqA0"], ["ssqA0"], scale=1.0 / 256, bias=EPS)
            self.ACT(ssq[:, 2:3], ssq[:, 2:3], AF.Sqrt, ["ssqA1"], ["ssqA1"], scale=1.0 / 128, bias=EPS)
            self.RCP(rstd[:, 1:3], ssq[:, 1:3], ["ssqA0", "ssqA1"], ["rstdA"])
            self.STT("dve", cqkv[:, 0:256], pA[:, 0:256], rstd[:, 1:2], gqa, ALU.mult, ALU.mult, ["pA", "rstdA", "gqa"], ["cqkv0"])
            self.STT("dve", cqkv[:, 256:384], pA[:, 256:384], rstd[:, 2:3], gkv, ALU.mult, ALU.mult, ["pA", "rstdA", "gkv"], ["cqkv1"])
            for k in range(3):
                self.TR(T1[:, k * 128:(k + 1) * 128], cqkv[:, k * 128:(k + 1) * 128], self.ident_b,
                        ["cqkv0", "cqkv1", "ident_b"], ["T1"])
            self.CP("act", cT, T1[:, 0:384].rearrange("p (k m) -> p k m", k=3), ["T1"], ["cT"])
            if KCUT == 3:
                return
            for k in range(2):
                self.MM(pQ[:, 0:384], cT[:, k, :], wqb[:, k, :], k == 0, k == 1, ["cT", "wqb"], ["pQ"])
            self.MM(pKV, cT[:, 2, :], wkvb, True, True, ["cT", "wkvb"], ["pKV"])
            if KCUT == 4:
                return
            self.CP("act", qf, pQ[:, 0:384], ["pQ"], ["qf"])
            self.CP("act", kvf, pKV, ["pKV"], ["kvf"])
            self.CP("act", krs, pA[:, 384:416], ["pA"], ["krs"])
            pQv = qf.rearrange("p (h r) -> p h r", h=4)
            pKVv = kvf.rearrange("p (h r) -> p h r", h=4)
            self.CP("pool", qa[:, :, 0:64], pQv[:, :, 0:64], ["qf"], ["qa0"])
            if KCUT == 41:
                return
            self.rope(pQv[:, :, 64:96], "qf", 4, 32, tab, tabk, 128, 160, t1, t2, [(qa[:, :, 64:96], 0, 4, "qa1")], "R")
            if KCUT == 42:
                return
            self.rope(krs.unsqueeze(1), "krs", 1, 32, tab, tabk, 128, 160, t1, t2, [(krr, 0, 1, "krr")], "R")
            if KCUT == 5:
                return
            self.CP("pool", ka[:, :, 0:64], pKVv[:, :, 0:64], ["kvf"], ["ka0"])
            self.CP("dve", ka[:, :, 64:96], krr.to_broadcast([128, 4, 32]), ["krr"], ["ka1"])
            vA = va[0][b]
            vAp = vA.rearrange("p (j two) d -> p j two d", two=2)
            pKVp = pKVv.rearrange("p (j two) r -> p j two r", two=2)
            self.CP("pool", vAp[:, :, 0, 0:64], pKVp[:, :, 0, 64:128], ["kvf"], ["va0_%d" % b])
            self.CP("pool", vAp[:, :, 1, 64:128], pKVp[:, :, 1, 64:128], ["kvf"], ["va0_%d" % b])
            if KCUT == 6:
                return
            for h in range(4):
                self.TR(T1[0:96, h * 128:(h + 1) * 128], qa[:, h, :], self.ident_b, ["qa0", "qa1", "ident_b"], ["T1"])
                self.TR(T1[0:96, (4 + h) * 128:(5 + h) * 128], ka[:, h, :], self.ident_b, ["ka0", "ka1", "ident_b"], ["T1"])
            sA = stA[b]
            self.CP("act", sA[0:96], T1[0:96, :].rearrange("p (k m) -> p k m", k=8), ["T1"], ["stA%d" % b])
            if KCUT == 7:
                return
            self.DMA("sp", self.QT[0][:, :, tok], sA[0:96, 0:4, :], ["stA%d" % b], [])
            self.DMA("sp", self.KT[0][:, :, tok], sA[0:96, 4:8, :], ["stA%d" % b], [])
            self.DMA("sp", self.VA[0][:, t, :, :], vA, ["va0_%d" % b], [])
            if KCUT == 8:
                return

            def gqa_v(src, srck, n):
                v = va[n][b]
                vp = v.rearrange("p (g two) d -> p g two d", two=2)
                sv = src[:, 384:512].rearrange("p (g r) -> p g r", g=2)
                self.CP("pool", vp[:, :, 0, 0:64], sv, [srck], ["va%d_%d" % (n, b)])
                self.CP("pool", vp[:, :, 1, 64:128], sv, [srck], ["va%d_%d" % (n, b)])
                return v

            def qk_store(src, srcks, n, tbank, tbk):
                for k in range(4):
                    self.TR(tbank[:, k * 128:(k + 1) * 128], src[:, k * 128:(k + 1) * 128], self.ident_b,
                            list(srcks) + ["ident_b"], [tbk])
                sp_ = stP[n % 2]
                spk = "stP%d" % (n % 2)
                self.CP("act", sp_, tbank[:, 0:512].rearrange("p (k m) -> p k m", k=4), [tbk], [spk])
                self.DMA("sp", self.QT[n][:, :, tok], sp_[:, 0:2, :], [spk], [])
                self.DMA("sp", self.KT[n][:, :, tok], sp_[:, 2:4, :], [spk], [])

            pBv = bdf[:, 0:384].rearrange("p (h r) -> p h r", h=6)
            qkb_d = qkb[:, 4:8, :].rearrange("p (g two) r -> p g two r", two=2)
            self.rope(pBv, "bdf", 6, 64, tab, tabk, 0, 64, t1, t2,
                      [(qkb[:, 0:4, :], 0, 4, "qkb0"), (qkb_d[:, :, 0, :], 4, 6, "qkb1"), (qkb_d[:, :, 1, :], 4, 6, "qkb2")], "R")
            vB = gqa_v(bdf, "bdf", 1)
            qk_store(qkb.rearrange("p h r -> p (h r)"), ["qkb0", "qkb1", "qkb2"], 1, T0, "T0")
            self.DMA("sp", self.VA[1][:, t, :, :], vB, ["va1_%d" % b], [])
            if KCUT == 9:
                return
            self.CP("act", qkc, pC1, ["pC1"], ["qkc"])
            vC = va[2][b]
            vCp = vC.rearrange("p (j two) d -> p j two d", two=2)
            self.CP("act", c2f, pC2[:, 0:256], ["pC2"], ["c2f"])
            pC2p = c2f.rearrange("p (j two r) -> p j two r", two=2, r=64)
            self.CP("pool", vCp[:, :, 0, 0:64], pC2p[:, :, 0, :], ["c2f"], ["va2_%d" % b])
            self.CP("pool", vCp[:, :, 1, 64:128], pC2p[:, :, 1, :], ["c2f"], ["va2_%d" % b])
            qk_store(qkc, ["qkc"], 2, T1, "T1")
            self.DMA("sp", self.VA[2][:, t, :, :], vC, ["va2_%d" % b], [])
            if KCUT == 10:
                return
            self.CP("act", bdfD, pBD, ["pBD"], ["bdfD"])
            pDv = bdfD[:, 0:384].rearrange("p (h r) -> p h r", h=6)
            sqv = sq.rearrange("p (h r) -> p h r", h=6)
            self.ACT(sq, pBD[:, 0:384], AF.Square, ["pBD"], ["sq"])
            self.RED(ssqD[:, 0:6], sqv, ALU.add, ["sq"], ["ssqD"])
            self.ACT(ssqD[:, 0:6], ssqD[:, 0:6], AF.Sqrt, ["ssqD"], ["ssqD"], scale=1.0 / 64, bias=EPS)
            self.RCP(rstdD[:, 0:6], ssqD[:, 0:6], ["ssqD"], ["rstdD"])
            self.TT("dve", qn, pDv, rstdD[:, 0:6].unsqueeze(2).to_broadcast([128, 6, 64]), ALU.mult, ["bdfD", "rstdD"], ["qn"])
            self.TT("dve", qn, qn, gd, ALU.mult, ["qn", "gd"], ["qn"])
            self.rope(qn, "qn", 6, 64, tab, tabk, 0, 64, t1, t2,
                      [(qkb[:, 0:4, :], 0, 4, "qkb0"), (qkb_d[:, :, 0, :], 4, 6, "qkb1"), (qkb_d[:, :, 1, :], 4, 6, "qkb2")], "R")
            vD = gqa_v(bdfD, "bdfD", 3)
            qk_store(qkb.rearrange("p h r -> p (h r)"), ["qkb0", "qkb1", "qkb2"], 3, T0, "T0")
            self.DMA("sp", self.VA[3][:, t, :, :], vD, ["va3_%d" % b], [])

    def phase_p2(self, l):
        cfg, A, I = self.cfg, self.A, self.I
        NT, NTL, T = cfg.NT, cfg.NTL, cfg.T
        LOOK = 3
        groups = [(g * 512, 512, g) for g in range(cfg.NG)]
        if self.with_ctx:
            groups.append((cfg.SEQ, cfg.CTX, None))
        ctx_tiles = list(range(NTL, NT))
        base_mark = A.mark()
        pt = [A.alloc([512], BF16) for _ in range(6)]
        tmpf = [A.alloc([512], F32) for _ in range(4)]
        rden = [A.alloc([512], F32) for _ in range(2)]
        rdt = A.alloc([512], F32)
        ost = [A.alloc([512], BF16) for _ in range(2)]
        qt = [A.alloc([512], BF16) for _ in range(2)]
        esink = A.alloc([4], F32)
        self.DMA("sp", esink, I["sink_b"][l:l + 1, :].partition_broadcast(128), (), ["esink"])
        self.ACT(esink, esink, AF.Exp, ["esink"], ["esink"])
        inner_mark = A.mark()
        cnt = {"pt": 0, "s": 0, "tf": 0, "grp": 0}
        for n in range(4):
            A.release(inner_mark)
            self.S.barrier()
            isA = n == 0
            scale = (96 ** -0.5) if isA else 0.125
            LOOK = 2 if n == 2 else 3
            if isA:
                kt = A.alloc([4, T], BF16)
                self.DMA("sp", kt[0:96], self.KT[0], (), ["kt"])
            else:
                kt = A.alloc([4, T], BF16)
                self.MEMSET("pool", kt, 0.0, ["kt"])
                for hh in range(4):
                    hp = (hh % 2) * 64
                    self.DMA("sp", kt[hp:hp + 64, hh, :], self.KT[n][hp:hp + 64, hh // 2, :], (), ["kt"])
            vat = A.alloc([NT, 4, 128], BF16)
            step = max(1, NT // 4)
            for t0 in range(0, NT, step):
                t1_ = min(NT, t0 + step)
                self.DMA("sp", vat[:, t0:t1_], self.VA[n][:, t0:t1_], (), ["vat"])
            mb = mc = bias = comb = None
            if n == 1:
                mb = A.alloc([6, 512], F32)
                self.DMA("sp", mb, I["maskb"].rearrange("r p q -> p r q"), (), ["mb"])
            if n == 2:
                mc = A.alloc([self.ncase * 8, 512], BF16)
                for ci in range(self.ncase):
                    self.DMA("pool", mc[:, ci * 8:(ci + 1) * 8, :], I["maskc"][ci].rearrange("r p q -> p r q"), (), ["mc"])
                bias = A.alloc([8, 512], F32)
                comb1 = A.alloc([8, 512], F32)
                comb = [comb1, comb1]
            G_list = []
            for h in range(4):
                for gi, (q0, nq, g) in enumerate(groups):
                    tiles = []
                    if g is None:
                        tiles = [(t_, None) for t_ in ctx_tiles]
                    elif n in (0, 3):
                        tiles = [(t_, None) for t_ in range(NT)]
                    elif n == 1:
                        for j in range(4 * g - 1, 4 * g + 5):
                            if 0 <= j < NTL:
                                tiles.append((j, ("mb", j - 4 * g + 1)))
                        tiles += [(t_, None) for t_ in ctx_tiles]
                    else:
                        for r in self.valid_c[g]:
                            tiles.append((4 * g + r - 2, ("comb", r)))
                        tiles += [(t_, None) for t_ in ctx_tiles]
                    gid = cnt["grp"]
                    cnt["grp"] += 1
                    G_list.append(dict(h=h, q0=q0, nq=nq, g=g, tiles=tiles, gid=gid, first_of_head=(gi == 0)))
            items = [(G, i) for G in G_list for i in range(len(G["tiles"]))]
            state = {}

            def emit_S(idx):
                G, i = items[idx]
                h, q0, nq, gid = G["h"], G["q0"], G["nq"], G["gid"]
                ph = (h % 2) * 64
                vr = slice(ph, ph + 64)
                qb = gid % 2
                qtb, qk_ = qt[qb], "qt%d" % qb
                if i == 0:
                    if n == 2 and G["first_of_head"]:
                        self.DMA("sp", bias, I["biasg"][l, h].rearrange("r p q -> p r q"), (), ["bias"])
                    if n == 2 and G["first_of_head"]:
                        state["built"] = None
                    if n == 2 and G["g"] is not None and state.get("built") != (self.case_of_g[G["g"]], tuple(self.valid_c[G["g"]])):
                        state["built"] = (self.case_of_g[G["g"]], tuple(self.valid_c[G["g"]]))
                        ci = self.case_of_g[G["g"]]
                        cb_ = comb[gid % 2]
                        for r in self.valid_c[G["g"]]:
                            self.TT("pool", cb_[:, r, :], bias[:, r, :], mc[:, ci * 8 + r, :], ALU.add,
                                    ["bias", "mc"], ["comb_%d" % r])
                    if isA:
                        self.DMA("sp", qtb[0:96, 0:nq], self.QT[0][:, h, q0:q0 + nq], (), [qk_])
                    else:
                        self.DMA("sp", qtb[:, 0:nq], self.QT[n][:, h // 2, q0:q0 + nq], (), [qk_])
                q_ap = qtb[0:96, 0:nq] if isA else qtb[:, 0:nq]
                tk = G["tiles"][i][0]
                sb_ = cnt["s"] % 4
                cnt["s"] += 1
                psS = self.bank(sb_)
                psk = "psS%d" % sb_
                if isA:
                    k_ap = kt[0:96, h, tk * 128:(tk + 1) * 128]
                else:
                    k_ap = kt[:, h, tk * 128:(tk + 1) * 128]
                self.MM(psS[:, 0:nq], k_ap, q_ap, True, True, ["kt", qk_], [psk])
                state[idx] = (psS, psk)

            for j in range(min(LOOK, len(items))):
                emit_S(j)
            for idx, (G, i) in enumerate(items):
                if idx + LOOK < len(items):
                    emit_S(idx + LOOK)
                h, q0, nq, gid = G["h"], G["q0"], G["nq"], G["gid"]
                ph = (h % 2) * 64
                vr = slice(ph, ph + 64)
                dr = slice(64 - ph, 128 - ph)
                psS, psk = state.pop(idx)
                tk, tabinfo = G["tiles"][i]
                ob = gid % 2
                psO = self.bank(6 + ob)
                pok = "psO%d" % ob
                last = len(G["tiles"]) - 1
                pb_ = cnt["pt"] % 6
                cnt["pt"] += 1
                ptb, ptk = pt[pb_], "pt%d" % pb_
                if tabinfo is None:
                    self.ACT(ptb[:, 0:nq], psS[:, 0:nq], AF.Exp, [psk], [ptk], scale=scale)
                else:
                    if tabinfo[0] == "mb":
                        tabv, tabk = mb[:, tabinfo[1], :], "mb"
                    else:
                        tabv, tabk = comb[gid % 2][:, tabinfo[1], :], "comb_%d" % tabinfo[1]
                    fb = cnt["tf"] % 4
                    cnt["tf"] += 1
                    self.STT("dve", tmpf[fb][:, 0:nq], psS[:, 0:nq], scale, tabv[:, 0:nq], ALU.mult, ALU.add,
                             [psk, tabk], ["tmpf%d" % fb])
                    self.ACT(ptb[:, 0:nq], tmpf[fb][:, 0:nq], AF.Exp, ["tmpf%d" % fb], [ptk])
                self.MM(psO[:, 0:nq], vat[:, tk, h, :], ptb[:, 0:nq], i == 0, i == last, ["vat", ptk], [pok])
                if i == last:
                    rb = gid % 2
                    rd, rdk = rden[rb], "rden%d" % rb
                    if n in (1, 2):
                        if n == 1:
                            self.ACT(rdt[dr, 0:nq], psO[dr, 0:nq], AF.Ln, [pok, "esink"], ["rdt"], bias=esink[dr, h:h + 1])
                        else:
                            self.ACT(rdt[dr, 0:nq], psO[dr, 0:nq], AF.Ln, [pok], ["rdt"])
                        self.ACT(rdt[dr, 0:nq], rdt[dr, 0:nq], AF.Exp, ["rdt"], ["rdt"], scale=-1.0)
                        self.CP("dve", rd[vr, 0:nq], rdt[dr, 0:nq], ["rdt"], [rdk])
                    else:
                        self.RCP(rd[vr, 0:nq], psO[dr, 0:nq], [pok], [rdk])
                    osb, osk = ost[rb], "ost%d" % rb
                    self.TT("dve", osb[vr, 0:nq], psO[vr, 0:nq], rd[vr, 0:nq], ALU.mult, [pok, rdk], [osk])
                    self.DMA("sp", self.OT[n * 2 + h // 2][vr, q0:q0 + nq], osb[vr, 0:nq], [osk], [])
        A.release(base_mark)

    def phase_p3(self, l):
        cfg, A, I = self.cfg, self.A, self.I
        NT, NTL = cfg.NT, cfg.NTL
        tiles = list(range(NT)) if self.with_ctx else list(range(NTL))
        wg = A.alloc([32, D], BF16)
        for n in range(4):
            for k in range(8):
                self.DMA("pool", wg[:, n * 8 + k, :], I["w_gate"][l, n, k * 128:(k + 1) * 128, :], (), ["wg"])
        bg = A.alloc([4 * D], BF16)
        self.DMA("pool", bg[0:1, :], I["b_gate"][l:l + 1, :], (), ["bg"])
        wbr = A.alloc([8, D], BF16)
        self.DMA("pool", wbr, I["w_branch"][l].rearrange("n (c p) m -> p (n c) m", p=128), (), ["wbr"])
        wo = A.alloc([8, D], BF16)
        self.DMA("pool", wo, I["w_out"][l].rearrange("(k p) m -> p k m", p=128), (), ["wo"])
        wr = A.alloc([8, 36], F32)
        self.DMA("sp", wr[:, :, 0:4], I["w_group"][l].rearrange("(k p) n -> p k n", p=128), (), ["wr"])
        self.DMA("sp", wr[:, :, 4:36], I["w_router"][l].rearrange("(k p) n -> p k n", p=128), (), ["wr"])
        br = A.alloc([36], F32)
        self.DMA("sp", br[0:1, 0:4], I["b_group"][l:l + 1, :], (), ["br"])
        self.DMA("sp", br[0:1, 4:36], I["b_router"][l:l + 1, :], (), ["br"])
        xt2 = [A.alloc([D], F32) for _ in range(3)]
        ot2 = [A.alloc([8, 128], BF16) for _ in range(2)]
        tmpA = A.alloc([D], F32)
        tmpB = A.alloc([D], F32)
        hb = A.alloc([D], BF16)
        hT = A.alloc([8, 128], BF16)
        gate = A.alloc([D], BF16)
        tmpn = A.alloc([D], BF16)
        ypre = A.alloc([D], F32)
        ypb = A.alloc([D], BF16)
        yT = A.alloc([8, 128], BF16)
        h2Tf = A.alloc([8, 128], F32)
        h2b = A.alloc([D], BF16)
        oh12b = A.alloc([64], BF16)
        csb = A.alloc([64], F32)
        tr0 = A.alloc([32], F32)
        tr1 = A.alloc([32], F32)
        self.MEMSET("dve", self.base, 0.0, ["base"])
        ssq = A.alloc([4], F32)
        rstd = A.alloc([4], F32)
        lg = A.alloc([36], F32)
        sm = A.alloc([16], F32)
        ohg = A.alloc([4], F32)
        pen = A.alloc([4], F32)
        em = A.alloc([32], F32)
        em2 = A.alloc([32], F32)
        oh1 = A.alloc([32], F32)
        oh2 = A.alloc([32], F32)
        c1 = A.alloc([32], F32)
        j4 = A.alloc([4], F32)
        PT = self.bank_bf(0)
        PB, PY = self.banks[2], self.banks[3]
        pR = self.bank(1)
        cntu = [0]

        def load(i):
            t = tiles[i]
            b = i % 2
            self.DMA("sp", xt2[i % 3], self.x_src(l, t), (), ["xt%d" % (i % 3)])
            self.DMA("sp", ot2[b], self.OT[:, :, t * 128:(t + 1) * 128].rearrange("b p t -> p b t"), (), ["ot%d" % b])

        def norm1(i):
            t = tiles[i]
            w = 0 if t < NTL else 1
            self.norm_mod(xt2[i % 3], "xt%d" % (i % 3), w, D, 0, hb, tmpA, hb, "hb", ssq[:, 0:1], rstd[:, 0:1], "n1",
                          jk="hb", tk="tmpA")

        def stageA(i, gen=None):
            t = tiles[i]
            b = i % 2
            xt, xk, ot, otk = xt2[i % 3], "xt%d" % (i % 3), ot2[b], "ot%d" % b
            w = 0 if t < NTL else 1
            m = self.mod[w]
            mk = "mod%d" % w
            tok = slice(t * 128, (t + 1) * 128)
            tmp = tmpA
            self.transpose8(hb, "hb", PT, "PT", hT, "hT")
            for n in range(4):
                for half in range(2):
                    cs = slice(half * 512, (half + 1) * 512)
                    r_ = cntu[0] % 2
                    cntu[0] += 1
                    PGh, pgk = self.bank(2 + r_), "PG%d" % r_
                    PBh, pbk = self.bank(4 + r_), "PB%d" % r_
                    for k in range(8):
                        self.MM(PGh, hT[:, k, :], wg[:, n * 8 + k, cs], k == 0, False, ["hT", "wg"], [pgk])
                    self.MM(PGh, self.ones_b[0:1, :], bg[0:1, n * D + half * 512:n * D + (half + 1) * 512], False, True,
                            ["ones_b", "bg"], [pgk])
                    for c in range(2):
                        self.MM(PBh, ot[:, n * 2 + c, :], wbr[:, n * 2 + c, cs], c == 0, c == 1, [otk, "wbr"], [pbk])
                    gh, ghk = gate[:, r_ * 512:(r_ + 1) * 512], "gate%d" % r_
                    self.ACT(gh, PGh, AF.Sigmoid, [pgk], [ghk])
                    yk = "ypre%d" % half
                    if n == 0:
                        self.TT("dve", ypre[:, cs], gh, PBh, ALU.mult, [ghk, pbk], [yk])
                    else:
                        tn, tnk = tmpn[:, r_ * 512:(r_ + 1) * 512], "tn%d" % r_
                        self.TT("dve", tn, gh, PBh, ALU.mult, [ghk, pbk], [tnk])
                        if n < 3:
                            self.TT("pool", ypre[:, cs], ypre[:, cs], tn, ALU.add, [yk, tnk], [yk])
                        else:
                            self.TT("pool", ypb[:, cs], ypre[:, cs], tn, ALU.add, [yk, tnk], ["ypb%d" % half])
                    u_ = n * 2 + half
                    if u_ in (1, 3, 5, 7) and gen is not None:
                        next(gen, None)
                    if u_ == 3 and i + 1 < len(tiles):
                        norm1(i + 1)
            if i + 2 < len(tiles):
                load(i + 2)
            self.transpose8(ypb, ["ypb0", "ypb1"], PT, "PT", yT, "yT")
            for half in range(2):
                cs = slice(half * 512, (half + 1) * 512)
                for k in range(8):
                    self.MM(PY[:, cs], yT[:, k, :], wo[:, k, cs], k == 0, k == 7, ["yT", "wo"], ["PY"])
            self.TT("dve", tmp, PY[:, :], m[:, 2 * D:3 * D], ALU.mult, ["PY", mk], ["tmpA"])
            self.TT("pool", xt, tmp, xt, ALU.add, ["tmpA", xk], [xk])
            self.DMA("sp", self.xcur[tok, :], xt, [xk], [])
            if gen is not None:
                next(gen, None)

        def stageB(i):
            t = tiles[i]
            xt, xk = xt2[i % 3], "xt%d" % (i % 3)
            w = 0 if t < NTL else 1
            tok = slice(t * 128, (t + 1) * 128)
            tmp = tmpB
            h2 = tmp
            self.norm_mod(xt, xk, w, 4 * D, 3 * D, h2b, tmp, h2, "tmpB", ssq[:, 1:2], rstd[:, 1:2], "n2", jk="h2b", tk="tmpB")
            yield
            for k in range(8):
                self.TR(PB[:, k * 128:(k + 1) * 128], h2[:, k * 128:(k + 1) * 128], self.ident_f, ["tmpB", "ident_f"], ["PB0", "PB1"])
            self.CP("act", h2Tf, PB[:, :].rearrange("p (k m) -> p k m", k=8), ["PB0", "PB1"], ["h2Tf"])
            self.CP("pool", h2b, h2, ["tmpB"], ["h2b"])
            self.DMA("sp", self.H2[tok, :], h2b, ["h2b"], [])
            yield
            for k in range(8):
                self.MM(pR[:, 0:36], h2Tf[:, k, :], wr[:, k, :], k == 0, False, ["h2Tf", "wr"], ["pR"])
            self.MM(pR[:, 0:36], self.ones_f[0:1, :], br[0:1, :], False, True, ["ones_f", "br"], ["pR"])
            self.CP("dve", lg, pR[:, 0:36], ["pR"], ["lg"])
            gl = lg[:, 0:4]
            el = lg[:, 4:36].rearrange("p (g e) -> p g e", g=4)
            gmax, negmax, se, gw, m1, m2, dd, w1, w1g, w2g = (sm[:, j:j + 1] for j in range(10))
            self.RED(gmax, gl, ALU.max, ["lg"], ["gmax"])
            self.TS("dve", ohg, gl, gmax, None, ALU.is_equal, None, ["lg", "gmax"], ["ohg"])
            self.TS("dve", negmax, gmax, -1.0, None, ALU.mult, None, ["gmax"], ["negmax"])
            self.ACT(j4, gl, AF.Exp, ["lg", "negmax"], ["j4", "se"], bias=negmax, accum=se)
            self.RCP(gw, se, ["se"], ["gw"])
            self.TS("dve", pen, ohg, -1.0, 1e9, ALU.add, ALU.mult, ["ohg"], ["pen"])
            emv = em.rearrange("p (g e) -> p g e", g=4)
            self.TT("dve", emv, el, pen.unsqueeze(2).to_broadcast([128, 4, 8]), ALU.add, ["lg", "pen"], ["em"])
            self.RED(m1, em, ALU.max, ["em"], ["m1"])
            self.TS("dve", oh1, em, m1, None, ALU.is_equal, None, ["em", "m1"], ["oh1"])
            self.STT("dve", em2, oh1, -2e9, em, ALU.mult, ALU.add, ["oh1", "em"], ["em2"])
            self.RED(m2, em2, ALU.max, ["em2"], ["m2"])
            self.TS("dve", oh2, em2, m2, None, ALU.is_equal, None, ["em2", "m2"], ["oh2"])
            yield
            self.TT("dve", dd, m2, m1, ALU.subtract, ["m1", "m2"], ["dd"])
            self.ACT(dd, dd, AF.Exp, ["dd"], ["dd"])
            self.TS("dve", dd, dd, 1.0, None, ALU.add, None, ["dd"], ["dd"])
            self.RCP(w1, dd, ["dd"], ["w1"])
            self.TT("dve", w1g, w1, gw, ALU.mult, ["w1", "gw"], ["w1g"])
            self.TT("dve", w2g, gw, w1g, ALU.subtract, ["w1g", "gw"], ["w2g"])
            rt, base, iota = self.rt, self.base, self.iota
            pRK = self.bank(1)[:, 64:192]
            self.CP("pool", oh12b[:, 0:32], oh1, ["oh1"], ["oh12b0"])
            self.CP("pool", oh12b[:, 32:64], oh2, ["oh2"], ["oh12b1"])
            yield
            self.MM(pRK[:, 0:64], self.ltb, oh12b, True, True, ["ltb", "oh12b0", "oh12b1"], ["pRK"])
            self.MM(pRK[:, 64:128], self.onesbb, oh12b, True, True, ["onesbb", "oh12b0", "oh12b1"], ["pRK"])
            self.CP("dve", csb, pRK[:, 64:128], ["pRK"], ["cs"])
            self.TT("dve", tr0, pRK[:, 0:32], base, ALU.add, ["pRK", "base"], ["tr0"])
            self.TT("dve", tr0, tr0, oh1, ALU.mult, ["tr0", "oh1"], ["tr0"])
            self.RED(rt[:, t, 4:5], tr0, ALU.add, ["tr0"], ["rt"])
            self.TT("dve", tr1, pRK[:, 32:64], base, ALU.add, ["pRK", "base"], ["tr1"])
            self.TT("dve", tr1, tr1, csb[:, 0:32], ALU.add, ["tr1", "cs"], ["tr1"])
            self.TT("dve", tr1, tr1, oh2, ALU.mult, ["tr1", "oh2"], ["tr1"])
            self.RED(rt[:, t, 5:6], tr1, ALU.add, ["tr1"], ["rt"])
            self.TT("dve", base, base, csb[:, 0:32], ALU.add, ["base", "cs"], ["base"])
            self.TT("dve", base, base, csb[:, 32:64], ALU.add, ["base", "cs"], ["base"])
            self.TT("dve", tr0, oh1, iota, ALU.mult, ["oh1", "iota", "tr0"], ["tr0"])
            self.RED(rt[:, t, 0:1], tr0, ALU.add, ["tr0"], ["rt"])
            self.TT("dve", tr1, oh2, iota, ALU.mult, ["oh2", "iota", "tr1"], ["tr1"])
            self.RED(rt[:, t, 1:2], tr1, ALU.add, ["tr1"], ["rt"])
            self.CP("dve", rt[:, t, 2:3], w1g, ["w1g"], ["rt"])
            self.CP("dve", rt[:, t, 3:4], w2g, ["w2g"], ["rt"])

        ntl_ = len(tiles)
        load(0)
        if ntl_ > 1:
            load(1)
        norm1(0)
        for i in range(ntl_):
            gen = stageB(i - 1) if i > 0 else None
            stageA(i, gen)
            if gen is not None:
                for _ in gen:
                    pass
        for _ in stageB(ntl_ - 1):
            pass

    def phase_p4(self, l):
        cfg, A, I = self.cfg, self.A, self.I
        NT, NTL = cfg.NT, cfg.NTL
        last = l == cfg.DEPTH - 1
        tiles = list(range(NT)) if self.with_ctx else list(range(NTL))
        ntl = len(tiles)
        NBLK = 2 * ntl + 32
        assert NBLK <= NBLK_MAX
        rt, base, iota = self.rt, self.base, self.iota
        m0 = A.mark()
        nbi = A.alloc([32], I32)
        pcnt = A.alloc([32], F32)
        pa = A.alloc([32], F32)
        pb = A.alloc([32], F32)
        bs = A.alloc([32], F32)
        oh3 = A.alloc([NT, 32], F32)
        sl = A.alloc([NT, 2], F32)
        cmp = A.alloc([NBLK_MAX, 32], F32)
        ble = A.alloc([NBLK_MAX], F32)
        zt = A.alloc([8192], BF16)
        h2t = [A.alloc([D], BF16) for _ in range(2)]
        self.MEMSET("pool", zt, 0.0, ["zt"])
        xsz = self.XS.rearrange("(p b) f -> p (b f)", p=128)
        tot = NBLK_MAX * D
        for c0 in range(0, tot, 8192):
            c1_ = min(tot, c0 + 8192)
            self.DMA("sp", xsz[:, c0:c1_], zt[:, 0:c1_ - c0], ["zt"], ["XS"])
        self.TS("dve", pcnt, base, 1.0 / 128, 0.496, ALU.mult, ALU.add, ["base"], ["pcnt"])
        self.CP("dve", nbi, pcnt, ["pcnt"], ["nbi"])
        self.CP("dve", pcnt, nbi, ["nbi"], ["pcnt"])
        self.TS("dve", pcnt, pcnt, 128.0, None, ALU.mult, None, ["pcnt"], ["pcnt"])
        self.CP("dve", pa, pcnt, ["pcnt"], ["pa"])
        src, dst, sk, dk = pa, pb, "pa", "pb"
        for sft in (1, 2, 4, 8, 16):
            self.CP("dve", dst[:, 0:sft], src[:, 0:sft], [sk], [dk])
            self.TT("dve", dst[:, sft:32], src[:, sft:32], src[:, 0:32 - sft], ALU.add, [sk], [dk])
            src, dst, sk, dk = dst, src, dk, sk
        pe, pek = src, sk
        self.TT("dve", bs, pe, pcnt, ALU.subtract, [pek, "pcnt"], ["bs"])
        for k in range(2):
            self.TT("dve", oh3, iota.unsqueeze(1).to_broadcast([128, NT, 32]),
                    rt[:, :, k:k + 1].to_broadcast([128, NT, 32]), ALU.is_equal, ["iota", "rt"], ["oh3"])
            self.TT("dve", oh3, oh3, bs.unsqueeze(1).to_broadcast([128, NT, 32]), ALU.mult, ["oh3", "bs"], ["oh3"])
            self.RED(sl[:, :, k], oh3, ALU.add, ["oh3"], ["sl%d" % k])
            self.TT("dve", sl[:, :, k], sl[:, :, k], rt[:, :, 4 + k], ALU.add, ["sl%d" % k, "rt"], ["sl%d" % k])
        self.CP("dve", self.slot_i, sl, ["sl0", "sl1"], ["slot_i"])
        self.TT("dve", cmp, self.thr.unsqueeze(2).to_broadcast([128, NBLK_MAX, 32]),
                pe.unsqueeze(1).to_broadcast([128, NBLK_MAX, 32]), ALU.is_ge, ["thr", pek], ["cmp"])
        self.RED(ble, cmp, ALU.add, ["cmp"], ["ble"])
        self.TS("dve", ble, ble, 31.0, 128.0, ALU.min, ALU.mult, ["ble"], ["ble"])
        self.TS("dve", ble, ble, self.pidx[:, 0:1], float(l * NE * 128), ALU.add, ALU.add, ["ble", "pidx"], ["ble"])
        self.CP("dve", self.widx, ble, ["ble"], ["widx"])
        for i, t in enumerate(tiles):
            hb, hk = h2t[i % 2], "h2t%d" % (i % 2)
            self.DMA("sp", hb, self.H2[t * 128:(t + 1) * 128, :], (), [hk])
            for k in range(2):
                self.S.add("pool", (lambda hb=hb, t=t, k=k: (lambda e: e.indirect_dma_start(
                    out=self.XS[:, :], out_offset=bass.IndirectOffsetOnAxis(ap=self.slot_i[:, t, k:k + 1], axis=0),
                    in_=hb, in_offset=None)))(), [hk, "slot_i", "XS"], ["XSs"], dma=True)
        self.S.barrier()
        A.release(m0)
        NWR = 4
        w13 = [A.alloc([8, 512], BF16) for _ in range(NWR)]
        w2 = [A.alloc([2, D], BF16) for _ in range(NWR)]
        xs = [A.alloc([D], BF16) for _ in range(NWR)]
        xsT = [A.alloc([8, 128], BF16) for _ in range(2)]
        sb = [A.alloc([256], F32) for _ in range(2)]
        ab = [A.alloc([256], BF16) for _ in range(2)]
        aT = [A.alloc([2, 128], BF16) for _ in range(2)]
        ys = [A.alloc([D], F32) for _ in range(2)]
        w13src = I["w13r"].rearrange("l r f -> (l r) f")
        w2src = I["w2r"].rearrange("l r f -> (l r) f")
        def wload(b):
            wr_ = b % NWR
            self.S.add("pool", (lambda b=b, wr_=wr_: (lambda e: e.indirect_dma_start(
                out=w13[wr_].rearrange("p k f -> p (k f)"), out_offset=None, in_=w13src,
                in_offset=bass.IndirectOffsetOnAxis(ap=self.widx[:, b:b + 1], axis=0))))(), ["widx"], ["w13_%d" % wr_], dma=True)
            self.S.add("pool", (lambda b=b, wr_=wr_: (lambda e: e.indirect_dma_start(
                out=w2[wr_].rearrange("p c f -> p (c f)"), out_offset=None, in_=w2src,
                in_offset=bass.IndirectOffsetOnAxis(ap=self.widx[:, b:b + 1], axis=0))))(), ["widx"], ["w2_%d" % wr_], dma=True)
            self.DMA("sp", xs[wr_], self.XS[b * 128:(b + 1) * 128, :], (), ["xsw%d" % wr_])

        for b in range(min(NWR - 1, NBLK)):
            wload(b)
        for b in range(NBLK):
            if b + NWR - 1 < NBLK:
                wload(b + NWR - 1)
            r = b % 2
            wr_ = b % NWR
            PTx = self.bank_bf(r)
            ptxk = "PTx%d" % r
            psH = self.bank(2 + r)
            phk = "psH%d" % r
            PTa = self.bank_bf(4)
            psO = self.banks[3]
            for k in range(8):
                self.TR(PTx[:, k * 128:(k + 1) * 128], xs[wr_][:, k * 128:(k + 1) * 128], self.ident_b, ["xsw%d" % wr_, "ident_b"], [ptxk])
            self.CP("act", xsT[r], PTx[:, :].rearrange("p (k m) -> p k m", k=8), [ptxk], ["xsT%d" % r])
            for k in range(8):
                self.MM(psH, xsT[r][:, k, :], w13[wr_][:, k, :], k == 0, k == 7, ["xsT%d" % r, "w13_%d" % wr_], [phk])
            self.ACT(sb[r], psH[:, 0:256], AF.Silu, [phk], ["sb%d" % r])
            self.TT("dve", ab[r], sb[r], psH[:, 256:512], ALU.mult, ["sb%d" % r, phk], ["ab%d" % r])
            for c in range(2):
                self.TR(PTa[:, c * 128:(c + 1) * 128], ab[r][:, c * 128:(c + 1) * 128], self.ident_b, ["ab%d" % r, "ident_b"], ["PTa"])
            self.CP("act", aT[r], PTa[:, 0:256].rearrange("p (c m) -> p c m", c=2), ["PTa"], ["aT%d" % r])
            for half in range(2):
                cs_ = slice(half * 512, (half + 1) * 512)
                for c in range(2):
                    self.MM(psO[:, cs_], aT[r][:, c, :], w2[wr_][:, c, cs_], c == 0, c == 1, ["aT%d" % r, "w2_%d" % wr_], ["psO"])
            self.CP("dve", ys[r], psO[:, :], ["psO"], ["ys%d" % r])
            self.DMA("sp", self.YS[b * 128:(b + 1) * 128, :], ys[r], ["ys%d" % r], [])
        self.S.barrier()
        A.release(m0)
        y0 = [A.alloc([D], F32) for _ in range(2)]
        y1 = [A.alloc([D], F32) for _ in range(2)]
        xt2 = [A.alloc([D], F32) for _ in range(2)]
        tmp = A.alloc([D], F32)
        junk = A.alloc([D], BF16)
        ssq = A.alloc([2], F32)
        rstd = A.alloc([2], F32)
        if last:
            self.gfin = A.alloc([D], F32)
            self.DMA("sp", self.gfin, I["g_final"].partition_broadcast(128), (), ["gfin"])
        for i, t in enumerate(tiles):
            b = i % 2
            w = 0 if t < NTL else 1
            tok = slice(t * 128, (t + 1) * 128)
            for k, yb in ((0, y0[b]), (1, y1[b])):
                self.S.add("pool", (lambda yb=yb, t=t, k=k: (lambda e: e.indirect_dma_start(
                    out=yb, out_offset=None, in_=self.YS[:, :],
                    in_offset=bass.IndirectOffsetOnAxis(ap=self.slot_i[:, t, k:k + 1], axis=0))))(), ["slot_i"], ["y%d_%d" % (k, b)], dma=True)
            self.DMA("sp", xt2[b], self.xcur[tok, :], (), ["xt%d" % b])
            self.TS("dve", tmp, y0[b], rt[:, t, 2:3], None, ALU.mult, None, ["y0_%d" % b, "rt"], ["tmp"])
            self.STT("dve", tmp, y1[b], rt[:, t, 3:4], tmp, ALU.mult, ALU.add, ["y1_%d" % b, "rt", "tmp"], ["tmp"])
            self.TT("pool", tmp, tmp, self.mod[w][:, 5 * D:6 * D], ALU.mult, ["tmp", "mod%d" % w], ["tmp"])
            self.TT("pool", xt2[b], tmp, xt2[b], ALU.add, ["tmp", "xt%d" % b], ["xt%d" % b])
            if not last:
                self.DMA("sp", self.xcur[tok, :], xt2[b], ["xt%d" % b], [])
            else:
                self.rstd_of(xt2[b], D, junk, ssq[:, 0:1], rstd[:, 0:1], ["xt%d" % b], "f")
                self.STT("dve", xt2[b], xt2[b], rstd[:, 0:1], self.gfin, ALU.mult, ALU.mult, ["xt%d" % b, "rstdf", "gfin"], ["xt%d" % b])
                o = self.DMA("sp", self.out[tok, :], xt2[b], ["xt%d" % b], [])
                self.S.out_ops.append(o)


_CACHE = {}


def _get_prog(cfg_key):
    if cfg_key not in _CACHE:
        cfg = Cfg(*cfg_key)
        p = Prog(cfg)
        p.build()
        _CACHE[cfg_key] = p
    return _CACHE[cfg_key]


def make_in_maps(prog, inputs, ncores):
    cfg = prog.cfg
    f = lambda a: np.ascontiguousarray(np.asarray(a, dtype=np.float32))
    shared = {}
    for k in ("w_mod", "b_mod", "g_norm_mix", "w_in", "g_q_a", "w_q_b", "g_kv_a", "w_kv_b", "sink_b", "g_q_d", "g_k_d",
              "w_gate", "w_branch", "w_out", "g_norm_ffn", "w_group", "b_group", "w_router", "b_router"):
        shared[k] = f(inputs[k])
    L = cfg.DEPTH
    shared["b_gate"] = f(inputs["b_gate"]).reshape(L, 4 * D)
    shared["g_final"] = f(inputs["g_final"]).reshape(1, D)
    shared["c_ctx"] = f(inputs["c_ctx"]).reshape(1, D)
    rpb = f(inputs["rpb_c"])
    shared["biasg"] = np.ascontiguousarray(rpb[:, :, prog.DR, prog.DC[None]])
    w13 = np.concatenate([f(inputs["w_ff1"]), f(inputs["w_ff3"])], axis=-1)
    w13 = w13.reshape(L, NE, 8, 128, 512).transpose(0, 1, 3, 2, 4)
    shared["w13r"] = np.ascontiguousarray(w13).reshape(L, NE * 128, 4096)
    w2 = f(inputs["w_ff2"]).reshape(L, NE, 2, 128, D).transpose(0, 1, 3, 2, 4)
    shared["w2r"] = np.ascontiguousarray(w2).reshape(L, NE * 128, 2048)
    shared["iota32"] = np.arange(32, dtype=np.float32).reshape(1, 32)
    shared["ltri"] = np.triu(np.ones((128, 128), np.float32), k=1)
    shared["pidx"] = np.arange(128, dtype=np.float32).reshape(128, 1)
    shared["thr"] = (128.0 * np.arange(NBLK_MAX, dtype=np.float32)).reshape(1, NBLK_MAX)
    shared["ident"] = np.eye(128, dtype=np.float32)
    shared["ropetab"] = rope_tab(cfg)
    shared["maskb"] = mask_b()
    shared["maskc"] = prog.maskc_np
    x = f(inputs["x"])
    c = f(inputs["c"])
    ctx = f(inputs["ctx"])
    maps = []
    for b in range(ncores):
        m = dict(shared)
        m["x"] = x[b]
        m["c"] = c[b:b + 1]
        m["ctx"] = ctx[b]
        maps.append(m)
    return maps


def kernel(**inputs):
    x = np.asarray(inputs["x"])
    B, SEQ, _ = x.shape
    CTX = np.asarray(inputs["ctx"]).shape[1]
    DEPTH = np.asarray(inputs["w_mod"]).shape[0]
    prog = _get_prog((SEQ, CTX, DEPTH))
    maps = make_in_maps(prog, inputs, B)
    res = run_bass_kernel_spmd(prog.nc, maps, core_ids=list(range(B)))
    out = np.stack([np.asarray(r["out"], dtype=np.float32) for r in res.results], axis=0)
    return out
```

```python
import contextlib
import numpy as np
import concourse.bass as bass
import concourse.mybir as mybir
from concourse.bass_utils import run_bass_kernel_spmd

F32 = mybir.dt.float32
BF16 = mybir.dt.bfloat16
AF = mybir.ActivationFunctionType
ALU = mybir.AluOpType
AX = mybir.AxisListType
I32 = mybir.dt.int32
NBLK_MAX = 100

SAME_ENGINE_SYNC = True
NDMA_SEMS = 10
import os
MAXOPS = int(os.environ.get('KMAXOPS', '100000000'))
D = 1024
EPS = 1e-6
NEG = -30000.0
IN_COLS = 2208
NE = 32


class Op:
    __slots__ = ("eng", "fn", "deps", "flag", "semv", "sem", "is_dma")

    def __init__(self, eng, fn, is_dma):
        self.eng = eng
        self.fn = fn
        self.deps = []
        self.flag = False
        self.semv = 0
        self.sem = None
        self.is_dma = is_dma


class Sched:
    ENGS = ("pe", "act", "dve", "pool", "sp")

    def __init__(self):
        self.ops = {e: [] for e in self.ENGS}
        self.last_w = {}
        self.readers = {}
        self.pending_barrier = {e: None for e in self.ENGS}
        self.out_ops = []

    def add(self, eng, fn, reads=(), writes=(), dma=False):
        op = Op(eng, fn, dma)
        self.nadd = getattr(self, "nadd", 0) + 1
        if self.nadd > MAXOPS:
            return op
        deps = {}
        raw = set()
        for k in reads:
            w = self.last_w.get(k)
            if w is not None:
                deps[id(w)] = w
                raw.add(id(w))
        for k in writes:
            w = self.last_w.get(k)
            if w is not None:
                deps[id(w)] = w
            for r in self.readers.get(k, ()):
                deps[id(r)] = r
        for k in reads:
            self.readers.setdefault(k, []).append(op)
        for k in writes:
            self.last_w[k] = op
            self.readers[k] = []
        pb = self.pending_barrier[eng]
        if pb is not None:
            for d in pb:
                deps[id(d)] = d
            self.pending_barrier[eng] = None
        for d in deps.values():
            if d is op:
                continue
            if (not d.is_dma) and d.eng == eng and (not dma) and (eng == "pe" or not SAME_ENGINE_SYNC):
                continue
            d.flag = True
            op.deps.append(d)
        self.ops[eng].append(op)
        return op

    def barrier(self):
        lst = []
        for e in self.ENGS:
            ops = self.ops[e]
            if not ops:
                continue
            nd = 0
            got_c = False
            for op in reversed(ops):
                if op.is_dma:
                    if nd < NDMA_SEMS:
                        lst.append(op)
                        nd += 1
                elif not got_c:
                    lst.append(op)
                    got_c = True
                if nd >= NDMA_SEMS and got_c:
                    break
        for e in self.ENGS:
            prev = self.pending_barrier[e]
            self.pending_barrier[e] = lst if prev is None else (prev + lst)
        self.last_w = {}
        self.readers = {}

    def emit(self, nc):
        final_ops = self.out_ops
        for o in final_ops:
            o.flag = True
        with contextlib.ExitStack() as es:
            csem = {}
            for e in ("pe", "act", "dve", "pool"):
                csem[e] = es.enter_context(nc.semaphore("s_" + e))
            dsem = {}
            for e in ("act", "pool", "sp"):
                dsem[e] = [es.enter_context(nc.semaphore("d_%s%d" % (e, i))) for i in range(NDMA_SEMS)]
            for e in self.ENGS:
                cnt = 0
                dcnt = 0
                dvals = [0] * NDMA_SEMS
                for op in self.ops[e]:
                    if op.is_dma:
                        slot = dcnt % NDMA_SEMS
                        dcnt += 1
                        dvals[slot] += 16
                        op.sem = dsem[e][slot]
                        op.semv = dvals[slot]
                    elif op.flag:
                        cnt += 1
                        op.sem = csem[e]
                        op.semv = cnt
            block = es.enter_context(nc.Block())

            def run_engine(e, eng):
                waited = {}
                prev_dma = [None] * NDMA_SEMS
                dcnt = 0
                for op in self.ops[e]:
                    for d in op.deps:
                        key = id(d.sem)
                        if waited.get(key, 0) >= d.semv:
                            continue
                        eng.wait_ge(d.sem, d.semv)
                        waited[key] = d.semv
                    if op.is_dma:
                        slot = dcnt % NDMA_SEMS
                        dcnt += 1
                        p = prev_dma[slot]
                        if p is not None:
                            key = id(p.sem)
                            if waited.get(key, 0) < p.semv:
                                eng.wait_ge(p.sem, p.semv)
                                waited[key] = p.semv
                        prev_dma[slot] = op
                        ins = op.fn(eng)
                        ins.then_inc(op.sem, 16)
                    else:
                        ins = op.fn(eng)
                        if op.flag:
                            ins.then_inc(op.sem, 1)
                if e == "sp":
                    for o in final_ops:
                        key = id(o.sem)
                        if waited.get(key, 0) < o.semv:
                            eng.wait_ge(o.sem, o.semv)
                            waited[key] = o.semv

            @block.tensor
            def _(eng):
                run_engine("pe", eng)

            @block.scalar
            def _(eng):
                run_engine("act", eng)

            @block.vector
            def _(eng):
                run_engine("dve", eng)

            @block.gpsimd
            def _(eng):
                run_engine("pool", eng)

            @block.sync
            def _(eng):
                run_engine("sp", eng)


class Arena:
    def __init__(self, t, nbytes):
        self.t = t
        self.cap = nbytes
        self.off = 0
        self.peak = 0

    def alloc(self, free_shape, dt):
        n = 1
        for s in free_shape:
            n *= s
        esz = 2 if dt == BF16 else 4
        off = (self.off + 63) // 64 * 64
        nb = n * esz
        assert off + nb <= self.cap, ("arena overflow", off + nb, self.cap)
        self.off = off + nb
        self.peak = max(self.peak, self.off)
        v = self.t[:, off // 2:(off + nb) // 2]
        if dt != BF16:
            v = v.bitcast(dt)
        if len(free_shape) > 1:
            names = ["a%d" % i for i in range(len(free_shape))]
            kw = {names[i]: free_shape[i] for i in range(len(free_shape))}
            v = v.rearrange("p (" + " ".join(names) + ") -> p " + " ".join(names), **kw)
        return v

    def mark(self):
        return self.off

    def release(self, m):
        self.off = m


class Cfg:
    def __init__(self, SEQ=4096, CTX=256, DEPTH=2):
        self.SEQ = SEQ
        self.CTX = CTX
        self.DEPTH = DEPTH
        self.T = SEQ + CTX
        self.NT = self.T // 128
        self.NTL = SEQ // 128
        self.NTC = CTX // 128
        self.ROWS = SEQ // 64
        self.NG = SEQ // 512


def rope_tab(cfg):
    t = np.arange(cfg.SEQ)
    rows = (t // 64).astype(np.float32)
    cols = (t % 64).astype(np.float32)
    out = np.zeros((cfg.T, 192), np.float32)
    out[cfg.SEQ:, 0:64] = 1.0
    out[cfg.SEQ:, 128:160] = 1.0
    off = 0
    for R in (64, 32):
        half = R // 2
        inv = (np.float32(10000.0) ** (-np.arange(0, half, 2, dtype=np.float32) / np.float32(half))).astype(np.float32)
        ar = rows[:, None] * inv
        ac = cols[:, None] * inv
        cr, sr, cc, sc = np.cos(ar), np.sin(ar), np.cos(ac), np.sin(ac)
        C = np.concatenate([cr, cr, cc, cc], axis=1)
        S_ = np.concatenate([-sr, sr, -sc, sc], axis=1)
        out[:cfg.SEQ, off:off + R] = C
        out[:cfg.SEQ, off + R:off + 2 * R] = S_
        off += 2 * R
    return out.astype(np.float32)


def mask_b():
    k = np.arange(128)[:, None]
    q = np.arange(512)[None, :]
    out = np.zeros((6, 128, 512), np.float32)
    for r in range(6):
        rel = r - 1
        ok = np.abs(q - 128 * rel - k) <= 128
        out[r] = np.where(ok, 0.0, NEG)
    return out


def c_tables(cfg):
    rows = cfg.ROWS
    kh = min(8, rows)
    k = np.arange(128)[:, None]
    q = np.arange(512)[None, :]
    k_col = k % 64
    q_col = q % 64
    c_start = np.clip(q_col - 8, 0, 64 - 16)
    col_ok = (k_col >= c_start) & (k_col < c_start + 16)
    DC = (np.clip(k_col - q_col, -15, 15) + 15).astype(np.int64) + np.zeros((128, 512), np.int64)
    DR = np.zeros((8, 128, 512), np.int64)
    for r in range(8):
        rel = r - 2
        d = 2 * rel + k // 64 - q // 64
        DR[r] = np.clip(d, -7, 7) + 7
    masks = []
    case_of_g = []
    valid = []
    for g in range(cfg.NG):
        m = np.full((8, 128, 512), NEG, np.float32)
        v = []
        for r in range(8):
            kt = 4 * g + r - 2
            if kt < 0 or kt >= cfg.NTL:
                continue
            k_row = 2 * kt + k // 64
            q_row = 8 * g + q // 64
            r_start = np.clip(q_row - kh // 2, 0, rows - kh)
            row_ok = (k_row >= r_start) & (k_row < r_start + kh)
            ok = row_ok & col_ok
            if ok.any():
                v.append(r)
            m[r] = np.where(ok, 0.0, NEG)
        found = None
        for ci, mm in enumerate(masks):
            if np.array_equal(mm, m):
                found = ci
                break
        if found is None:
            masks.append(m)
            found = len(masks) - 1
        case_of_g.append(found)
        valid.append(v)
    return np.stack(masks).astype(np.float32), case_of_g, valid, DR, DC


class Prog:
    def __init__(self, cfg):
        self.cfg = cfg
        self.S = Sched()
        self.nc = bass.Bass("TRN2", target_bir_lowering=False)
        self.maskc_np, self.case_of_g, self.valid_c, self.DR, self.DC = c_tables(cfg)
        self.ncase = self.maskc_np.shape[0]

    def MM(self, out, lhsT, rhs, start, stop, r, w):
        return self.S.add("pe", lambda e: e.matmul(out, lhsT=lhsT, rhs=rhs, start=start, stop=stop), r, w)

    def TR(self, out, in_, ident, r, w):
        return self.S.add("pe", lambda e: e.transpose(out=out, in_=in_, identity=ident), r, w)

    def ACT(self, out, in_, func, r, w, scale=1.0, bias=0.0, accum=None):
        if accum is None:
            return self.S.add("act", lambda e: e.activation(out=out, in_=in_, func=func, bias=bias, scale=scale), r, w)
        return self.S.add("act", lambda e: e.activation(out=out, in_=in_, func=func, bias=bias, scale=scale,
                                                        accum_out=accum), r, w)

    def TT(self, eng, out, in0, in1, op, r, w):
        return self.S.add(eng, lambda e: e.tensor_tensor(out=out, in0=in0, in1=in1, op=op), r, w)

    def TS(self, eng, out, in0, s1, s2, op0, op1, r, w):
        if s2 is None:
            return self.S.add(eng, lambda e: e.tensor_scalar(out=out, in0=in0, scalar1=s1, scalar2=None, op0=op0), r, w)
        return self.S.add(eng, lambda e: e.tensor_scalar(out=out, in0=in0, scalar1=s1, scalar2=s2, op0=op0, op1=op1), r, w)

    def STT(self, eng, out, in0, scalar, in1, op0, op1, r, w):
        return self.S.add(eng, lambda e: e.scalar_tensor_tensor(out=out, in0=in0, scalar=scalar, in1=in1,
                                                                 op0=op0, op1=op1), r, w)

    def CP(self, eng, out, in_, r, w):
        if eng == "act":
            return self.S.add("act", lambda e: e.copy(out=out, in_=in_), r, w)
        return self.S.add(eng, lambda e: e.tensor_copy(out=out, in_=in_), r, w)

    def RED(self, out, in_, op, r, w):
        return self.S.add("dve", lambda e: e.tensor_reduce(out=out, in_=in_, axis=AX.X, op=op), r, w)

    def RCP(self, out, in_, r, w):
        return self.S.add("dve", lambda e: e.reciprocal(out=out, in_=in_), r, w)

    def DMA(self, q, out, in_, r, w):
        return self.S.add(q, lambda e: e.dma_start(out=out, in_=in_), r, w, dma=True)

    def MEMSET(self, eng, ap, val, w):
        return self.S.add(eng, lambda e: e.memset(ap, val), (), w)

    def build(self):
        cfg = self.cfg
        nc = self.nc
        L = cfg.DEPTH
        T, NT, NTL = cfg.T, cfg.NT, cfg.NTL

        def din(name, shape):
            return nc.dram_tensor(name, list(shape), F32, kind="ExternalInput").ap()

        self.I = I = {}
        I["x"] = din("x", [cfg.SEQ, D])
        I["c"] = din("c", [1, D])
        I["ctx"] = din("ctx", [cfg.CTX, D])
        I["c_ctx"] = din("c_ctx", [1, D])
        I["w_mod"] = din("w_mod", [L, D, 6 * D])
        I["b_mod"] = din("b_mod", [L, 6 * D])
        I["g_norm_mix"] = din("g_norm_mix", [L, D])
        I["w_in"] = din("w_in", [L, D, IN_COLS])
        I["g_q_a"] = din("g_q_a", [L, 256])
        I["w_q_b"] = din("w_q_b", [L, 256, 384])
        I["g_kv_a"] = din("g_kv_a", [L, 128])
        I["w_kv_b"] = din("w_kv_b", [L, 128, 512])
        I["sink_b"] = din("sink_b", [L, 4])
        I["biasg"] = din("biasg", [L, 4, 8, 128, 512])
        I["g_q_d"] = din("g_q_d", [L, 64])
        I["g_k_d"] = din("g_k_d", [L, 64])
        I["w_gate"] = din("w_gate", [L, 4, D, D])
        I["b_gate"] = din("b_gate", [L, 4 * D])
        I["w_branch"] = din("w_branch", [L, 4, 256, D])
        I["w_out"] = din("w_out", [L, D, D])
        I["g_norm_ffn"] = din("g_norm_ffn", [L, D])
        I["w_group"] = din("w_group", [L, D, 4])
        I["b_group"] = din("b_group", [L, 4])
        I["w_router"] = din("w_router", [L, D, 32])
        I["b_router"] = din("b_router", [L, 32])
        I["w13r"] = din("w13r", [L, NE * 128, 4096])
        I["w2r"] = din("w2r", [L, NE * 128, 2048])
        I["iota32"] = din("iota32", [1, 32])
        I["ltri"] = din("ltri", [128, 128])
        I["pidx"] = din("pidx", [128, 1])
        I["thr"] = din("thr", [1, NBLK_MAX])
        I["g_final"] = din("g_final", [1, D])
        I["ident"] = din("ident", [128, 128])
        I["ropetab"] = din("ropetab", [T, 192])
        I["maskb"] = din("maskb", [6, 128, 512])
        I["maskc"] = din("maskc", [self.ncase, 8, 128, 512])
        self.out = nc.dram_tensor("out", [cfg.SEQ, D], F32, kind="ExternalOutput").ap()

        def dscr(name, shape, dt):
            return nc.dram_tensor(name, list(shape), dt, kind="Internal").ap()

        self.xcur = dscr("xcur", [T, D], F32)
        self.QT = [dscr("QT_A", [96, 4, T], BF16)] + [dscr("QT_%d" % n, [128, 2, T], BF16) for n in (1, 2, 3)]
        self.KT = [dscr("KT_A", [96, 4, T], BF16)] + [dscr("KT_%d" % n, [128, 2, T], BF16) for n in (1, 2, 3)]
        self.VA = [dscr("VA_%d" % n, [128, NT, 4, 128], BF16) for n in range(4)]
        self.OT = dscr("OT", [8, 128, T], BF16)
        self.H2 = dscr("H2", [T, D], BF16)
        self.XS = dscr("XS", [NBLK_MAX * 128, D], BF16)
        self.YS = dscr("YS", [NBLK_MAX * 128, D], F32)

        with contextlib.ExitStack() as es:
            ARENA_BYTES = 207 * 1024
            at = es.enter_context(nc.sbuf_tensor("arena", [128, ARENA_BYTES // 2], BF16))
            self.A = Arena(at, ARENA_BYTES)
            self.banks = [es.enter_context(nc.psum_tensor("pb%d" % i, [128, 1024], F32)) for i in range(4)]
            self.setup()
            for l in range(L):
                self.layer(l)
            self.S.emit(nc)
        return nc

    def bank(self, i):
        return self.banks[i // 2][:, (i % 2) * 512:(i % 2 + 1) * 512]

    def bank_bf(self, i):
        return self.bank(i).bitcast(BF16)

    def setup(self):
        A, I = self.A, self.I
        self.ident_f = A.alloc([128], F32)
        self.ident_b = A.alloc([128], BF16)
        self.ones_f = A.alloc([128], F32)
        self.ones_b = A.alloc([128], BF16)
        self.DMA("sp", self.ident_f, I["ident"], (), ["ident_f"])
        self.DMA("pool", self.ident_b, I["ident"], (), ["ident_b"])
        self.MEMSET("dve", self.ones_f, 1.0, ["ones_f"])
        self.MEMSET("dve", self.ones_b, 1.0, ["ones_b"])
        self.mod = [A.alloc([6 * D], F32), A.alloc([6 * D], F32)]
        self.rt = A.alloc([self.cfg.NT, 8], F32)
        self.base = A.alloc([32], F32)
        self.iota = A.alloc([32], F32)
        self.ltb = A.alloc([128], BF16)
        self.onesbb = A.alloc([128], BF16)
        self.pidx = A.alloc([1], F32)
        self.thr = A.alloc([NBLK_MAX], F32)
        self.slot_i = A.alloc([self.cfg.NT, 2], I32)
        self.widx = A.alloc([NBLK_MAX], I32)
        self.DMA("sp", self.iota, I["iota32"].partition_broadcast(128), (), ["iota"])
        self.DMA("pool", self.ltb, I["ltri"], (), ["ltb"])
        self.MEMSET("dve", self.onesbb, 1.0, ["onesbb"])
        self.DMA("sp", self.pidx, I["pidx"], (), ["pidx"])
        self.DMA("sp", self.thr, I["thr"].partition_broadcast(128), (), ["thr"])
        self.pers_mark = A.mark()

    def layer(self, l):
        import os
        cfg = self.cfg
        stop = int(os.environ.get("KSTOP", "99"))
        self.with_ctx = l < cfg.DEPTH - 1
        phases = [self.phase_mod, self.phase_p1, self.phase_p2, self.phase_p3, self.phase_p4]
        for pi, ph in enumerate(phases):
            if l * 5 + pi > stop:
                return
            self.S.barrier()
            self.A.release(self.pers_mark)
            ph(l)

    def phase_mod(self, l):
        A, I = self.A, self.I
        cb = [A.alloc([D], F32), A.alloc([D], F32)]
        lh = [A.alloc([8, 128], F32), A.alloc([8, 128], F32)]
        bmod = A.alloc([6 * D], F32)
        gm = A.alloc([D], F32)
        gf = A.alloc([D], F32)
        wm = [A.alloc([8, 512], F32), A.alloc([8, 512], F32)]
        self.DMA("sp", cb[0], I["c"].partition_broadcast(128), (), ["cb0"])
        self.DMA("sp", cb[1], I["c_ctx"].partition_broadcast(128), (), ["cb1"])
        self.DMA("sp", bmod[0:1, :], I["b_mod"][l:l + 1, :], (), ["bmod"])
        self.DMA("sp", gm, I["g_norm_mix"][l:l + 1, :].partition_broadcast(128), (), ["gm"])
        self.DMA("sp", gf, I["g_norm_ffn"][l:l + 1, :].partition_broadcast(128), (), ["gf"])
        for w in range(2):
            self.ACT(cb[w], cb[w], AF.Silu, ["cb%d" % w], ["cb%d" % w])
            for k in range(8):
                bk = self.banks[w][:, k * 128:(k + 1) * 128]
                self.TR(bk, cb[w][:, k * 128:(k + 1) * 128], self.ident_f, ["cb%d" % w, "ident_f"], ["pbk%d" % w])
            self.CP("act", lh[w], self.banks[w][:, :].rearrange("p (k m) -> p k m", k=8), ["pbk%d" % w], ["lh%d" % w])
        for j in range(12):
            wmj = wm[j % 2]
            wk = "wm%d" % (j % 2)
            self.DMA("sp", wmj, I["w_mod"][l, :, j * 512:(j + 1) * 512].rearrange("(k p) n -> p k n", p=128), (), [wk])
            for w in range(2):
                bi = 4 + (j * 2 + w) % 4
                bk = self.bank(bi)
                bkk = "bank%d" % bi
                for k in range(8):
                    self.MM(bk, lh[w][:, k, :], wmj[:, k, :], k == 0, False, ["lh%d" % w, wk], [bkk])
                self.MM(bk, self.ones_f[0:1, :], bmod[0:1, j * 512:(j + 1) * 512], False, True, ["ones_f", "bmod"], [bkk])
                self.CP("dve" if w == 0 else "act", self.mod[w][:, j * 512:(j + 1) * 512], bk, [bkk], ["mod%d" % w])
        for w in range(2):
            m = self.mod[w]
            self.STT("dve", m[:, D:2 * D], m[:, D:2 * D], 1.0, gm, ALU.add, ALU.mult, ["mod%d" % w, "gm"], ["mod%d" % w])
            self.STT("dve", m[:, 4 * D:5 * D], m[:, 4 * D:5 * D], 1.0, gf, ALU.add, ALU.mult, ["mod%d" % w, "gf"], ["mod%d" % w])

    def x_src(self, l, t):
        cfg = self.cfg
        if l == 0:
            if t < cfg.NTL:
                return self.I["x"][t * 128:(t + 1) * 128, :]
            return self.I["ctx"][(t - cfg.NTL) * 128:(t - cfg.NTL + 1) * 128, :]
        return self.xcur[t * 128:(t + 1) * 128, :]

    def rstd_of(self, src, n, junk, ssq, rstd, rk, tag, jk="junk"):
        self.ACT(junk, src, AF.Square, rk, [jk, "ssq" + tag], accum=ssq)
        self.ACT(ssq, ssq, AF.Sqrt, ["ssq" + tag], ["ssq" + tag], scale=1.0 / n, bias=EPS)
        self.RCP(rstd, ssq, ["ssq" + tag], ["rstd" + tag])

    def norm_mod(self, xt, xk, w, goff, soff, junk, tmp, out, outk, ssq, rstd, tag, jk="junk", tk="tmp"):
        self.rstd_of(xt, D, junk, ssq, rstd, [xk], tag, jk=jk)
        m = self.mod[w]
        self.STT("dve", tmp, xt, rstd, m[:, goff:goff + D], ALU.mult, ALU.mult, [xk, "rstd" + tag, "mod%d" % w], [tk])
        self.TT("dve", out, tmp, m[:, soff:soff + D], ALU.add, [tk, "mod%d" % w], [outk])

    def transpose8(self, src, srck, bank_bf, bankk, dst, dstk, nblk=8, ident=None, identk="ident_b"):
        ident = self.ident_b if ident is None else ident
        srcks = [srck] if isinstance(srck, str) else list(srck)
        for k in range(nblk):
            self.TR(bank_bf[:, k * 128:(k + 1) * 128], src[:, k * 128:(k + 1) * 128], ident, srcks + [identk], [bankk])
        self.CP("act", dst, bank_bf[:, 0:nblk * 128].rearrange("p (k m) -> p k m", k=nblk), [bankk], [dstk])

    def rope(self, src, srck, H, R, tab, tabk, coff, soff, t1, t2, outs, tag):
        w = R // 4
        C = tab[:, coff:coff + R]
        Sg = tab[:, soff:soff + R].rearrange("p (a b w) -> p a b w", a=2, b=2)
        t1v = t1[:, 0:H * R].rearrange("p (h r) -> p h r", h=H)
        t2v = t2[:, 0:H * R].rearrange("p (h r) -> p h r", h=H)
        self.TT("dve", t1v, src, C.unsqueeze(1).to_broadcast([128, H, R]), ALU.mult, [srck, tabk], ["t1" + tag])
        s5 = src.rearrange("p h (a b w) -> p h a b w", a=2, b=2)
        t5 = t2v.rearrange("p h (a b w) -> p h a b w", a=2, b=2)
        self.TT("dve", t5[:, :, :, 0, :], s5[:, :, :, 1, :], Sg[:, :, 0, :].unsqueeze(1).to_broadcast([128, H, 2, w]),
                ALU.mult, [srck, tabk], ["t2a" + tag])
        self.TT("dve", t5[:, :, :, 1, :], s5[:, :, :, 0, :], Sg[:, :, 1, :].unsqueeze(1).to_broadcast([128, H, 2, w]),
                ALU.mult, [srck, tabk], ["t2b" + tag])
        for (dst, h0, h1, dk) in outs:
            self.TT("dve", dst, t1v[:, h0:h1, :], t2v[:, h0:h1, :], ALU.add, ["t1" + tag, "t2a" + tag, "t2b" + tag], [dk])

    def phase_p1(self, l):
        cfg, A, I = self.cfg, self.A, self.I
        NT, NTL = cfg.NT, cfg.NTL
        win = A.alloc([8, IN_COLS], BF16)
        for k in range(8):
            self.DMA("pool", win[:, k, :], I["w_in"][l, k * 128:(k + 1) * 128, :], (), ["win"])
        wqb = A.alloc([2, 384], BF16)
        self.DMA("pool", wqb, I["w_q_b"][l].rearrange("(k p) n -> p k n", p=128), (), ["wqb"])
        wkvb = A.alloc([512], BF16)
        self.DMA("pool", wkvb, I["w_kv_b"][l], (), ["wkvb"])
        gqa = A.alloc([256], F32)
        gkv = A.alloc([128], F32)
        gd = A.alloc([6, 64], F32)
        self.DMA("sp", gqa, I["g_q_a"][l:l + 1, :].partition_broadcast(128), (), ["gqa"])
        self.DMA("sp", gkv, I["g_kv_a"][l:l + 1, :].partition_broadcast(128), (), ["gkv"])
        for h in range(6):
            src = I["g_q_d"] if h < 4 else I["g_k_d"]
            self.DMA("sp", gd[:, h, :], src[l:l + 1, :].partition_broadcast(128), (), ["gd"])
        xt2 = [A.alloc([D], F32) for _ in range(2)]
        tab2 = [A.alloc([192], F32) for _ in range(2)]
        junk = A.alloc([D], F32)
        tmp = A.alloc([D], F32)
        hb = A.alloc([D], BF16)
        hT = A.alloc([8, 128], BF16)
        ssq = A.alloc([8], F32)
        rstd = A.alloc([8], F32)
        ssqD = A.alloc([8], F32)
        krs = A.alloc([32], F32)
        qf = A.alloc([384], F32)
        kvf = A.alloc([512], F32)
        bdf = A.alloc([512], F32)
        bdfD = A.alloc([512], F32)
        c2f = A.alloc([256], F32)
        rstdD = A.alloc([8], F32)
        cqkv = A.alloc([384], BF16)
        cT = A.alloc([3, 128], BF16)
        qa = A.alloc([4, 96], BF16)
        ka = A.alloc([4, 96], BF16)
        krr = A.alloc([1, 32], F32)
        t1 = A.alloc([512], F32)
        t2 = A.alloc([512], F32)
        sq = A.alloc([384], F32)
        qn = A.alloc([6, 64], F32)
        qkb = A.alloc([8, 64], BF16)
        qkc = A.alloc([512], BF16)
        stA = [A.alloc([8, 128], BF16) for _ in range(2)]
        stP = [A.alloc([4, 128], BF16) for _ in range(2)]
        va = [[A.alloc([4, 128], BF16) for _ in range(2)] for _ in range(4)]
        for n in range(4):
            for b in range(2):
                self.MEMSET("pool", va[n][b], 1.0, ["va%d_%d" % (n, b)])
        T0, T1 = self.bank_bf(0), self.bank_bf(1)
        pA, pBD, pC1, pC2, pQ, pKV = (self.bank(i) for i in (2, 3, 4, 5, 6, 7))

        def load(t):
            b = t % 2
            self.DMA("sp", xt2[b], self.x_src(l, t), (), ["xt%d" % b])
            self.DMA("sp", tab2[b], I["ropetab"][t * 128:(t + 1) * 128, :], (), ["tab%d" % b])

        import os
        KCUT = int(os.environ.get("KCUT", "99"))
        load(0)
        for t in range(NT):
            if t + 1 < NT:
                load(t + 1)
            b = t % 2
            xt, xk, tab, tabk = xt2[b], "xt%d" % b, tab2[b], "tab%d" % b
            w = 0 if t < NTL else 1
            tok = slice(t * 128, (t + 1) * 128)
            self.norm_mod(xt, xk, w, D, 0, junk, tmp, hb, "hb", ssq[:, 0:1], rstd[:, 0:1], "n1")
            self.transpose8(hb, "hb", T0, "T0", hT, "hT")
            if KCUT == 1:
                return
            def proj(dst, dk, c0, c1):
                for k in range(8):
                    self.MM(dst[:, 0:c1 - c0], hT[:, k, :], win[:, k, c0:c1], k == 0, k == 7, ["hT", "win"], [dk])
            proj(pA, "pA", 0, 416)
            proj(pBD, "pBD", 416, 928)
            self.CP("act", bdf, pBD, ["pBD"], ["bdf"])
            proj(pC1, "pC1", 928, 1440)
            proj(pC2, "pC2", 1440, 1696)
            proj(pBD, "pBD", 1696, 2208)
            if KCUT == 2:
                return
            self.ACT(junk[:, 0:256], pA[:, 0:256], AF.Square, ["pA"], ["junk", "ssqA0"], accum=ssq[:, 1:2])
            self.ACT(junk[:, 256:384], pA[:, 256:384], AF.Square, ["pA"], ["junk", "ssqA1"], accum=ssq[:, 2:3])
            self.ACT(ssq[:, 1:2], ssq[:, 1:2], AF.Sqrt, ["ssqA0"], ["ssqA0"], scale=1.0 / 256, bias=EPS)
            self.ACT(ssq[:, 2:3], ssq[:, 2:3], AF.Sqrt, ["ssqA1"], ["ssqA1"], scale=1.0 / 128, bias=EPS)
            self.RCP(rstd[:, 1:3], ssq[:, 1:3], ["ssqA0", "ssqA1"], ["rstdA"])
            self.STT("dve", cqkv[:, 0:256], pA[:, 0:256], rstd[:, 1:2], gqa, ALU.mult, ALU.mult, ["pA", "rstdA", "gqa"], ["cqkv0"])
            self.STT("dve", cqkv[:, 256:384], pA[:, 256:384], rstd[:, 2:3], gkv, ALU.mult, ALU.mult, ["pA", "rstdA", "gkv"], ["cqkv1"])
            for k in range(3):
                self.TR(T1[:, k * 128:(k + 1) * 128], cqkv[:, k * 128:(k + 1) * 128], self.ident_b,
                        ["cqkv0", "cqkv1", "ident_b"], ["T1"])
            self.CP("act", cT, T1[:, 0:384].rearrange("p (k m) -> p k m", k=3), ["T1"], ["cT"])
            if KCUT == 3:
                return
            for k in range(2):
                self.MM(pQ[:, 0:384], cT[:, k, :], wqb[:, k, :], k == 0, k == 1, ["cT", "wqb"], ["pQ"])
            self.MM(pKV, cT[:, 2, :], wkvb, True, True, ["cT", "wkvb"], ["pKV"])
            if KCUT == 4:
                return
            self.CP("act", qf, pQ[:, 0:384], ["pQ"], ["qf"])
            self.CP("act", kvf, pKV, ["pKV"], ["kvf"])
            self.CP("act", krs, pA[:, 384:416], ["pA"], ["krs"])
            pQv = qf.rearrange("p (h r) -> p h r", h=4)
            pKVv = kvf.rearrange("p (h r) -> p h r", h=4)
            self.CP("pool", qa[:, :, 0:64], pQv[:, :, 0:64], ["qf"], ["qa0"])
            if KCUT == 41:
                return
            self.rope(pQv[:, :, 64:96], "qf", 4, 32, tab, tabk, 128, 160, t1, t2, [(qa[:, :, 64:96], 0, 4, "qa1")], "R")
            if KCUT == 42:
                return
            self.rope(krs.unsqueeze(1), "krs", 1, 32, tab, tabk, 128, 160, t1, t2, [(krr, 0, 1, "krr")], "R")
            if KCUT == 5:
                return
            self.CP("pool", ka[:, :, 0:64], pKVv[:, :, 0:64], ["kvf"], ["ka0"])
            self.CP("dve", ka[:, :, 64:96], krr.to_broadcast([128, 4, 32]), ["krr"], ["ka1"])
            vA = va[0][b]
            vAp = vA.rearrange("p (j two) d -> p j two d", two=2)
            pKVp = pKVv.rearrange("p (j two) r -> p j two r", two=2)
            self.CP("pool", vAp[:, :, 0, 0:64], pKVp[:, :, 0, 64:128], ["kvf"], ["va0_%d" % b])
            self.CP("pool", vAp[:, :, 1, 64:128], pKVp[:, :, 1, 64:128], ["kvf"], ["va0_%d" % b])
            if KCUT == 6:
                return
            for h in range(4):
                self.TR(T1[0:96, h * 128:(h + 1) * 128], qa[:, h, :], self.ident_b, ["qa0", "qa1", "ident_b"], ["T1"])
                self.TR(T1[0:96, (4 + h) * 128:(5 + h) * 128], ka[:, h, :], self.ident_b, ["ka0", "ka1", "ident_b"], ["T1"])
            sA = stA[b]
            self.CP("act", sA[0:96], T1[0:96, :].rearrange("p (k m) -> p k m", k=8), ["T1"], ["stA%d" % b])
            if KCUT == 7:
                return
            self.DMA("sp", self.QT[0][:, :, tok], sA[0:96, 0:4, :], ["stA%d" % b], [])
            self.DMA("sp", self.KT[0][:, :, tok], sA[0:96, 4:8, :], ["stA%d" % b], [])
            self.DMA("sp", self.VA[0][:, t, :, :], vA, ["va0_%d" % b], [])
            if KCUT == 8:
                return

            def gqa_v(src, srck, n):
                v = va[n][b]
                vp = v.rearrange("p (g two) d -> p g two d", two=2)
                sv = src[:, 384:512].rearrange("p (g r) -> p g r", g=2)
                self.CP("pool", vp[:, :, 0, 0:64], sv, [srck], ["va%d_%d" % (n, b)])
                self.CP("pool", vp[:, :, 1, 64:128], sv, [srck], ["va%d_%d" % (n, b)])
                return v

            def qk_store(src, srcks, n, tbank, tbk):
                for k in range(4):
                    self.TR(tbank[:, k * 128:(k + 1) * 128], src[:, k * 128:(k + 1) * 128], self.ident_b,
                            list(srcks) + ["ident_b"], [tbk])
                sp_ = stP[n % 2]
                spk = "stP%d" % (n % 2)
                self.CP("act", sp_, tbank[:, 0:512].rearrange("p (k m) -> p k m", k=4), [tbk], [spk])
                self.DMA("sp", self.QT[n][:, :, tok], sp_[:, 0:2, :], [spk], [])
                self.DMA("sp", self.KT[n][:, :, tok], sp_[:, 2:4, :], [spk], [])

            pBv = bdf[:, 0:384].rearrange("p (h r) -> p h r", h=6)
            qkb_d = qkb[:, 4:8, :].rearrange("p (g two) r -> p g two r", two=2)
            self.rope(pBv, "bdf", 6, 64, tab, tabk, 0, 64, t1, t2,
                      [(qkb[:, 0:4, :], 0, 4, "qkb0"), (qkb_d[:, :, 0, :], 4, 6, "qkb1"), (qkb_d[:, :, 1, :], 4, 6, "qkb2")], "R")
            vB = gqa_v(bdf, "bdf", 1)
            qk_store(qkb.rearrange("p h r -> p (h r)"), ["qkb0", "qkb1", "qkb2"], 1, T0, "T0")
            self.DMA("sp", self.VA[1][:, t, :, :], vB, ["va1_%d" % b], [])
            if KCUT == 9:
                return
            self.CP("act", qkc, pC1, ["pC1"], ["qkc"])
            vC = va[2][b]
            vCp = vC.rearrange("p (j two) d -> p j two d", two=2)
            self.CP("act", c2f, pC2[:, 0:256], ["pC2"], ["c2f"])
            pC2p = c2f.rearrange("p (j two r) -> p j two r", two=2, r=64)
            self.CP("pool", vCp[:, :, 0, 0:64], pC2p[:, :, 0, :], ["c2f"], ["va2_%d" % b])
            self.CP("pool", vCp[:, :, 1, 64:128], pC2p[:, :, 1, :], ["c2f"], ["va2_%d" % b])
            qk_store(qkc, ["qkc"], 2, T1, "T1")
            self.DMA("sp", self.VA[2][:, t, :, :], vC, ["va2_%d" % b], [])
            if KCUT == 10:
                return
            self.CP("act", bdfD, pBD, ["pBD"], ["bdfD"])
            pDv = bdfD[:, 0:384].rearrange("p (h r) -> p h r", h=6)
            sqv = sq.rearrange("p (h r) -> p h r", h=6)
            self.ACT(sq, pBD[:, 0:384], AF.Square, ["pBD"], ["sq"])
            self.RED(ssqD[:, 0:6], sqv, ALU.add, ["sq"], ["ssqD"])
            self.ACT(ssqD[:, 0:6], ssqD[:, 0:6], AF.Sqrt, ["ssqD"], ["ssqD"], scale=1.0 / 64, bias=EPS)
            self.RCP(rstdD[:, 0:6], ssqD[:, 0:6], ["ssqD"], ["rstdD"])
            self.TT("dve", qn, pDv, rstdD[:, 0:6].unsqueeze(2).to_broadcast([128, 6, 64]), ALU.mult, ["bdfD", "rstdD"], ["qn"])
            self.TT("dve", qn, qn, gd, ALU.mult, ["qn", "gd"], ["qn"])
            self.rope(qn, "qn", 6, 64, tab, tabk, 0, 64, t1, t2,
                      [(qkb[:, 0:4, :], 0, 4, "qkb0"), (qkb_d[:, :, 0, :], 4, 6, "qkb1"), (qkb_d[:, :, 1, :], 4, 6, "qkb2")], "R")
            vD = gqa_v(bdfD, "bdfD", 3)
            qk_store(qkb.rearrange("p h r -> p (h r)"), ["qkb0", "qkb1", "qkb2"], 3, T0, "T0")
            self.DMA("sp", self.VA[3][:, t, :, :], vD, ["va3_%d" % b], [])

    def phase_p2(self, l):
        cfg, A, I = self.cfg, self.A, self.I
        NT, NTL, T = cfg.NT, cfg.NTL, cfg.T
        LOOK = 3
        groups = [(g * 512, 512, g) for g in range(cfg.NG)]
        if self.with_ctx:
            groups.append((cfg.SEQ, cfg.CTX, None))
        ctx_tiles = list(range(NTL, NT))
        base_mark = A.mark()
        pt = [A.alloc([512], BF16) for _ in range(6)]
        tmpf = [A.alloc([512], F32) for _ in range(4)]
        rden = [A.alloc([512], F32) for _ in range(2)]
        rdt = A.alloc([512], F32)
        ost = [A.alloc([512], BF16) for _ in range(2)]
        qt = [A.alloc([512], BF16) for _ in range(2)]
        esink = A.alloc([4], F32)
        self.DMA("sp", esink, I["sink_b"][l:l + 1, :].partition_broadcast(128), (), ["esink"])
        self.ACT(esink, esink, AF.Exp, ["esink"], ["esink"])
        inner_mark = A.mark()
        cnt = {"pt": 0, "s": 0, "tf": 0, "grp": 0}
        for n in range(4):
            A.release(inner_mark)
            self.S.barrier()
            isA = n == 0
            scale = (96 ** -0.5) if isA else 0.125
            LOOK = 2 if n == 2 else 3
            if isA:
                kt = A.alloc([4, T], BF16)
                self.DMA("sp", kt[0:96], self.KT[0], (), ["kt"])
            else:
                kt = A.alloc([4, T], BF16)
                self.MEMSET("pool", kt, 0.0, ["kt"])
                for hh in range(4):
                    hp = (hh % 2) * 64
                    self.DMA("sp", kt[hp:hp + 64, hh, :], self.KT[n][hp:hp + 64, hh // 2, :], (), ["kt"])
            vat = A.alloc([NT, 4, 128], BF16)
            step = max(1, NT // 4)
            for t0 in range(0, NT, step):
                t1_ = min(NT, t0 + step)
                self.DMA("sp", vat[:, t0:t1_], self.VA[n][:, t0:t1_], (), ["vat"])
            mb = mc = bias = comb = None
            if n == 1:
                mb = A.alloc([6, 512], F32)
                self.DMA("sp", mb, I["maskb"].rearrange("r p q -> p r q"), (), ["mb"])
            if n == 2:
                mc = A.alloc([self.ncase * 8, 512], BF16)
                for ci in range(self.ncase):
                    self.DMA("pool", mc[:, ci * 8:(ci + 1) * 8, :], I["maskc"][ci].rearrange("r p q -> p r q"), (), ["mc"])
                bias = A.alloc([8, 512], F32)
                comb1 = A.alloc([8, 512], F32)
                comb = [comb1, comb1]
            G_list = []
            for h in range(4):
                for gi, (q0, nq, g) in enumerate(groups):
                    tiles = []
                    if g is None:
                        tiles = [(t_, None) for t_ in ctx_tiles]
                    elif n in (0, 3):
                        tiles = [(t_, None) for t_ in range(NT)]
                    elif n == 1:
                        for j in range(4 * g - 1, 4 * g + 5):
                            if 0 <= j < NTL:
                                tiles.append((j, ("mb", j - 4 * g + 1)))
                        tiles += [(t_, None) for t_ in ctx_tiles]
                    else:
                        for r in self.valid_c[g]:
                            tiles.append((4 * g + r - 2, ("comb", r)))
                        tiles += [(t_, None) for t_ in ctx_tiles]
                    gid = cnt["grp"]
                    cnt["grp"] += 1
                    G_list.append(dict(h=h, q0=q0, nq=nq, g=g, tiles=tiles, gid=gid, first_of_head=(gi == 0)))
            items = [(G, i) for G in G_list for i in range(len(G["tiles"]))]
            state = {}

            def emit_S(idx):
                G, i = items[idx]
                h, q0, nq, gid = G["h"], G["q0"], G["nq"], G["gid"]
                ph = (h % 2) * 64
                vr = slice(ph, ph + 64)
                qb = gid % 2
                qtb, qk_ = qt[qb], "qt%d" % qb
                if i == 0:
                    if n == 2 and G["first_of_head"]:
                        self.DMA("sp", bias, I["biasg"][l, h].rearrange("r p q -> p r q"), (), ["bias"])
                    if n == 2 and G["first_of_head"]:
                        state["built"] = None
                    if n == 2 and G["g"] is not None and state.get("built") != (self.case_of_g[G["g"]], tuple(self.valid_c[G["g"]])):
                        state["built"] = (self.case_of_g[G["g"]], tuple(self.valid_c[G["g"]]))
                        ci = self.case_of_g[G["g"]]
                        cb_ = comb[gid % 2]
                        for r in self.valid_c[G["g"]]:
                            self.TT("pool", cb_[:, r, :], bias[:, r, :], mc[:, ci * 8 + r, :], ALU.add,
                                    ["bias", "mc"], ["comb_%d" % r])
                    if isA:
                        self.DMA("sp", qtb[0:96, 0:nq], self.QT[0][:, h, q0:q0 + nq], (), [qk_])
                    else:
                        self.DMA("sp", qtb[:, 0:nq], self.QT[n][:, h // 2, q0:q0 + nq], (), [qk_])
                q_ap = qtb[0:96, 0:nq] if isA else qtb[:, 0:nq]
                tk = G["tiles"][i][0]
                sb_ = cnt["s"] % 4
                cnt["s"] += 1
                psS = self.bank(sb_)
                psk = "psS%d" % sb_
                if isA:
                    k_ap = kt[0:96, h, tk * 128:(tk + 1) * 128]
                else:
                    k_ap = kt[:, h, tk * 128:(tk + 1) * 128]
                self.MM(psS[:, 0:nq], k_ap, q_ap, True, True, ["kt", qk_], [psk])
                state[idx] = (psS, psk)

            for j in range(min(LOOK, len(items))):
                emit_S(j)
            for idx, (G, i) in enumerate(items):
                if idx + LOOK < len(items):
                    emit_S(idx + LOOK)
                h, q0, nq, gid = G["h"], G["q0"], G["nq"], G["gid"]
                ph = (h % 2) * 64
                vr = slice(ph, ph + 64)
                dr = slice(64 - ph, 128 - ph)
                psS, psk = state.pop(idx)
                tk, tabinfo = G["tiles"][i]
                ob = gid % 2
                psO = self.bank(6 + ob)
                pok = "psO%d" % ob
                last = len(G["tiles"]) - 1
                pb_ = cnt["pt"] % 6
                cnt["pt"] += 1
                ptb, ptk = pt[pb_], "pt%d" % pb_
                if tabinfo is None:
                    self.ACT(ptb[:, 0:nq], psS[:, 0:nq], AF.Exp, [psk], [ptk], scale=scale)
                else:
                    if tabinfo[0] == "mb":
                        tabv, tabk = mb[:, tabinfo[1], :], "mb"
                    else:
                        tabv, tabk = comb[gid % 2][:, tabinfo[1], :], "comb_%d" % tabinfo[1]
                    fb = cnt["tf"] % 4
                    cnt["tf"] += 1
                    self.STT("dve", tmpf[fb][:, 0:nq], psS[:, 0:nq], scale, tabv[:, 0:nq], ALU.mult, ALU.add,
                             [psk, tabk], ["tmpf%d" % fb])
                    self.ACT(ptb[:, 0:nq], tmpf[fb][:, 0:nq], AF.Exp, ["tmpf%d" % fb], [ptk])
                self.MM(psO[:, 0:nq], vat[:, tk, h, :], ptb[:, 0:nq], i == 0, i == last, ["vat", ptk], [pok])
                if i == min(2, last) and state.get("pending") is not None:
                    state.pop("pending")()
                if i == last:
                  def epilogue(n=n, h=h, gid=gid, nq=nq, q0=q0, vr=vr, dr=dr, psO=psO, pok=pok):
                    rb = gid % 2
                    rd, rdk = rden[rb], "rden%d" % rb
                    if n in (1, 2):
                        if n == 1:
                            self.ACT(rdt[dr, 0:nq], psO[dr, 0:nq], AF.Ln, [pok, "esink"], ["rdt"], bias=esink[dr, h:h + 1])
                        else:
                            self.ACT(rdt[dr, 0:nq], psO[dr, 0:nq], AF.Ln, [pok], ["rdt"])
                        self.ACT(rdt[dr, 0:nq], rdt[dr, 0:nq], AF.Exp, ["rdt"], ["rdt"], scale=-1.0)
                        self.CP("dve", rd[vr, 0:nq], rdt[dr, 0:nq], ["rdt"], [rdk])
                    else:
                        self.RCP(rd[vr, 0:nq], psO[dr, 0:nq], [pok], [rdk])
                    osb, osk = ost[rb], "ost%d" % rb
                    self.TT("dve", osb[vr, 0:nq], psO[vr, 0:nq], rd[vr, 0:nq], ALU.mult, [pok, rdk], [osk])
                    self.DMA("sp", self.OT[n * 2 + h // 2][vr, q0:q0 + nq], osb[vr, 0:nq], [osk], [])
                  if state.get("pending") is not None:
                      state.pop("pending")()
                  state["pending"] = epilogue
            if state.get("pending") is not None:
                state.pop("pending")()
        A.release(base_mark)

    def phase_p3(self, l):
        cfg, A, I = self.cfg, self.A, self.I
        NT, NTL = cfg.NT, cfg.NTL
        tiles = list(range(NT)) if self.with_ctx else list(range(NTL))
        wg = A.alloc([32, D], BF16)
        for n in range(4):
            for k in range(8):
                self.DMA("pool", wg[:, n * 8 + k, :], I["w_gate"][l, n, k * 128:(k + 1) * 128, :], (), ["wg"])
        bg = A.alloc([4 * D], BF16)
        self.DMA("pool", bg[0:1, :], I["b_gate"][l:l + 1, :], (), ["bg"])
        wbr = A.alloc([8, D], BF16)
        self.DMA("pool", wbr, I["w_branch"][l].rearrange("n (c p) m -> p (n c) m", p=128), (), ["wbr"])
        wo = A.alloc([8, D], BF16)
        self.DMA("pool", wo, I["w_out"][l].rearrange("(k p) m -> p k m", p=128), (), ["wo"])
        wr = A.alloc([8, 36], F32)
        self.DMA("sp", wr[:, :, 0:4], I["w_group"][l].rearrange("(k p) n -> p k n", p=128), (), ["wr"])
        self.DMA("sp", wr[:, :, 4:36], I["w_router"][l].rearrange("(k p) n -> p k n", p=128), (), ["wr"])
        br = A.alloc([36], F32)
        self.DMA("sp", br[0:1, 0:4], I["b_group"][l:l + 1, :], (), ["br"])
        self.DMA("sp", br[0:1, 4:36], I["b_router"][l:l + 1, :], (), ["br"])
        xt2 = [A.alloc([D], F32) for _ in range(3)]
        ot2 = [A.alloc([8, 128], BF16) for _ in range(2)]
        tmpA = A.alloc([D], F32)
        tmpB = A.alloc([D], F32)
        hb = A.alloc([D], BF16)
        hT = A.alloc([8, 128], BF16)
        gate = A.alloc([D], BF16)
        tmpn = A.alloc([D], BF16)
        ypre = A.alloc([D], F32)
        ypb = A.alloc([D], BF16)
        yT = A.alloc([8, 128], BF16)
        h2Tf = A.alloc([8, 128], F32)
        h2b = A.alloc([D], BF16)
        oh12b = A.alloc([64], BF16)
        csb = A.alloc([64], F32)
        tr0 = A.alloc([32], F32)
        tr1 = A.alloc([32], F32)
        self.MEMSET("dve", self.base, 0.0, ["base"])
        ssq = A.alloc([4], F32)
        rstd = A.alloc([4], F32)
        lg = A.alloc([36], F32)
        sm = A.alloc([16], F32)
        ohg = A.alloc([4], F32)
        pen = A.alloc([4], F32)
        em = A.alloc([32], F32)
        em2 = A.alloc([32], F32)
        oh1 = A.alloc([32], F32)
        oh2 = A.alloc([32], F32)
        c1 = A.alloc([32], F32)
        j4 = A.alloc([4], F32)
        PT = self.bank_bf(0)
        PB, PY = self.banks[2], self.banks[3]
        pR = self.bank(1)
        cntu = [0]

        def load(i):
            t = tiles[i]
            b = i % 2
            self.DMA("sp", xt2[i % 3], self.x_src(l, t), (), ["xt%d" % (i % 3)])
            self.DMA("sp", ot2[b], self.OT[:, :, t * 128:(t + 1) * 128].rearrange("b p t -> p b t"), (), ["ot%d" % b])

        def norm1(i):
            t = tiles[i]
            w = 0 if t < NTL else 1
            self.norm_mod(xt2[i % 3], "xt%d" % (i % 3), w, D, 0, hb, tmpA, hb, "hb", ssq[:, 0:1], rstd[:, 0:1], "n1",
                          jk="hb", tk="tmpA")

        def stageA(i, gen=None):
            t = tiles[i]
            b = i % 2
            xt, xk, ot, otk = xt2[i % 3], "xt%d" % (i % 3), ot2[b], "ot%d" % b
            w = 0 if t < NTL else 1
            m = self.mod[w]
            mk = "mod%d" % w
            tok = slice(t * 128, (t + 1) * 128)
            tmp = tmpA
            self.transpose8(hb, "hb", PT, "PT", hT, "hT")
            for n in range(4):
                for half in range(2):
                    cs = slice(half * 512, (half + 1) * 512)
                    r_ = cntu[0] % 2
                    cntu[0] += 1
                    PGh, pgk = self.bank(2 + r_), "PG%d" % r_
                    PBh, pbk = self.bank(4 + r_), "PB%d" % r_
                    for k in range(8):
                        self.MM(PGh, hT[:, k, :], wg[:, n * 8 + k, cs], k == 0, False, ["hT", "wg"], [pgk])
                    self.MM(PGh, self.ones_b[0:1, :], bg[0:1, n * D + half * 512:n * D + (half + 1) * 512], False, True,
                            ["ones_b", "bg"], [pgk])
                    for c in range(2):
                        self.MM(PBh, ot[:, n * 2 + c, :], wbr[:, n * 2 + c, cs], c == 0, c == 1, [otk, "wbr"], [pbk])
                    gh, ghk = gate[:, r_ * 512:(r_ + 1) * 512], "gate%d" % r_
                    self.ACT(gh, PGh, AF.Sigmoid, [pgk], [ghk])
                    yk = "ypre%d" % half
                    if n == 0:
                        self.TT("dve", ypre[:, cs], gh, PBh, ALU.mult, [ghk, pbk], [yk])
                    else:
                        tn, tnk = tmpn[:, r_ * 512:(r_ + 1) * 512], "tn%d" % r_
                        self.TT("dve", tn, gh, PBh, ALU.mult, [ghk, pbk], [tnk])
                        if n < 3:
                            self.TT("pool", ypre[:, cs], ypre[:, cs], tn, ALU.add, [yk, tnk], [yk])
                        else:
                            self.TT("pool", ypb[:, cs], ypre[:, cs], tn, ALU.add, [yk, tnk], ["ypb%d" % half])
                    u_ = n * 2 + half
                    if u_ in (1, 3, 5, 7) and gen is not None:
                        next(gen, None)
                    if u_ == 3 and i + 1 < len(tiles):
                        norm1(i + 1)
            if i + 2 < len(tiles):
                load(i + 2)
            self.transpose8(ypb, ["ypb0", "ypb1"], PT, "PT", yT, "yT")
            for half in range(2):
                cs = slice(half * 512, (half + 1) * 512)
                for k in range(8):
                    self.MM(PY[:, cs], yT[:, k, :], wo[:, k, cs], k == 0, k == 7, ["yT", "wo"], ["PY"])
            self.TT("dve", tmp, PY[:, :], m[:, 2 * D:3 * D], ALU.mult, ["PY", mk], ["tmpA"])
            self.TT("pool", xt, tmp, xt, ALU.add, ["tmpA", xk], [xk])
            self.DMA("sp", self.xcur[tok, :], xt, [xk], [])
            if gen is not None:
                next(gen, None)

        def stageB(i):
            t = tiles[i]
            xt, xk = xt2[i % 3], "xt%d" % (i % 3)
            w = 0 if t < NTL else 1
            tok = slice(t * 128, (t + 1) * 128)
            tmp = tmpB
            h2 = tmp
            self.norm_mod(xt, xk, w, 4 * D, 3 * D, h2b, tmp, h2, "tmpB", ssq[:, 1:2], rstd[:, 1:2], "n2", jk="h2b", tk="tmpB")
            yield
            for k in range(8):
                self.TR(PB[:, k * 128:(k + 1) * 128], h2[:, k * 128:(k + 1) * 128], self.ident_f, ["tmpB", "ident_f"], ["PB0", "PB1"])
            self.CP("act", h2Tf, PB[:, :].rearrange("p (k m) -> p k m", k=8), ["PB0", "PB1"], ["h2Tf"])
            self.CP("pool", h2b, h2, ["tmpB"], ["h2b"])
            self.DMA("sp", self.H2[tok, :], h2b, ["h2b"], [])
            yield
            for k in range(8):
                self.MM(pR[:, 0:36], h2Tf[:, k, :], wr[:, k, :], k == 0, False, ["h2Tf", "wr"], ["pR"])
            self.MM(pR[:, 0:36], self.ones_f[0:1, :], br[0:1, :], False, True, ["ones_f", "br"], ["pR"])
            self.CP("dve", lg, pR[:, 0:36], ["pR"], ["lg"])
            gl = lg[:, 0:4]
            el = lg[:, 4:36].rearrange("p (g e) -> p g e", g=4)
            gmax, negmax, se, gw, m1, m2, dd, w1, w1g, w2g = (sm[:, j:j + 1] for j in range(10))
            self.RED(gmax, gl, ALU.max, ["lg"], ["gmax"])
            self.TS("dve", ohg, gl, gmax, None, ALU.is_equal, None, ["lg", "gmax"], ["ohg"])
            self.TS("dve", negmax, gmax, -1.0, None, ALU.mult, None, ["gmax"], ["negmax"])
            self.ACT(j4, gl, AF.Exp, ["lg", "negmax"], ["j4", "se"], bias=negmax, accum=se)
            self.RCP(gw, se, ["se"], ["gw"])
            self.TS("dve", pen, ohg, -1.0, 1e9, ALU.add, ALU.mult, ["ohg"], ["pen"])
            emv = em.rearrange("p (g e) -> p g e", g=4)
            self.TT("dve", emv, el, pen.unsqueeze(2).to_broadcast([128, 4, 8]), ALU.add, ["lg", "pen"], ["em"])
            self.RED(m1, em, ALU.max, ["em"], ["m1"])
            self.TS("dve", oh1, em, m1, None, ALU.is_equal, None, ["em", "m1"], ["oh1"])
            self.STT("dve", em2, oh1, -2e9, em, ALU.mult, ALU.add, ["oh1", "em"], ["em2"])
            self.RED(m2, em2, ALU.max, ["em2"], ["m2"])
            self.TS("dve", oh2, em2, m2, None, ALU.is_equal, None, ["em2", "m2"], ["oh2"])
            yield
            self.TT("dve", dd, m2, m1, ALU.subtract, ["m1", "m2"], ["dd"])
            self.ACT(dd, dd, AF.Exp, ["dd"], ["dd"])
            self.TS("dve", dd, dd, 1.0, None, ALU.add, None, ["dd"], ["dd"])
            self.RCP(w1, dd, ["dd"], ["w1"])
            self.TT("dve", w1g, w1, gw, ALU.mult, ["w1", "gw"], ["w1g"])
            self.TT("dve", w2g, gw, w1g, ALU.subtract, ["w1g", "gw"], ["w2g"])
            rt, base, iota = self.rt, self.base, self.iota
            pRK = self.bank(1)[:, 64:192]
            self.CP("pool", oh12b[:, 0:32], oh1, ["oh1"], ["oh12b0"])
            self.CP("pool", oh12b[:, 32:64], oh2, ["oh2"], ["oh12b1"])
            yield
            self.MM(pRK[:, 0:64], self.ltb, oh12b, True, True, ["ltb", "oh12b0", "oh12b1"], ["pRK"])
            self.MM(pRK[:, 64:128], self.onesbb, oh12b, True, True, ["onesbb", "oh12b0", "oh12b1"], ["pRK"])
            self.CP("dve", csb, pRK[:, 64:128], ["pRK"], ["cs"])
            self.TT("dve", tr0, pRK[:, 0:32], base, ALU.add, ["pRK", "base"], ["tr0"])
            self.TT("dve", tr0, tr0, oh1, ALU.mult, ["tr0", "oh1"], ["tr0"])
            self.RED(rt[:, t, 4:5], tr0, ALU.add, ["tr0"], ["rt"])
            self.TT("dve", tr1, pRK[:, 32:64], base, ALU.add, ["pRK", "base"], ["tr1"])
            self.TT("dve", tr1, tr1, csb[:, 0:32], ALU.add, ["tr1", "cs"], ["tr1"])
            self.TT("dve", tr1, tr1, oh2, ALU.mult, ["tr1", "oh2"], ["tr1"])
            self.RED(rt[:, t, 5:6], tr1, ALU.add, ["tr1"], ["rt"])
            self.TT("dve", base, base, csb[:, 0:32], ALU.add, ["base", "cs"], ["base"])
            self.TT("dve", base, base, csb[:, 32:64], ALU.add, ["base", "cs"], ["base"])
            self.TT("dve", tr0, oh1, iota, ALU.mult, ["oh1", "iota", "tr0"], ["tr0"])
            self.RED(rt[:, t, 0:1], tr0, ALU.add, ["tr0"], ["rt"])
            self.TT("dve", tr1, oh2, iota, ALU.mult, ["oh2", "iota", "tr1"], ["tr1"])
            self.RED(rt[:, t, 1:2], tr1, ALU.add, ["tr1"], ["rt"])
            self.CP("dve", rt[:, t, 2:3], w1g, ["w1g"], ["rt"])
            self.CP("dve", rt[:, t, 3:4], w2g, ["w2g"], ["rt"])

        ntl_ = len(tiles)
        load(0)
        if ntl_ > 1:
            load(1)
        norm1(0)
        for i in range(ntl_):
            gen = stageB(i - 1) if i > 0 else None
            stageA(i, gen)
            if gen is not None:
                for _ in gen:
                    pass
        for _ in stageB(ntl_ - 1):
            pass

    def phase_p4(self, l):
        cfg, A, I = self.cfg, self.A, self.I
        NT, NTL = cfg.NT, cfg.NTL
        last = l == cfg.DEPTH - 1
        tiles = list(range(NT)) if self.with_ctx else list(range(NTL))
        ntl = len(tiles)
        NBLK = 2 * ntl + 32
        assert NBLK <= NBLK_MAX
        rt, base, iota = self.rt, self.base, self.iota
        m0 = A.mark()
        nbi = A.alloc([32], I32)
        pcnt = A.alloc([32], F32)
        pa = A.alloc([32], F32)
        pb = A.alloc([32], F32)
        bs = A.alloc([32], F32)
        oh3 = A.alloc([NT, 32], F32)
        sl = A.alloc([NT, 2], F32)
        cmp = A.alloc([NBLK_MAX, 32], F32)
        ble = A.alloc([NBLK_MAX], F32)
        zt = A.alloc([8192], BF16)
        h2t = [A.alloc([D], BF16) for _ in range(2)]
        self.MEMSET("pool", zt, 0.0, ["zt"])
        xsz = self.XS.rearrange("(p b) f -> p (b f)", p=128)
        tot = NBLK_MAX * D
        for c0 in range(0, tot, 8192):
            c1_ = min(tot, c0 + 8192)
            self.DMA("sp", xsz[:, c0:c1_], zt[:, 0:c1_ - c0], ["zt"], ["XS"])
        self.TS("dve", pcnt, base, 1.0 / 128, 0.496, ALU.mult, ALU.add, ["base"], ["pcnt"])
        self.CP("dve", nbi, pcnt, ["pcnt"], ["nbi"])
        self.CP("dve", pcnt, nbi, ["nbi"], ["pcnt"])
        self.TS("dve", pcnt, pcnt, 128.0, None, ALU.mult, None, ["pcnt"], ["pcnt"])
        self.CP("dve", pa, pcnt, ["pcnt"], ["pa"])
        src, dst, sk, dk = pa, pb, "pa", "pb"
        for sft in (1, 2, 4, 8, 16):
            self.CP("dve", dst[:, 0:sft], src[:, 0:sft], [sk], [dk])
            self.TT("dve", dst[:, sft:32], src[:, sft:32], src[:, 0:32 - sft], ALU.add, [sk], [dk])
            src, dst, sk, dk = dst, src, dk, sk
        pe, pek = src, sk
        self.TT("dve", bs, pe, pcnt, ALU.subtract, [pek, "pcnt"], ["bs"])
        for k in range(2):
            self.TT("dve", oh3, iota.unsqueeze(1).to_broadcast([128, NT, 32]),
                    rt[:, :, k:k + 1].to_broadcast([128, NT, 32]), ALU.is_equal, ["iota", "rt"], ["oh3"])
            self.TT("dve", oh3, oh3, bs.unsqueeze(1).to_broadcast([128, NT, 32]), ALU.mult, ["oh3", "bs"], ["oh3"])
            self.RED(sl[:, :, k], oh3, ALU.add, ["oh3"], ["sl%d" % k])
            self.TT("dve", sl[:, :, k], sl[:, :, k], rt[:, :, 4 + k], ALU.add, ["sl%d" % k, "rt"], ["sl%d" % k])
        self.CP("dve", self.slot_i, sl, ["sl0", "sl1"], ["slot_i"])
        self.TT("dve", cmp, self.thr.unsqueeze(2).to_broadcast([128, NBLK_MAX, 32]),
                pe.unsqueeze(1).to_broadcast([128, NBLK_MAX, 32]), ALU.is_ge, ["thr", pek], ["cmp"])
        self.RED(ble, cmp, ALU.add, ["cmp"], ["ble"])
        self.TS("dve", ble, ble, 31.0, 128.0, ALU.min, ALU.mult, ["ble"], ["ble"])
        self.TS("dve", ble, ble, self.pidx[:, 0:1], float(l * NE * 128), ALU.add, ALU.add, ["ble", "pidx"], ["ble"])
        self.CP("dve", self.widx, ble, ["ble"], ["widx"])
        for i, t in enumerate(tiles):
            hb, hk = h2t[i % 2], "h2t%d" % (i % 2)
            self.DMA("sp", hb, self.H2[t * 128:(t + 1) * 128, :], (), [hk])
            for k in range(2):
                self.S.add("pool", (lambda hb=hb, t=t, k=k: (lambda e: e.indirect_dma_start(
                    out=self.XS[:, :], out_offset=bass.IndirectOffsetOnAxis(ap=self.slot_i[:, t, k:k + 1], axis=0),
                    in_=hb, in_offset=None)))(), [hk, "slot_i", "XS"], ["XSs"], dma=True)
        self.S.barrier()
        A.release(m0)
        NWR = 4
        w13 = [A.alloc([8, 512], BF16) for _ in range(NWR)]
        w2 = [A.alloc([2, D], BF16) for _ in range(NWR)]
        xs = [A.alloc([D], BF16) for _ in range(NWR)]
        xsT = [A.alloc([8, 128], BF16) for _ in range(2)]
        sb = [A.alloc([256], F32) for _ in range(2)]
        ab = [A.alloc([256], BF16) for _ in range(2)]
        aT = [A.alloc([2, 128], BF16) for _ in range(2)]
        ys = [A.alloc([D], F32) for _ in range(2)]
        w13src = I["w13r"].rearrange("l r f -> (l r) f")
        w2src = I["w2r"].rearrange("l r f -> (l r) f")
        def wload(b):
            wr_ = b % NWR
            self.S.add("pool", (lambda b=b, wr_=wr_: (lambda e: e.indirect_dma_start(
                out=w13[wr_].rearrange("p k f -> p (k f)"), out_offset=None, in_=w13src,
                in_offset=bass.IndirectOffsetOnAxis(ap=self.widx[:, b:b + 1], axis=0))))(), ["widx"], ["w13_%d" % wr_], dma=True)
            self.S.add("pool", (lambda b=b, wr_=wr_: (lambda e: e.indirect_dma_start(
                out=w2[wr_].rearrange("p c f -> p (c f)"), out_offset=None, in_=w2src,
                in_offset=bass.IndirectOffsetOnAxis(ap=self.widx[:, b:b + 1], axis=0))))(), ["widx"], ["w2_%d" % wr_], dma=True)
            self.DMA("sp", xs[wr_], self.XS[b * 128:(b + 1) * 128, :], (), ["xsw%d" % wr_])

        for b in range(min(NWR - 1, NBLK)):
            wload(b)
        for b in range(NBLK):
            if b + NWR - 1 < NBLK:
                wload(b + NWR - 1)
            r = b % 2
            wr_ = b % NWR
            PTx = self.bank_bf(r)
            ptxk = "PTx%d" % r
            psH = self.bank(2 + r)
            phk = "psH%d" % r
            PTa = self.bank_bf(4)
            psO = self.banks[3]
            for k in range(8):
                self.TR(PTx[:, k * 128:(k + 1) * 128], xs[wr_][:, k * 128:(k + 1) * 128], self.ident_b, ["xsw%d" % wr_, "ident_b"], [ptxk])
            self.CP("act", xsT[r], PTx[:, :].rearrange("p (k m) -> p k m", k=8), [ptxk], ["xsT%d" % r])
            for k in range(8):
                self.MM(psH, xsT[r][:, k, :], w13[wr_][:, k, :], k == 0, k == 7, ["xsT%d" % r, "w13_%d" % wr_], [phk])
            self.ACT(sb[r], psH[:, 0:256], AF.Silu, [phk], ["sb%d" % r])
            self.TT("dve", ab[r], sb[r], psH[:, 256:512], ALU.mult, ["sb%d" % r, phk], ["ab%d" % r])
            for c in range(2):
                self.TR(PTa[:, c * 128:(c + 1) * 128], ab[r][:, c * 128:(c + 1) * 128], self.ident_b, ["ab%d" % r, "ident_b"], ["PTa"])
            self.CP("act", aT[r], PTa[:, 0:256].rearrange("p (c m) -> p c m", c=2), ["PTa"], ["aT%d" % r])
            for half in range(2):
                cs_ = slice(half * 512, (half + 1) * 512)
                for c in range(2):
                    self.MM(psO[:, cs_], aT[r][:, c, :], w2[wr_][:, c, cs_], c == 0, c == 1, ["aT%d" % r, "w2_%d" % wr_], ["psO"])
            self.CP("dve", ys[r], psO[:, :], ["psO"], ["ys%d" % r])
            self.DMA("sp", self.YS[b * 128:(b + 1) * 128, :], ys[r], ["ys%d" % r], [])
        self.S.barrier()
        A.release(m0)
        y0 = [A.alloc([D], F32) for _ in range(2)]
        y1 = [A.alloc([D], F32) for _ in range(2)]
        xt2 = [A.alloc([D], F32) for _ in range(2)]
        tmp = A.alloc([D], F32)
        junk = A.alloc([D], BF16)
        ssq = A.alloc([2], F32)
        rstd = A.alloc([2], F32)
        if last:
            self.gfin = A.alloc([D], F32)
            self.DMA("sp", self.gfin, I["g_final"].partition_broadcast(128), (), ["gfin"])
        for i, t in enumerate(tiles):
            b = i % 2
            w = 0 if t < NTL else 1
            tok = slice(t * 128, (t + 1) * 128)
            for k, yb in ((0, y0[b]), (1, y1[b])):
                self.S.add("pool", (lambda yb=yb, t=t, k=k: (lambda e: e.indirect_dma_start(
                    out=yb, out_offset=None, in_=self.YS[:, :],
                    in_offset=bass.IndirectOffsetOnAxis(ap=self.slot_i[:, t, k:k + 1], axis=0))))(), ["slot_i"], ["y%d_%d" % (k, b)], dma=True)
            self.DMA("sp", xt2[b], self.xcur[tok, :], (), ["xt%d" % b])
            self.TS("dve", tmp, y0[b], rt[:, t, 2:3], None, ALU.mult, None, ["y0_%d" % b, "rt"], ["tmp"])
            self.STT("dve", tmp, y1[b], rt[:, t, 3:4], tmp, ALU.mult, ALU.add, ["y1_%d" % b, "rt", "tmp"], ["tmp"])
            self.TT("pool", tmp, tmp, self.mod[w][:, 5 * D:6 * D], ALU.mult, ["tmp", "mod%d" % w], ["tmp"])
            self.TT("pool", xt2[b], tmp, xt2[b], ALU.add, ["tmp", "xt%d" % b], ["xt%d" % b])
            if not last:
                self.DMA("sp", self.xcur[tok, :], xt2[b], ["xt%d" % b], [])
            else:
                self.rstd_of(xt2[b], D, junk, ssq[:, 0:1], rstd[:, 0:1], ["xt%d" % b], "f")
                self.STT("dve", xt2[b], xt2[b], rstd[:, 0:1], self.gfin, ALU.mult, ALU.mult, ["xt%d" % b, "rstdf", "gfin"], ["xt%d" % b])
                o = self.DMA("sp", self.out[tok, :], xt2[b], ["xt%d" % b], [])
                self.S.out_ops.append(o)


_CACHE = {}


def _get_prog(cfg_key):
    if cfg_key not in _CACHE:
        cfg = Cfg(*cfg_key)
        p = Prog(cfg)
        p.build()
        _CACHE[cfg_key] = p
    return _CACHE[cfg_key]


def make_in_maps(prog, inputs, ncores):
    cfg = prog.cfg
    f = lambda a: np.ascontiguousarray(np.asarray(a, dtype=np.float32))
    shared = {}
    for k in ("w_mod", "b_mod", "g_norm_mix", "w_in", "g_q_a", "w_q_b", "g_kv_a", "w_kv_b", "sink_b", "g_q_d", "g_k_d",
              "w_gate", "w_branch", "w_out", "g_norm_ffn", "w_group", "b_group", "w_router", "b_router"):
        shared[k] = f(inputs[k])
    L = cfg.DEPTH
    shared["b_gate"] = f(inputs["b_gate"]).reshape(L, 4 * D)
    shared["g_final"] = f(inputs["g_final"]).reshape(1, D)
    shared["c_ctx"] = f(inputs["c_ctx"]).reshape(1, D)
    rpb = f(inputs["rpb_c"])
    shared["biasg"] = np.ascontiguousarray(rpb[:, :, prog.DR, prog.DC[None]])
    w13 = np.concatenate([f(inputs["w_ff1"]), f(inputs["w_ff3"])], axis=-1)
    w13 = w13.reshape(L, NE, 8, 128, 512).transpose(0, 1, 3, 2, 4)
    shared["w13r"] = np.ascontiguousarray(w13).reshape(L, NE * 128, 4096)
    w2 = f(inputs["w_ff2"]).reshape(L, NE, 2, 128, D).transpose(0, 1, 3, 2, 4)
    shared["w2r"] = np.ascontiguousarray(w2).reshape(L, NE * 128, 2048)
    shared["iota32"] = np.arange(32, dtype=np.float32).reshape(1, 32)
    shared["ltri"] = np.triu(np.ones((128, 128), np.float32), k=1)
    shared["pidx"] = np.arange(128, dtype=np.float32).reshape(128, 1)
    shared["thr"] = (128.0 * np.arange(NBLK_MAX, dtype=np.float32)).reshape(1, NBLK_MAX)
    shared["ident"] = np.eye(128, dtype=np.float32)
    shared["ropetab"] = rope_tab(cfg)
    shared["maskb"] = mask_b()
    shared["maskc"] = prog.maskc_np
    x = f(inputs["x"])
    c = f(inputs["c"])
    ctx = f(inputs["ctx"])
    maps = []
    for b in range(ncores):
        m = dict(shared)
        m["x"] = x[b]
        m["c"] = c[b:b + 1]
        m["ctx"] = ctx[b]
        maps.append(m)
    return maps


def kernel(**inputs):
    x = np.asarray(inputs["x"])
    B, SEQ, _ = x.shape
    CTX = np.asarray(inputs["ctx"]).shape[1]
    DEPTH = np.asarray(inputs["w_mod"]).shape[0]
    prog = _get_prog((SEQ, CTX, DEPTH))
    maps = make_in_maps(prog, inputs, B)
    res = run_bass_kernel_spmd(prog.nc, maps, core_ids=list(range(B)))
    out = np.stack([np.asarray(r["out"], dtype=np.float32) for r in res.results], axis=0)
    return out
```

```python
import contextlib
import numpy as np
import concourse.bass as bass
import concourse.mybir as mybir
from concourse.bass_utils import run_bass_kernel_spmd

F32 = mybir.dt.float32
BF16 = mybir.dt.bfloat16
AF = mybir.ActivationFunctionType
ALU = mybir.AluOpType
AX = mybir.AxisListType
I32 = mybir.dt.int32
NBLK_MAX = 100

SAME_ENGINE_SYNC = True
NDMA_SEMS = 10
import os
MAXOPS = int(os.environ.get('KMAXOPS', '100000000'))
D = 1024
EPS = 1e-6
NEG = -30000.0
IN_COLS = 2208
NE = 32


class Op:
    __slots__ = ("eng", "fn", "deps", "flag", "semv", "sem", "is_dma")

    def __init__(self, eng, fn, is_dma):
        self.eng = eng
        self.fn = fn
        self.deps = []
        self.flag = False
        self.semv = 0
        self.sem = None
        self.is_dma = is_dma


class Sched:
    ENGS = ("pe", "act", "dve", "pool", "sp")

    def __init__(self):
        self.ops = {e: [] for e in self.ENGS}
        self.last_w = {}
        self.readers = {}
        self.pending_barrier = {e: None for e in self.ENGS}
        self.out_ops = []

    def add(self, eng, fn, reads=(), writes=(), dma=False):
        op = Op(eng, fn, dma)
        self.nadd = getattr(self, "nadd", 0) + 1
        if self.nadd > MAXOPS:
            return op
        deps = {}
        raw = set()
        for k in reads:
            w = self.last_w.get(k)
            if w is not None:
                deps[id(w)] = w
                raw.add(id(w))
        for k in writes:
            w = self.last_w.get(k)
            if w is not None:
                deps[id(w)] = w
            for r in self.readers.get(k, ()):
                deps[id(r)] = r
        for k in reads:
            self.readers.setdefault(k, []).append(op)
        for k in writes:
            self.last_w[k] = op
            self.readers[k] = []
        pb = self.pending_barrier[eng]
        if pb is not None:
            for d in pb:
                deps[id(d)] = d
            self.pending_barrier[eng] = None
        for d in deps.values():
            if d is op:
                continue
            if (not d.is_dma) and d.eng == eng and (not dma) and (eng == "pe" or not SAME_ENGINE_SYNC):
                continue
            d.flag = True
            op.deps.append(d)
        self.ops[eng].append(op)
        return op

    def barrier(self):
        lst = []
        for e in self.ENGS:
            ops = self.ops[e]
            if not ops:
                continue
            nd = 0
            got_c = False
            for op in reversed(ops):
                if op.is_dma:
                    if nd < NDMA_SEMS:
                        lst.append(op)
                        nd += 1
                elif not got_c:
                    lst.append(op)
                    got_c = True
                if nd >= NDMA_SEMS and got_c:
                    break
        for e in self.ENGS:
            prev = self.pending_barrier[e]
            self.pending_barrier[e] = lst if prev is None else (prev + lst)
        self.last_w = {}
        self.readers = {}

    def emit(self, nc):
        final_ops = self.out_ops
        for o in final_ops:
            o.flag = True
        with contextlib.ExitStack() as es:
            csem = {}
            for e in ("pe", "act", "dve", "pool"):
                csem[e] = es.enter_context(nc.semaphore("s_" + e))
            dsem = {}
            for e in ("act", "pool", "sp"):
                dsem[e] = [es.enter_context(nc.semaphore("d_%s%d" % (e, i))) for i in range(NDMA_SEMS)]
            for e in self.ENGS:
                cnt = 0
                dcnt = 0
                dvals = [0] * NDMA_SEMS
                for op in self.ops[e]:
                    if op.is_dma:
                        slot = dcnt % NDMA_SEMS
                        dcnt += 1
                        dvals[slot] += 16
                        op.sem = dsem[e][slot]
                        op.semv = dvals[slot]
                    elif op.flag:
                        cnt += 1
                        op.sem = csem[e]
                        op.semv = cnt
            block = es.enter_context(nc.Block())

            def run_engine(e, eng):
                waited = {}
                prev_dma = [None] * NDMA_SEMS
                dcnt = 0
                for op in self.ops[e]:
                    for d in op.deps:
                        key = id(d.sem)
                        if waited.get(key, 0) >= d.semv:
                            continue
                        eng.wait_ge(d.sem, d.semv)
                        waited[key] = d.semv
                    if op.is_dma:
                        slot = dcnt % NDMA_SEMS
                        dcnt += 1
                        p = prev_dma[slot]
                        if p is not None:
                            key = id(p.sem)
                            if waited.get(key, 0) < p.semv:
                                eng.wait_ge(p.sem, p.semv)
                                waited[key] = p.semv
                        prev_dma[slot] = op
                        ins = op.fn(eng)
                        ins.then_inc(op.sem, 16)
                    else:
                        ins = op.fn(eng)
                        if op.flag:
                            ins.then_inc(op.sem, 1)
                if e == "sp":
                    for o in final_ops:
                        key = id(o.sem)
                        if waited.get(key, 0) < o.semv:
                            eng.wait_ge(o.sem, o.semv)
                            waited[key] = o.semv

            @block.tensor
            def _(eng):
                run_engine("pe", eng)

            @block.scalar
            def _(eng):
                run_engine("act", eng)

            @block.vector
            def _(eng):
                run_engine("dve", eng)

            @block.gpsimd
            def _(eng):
                run_engine("pool", eng)

            @block.sync
            def _(eng):
                run_engine("sp", eng)


class Arena:
    def __init__(self, t, nbytes):
        self.t = t
        self.cap = nbytes
        self.off = 0
        self.peak = 0

    def alloc(self, free_shape, dt):
        n = 1
        for s in free_shape:
            n *= s
        esz = 2 if dt == BF16 else 4
        off = (self.off + 63) // 64 * 64
        nb = n * esz
        assert off + nb <= self.cap, ("arena overflow", off + nb, self.cap)
        self.off = off + nb
        self.peak = max(self.peak, self.off)
        v = self.t[:, off // 2:(off + nb) // 2]
        if dt != BF16:
            v = v.bitcast(dt)
        if len(free_shape) > 1:
            names = ["a%d" % i for i in range(len(free_shape))]
            kw = {names[i]: free_shape[i] for i in range(len(free_shape))}
            v = v.rearrange("p (" + " ".join(names) + ") -> p " + " ".join(names), **kw)
        return v

    def mark(self):
        return self.off

    def release(self, m):
        self.off = m


class Cfg:
    def __init__(self, SEQ=4096, CTX=256, DEPTH=2):
        self.SEQ = SEQ
        self.CTX = CTX
        self.DEPTH = DEPTH
        self.T = SEQ + CTX
        self.NT = self.T // 128
        self.NTL = SEQ // 128
        self.NTC = CTX // 128
        self.ROWS = SEQ // 64
        self.NG = SEQ // 512


def rope_tab(cfg):
    t = np.arange(cfg.SEQ)
    rows = (t // 64).astype(np.float32)
    cols = (t % 64).astype(np.float32)
    out = np.zeros((cfg.T, 192), np.float32)
    out[cfg.SEQ:, 0:64] = 1.0
    out[cfg.SEQ:, 128:160] = 1.0
    off = 0
    for R in (64, 32):
        half = R // 2
        inv = (np.float32(10000.0) ** (-np.arange(0, half, 2, dtype=np.float32) / np.float32(half))).astype(np.float32)
        ar = rows[:, None] * inv
        ac = cols[:, None] * inv
        cr, sr, cc, sc = np.cos(ar), np.sin(ar), np.cos(ac), np.sin(ac)
        C = np.concatenate([cr, cr, cc, cc], axis=1)
        S_ = np.concatenate([-sr, sr, -sc, sc], axis=1)
        out[:cfg.SEQ, off:off + R] = C
        out[:cfg.SEQ, off + R:off + 2 * R] = S_
        off += 2 * R
    return out.astype(np.float32)


def mask_b():
    k = np.arange(128)[:, None]
    q = np.arange(512)[None, :]
    out = np.zeros((6, 128, 512), np.float32)
    for r in range(6):
        rel = r - 1
        ok = np.abs(q - 128 * rel - k) <= 128
        out[r] = np.where(ok, 0.0, NEG)
    return out


def c_tables(cfg):
    rows = cfg.ROWS
    kh = min(8, rows)
    k = np.arange(128)[:, None]
    q = np.arange(512)[None, :]
    k_col = k % 64
    q_col = q % 64
    c_start = np.clip(q_col - 8, 0, 64 - 16)
    col_ok = (k_col >= c_start) & (k_col < c_start + 16)
    DC = (np.clip(k_col - q_col, -15, 15) + 15).astype(np.int64) + np.zeros((128, 512), np.int64)
    DR = np.zeros((8, 128, 512), np.int64)
    for r in range(8):
        rel = r - 2
        d = 2 * rel + k // 64 - q // 64
        DR[r] = np.clip(d, -7, 7) + 7
    masks = []
    case_of_g = []
    valid = []
    for g in range(cfg.NG):
        m = np.full((8, 128, 512), NEG, np.float32)
        v = []
        for r in range(8):
            kt = 4 * g + r - 2
            if kt < 0 or kt >= cfg.NTL:
                continue
            k_row = 2 * kt + k // 64
            q_row = 8 * g + q // 64
            r_start = np.clip(q_row - kh // 2, 0, rows - kh)
            row_ok = (k_row >= r_start) & (k_row < r_start + kh)
            ok = row_ok & col_ok
            if ok.any():
                v.append(r)
            m[r] = np.where(ok, 0.0, NEG)
        found = None
        for ci, mm in enumerate(masks):
            if np.array_equal(mm, m):
                found = ci
                break
        if found is None:
            masks.append(m)
            found = len(masks) - 1
        case_of_g.append(found)
        valid.append(v)
    return np.stack(masks).astype(np.float32), case_of_g, valid, DR, DC


class Prog:
    def __init__(self, cfg):
        self.cfg = cfg
        self.S = Sched()
        self.nc = bass.Bass("TRN2", target_bir_lowering=False)
        self.maskc_np, self.case_of_g, self.valid_c, self.DR, self.DC = c_tables(cfg)
        self.ncase = self.maskc_np.shape[0]

    def MM(self, out, lhsT, rhs, start, stop, r, w):
        return self.S.add("pe", lambda e: e.matmul(out, lhsT=lhsT, rhs=rhs, start=start, stop=stop), r, w)

    def TR(self, out, in_, ident, r, w):
        return self.S.add("pe", lambda e: e.transpose(out=out, in_=in_, identity=ident), r, w)

    def ACT(self, out, in_, func, r, w, scale=1.0, bias=0.0, accum=None):
        if accum is None:
            return self.S.add("act", lambda e: e.activation(out=out, in_=in_, func=func, bias=bias, scale=scale), r, w)
        return self.S.add("act", lambda e: e.activation(out=out, in_=in_, func=func, bias=bias, scale=scale,
                                                        accum_out=accum), r, w)

    def TT(self, eng, out, in0, in1, op, r, w):
        return self.S.add(eng, lambda e: e.tensor_tensor(out=out, in0=in0, in1=in1, op=op), r, w)

    def TS(self, eng, out, in0, s1, s2, op0, op1, r, w):
        if s2 is None:
            return self.S.add(eng, lambda e: e.tensor_scalar(out=out, in0=in0, scalar1=s1, scalar2=None, op0=op0), r, w)
        return self.S.add(eng, lambda e: e.tensor_scalar(out=out, in0=in0, scalar1=s1, scalar2=s2, op0=op0, op1=op1), r, w)

    def STT(self, eng, out, in0, scalar, in1, op0, op1, r, w):
        return self.S.add(eng, lambda e: e.scalar_tensor_tensor(out=out, in0=in0, scalar=scalar, in1=in1,
                                                                 op0=op0, op1=op1), r, w)

    def CP(self, eng, out, in_, r, w):
        if eng == "act":
            return self.S.add("act", lambda e: e.copy(out=out, in_=in_), r, w)
        return self.S.add(eng, lambda e: e.tensor_copy(out=out, in_=in_), r, w)

    def RED(self, out, in_, op, r, w):
        return self.S.add("dve", lambda e: e.tensor_reduce(out=out, in_=in_, axis=AX.X, op=op), r, w)

    def RCP(self, out, in_, r, w):
        return self.S.add("dve", lambda e: e.reciprocal(out=out, in_=in_), r, w)

    def DMA(self, q, out, in_, r, w):
        return self.S.add(q, lambda e: e.dma_start(out=out, in_=in_), r, w, dma=True)

    def MEMSET(self, eng, ap, val, w):
        return self.S.add(eng, lambda e: e.memset(ap, val), (), w)

    def build(self):
        cfg = self.cfg
        nc = self.nc
        L = cfg.DEPTH
        T, NT, NTL = cfg.T, cfg.NT, cfg.NTL

        def din(name, shape):
            return nc.dram_tensor(name, list(shape), F32, kind="ExternalInput").ap()

        self.I = I = {}
        I["x"] = din("x", [cfg.SEQ, D])
        I["c"] = din("c", [1, D])
        I["ctx"] = din("ctx", [cfg.CTX, D])
        I["c_ctx"] = din("c_ctx", [1, D])
        I["w_mod"] = din("w_mod", [L, D, 6 * D])
        I["b_mod"] = din("b_mod", [L, 6 * D])
        I["g_norm_mix"] = din("g_norm_mix", [L, D])
        I["w_in"] = din("w_in", [L, D, IN_COLS])
        I["g_q_a"] = din("g_q_a", [L, 256])
        I["w_q_b"] = din("w_q_b", [L, 256, 384])
        I["g_kv_a"] = din("g_kv_a", [L, 128])
        I["w_kv_b"] = din("w_kv_b", [L, 128, 512])
        I["sink_b"] = din("sink_b", [L, 4])
        I["biasg"] = din("biasg", [L, 4, 8, 128, 512])
        I["g_q_d"] = din("g_q_d", [L, 64])
        I["g_k_d"] = din("g_k_d", [L, 64])
        I["w_gate"] = din("w_gate", [L, 4, D, D])
        I["b_gate"] = din("b_gate", [L, 4 * D])
        I["w_branch"] = din("w_branch", [L, 4, 256, D])
        I["w_out"] = din("w_out", [L, D, D])
        I["g_norm_ffn"] = din("g_norm_ffn", [L, D])
        I["w_group"] = din("w_group", [L, D, 4])
        I["b_group"] = din("b_group", [L, 4])
        I["w_router"] = din("w_router", [L, D, 32])
        I["b_router"] = din("b_router", [L, 32])
        I["w13r"] = din("w13r", [L, NE * 128, 4096])
        I["w2r"] = din("w2r", [L, NE * 128, 2048])
        I["iota32"] = din("iota32", [1, 32])
        I["ltri"] = din("ltri", [128, 128])
        I["pidx"] = din("pidx", [128, 1])
        I["thr"] = din("thr", [1, NBLK_MAX])
        I["g_final"] = din("g_final", [1, D])
        I["ident"] = din("ident", [128, 128])
        I["ropetab"] = din("ropetab", [T, 192])
        I["maskb"] = din("maskb", [6, 128, 512])
        I["maskc"] = din("maskc", [self.ncase, 8, 128, 512])
        self.out = nc.dram_tensor("out", [cfg.SEQ, D], F32, kind="ExternalOutput").ap()

        def dscr(name, shape, dt):
            return nc.dram_tensor(name, list(shape), dt, kind="Internal").ap()

        self.xcur = dscr("xcur", [T, D], F32)
        self.QT = [dscr("QT_A", [96, 4, T], BF16)] + [dscr("QT_%d" % n, [128, 2, T], BF16) for n in (1, 2, 3)]
        self.KT = [dscr("KT_A", [96, 4, T], BF16)] + [dscr("KT_%d" % n, [128, 2, T], BF16) for n in (1, 2, 3)]
        self.VA = [dscr("VA_%d" % n, [128, NT, 4, 128], BF16) for n in range(4)]
        self.OT = dscr("OT", [8, 128, T], BF16)
        self.H2 = dscr("H2", [T, D], BF16)
        self.XS = dscr("XS", [NBLK_MAX * 128, D], BF16)
        self.YS = dscr("YS", [NBLK_MAX * 128, D], BF16)

        with contextlib.ExitStack() as es:
            ARENA_BYTES = 207 * 1024
            at = es.enter_context(nc.sbuf_tensor("arena", [128, ARENA_BYTES // 2], BF16))
            self.A = Arena(at, ARENA_BYTES)
            self.banks = [es.enter_context(nc.psum_tensor("pb%d" % i, [128, 1024], F32)) for i in range(4)]
            self.setup()
            for l in range(L):
                self.layer(l)
            self.S.emit(nc)
        return nc

    def bank(self, i):
        return self.banks[i // 2][:, (i % 2) * 512:(i % 2 + 1) * 512]

    def bank_bf(self, i):
        return self.bank(i).bitcast(BF16)

    def setup(self):
        A, I = self.A, self.I
        self.ident_f = A.alloc([128], F32)
        self.ident_b = A.alloc([128], BF16)
        self.ones_f = A.alloc([128], F32)
        self.ones_b = A.alloc([128], BF16)
        self.DMA("sp", self.ident_f, I["ident"], (), ["ident_f"])
        self.DMA("pool", self.ident_b, I["ident"], (), ["ident_b"])
        self.MEMSET("dve", self.ones_f, 1.0, ["ones_f"])
        self.MEMSET("dve", self.ones_b, 1.0, ["ones_b"])
        self.mod = [A.alloc([6 * D], F32), A.alloc([6 * D], F32)]
        self.rt = A.alloc([self.cfg.NT, 8], F32)
        self.base = A.alloc([32], F32)
        self.iota = A.alloc([32], F32)
        self.ltb = A.alloc([128], BF16)
        self.onesbb = A.alloc([128], BF16)
        self.pidx = A.alloc([1], F32)
        self.thr = A.alloc([NBLK_MAX], F32)
        self.slot_i = A.alloc([self.cfg.NT, 2], I32)
        self.widx = A.alloc([NBLK_MAX], I32)
        self.DMA("sp", self.iota, I["iota32"].partition_broadcast(128), (), ["iota"])
        self.DMA("pool", self.ltb, I["ltri"], (), ["ltb"])
        self.MEMSET("dve", self.onesbb, 1.0, ["onesbb"])
        self.DMA("sp", self.pidx, I["pidx"], (), ["pidx"])
        self.DMA("sp", self.thr, I["thr"].partition_broadcast(128), (), ["thr"])
        self.pers_mark = A.mark()

    def layer(self, l):
        import os
        cfg = self.cfg
        stop = int(os.environ.get("KSTOP", "99"))
        self.with_ctx = l < cfg.DEPTH - 1
        phases = [self.phase_mod, self.phase_p1, self.phase_p2, self.phase_p3, self.phase_p4]
        for pi, ph in enumerate(phases):
            if l * 5 + pi > stop:
                return
            self.S.barrier()
            self.A.release(self.pers_mark)
            ph(l)

    def phase_mod(self, l):
        A, I = self.A, self.I
        cb = [A.alloc([D], F32), A.alloc([D], F32)]
        lh = [A.alloc([8, 128], F32), A.alloc([8, 128], F32)]
        bmod = A.alloc([6 * D], F32)
        gm = A.alloc([D], F32)
        gf = A.alloc([D], F32)
        wm = [A.alloc([8, 512], F32), A.alloc([8, 512], F32)]
        self.DMA("sp", cb[0], I["c"].partition_broadcast(128), (), ["cb0"])
        self.DMA("sp", cb[1], I["c_ctx"].partition_broadcast(128), (), ["cb1"])
        self.DMA("sp", bmod[0:1, :], I["b_mod"][l:l + 1, :], (), ["bmod"])
        self.DMA("sp", gm, I["g_norm_mix"][l:l + 1, :].partition_broadcast(128), (), ["gm"])
        self.DMA("sp", gf, I["g_norm_ffn"][l:l + 1, :].partition_broadcast(128), (), ["gf"])
        for w in range(2):
            self.ACT(cb[w], cb[w], AF.Silu, ["cb%d" % w], ["cb%d" % w])
            for k in range(8):
                bk = self.banks[w][:, k * 128:(k + 1) * 128]
                self.TR(bk, cb[w][:, k * 128:(k + 1) * 128], self.ident_f, ["cb%d" % w, "ident_f"], ["pbk%d" % w])
            self.CP("act", lh[w], self.banks[w][:, :].rearrange("p (k m) -> p k m", k=8), ["pbk%d" % w], ["lh%d" % w])
        for j in range(12):
            wmj = wm[j % 2]
            wk = "wm%d" % (j % 2)
            self.DMA("sp", wmj, I["w_mod"][l, :, j * 512:(j + 1) * 512].rearrange("(k p) n -> p k n", p=128), (), [wk])
            for w in range(2):
                bi = 4 + (j * 2 + w) % 4
                bk = self.bank(bi)
                bkk = "bank%d" % bi
                for k in range(8):
                    self.MM(bk, lh[w][:, k, :], wmj[:, k, :], k == 0, False, ["lh%d" % w, wk], [bkk])
                self.MM(bk, self.ones_f[0:1, :], bmod[0:1, j * 512:(j + 1) * 512], False, True, ["ones_f", "bmod"], [bkk])
                self.CP("dve" if w == 0 else "act", self.mod[w][:, j * 512:(j + 1) * 512], bk, [bkk], ["mod%d" % w])
        for w in range(2):
            m = self.mod[w]
            self.STT("dve", m[:, D:2 * D], m[:, D:2 * D], 1.0, gm, ALU.add, ALU.mult, ["mod%d" % w, "gm"], ["mod%d" % w])
            self.STT("dve", m[:, 4 * D:5 * D], m[:, 4 * D:5 * D], 1.0, gf, ALU.add, ALU.mult, ["mod%d" % w, "gf"], ["mod%d" % w])

    def x_src(self, l, t):
        cfg = self.cfg
        if l == 0:
            if t < cfg.NTL:
                return self.I["x"][t * 128:(t + 1) * 128, :]
            return self.I["ctx"][(t - cfg.NTL) * 128:(t - cfg.NTL + 1) * 128, :]
        return self.xcur[t * 128:(t + 1) * 128, :]

    def rstd_of(self, src, n, junk, ssq, rstd, rk, tag, jk="junk"):
        self.ACT(junk, src, AF.Square, rk, [jk, "ssq" + tag], accum=ssq)
        self.ACT(ssq, ssq, AF.Sqrt, ["ssq" + tag], ["ssq" + tag], scale=1.0 / n, bias=EPS)
        self.RCP(rstd, ssq, ["ssq" + tag], ["rstd" + tag])

    def norm_mod(self, xt, xk, w, goff, soff, junk, tmp, out, outk, ssq, rstd, tag, jk="junk", tk="tmp"):
        self.rstd_of(xt, D, junk, ssq, rstd, [xk], tag, jk=jk)
        m = self.mod[w]
        self.STT("dve", tmp, xt, rstd, m[:, goff:goff + D], ALU.mult, ALU.mult, [xk, "rstd" + tag, "mod%d" % w], [tk])
        self.TT("dve", out, tmp, m[:, soff:soff + D], ALU.add, [tk, "mod%d" % w], [outk])

    def transpose8(self, src, srck, bank_bf, bankk, dst, dstk, nblk=8, ident=None, identk="ident_b"):
        ident = self.ident_b if ident is None else ident
        srcks = [srck] if isinstance(srck, str) else list(srck)
        for k in range(nblk):
            self.TR(bank_bf[:, k * 128:(k + 1) * 128], src[:, k * 128:(k + 1) * 128], ident, srcks + [identk], [bankk])
        self.CP("act", dst, bank_bf[:, 0:nblk * 128].rearrange("p (k m) -> p k m", k=nblk), [bankk], [dstk])

    def rope(self, src, srck, H, R, tab, tabk, coff, soff, t1, t2, outs, tag):
        w = R // 4
        C = tab[:, coff:coff + R]
        Sg = tab[:, soff:soff + R].rearrange("p (a b w) -> p a b w", a=2, b=2)
        t1v = t1[:, 0:H * R].rearrange("p (h r) -> p h r", h=H)
        t2v = t2[:, 0:H * R].rearrange("p (h r) -> p h r", h=H)
        self.TT("dve", t1v, src, C.unsqueeze(1).to_broadcast([128, H, R]), ALU.mult, [srck, tabk], ["t1" + tag])
        s5 = src.rearrange("p h (a b w) -> p h a b w", a=2, b=2)
        t5 = t2v.rearrange("p h (a b w) -> p h a b w", a=2, b=2)
        self.TT("dve", t5[:, :, :, 0, :], s5[:, :, :, 1, :], Sg[:, :, 0, :].unsqueeze(1).to_broadcast([128, H, 2, w]),
                ALU.mult, [srck, tabk], ["t2a" + tag])
        self.TT("dve", t5[:, :, :, 1, :], s5[:, :, :, 0, :], Sg[:, :, 1, :].unsqueeze(1).to_broadcast([128, H, 2, w]),
                ALU.mult, [srck, tabk], ["t2b" + tag])
        for (dst, h0, h1, dk) in outs:
            self.TT("dve", dst, t1v[:, h0:h1, :], t2v[:, h0:h1, :], ALU.add, ["t1" + tag, "t2a" + tag, "t2b" + tag], [dk])

    def phase_p1(self, l):
        cfg, A, I = self.cfg, self.A, self.I
        NT, NTL = cfg.NT, cfg.NTL
        win = A.alloc([8, IN_COLS], BF16)
        for k in range(8):
            self.DMA("pool", win[:, k, :], I["w_in"][l, k * 128:(k + 1) * 128, :], (), ["win"])
        wqb = A.alloc([2, 384], BF16)
        self.DMA("pool", wqb, I["w_q_b"][l].rearrange("(k p) n -> p k n", p=128), (), ["wqb"])
        wkvb = A.alloc([512], BF16)
        self.DMA("pool", wkvb, I["w_kv_b"][l], (), ["wkvb"])
        gqa = A.alloc([256], F32)
        gkv = A.alloc([128], F32)
        gd = A.alloc([6, 64], F32)
        self.DMA("sp", gqa, I["g_q_a"][l:l + 1, :].partition_broadcast(128), (), ["gqa"])
        self.DMA("sp", gkv, I["g_kv_a"][l:l + 1, :].partition_broadcast(128), (), ["gkv"])
        for h in range(6):
            src = I["g_q_d"] if h < 4 else I["g_k_d"]
            self.DMA("sp", gd[:, h, :], src[l:l + 1, :].partition_broadcast(128), (), ["gd"])
        xt2 = [A.alloc([D], F32) for _ in range(2)]
        tab2 = [A.alloc([192], F32) for _ in range(2)]
        junk = A.alloc([D], F32)
        tmp = A.alloc([D], F32)
        hb = A.alloc([D], BF16)
        hT = A.alloc([8, 128], BF16)
        ssq = A.alloc([8], F32)
        rstd = A.alloc([8], F32)
        ssqD = A.alloc([8], F32)
        krs = A.alloc([32], F32)
        qf = A.alloc([384], F32)
        kvf = A.alloc([512], F32)
        bdf = A.alloc([512], F32)
        bdfD = A.alloc([512], F32)
        c2f = A.alloc([256], F32)
        rstdD = A.alloc([8], F32)
        cqkv = A.alloc([384], BF16)
        cT = A.alloc([3, 128], BF16)
        qa = A.alloc([4, 96], BF16)
        ka = A.alloc([4, 96], BF16)
        krr = A.alloc([1, 32], F32)
        t1 = A.alloc([512], F32)
        t2 = A.alloc([512], F32)
        sq = A.alloc([384], F32)
        qn = A.alloc([6, 64], F32)
        qkb = A.alloc([8, 64], BF16)
        qkc = A.alloc([512], BF16)
        stA = [A.alloc([8, 128], BF16) for _ in range(2)]
        stP = [A.alloc([4, 128], BF16) for _ in range(2)]
        va = [[A.alloc([4, 128], BF16) for _ in range(2)] for _ in range(4)]
        for n in range(4):
            for b in range(2):
                self.MEMSET("pool", va[n][b], 1.0, ["va%d_%d" % (n, b)])
        T0, T1 = self.bank_bf(0), self.bank_bf(1)
        pA, pBD, pC1, pC2, pQ, pKV = (self.bank(i) for i in (2, 3, 4, 5, 6, 7))

        def load(t):
            b = t % 2
            self.DMA("sp", xt2[b], self.x_src(l, t), (), ["xt%d" % b])
            self.DMA("sp", tab2[b], I["ropetab"][t * 128:(t + 1) * 128, :], (), ["tab%d" % b])

        import os
        KCUT = int(os.environ.get("KCUT", "99"))
        load(0)
        for t in range(NT):
            if t + 1 < NT:
                load(t + 1)
            b = t % 2
            xt, xk, tab, tabk = xt2[b], "xt%d" % b, tab2[b], "tab%d" % b
            w = 0 if t < NTL else 1
            tok = slice(t * 128, (t + 1) * 128)
            self.norm_mod(xt, xk, w, D, 0, junk, tmp, hb, "hb", ssq[:, 0:1], rstd[:, 0:1], "n1")
            self.transpose8(hb, "hb", T0, "T0", hT, "hT")
            if KCUT == 1:
                return
            def proj(dst, dk, c0, c1):
                for k in range(8):
                    self.MM(dst[:, 0:c1 - c0], hT[:, k, :], win[:, k, c0:c1], k == 0, k == 7, ["hT", "win"], [dk])
            proj(pA, "pA", 0, 416)
            proj(pBD, "pBD", 416, 928)
            self.CP("act", bdf, pBD, ["pBD"], ["bdf"])
            proj(pC1, "pC1", 928, 1440)
            proj(pC2, "pC2", 1440, 1696)
            proj(pBD, "pBD", 1696, 2208)
            if KCUT == 2:
                return
            self.ACT(junk[:, 0:256], pA[:, 0:256], AF.Square, ["pA"], ["junk", "ssqA0"], accum=ssq[:, 1:2])
            self.ACT(junk[:, 256:384], pA[:, 256:384], AF.Square, ["pA"], ["junk", "ssqA1"], accum=ssq[:, 2:3])
            self.ACT(ssq[:, 1:2], ssq[:, 1:2], AF.Sqrt, ["ssqA0"], ["ssqA0"], scale=1.0 / 256, bias=EPS)
            self.ACT(ssq[:, 2:3], ssq[:, 2:3], AF.Sqrt, ["ssqA1"], ["ssqA1"], scale=1.0 / 128, bias=EPS)
            self.RCP(rstd[:, 1:3], ssq[:, 1:3], ["ssqA0", "ssqA1"], ["rstdA"])
            self.STT("dve", cqkv[:, 0:256], pA[:, 0:256], rstd[:, 1:2], gqa, ALU.mult, ALU.mult, ["pA", "rstdA", "gqa"], ["cqkv0"])
            self.STT("dve", cqkv[:, 256:384], pA[:, 256:384], rstd[:, 2:3], gkv, ALU.mult, ALU.mult, ["pA", "rstdA", "gkv"], ["cqkv1"])
            for k in range(3):
                self.TR(T1[:, k * 128:(k + 1) * 128], cqkv[:, k * 128:(k + 1) * 128], self.ident_b,
                        ["cqkv0", "cqkv1", "ident_b"], ["T1"])
            self.CP("act", cT, T1[:, 0:384].rearrange("p (k m) -> p k m", k=3), ["T1"], ["cT"])
            if KCUT == 3:
                return
            for k in range(2):
                self.MM(pQ[:, 0:384], cT[:, k, :], wqb[:, k, :], k == 0, k == 1, ["cT", "wqb"], ["pQ"])
            self.MM(pKV, cT[:, 2, :], wkvb, True, True, ["cT", "wkvb"], ["pKV"])
            if KCUT == 4:
                return
            self.CP("act", qf, pQ[:, 0:384], ["pQ"], ["qf"])
            self.CP("act", kvf, pKV, ["pKV"], ["kvf"])
            self.CP("act", krs, pA[:, 384:416], ["pA"], ["krs"])
            pQv = qf.rearrange("p (h r) -> p h r", h=4)
            pKVv = kvf.rearrange("p (h r) -> p h r", h=4)
            self.CP("pool", qa[:, :, 0:64], pQv[:, :, 0:64], ["qf"], ["qa0"])
            if KCUT == 41:
                return
            self.rope(pQv[:, :, 64:96], "qf", 4, 32, tab, tabk, 128, 160, t1, t2, [(qa[:, :, 64:96], 0, 4, "qa1")], "R")
            if KCUT == 42:
                return
            self.rope(krs.unsqueeze(1), "krs", 1, 32, tab, tabk, 128, 160, t1, t2, [(krr, 0, 1, "krr")], "R")
            if KCUT == 5:
                return
            self.CP("pool", ka[:, :, 0:64], pKVv[:, :, 0:64], ["kvf"], ["ka0"])
            self.CP("dve", ka[:, :, 64:96], krr.to_broadcast([128, 4, 32]), ["krr"], ["ka1"])
            vA = va[0][b]
            vAp = vA.rearrange("p (j two) d -> p j two d", two=2)
            pKVp = pKVv.rearrange("p (j two) r -> p j two r", two=2)
            self.CP("pool", vAp[:, :, 0, 0:64], pKVp[:, :, 0, 64:128], ["kvf"], ["va0_%d" % b])
            self.CP("pool", vAp[:, :, 1, 64:128], pKVp[:, :, 1, 64:128], ["kvf"], ["va0_%d" % b])
            if KCUT == 6:
                return
            for h in range(4):
                self.TR(T1[0:96, h * 128:(h + 1) * 128], qa[:, h, :], self.ident_b, ["qa0", "qa1", "ident_b"], ["T1"])
                self.TR(T1[0:96, (4 + h) * 128:(5 + h) * 128], ka[:, h, :], self.ident_b, ["ka0", "ka1", "ident_b"], ["T1"])
            sA = stA[b]
            self.CP("act", sA[0:96], T1[0:96, :].rearrange("p (k m) -> p k m", k=8), ["T1"], ["stA%d" % b])
            if KCUT == 7:
                return
            self.DMA("sp", self.QT[0][:, :, tok], sA[0:96, 0:4, :], ["stA%d" % b], [])
            self.DMA("sp", self.KT[0][:, :, tok], sA[0:96, 4:8, :], ["stA%d" % b], [])
            self.DMA("sp", self.VA[0][:, t, :, :], vA, ["va0_%d" % b], [])
            if KCUT == 8:
                return

            def gqa_v(src, srck, n):
                v = va[n][b]
                vp = v.rearrange("p (g two) d -> p g two d", two=2)
                sv = src[:, 384:512].rearrange("p (g r) -> p g r", g=2)
                self.CP("pool", vp[:, :, 0, 0:64], sv, [srck], ["va%d_%d" % (n, b)])
                self.CP("pool", vp[:, :, 1, 64:128], sv, [srck], ["va%d_%d" % (n, b)])
                return v

            def qk_store(src, srcks, n, tbank, tbk):
                for k in range(4):
                    self.TR(tbank[:, k * 128:(k + 1) * 128], src[:, k * 128:(k + 1) * 128], self.ident_b,
                            list(srcks) + ["ident_b"], [tbk])
                sp_ = stP[n % 2]
                spk = "stP%d" % (n % 2)
                self.CP("act", sp_, tbank[:, 0:512].rearrange("p (k m) -> p k m", k=4), [tbk], [spk])
                self.DMA("sp", self.QT[n][:, :, tok], sp_[:, 0:2, :], [spk], [])
                self.DMA("sp", self.KT[n][:, :, tok], sp_[:, 2:4, :], [spk], [])

            pBv = bdf[:, 0:384].rearrange("p (h r) -> p h r", h=6)
            qkb_d = qkb[:, 4:8, :].rearrange("p (g two) r -> p g two r", two=2)
            self.rope(pBv, "bdf", 6, 64, tab, tabk, 0, 64, t1, t2,
                      [(qkb[:, 0:4, :], 0, 4, "qkb0"), (qkb_d[:, :, 0, :], 4, 6, "qkb1"), (qkb_d[:, :, 1, :], 4, 6, "qkb2")], "R")
            vB = gqa_v(bdf, "bdf", 1)
            qk_store(qkb.rearrange("p h r -> p (h r)"), ["qkb0", "qkb1", "qkb2"], 1, T0, "T0")
            self.DMA("sp", self.VA[1][:, t, :, :], vB, ["va1_%d" % b], [])
            if KCUT == 9:
                return
            self.CP("act", qkc, pC1, ["pC1"], ["qkc"])
            vC = va[2][b]
            vCp = vC.rearrange("p (j two) d -> p j two d", two=2)
            self.CP("act", c2f, pC2[:, 0:256], ["pC2"], ["c2f"])
            pC2p = c2f.rearrange("p (j two r) -> p j two r", two=2, r=64)
            self.CP("pool", vCp[:, :, 0, 0:64], pC2p[:, :, 0, :], ["c2f"], ["va2_%d" % b])
            self.CP("pool", vCp[:, :, 1, 64:128], pC2p[:, :, 1, :], ["c2f"], ["va2_%d" % b])
            qk_store(qkc, ["qkc"], 2, T1, "T1")
            self.DMA("sp", self.VA[2][:, t, :, :], vC, ["va2_%d" % b], [])
            if KCUT == 10:
                return
            self.CP("act", bdfD, pBD, ["pBD"], ["bdfD"])
            pDv = bdfD[:, 0:384].rearrange("p (h r) -> p h r", h=6)
            sqv = sq.rearrange("p (h r) -> p h r", h=6)
            self.ACT(sq, pBD[:, 0:384], AF.Square, ["pBD"], ["sq"])
            self.RED(ssqD[:, 0:6], sqv, ALU.add, ["sq"], ["ssqD"])
            self.ACT(ssqD[:, 0:6], ssqD[:, 0:6], AF.Sqrt, ["ssqD"], ["ssqD"], scale=1.0 / 64, bias=EPS)
            self.RCP(rstdD[:, 0:6], ssqD[:, 0:6], ["ssqD"], ["rstdD"])
            self.TT("dve", qn, pDv, rstdD[:, 0:6].unsqueeze(2).to_broadcast([128, 6, 64]), ALU.mult, ["bdfD", "rstdD"], ["qn"])
            self.TT("dve", qn, qn, gd, ALU.mult, ["qn", "gd"], ["qn"])
            self.rope(qn, "qn", 6, 64, tab, tabk, 0, 64, t1, t2,
                      [(qkb[:, 0:4, :], 0, 4, "qkb0"), (qkb_d[:, :, 0, :], 4, 6, "qkb1"), (qkb_d[:, :, 1, :], 4, 6, "qkb2")], "R")
            vD = gqa_v(bdfD, "bdfD", 3)
            qk_store(qkb.rearrange("p h r -> p (h r)"), ["qkb0", "qkb1", "qkb2"], 3, T0, "T0")
            self.DMA("sp", self.VA[3][:, t, :, :], vD, ["va3_%d" % b], [])

    def phase_p2(self, l):
        cfg, A, I = self.cfg, self.A, self.I
        NT, NTL, T = cfg.NT, cfg.NTL, cfg.T
        LOOK = 3
        groups = [(g * 512, 512, g) for g in range(cfg.NG)]
        if self.with_ctx:
            groups.append((cfg.SEQ, cfg.CTX, None))
        ctx_tiles = list(range(NTL, NT))
        base_mark = A.mark()
        pt = [A.alloc([512], BF16) for _ in range(6)]
        tmpf = [A.alloc([512], F32) for _ in range(4)]
        rden = [A.alloc([512], F32) for _ in range(2)]
        rdt = A.alloc([512], F32)
        ost = [A.alloc([512], BF16) for _ in range(2)]
        qt = [A.alloc([512], BF16) for _ in range(2)]
        esink = A.alloc([4], F32)
        self.DMA("sp", esink, I["sink_b"][l:l + 1, :].partition_broadcast(128), (), ["esink"])
        self.ACT(esink, esink, AF.Exp, ["esink"], ["esink"])
        inner_mark = A.mark()
        cnt = {"pt": 0, "s": 0, "tf": 0, "grp": 0}
        for n in range(4):
            A.release(inner_mark)
            self.S.barrier()
            isA = n == 0
            scale = (96 ** -0.5) if isA else 0.125
            LOOK = 2 if n == 2 else 3
            if isA:
                kt = A.alloc([4, T], BF16)
                self.DMA("sp", kt[0:96], self.KT[0], (), ["kt"])
            else:
                kt = A.alloc([4, T], BF16)
                self.MEMSET("pool", kt, 0.0, ["kt"])
                for hh in range(4):
                    hp = (hh % 2) * 64
                    self.DMA("sp", kt[hp:hp + 64, hh, :], self.KT[n][hp:hp + 64, hh // 2, :], (), ["kt"])
            vat = A.alloc([NT, 4, 128], BF16)
            step = max(1, NT // 4)
            for t0 in range(0, NT, step):
                t1_ = min(NT, t0 + step)
                self.DMA("sp", vat[:, t0:t1_], self.VA[n][:, t0:t1_], (), ["vat"])
            mb = mc = bias = comb = None
            if n == 1:
                mb = A.alloc([6, 512], F32)
                self.DMA("sp", mb, I["maskb"].rearrange("r p q -> p r q"), (), ["mb"])
            if n == 2:
                mc = A.alloc([self.ncase * 8, 512], BF16)
                for ci in range(self.ncase):
                    self.DMA("pool", mc[:, ci * 8:(ci + 1) * 8, :], I["maskc"][ci].rearrange("r p q -> p r q"), (), ["mc"])
                bias = A.alloc([8, 512], F32)
                comb1 = A.alloc([8, 512], F32)
                comb = [comb1, comb1]
            G_list = []
            for h in range(4):
                for gi, (q0, nq, g) in enumerate(groups):
                    tiles = []
                    if g is None:
                        tiles = [(t_, None) for t_ in ctx_tiles]
                    elif n in (0, 3):
                        tiles = [(t_, None) for t_ in range(NT)]
                    elif n == 1:
                        for j in range(4 * g - 1, 4 * g + 5):
                            if 0 <= j < NTL:
                                tiles.append((j, ("mb", j - 4 * g + 1)))
                        tiles += [(t_, None) for t_ in ctx_tiles]
                    else:
                        for r in self.valid_c[g]:
                            tiles.append((4 * g + r - 2, ("comb", r)))
                        tiles += [(t_, None) for t_ in ctx_tiles]
                    gid = cnt["grp"]
                    cnt["grp"] += 1
                    G_list.append(dict(h=h, q0=q0, nq=nq, g=g, tiles=tiles, gid=gid, first_of_head=(gi == 0)))
            items = [(G, i) for G in G_list for i in range(len(G["tiles"]))]
            state = {}

            def emit_S(idx):
                G, i = items[idx]
                h, q0, nq, gid = G["h"], G["q0"], G["nq"], G["gid"]
                ph = (h % 2) * 64
                vr = slice(ph, ph + 64)
                qb = gid % 2
                qtb, qk_ = qt[qb], "qt%d" % qb
                if i == 0:
                    if n == 2 and G["first_of_head"]:
                        self.DMA("sp", bias, I["biasg"][l, h].rearrange("r p q -> p r q"), (), ["bias"])
                    if n == 2 and G["first_of_head"]:
                        state["built"] = None
                    if n == 2 and G["g"] is not None and state.get("built") != (self.case_of_g[G["g"]], tuple(self.valid_c[G["g"]])):
                        state["built"] = (self.case_of_g[G["g"]], tuple(self.valid_c[G["g"]]))
                        ci = self.case_of_g[G["g"]]
                        cb_ = comb[gid % 2]
                        for r in self.valid_c[G["g"]]:
                            self.TT("pool", cb_[:, r, :], bias[:, r, :], mc[:, ci * 8 + r, :], ALU.add,
                                    ["bias", "mc"], ["comb_%d" % r])
                    if isA:
                        self.DMA("sp", qtb[0:96, 0:nq], self.QT[0][:, h, q0:q0 + nq], (), [qk_])
                    else:
                        self.DMA("sp", qtb[:, 0:nq], self.QT[n][:, h // 2, q0:q0 + nq], (), [qk_])
                q_ap = qtb[0:96, 0:nq] if isA else qtb[:, 0:nq]
                tk = G["tiles"][i][0]
                sb_ = cnt["s"] % 4
                cnt["s"] += 1
                psS = self.bank(sb_)
                psk = "psS%d" % sb_
                if isA:
                    k_ap = kt[0:96, h, tk * 128:(tk + 1) * 128]
                else:
                    k_ap = kt[:, h, tk * 128:(tk + 1) * 128]
                self.MM(psS[:, 0:nq], k_ap, q_ap, True, True, ["kt", qk_], [psk])
                state[idx] = (psS, psk)

            for j in range(min(LOOK, len(items))):
                emit_S(j)
            for idx, (G, i) in enumerate(items):
                if idx + LOOK < len(items):
                    emit_S(idx + LOOK)
                h, q0, nq, gid = G["h"], G["q0"], G["nq"], G["gid"]
                ph = (h % 2) * 64
                vr = slice(ph, ph + 64)
                dr = slice(64 - ph, 128 - ph)
                psS, psk = state.pop(idx)
                tk, tabinfo = G["tiles"][i]
                ob = gid % 2
                psO = self.bank(6 + ob)
                pok = "psO%d" % ob
                last = len(G["tiles"]) - 1
                pb_ = cnt["pt"] % 6
                cnt["pt"] += 1
                ptb, ptk = pt[pb_], "pt%d" % pb_
                if tabinfo is None:
                    self.ACT(ptb[:, 0:nq], psS[:, 0:nq], AF.Exp, [psk], [ptk], scale=scale)
                else:
                    if tabinfo[0] == "mb":
                        tabv, tabk = mb[:, tabinfo[1], :], "mb"
                    else:
                        tabv, tabk = comb[gid % 2][:, tabinfo[1], :], "comb_%d" % tabinfo[1]
                    fb = cnt["tf"] % 4
                    cnt["tf"] += 1
                    self.STT("dve", tmpf[fb][:, 0:nq], psS[:, 0:nq], scale, tabv[:, 0:nq], ALU.mult, ALU.add,
                             [psk, tabk], ["tmpf%d" % fb])
                    self.ACT(ptb[:, 0:nq], tmpf[fb][:, 0:nq], AF.Exp, ["tmpf%d" % fb], [ptk])
                self.MM(psO[:, 0:nq], vat[:, tk, h, :], ptb[:, 0:nq], i == 0, i == last, ["vat", ptk], [pok])
                if i == last:
                    rb = gid % 2
                    rd, rdk = rden[rb], "rden%d" % rb
                    if n in (1, 2):
                        if n == 1:
                            self.ACT(rdt[dr, 0:nq], psO[dr, 0:nq], AF.Ln, [pok, "esink"], ["rdt"], bias=esink[dr, h:h + 1])
                        else:
                            self.ACT(rdt[dr, 0:nq], psO[dr, 0:nq], AF.Ln, [pok], ["rdt"])
                        self.ACT(rdt[dr, 0:nq], rdt[dr, 0:nq], AF.Exp, ["rdt"], ["rdt"], scale=-1.0)
                        self.CP("dve", rd[vr, 0:nq], rdt[dr, 0:nq], ["rdt"], [rdk])
                    else:
                        self.RCP(rd[vr, 0:nq], psO[dr, 0:nq], [pok], [rdk])
                    osb, osk = ost[rb], "ost%d" % rb
                    self.TT("dve", osb[vr, 0:nq], psO[vr, 0:nq], rd[vr, 0:nq], ALU.mult, [pok, rdk], [osk])
                    self.DMA("sp", self.OT[n * 2 + h // 2][vr, q0:q0 + nq], osb[vr, 0:nq], [osk], [])
        A.release(base_mark)

    def phase_p3(self, l):
        cfg, A, I = self.cfg, self.A, self.I
        NT, NTL = cfg.NT, cfg.NTL
        tiles = list(range(NT)) if self.with_ctx else list(range(NTL))
        wg = A.alloc([32, D], BF16)
        for n in range(4):
            for k in range(8):
                self.DMA("pool", wg[:, n * 8 + k, :], I["w_gate"][l, n, k * 128:(k + 1) * 128, :], (), ["wg"])
        bg = A.alloc([4 * D], BF16)
        self.DMA("pool", bg[0:1, :], I["b_gate"][l:l + 1, :], (), ["bg"])
        wbr = A.alloc([8, D], BF16)
        self.DMA("pool", wbr, I["w_branch"][l].rearrange("n (c p) m -> p (n c) m", p=128), (), ["wbr"])
        wo = A.alloc([8, D], BF16)
        self.DMA("pool", wo, I["w_out"][l].rearrange("(k p) m -> p k m", p=128), (), ["wo"])
        wr = A.alloc([8, 36], F32)
        self.DMA("sp", wr[:, :, 0:4], I["w_group"][l].rearrange("(k p) n -> p k n", p=128), (), ["wr"])
        self.DMA("sp", wr[:, :, 4:36], I["w_router"][l].rearrange("(k p) n -> p k n", p=128), (), ["wr"])
        br = A.alloc([36], F32)
        self.DMA("sp", br[0:1, 0:4], I["b_group"][l:l + 1, :], (), ["br"])
        self.DMA("sp", br[0:1, 4:36], I["b_router"][l:l + 1, :], (), ["br"])
        xt2 = [A.alloc([D], F32) for _ in range(3)]
        ot2 = [A.alloc([8, 128], BF16) for _ in range(2)]
        tmpA = A.alloc([D], F32)
        tmpB = A.alloc([D], F32)
        hb = A.alloc([D], BF16)
        hT = A.alloc([8, 128], BF16)
        gate = A.alloc([D], BF16)
        tmpn = A.alloc([D], BF16)
        ypre = A.alloc([D], F32)
        ypb = A.alloc([D], BF16)
        yT = A.alloc([8, 128], BF16)
        h2Tf = A.alloc([8, 128], F32)
        h2b = A.alloc([D], BF16)
        oh12b = A.alloc([64], BF16)
        csb = A.alloc([64], F32)
        tr0 = A.alloc([32], F32)
        tr1 = A.alloc([32], F32)
        self.MEMSET("dve", self.base, 0.0, ["base"])
        ssq = A.alloc([4], F32)
        rstd = A.alloc([4], F32)
        lg = A.alloc([36], F32)
        sm = A.alloc([16], F32)
        ohg = A.alloc([4], F32)
        pen = A.alloc([4], F32)
        em = A.alloc([32], F32)
        em2 = A.alloc([32], F32)
        oh1 = A.alloc([32], F32)
        oh2 = A.alloc([32], F32)
        c1 = A.alloc([32], F32)
        j4 = A.alloc([4], F32)
        PT = self.bank_bf(0)
        PB, PY = self.banks[2], self.banks[3]
        pR = self.bank(1)
        cntu = [0]

        def load(i):
            t = tiles[i]
            b = i % 2
            self.DMA("sp", xt2[i % 3], self.x_src(l, t), (), ["xt%d" % (i % 3)])
            self.DMA("sp", ot2[b], self.OT[:, :, t * 128:(t + 1) * 128].rearrange("b p t -> p b t"), (), ["ot%d" % b])

        def norm1(i):
            t = tiles[i]
            w = 0 if t < NTL else 1
            self.norm_mod(xt2[i % 3], "xt%d" % (i % 3), w, D, 0, hb, tmpA, hb, "hb", ssq[:, 0:1], rstd[:, 0:1], "n1",
                          jk="hb", tk="tmpA")

        def stageA(i, gen=None):
            t = tiles[i]
            b = i % 2
            xt, xk, ot, otk = xt2[i % 3], "xt%d" % (i % 3), ot2[b], "ot%d" % b
            w = 0 if t < NTL else 1
            m = self.mod[w]
            mk = "mod%d" % w
            tok = slice(t * 128, (t + 1) * 128)
            tmp = tmpA
            self.transpose8(hb, "hb", PT, "PT", hT, "hT")
            for n in range(4):
                for half in range(2):
                    cs = slice(half * 512, (half + 1) * 512)
                    r_ = cntu[0] % 2
                    cntu[0] += 1
                    PGh, pgk = self.bank(2 + r_), "PG%d" % r_
                    PBh, pbk = self.bank(4 + r_), "PB%d" % r_
                    for k in range(8):
                        self.MM(PGh, hT[:, k, :], wg[:, n * 8 + k, cs], k == 0, False, ["hT", "wg"], [pgk])
                    self.MM(PGh, self.ones_b[0:1, :], bg[0:1, n * D + half * 512:n * D + (half + 1) * 512], False, True,
                            ["ones_b", "bg"], [pgk])
                    for c in range(2):
                        self.MM(PBh, ot[:, n * 2 + c, :], wbr[:, n * 2 + c, cs], c == 0, c == 1, [otk, "wbr"], [pbk])
                    gh, ghk = gate[:, r_ * 512:(r_ + 1) * 512], "gate%d" % r_
                    self.ACT(gh, PGh, AF.Sigmoid, [pgk], [ghk])
                    yk = "ypre%d" % half
                    if n == 0:
                        self.TT("dve", ypre[:, cs], gh, PBh, ALU.mult, [ghk, pbk], [yk])
                    else:
                        tn, tnk = tmpn[:, r_ * 512:(r_ + 1) * 512], "tn%d" % r_
                        self.TT("dve", tn, gh, PBh, ALU.mult, [ghk, pbk], [tnk])
                        if n < 3:
                            self.TT("pool", ypre[:, cs], ypre[:, cs], tn, ALU.add, [yk, tnk], [yk])
                        else:
                            self.TT("pool", ypb[:, cs], ypre[:, cs], tn, ALU.add, [yk, tnk], ["ypb%d" % half])
                    u_ = n * 2 + half
                    if u_ in (1, 3, 5, 7) and gen is not None:
                        next(gen, None)
                    if u_ == 3 and i + 1 < len(tiles):
                        norm1(i + 1)
            if i + 2 < len(tiles):
                load(i + 2)
            self.transpose8(ypb, ["ypb0", "ypb1"], PT, "PT", yT, "yT")
            for half in range(2):
                cs = slice(half * 512, (half + 1) * 512)
                for k in range(8):
                    self.MM(PY[:, cs], yT[:, k, :], wo[:, k, cs], k == 0, k == 7, ["yT", "wo"], ["PY"])
            self.TT("dve", tmp, PY[:, :], m[:, 2 * D:3 * D], ALU.mult, ["PY", mk], ["tmpA"])
            self.TT("pool", xt, tmp, xt, ALU.add, ["tmpA", xk], [xk])
            self.DMA("sp", self.xcur[tok, :], xt, [xk], [])
            if gen is not None:
                next(gen, None)

        def stageB(i):
            t = tiles[i]
            xt, xk = xt2[i % 3], "xt%d" % (i % 3)
            w = 0 if t < NTL else 1
            tok = slice(t * 128, (t + 1) * 128)
            tmp = tmpB
            h2 = tmp
            self.norm_mod(xt, xk, w, 4 * D, 3 * D, h2b, tmp, h2, "tmpB", ssq[:, 1:2], rstd[:, 1:2], "n2", jk="h2b", tk="tmpB")
            yield
            for k in range(8):
                self.TR(PB[:, k * 128:(k + 1) * 128], h2[:, k * 128:(k + 1) * 128], self.ident_f, ["tmpB", "ident_f"], ["PB0", "PB1"])
            self.CP("act", h2Tf, PB[:, :].rearrange("p (k m) -> p k m", k=8), ["PB0", "PB1"], ["h2Tf"])
            self.CP("pool", h2b, h2, ["tmpB"], ["h2b"])
            self.DMA("sp", self.H2[tok, :], h2b, ["h2b"], [])
            yield
            for k in range(8):
                self.MM(pR[:, 0:36], h2Tf[:, k, :], wr[:, k, :], k == 0, False, ["h2Tf", "wr"], ["pR"])
            self.MM(pR[:, 0:36], self.ones_f[0:1, :], br[0:1, :], False, True, ["ones_f", "br"], ["pR"])
            self.CP("dve", lg, pR[:, 0:36], ["pR"], ["lg"])
            gl = lg[:, 0:4]
            el = lg[:, 4:36].rearrange("p (g e) -> p g e", g=4)
            gmax, negmax, se, gw, m1, m2, dd, w1, w1g, w2g = (sm[:, j:j + 1] for j in range(10))
            self.RED(gmax, gl, ALU.max, ["lg"], ["gmax"])
            self.TS("dve", ohg, gl, gmax, None, ALU.is_equal, None, ["lg", "gmax"], ["ohg"])
            self.TS("dve", negmax, gmax, -1.0, None, ALU.mult, None, ["gmax"], ["negmax"])
            self.ACT(j4, gl, AF.Exp, ["lg", "negmax"], ["j4", "se"], bias=negmax, accum=se)
            self.RCP(gw, se, ["se"], ["gw"])
            self.TS("dve", pen, ohg, -1.0, 1e9, ALU.add, ALU.mult, ["ohg"], ["pen"])
            emv = em.rearrange("p (g e) -> p g e", g=4)
            self.TT("dve", emv, el, pen.unsqueeze(2).to_broadcast([128, 4, 8]), ALU.add, ["lg", "pen"], ["em"])
            self.RED(m1, em, ALU.max, ["em"], ["m1"])
            self.TS("dve", oh1, em, m1, None, ALU.is_equal, None, ["em", "m1"], ["oh1"])
            self.STT("dve", em2, oh1, -2e9, em, ALU.mult, ALU.add, ["oh1", "em"], ["em2"])
            self.RED(m2, em2, ALU.max, ["em2"], ["m2"])
            self.TS("dve", oh2, em2, m2, None, ALU.is_equal, None, ["em2", "m2"], ["oh2"])
            yield
            self.TT("dve", dd, m2, m1, ALU.subtract, ["m1", "m2"], ["dd"])
            self.ACT(dd, dd, AF.Exp, ["dd"], ["dd"])
            self.TS("dve", dd, dd, 1.0, None, ALU.add, None, ["dd"], ["dd"])
            self.RCP(w1, dd, ["dd"], ["w1"])
            self.TT("dve", w1g, w1, gw, ALU.mult, ["w1", "gw"], ["w1g"])
            self.TT("dve", w2g, gw, w1g, ALU.subtract, ["w1g", "gw"], ["w2g"])
            rt, base, iota = self.rt, self.base, self.iota
            pRK = self.bank(1)[:, 64:192]
            self.CP("pool", oh12b[:, 0:32], oh1, ["oh1"], ["oh12b0"])
            self.CP("pool", oh12b[:, 32:64], oh2, ["oh2"], ["oh12b1"])
            yield
            self.MM(pRK[:, 0:64], self.ltb, oh12b, True, True, ["ltb", "oh12b0", "oh12b1"], ["pRK"])
            self.MM(pRK[:, 64:128], self.onesbb, oh12b, True, True, ["onesbb", "oh12b0", "oh12b1"], ["pRK"])
            self.CP("dve", csb, pRK[:, 64:128], ["pRK"], ["cs"])
            self.TT("dve", tr0, pRK[:, 0:32], base, ALU.add, ["pRK", "base"], ["tr0"])
            self.TT("dve", tr0, tr0, oh1, ALU.mult, ["tr0", "oh1"], ["tr0"])
            self.RED(rt[:, t, 4:5], tr0, ALU.add, ["tr0"], ["rt"])
            self.TT("dve", tr1, pRK[:, 32:64], base, ALU.add, ["pRK", "base"], ["tr1"])
            self.TT("dve", tr1, tr1, csb[:, 0:32], ALU.add, ["tr1", "cs"], ["tr1"])
            self.TT("dve", tr1, tr1, oh2, ALU.mult, ["tr1", "oh2"], ["tr1"])
            self.RED(rt[:, t, 5:6], tr1, ALU.add, ["tr1"], ["rt"])
            self.TT("dve", base, base, csb[:, 0:32], ALU.add, ["base", "cs"], ["base"])
            self.TT("dve", base, base, csb[:, 32:64], ALU.add, ["base", "cs"], ["base"])
            self.TT("dve", tr0, oh1, iota, ALU.mult, ["oh1", "iota", "tr0"], ["tr0"])
            self.RED(rt[:, t, 0:1], tr0, ALU.add, ["tr0"], ["rt"])
            self.TT("dve", tr1, oh2, iota, ALU.mult, ["oh2", "iota", "tr1"], ["tr1"])
            self.RED(rt[:, t, 1:2], tr1, ALU.add, ["tr1"], ["rt"])
            self.CP("dve", rt[:, t, 2:3], w1g, ["w1g"], ["rt"])
            self.CP("dve", rt[:, t, 3:4], w2g, ["w2g"], ["rt"])

        ntl_ = len(tiles)
        load(0)
        if ntl_ > 1:
            load(1)
        norm1(0)
        for i in range(ntl_):
            gen = stageB(i - 1) if i > 0 else None
            stageA(i, gen)
            if gen is not None:
                for _ in gen:
                    pass
        for _ in stageB(ntl_ - 1):
            pass

    def phase_p4(self, l):
        cfg, A, I = self.cfg, self.A, self.I
        NT, NTL = cfg.NT, cfg.NTL
        last = l == cfg.DEPTH - 1
        tiles = list(range(NT)) if self.with_ctx else list(range(NTL))
        ntl = len(tiles)
        NBLK = 2 * ntl + 32
        assert NBLK <= NBLK_MAX
        rt, base, iota = self.rt, self.base, self.iota
        m0 = A.mark()
        nbi = A.alloc([32], I32)
        pcnt = A.alloc([32], F32)
        pa = A.alloc([32], F32)
        pb = A.alloc([32], F32)
        bs = A.alloc([32], F32)
        oh3 = A.alloc([NT, 32], F32)
        sl = A.alloc([NT, 2], F32)
        cmp = A.alloc([NBLK_MAX, 32], F32)
        ble = A.alloc([NBLK_MAX], F32)
        zt = A.alloc([8192], BF16)
        h2t = [A.alloc([D], BF16) for _ in range(2)]
        self.MEMSET("pool", zt, 0.0, ["zt"])
        xsz = self.XS.rearrange("(p b) f -> p (b f)", p=128)
        tot = NBLK_MAX * D
        for c0 in range(0, tot, 8192):
            c1_ = min(tot, c0 + 8192)
            self.DMA("sp", xsz[:, c0:c1_], zt[:, 0:c1_ - c0], ["zt"], ["XS"])
        self.TS("dve", pcnt, base, 1.0 / 128, 0.496, ALU.mult, ALU.add, ["base"], ["pcnt"])
        self.CP("dve", nbi, pcnt, ["pcnt"], ["nbi"])
        self.CP("dve", pcnt, nbi, ["nbi"], ["pcnt"])
        self.TS("dve", pcnt, pcnt, 128.0, None, ALU.mult, None, ["pcnt"], ["pcnt"])
        self.CP("dve", pa, pcnt, ["pcnt"], ["pa"])
        src, dst, sk, dk = pa, pb, "pa", "pb"
        for sft in (1, 2, 4, 8, 16):
            self.CP("dve", dst[:, 0:sft], src[:, 0:sft], [sk], [dk])
            self.TT("dve", dst[:, sft:32], src[:, sft:32], src[:, 0:32 - sft], ALU.add, [sk], [dk])
            src, dst, sk, dk = dst, src, dk, sk
        pe, pek = src, sk
        self.TT("dve", bs, pe, pcnt, ALU.subtract, [pek, "pcnt"], ["bs"])
        for k in range(2):
            self.TT("dve", oh3, iota.unsqueeze(1).to_broadcast([128, NT, 32]),
                    rt[:, :, k:k + 1].to_broadcast([128, NT, 32]), ALU.is_equal, ["iota", "rt"], ["oh3"])
            self.TT("dve", oh3, oh3, bs.unsqueeze(1).to_broadcast([128, NT, 32]), ALU.mult, ["oh3", "bs"], ["oh3"])
            self.RED(sl[:, :, k], oh3, ALU.add, ["oh3"], ["sl%d" % k])
            self.TT("dve", sl[:, :, k], sl[:, :, k], rt[:, :, 4 + k], ALU.add, ["sl%d" % k, "rt"], ["sl%d" % k])
        self.CP("dve", self.slot_i, sl, ["sl0", "sl1"], ["slot_i"])
        self.TT("dve", cmp, self.thr.unsqueeze(2).to_broadcast([128, NBLK_MAX, 32]),
                pe.unsqueeze(1).to_broadcast([128, NBLK_MAX, 32]), ALU.is_ge, ["thr", pek], ["cmp"])
        self.RED(ble, cmp, ALU.add, ["cmp"], ["ble"])
        self.TS("dve", ble, ble, 31.0, 128.0, ALU.min, ALU.mult, ["ble"], ["ble"])
        self.TS("dve", ble, ble, self.pidx[:, 0:1], float(l * NE * 128), ALU.add, ALU.add, ["ble", "pidx"], ["ble"])
        self.CP("dve", self.widx, ble, ["ble"], ["widx"])
        for i, t in enumerate(tiles):
            hb, hk = h2t[i % 2], "h2t%d" % (i % 2)
            self.DMA("sp", hb, self.H2[t * 128:(t + 1) * 128, :], (), [hk])
            for k in range(2):
                self.S.add("pool", (lambda hb=hb, t=t, k=k: (lambda e: e.indirect_dma_start(
                    out=self.XS[:, :], out_offset=bass.IndirectOffsetOnAxis(ap=self.slot_i[:, t, k:k + 1], axis=0),
                    in_=hb, in_offset=None)))(), [hk, "slot_i", "XS"], ["XSs"], dma=True)
        self.S.barrier()
        A.release(m0)
        NWR = 4
        w13 = [A.alloc([8, 512], BF16) for _ in range(NWR)]
        w2 = [A.alloc([2, D], BF16) for _ in range(NWR)]
        xs = [A.alloc([D], BF16) for _ in range(NWR)]
        xsT = [A.alloc([8, 128], BF16) for _ in range(2)]
        sb = [A.alloc([256], F32) for _ in range(2)]
        ab = [A.alloc([256], BF16) for _ in range(2)]
        aT = [A.alloc([2, 128], BF16) for _ in range(2)]
        ys = [A.alloc([D], BF16) for _ in range(2)]
        w13src = I["w13r"].rearrange("l r f -> (l r) f")
        w2src = I["w2r"].rearrange("l r f -> (l r) f")
        def wload(b):
            wr_ = b % NWR
            self.S.add("pool", (lambda b=b, wr_=wr_: (lambda e: e.indirect_dma_start(
                out=w13[wr_].rearrange("p k f -> p (k f)"), out_offset=None, in_=w13src,
                in_offset=bass.IndirectOffsetOnAxis(ap=self.widx[:, b:b + 1], axis=0))))(), ["widx"], ["w13_%d" % wr_], dma=True)
            self.S.add("pool", (lambda b=b, wr_=wr_: (lambda e: e.indirect_dma_start(
                out=w2[wr_].rearrange("p c f -> p (c f)"), out_offset=None, in_=w2src,
                in_offset=bass.IndirectOffsetOnAxis(ap=self.widx[:, b:b + 1], axis=0))))(), ["widx"], ["w2_%d" % wr_], dma=True)
            self.DMA("sp", xs[wr_], self.XS[b * 128:(b + 1) * 128, :], (), ["xsw%d" % wr_])

        for b in range(min(NWR - 1, NBLK)):
            wload(b)
        for b in range(NBLK):
            if b + NWR - 1 < NBLK:
                wload(b + NWR - 1)
            r = b % 2
            wr_ = b % NWR
            PTx = self.bank_bf(r)
            ptxk = "PTx%d" % r
            psH = self.bank(2 + r)
            phk = "psH%d" % r
            PTa = self.bank_bf(4)
            psO = self.banks[3]
            for k in range(8):
                self.TR(PTx[:, k * 128:(k + 1) * 128], xs[wr_][:, k * 128:(k + 1) * 128], self.ident_b, ["xsw%d" % wr_, "ident_b"], [ptxk])
            self.CP("act", xsT[r], PTx[:, :].rearrange("p (k m) -> p k m", k=8), [ptxk], ["xsT%d" % r])
            for k in range(8):
                self.MM(psH, xsT[r][:, k, :], w13[wr_][:, k, :], k == 0, k == 7, ["xsT%d" % r, "w13_%d" % wr_], [phk])
            self.ACT(sb[r], psH[:, 0:256], AF.Silu, [phk], ["sb%d" % r])
            self.TT("dve", ab[r], sb[r], psH[:, 256:512], ALU.mult, ["sb%d" % r, phk], ["ab%d" % r])
            for c in range(2):
                self.TR(PTa[:, c * 128:(c + 1) * 128], ab[r][:, c * 128:(c + 1) * 128], self.ident_b, ["ab%d" % r, "ident_b"], ["PTa"])
            self.CP("act", aT[r], PTa[:, 0:256].rearrange("p (c m) -> p c m", c=2), ["PTa"], ["aT%d" % r])
            for half in range(2):
                cs_ = slice(half * 512, (half + 1) * 512)
                for c in range(2):
                    self.MM(psO[:, cs_], aT[r][:, c, :], w2[wr_][:, c, cs_], c == 0, c == 1, ["aT%d" % r, "w2_%d" % wr_], ["psO"])
            self.CP("act", ys[r], psO[:, :], ["psO"], ["ys%d" % r])
            self.DMA("sp", self.YS[b * 128:(b + 1) * 128, :], ys[r], ["ys%d" % r], [])
        self.S.barrier()
        A.release(m0)
        y0 = [A.alloc([D], BF16) for _ in range(2)]
        y1 = [A.alloc([D], BF16) for _ in range(2)]
        xt2 = [A.alloc([D], F32) for _ in range(2)]
        tmp = A.alloc([D], F32)
        junk = A.alloc([D], BF16)
        ssq = A.alloc([2], F32)
        rstd = A.alloc([2], F32)
        if last:
            self.gfin = A.alloc([D], F32)
            self.DMA("sp", self.gfin, I["g_final"].partition_broadcast(128), (), ["gfin"])
        for i, t in enumerate(tiles):
            b = i % 2
            w = 0 if t < NTL else 1
            tok = slice(t * 128, (t + 1) * 128)
            for k, yb in ((0, y0[b]), (1, y1[b])):
                self.S.add("pool", (lambda yb=yb, t=t, k=k: (lambda e: e.indirect_dma_start(
                    out=yb, out_offset=None, in_=self.YS[:, :],
                    in_offset=bass.IndirectOffsetOnAxis(ap=self.slot_i[:, t, k:k + 1], axis=0))))(), ["slot_i"], ["y%d_%d" % (k, b)], dma=True)
            self.DMA("sp", xt2[b], self.xcur[tok, :], (), ["xt%d" % b])
            self.TS("dve", tmp, y0[b], rt[:, t, 2:3], None, ALU.mult, None, ["y0_%d" % b, "rt"], ["tmp"])
            self.STT("dve", tmp, y1[b], rt[:, t, 3:4], tmp, ALU.mult, ALU.add, ["y1_%d" % b, "rt", "tmp"], ["tmp"])
            self.TT("pool", tmp, tmp, self.mod[w][:, 5 * D:6 * D], ALU.mult, ["tmp", "mod%d" % w], ["tmp"])
            self.TT("pool", xt2[b], tmp, xt2[b], ALU.add, ["tmp", "xt%d" % b], ["xt%d" % b])
            if not last:
                self.DMA("sp", self.xcur[tok, :], xt2[b], ["xt%d" % b], [])
            else:
                self.rstd_of(xt2[b], D, junk, ssq[:, 0:1], rstd[:, 0:1], ["xt%d" % b], "f")
                self.STT("dve", xt2[b], xt2[b], rstd[:, 0:1], self.gfin, ALU.mult, ALU.mult, ["xt%d" % b, "rstdf", "gfin"], ["xt%d" % b])
                o = self.DMA("sp", self.out[tok, :], xt2[b], ["xt%d" % b], [])
                self.S.out_ops.append(o)


_CACHE = {}


def _get_prog(cfg_key):
    if cfg_key not in _CACHE:
        cfg = Cfg(*cfg_key)
        p = Prog(cfg)
        p.build()
        _CACHE[cfg_key] = p
    return _CACHE[cfg_key]


def make_in_maps(prog, inputs, ncores):
    cfg = prog.cfg
    f = lambda a: np.ascontiguousarray(np.asarray(a, dtype=np.float32))
    shared = {}
    for k in ("w_mod", "b_mod", "g_norm_mix", "w_in", "g_q_a", "w_q_b", "g_kv_a", "w_kv_b", "sink_b", "g_q_d", "g_k_d",
              "w_gate", "w_branch", "w_out", "g_norm_ffn", "w_group", "b_group", "w_router", "b_router"):
        shared[k] = f(inputs[k])
    L = cfg.DEPTH
    shared["b_gate"] = f(inputs["b_gate"]).reshape(L, 4 * D)
    shared["g_final"] = f(inputs["g_final"]).reshape(1, D)
    shared["c_ctx"] = f(inputs["c_ctx"]).reshape(1, D)
    rpb = f(inputs["rpb_c"])
    shared["biasg"] = np.ascontiguousarray(rpb[:, :, prog.DR, prog.DC[None]])
    w13 = np.concatenate([f(inputs["w_ff1"]), f(inputs["w_ff3"])], axis=-1)
    w13 = w13.reshape(L, NE, 8, 128, 512).transpose(0, 1, 3, 2, 4)
    shared["w13r"] = np.ascontiguousarray(w13).reshape(L, NE * 128, 4096)
    w2 = f(inputs["w_ff2"]).reshape(L, NE, 2, 128, D).transpose(0, 1, 3, 2, 4)
    shared["w2r"] = np.ascontiguousarray(w2).reshape(L, NE * 128, 2048)
    shared["iota32"] = np.arange(32, dtype=np.float32).reshape(1, 32)
    shared["ltri"] = np.triu(np.ones((128, 128), np.float32), k=1)
    shared["pidx"] = np.arange(128, dtype=np.float32).reshape(128, 1)
    shared["thr"] = (128.0 * np.arange(NBLK_MAX, dtype=np.float32)).reshape(1, NBLK_MAX)
    shared["ident"] = np.eye(128, dtype=np.float32)
    shared["ropetab"] = rope_tab(cfg)
    shared["maskb"] = mask_b()
    shared["maskc"] = prog.maskc_np
    x = f(inputs["x"])
    c = f(inputs["c"])
    ctx = f(inputs["ctx"])
    maps = []
    for b in range(ncores):
        m = dict(shared)
        m["x"] = x[b]
        m["c"] = c[b:b + 1]
        m["ctx"] = ctx[b]
        maps.append(m)
    return maps


def kernel(**inputs):
    x = np.asarray(inputs["x"])
    B, SEQ, _ = x.shape
    CTX = np.asarray(inputs["ctx"]).shape[1]
    DEPTH = np.asarray(inputs["w_mod"]).shape[0]
    prog = _get_prog((SEQ, CTX, DEPTH))
    maps = make_in_maps(prog, inputs, B)
    res = run_bass_kernel_spmd(prog.nc, maps, core_ids=list(range(B)))
    out = np.stack([np.asarray(r["out"], dtype=np.float32) for r in res.results], axis=0)
    return out
```

```python
import contextlib
import numpy as np
import concourse.bass as bass
import concourse.mybir as mybir
from concourse.bass_utils import run_bass_kernel_spmd

F32 = mybir.dt.float32
BF16 = mybir.dt.bfloat16
AF = mybir.ActivationFunctionType
ALU = mybir.AluOpType
AX = mybir.AxisListType
I32 = mybir.dt.int32
NBLK_MAX = 100

SAME_ENGINE_SYNC = True
NDMA_SEMS = 10
import os
MAXOPS = int(os.environ.get('KMAXOPS', '100000000'))
D = 1024
EPS = 1e-6
NEG = -30000.0
IN_COLS = 2208
NE = 32


class Op:
    __slots__ = ("eng", "fn", "deps", "flag", "semv", "sem", "is_dma")

    def __init__(self, eng, fn, is_dma):
        self.eng = eng
        self.fn = fn
        self.deps = []
        self.flag = False
        self.semv = 0
        self.sem = None
        self.is_dma = is_dma


class Sched:
    ENGS = ("pe", "act", "dve", "pool", "sp")

    def __init__(self):
        self.ops = {e: [] for e in self.ENGS}
        self.last_w = {}
        self.readers = {}
        self.pending_barrier = {e: None for e in self.ENGS}
        self.out_ops = []

    def add(self, eng, fn, reads=(), writes=(), dma=False):
        op = Op(eng, fn, dma)
        self.nadd = getattr(self, "nadd", 0) + 1
        if self.nadd > MAXOPS:
            return op
        deps = {}
        raw = set()
        for k in reads:
            w = self.last_w.get(k)
            if w is not None:
                deps[id(w)] = w
                raw.add(id(w))
        for k in writes:
            w = self.last_w.get(k)
            if w is not None:
                deps[id(w)] = w
            for r in self.readers.get(k, ()):
                deps[id(r)] = r
        for k in reads:
            self.readers.setdefault(k, []).append(op)
        for k in writes:
            self.last_w[k] = op
            self.readers[k] = []
        pb = self.pending_barrier[eng]
        if pb is not None:
            for d in pb:
                deps[id(d)] = d
            self.pending_barrier[eng] = None
        for d in deps.values():
            if d is op:
                continue
            if (not d.is_dma) and d.eng == eng and (not dma) and (eng == "pe" or not SAME_ENGINE_SYNC):
                continue
            d.flag = True
            op.deps.append(d)
        self.ops[eng].append(op)
        return op

    def barrier(self):
        lst = []
        for e in self.ENGS:
            ops = self.ops[e]
            if not ops:
                continue
            nd = 0
            got_c = False
            for op in reversed(ops):
                if op.is_dma:
                    if nd < NDMA_SEMS:
                        lst.append(op)
                        nd += 1
                elif not got_c:
                    lst.append(op)
                    got_c = True
                if nd >= NDMA_SEMS and got_c:
                    break
        for e in self.ENGS:
            prev = self.pending_barrier[e]
            self.pending_barrier[e] = lst if prev is None else (prev + lst)
        self.last_w = {}
        self.readers = {}

    def emit(self, nc):
        final_ops = self.out_ops
        for o in final_ops:
            o.flag = True
        with contextlib.ExitStack() as es:
            csem = {}
            for e in ("pe", "act", "dve", "pool"):
                csem[e] = es.enter_context(nc.semaphore("s_" + e))
            dsem = {}
            for e in ("act", "pool", "sp"):
                dsem[e] = [es.enter_context(nc.semaphore("d_%s%d" % (e, i))) for i in range(NDMA_SEMS)]
            for e in self.ENGS:
                cnt = 0
                dcnt = 0
                dvals = [0] * NDMA_SEMS
                for op in self.ops[e]:
                    if op.is_dma:
                        slot = dcnt % NDMA_SEMS
                        dcnt += 1
                        dvals[slot] += 16
                        op.sem = dsem[e][slot]
                        op.semv = dvals[slot]
                    elif op.flag:
                        cnt += 1
                        op.sem = csem[e]
                        op.semv = cnt
            block = es.enter_context(nc.Block())

            def run_engine(e, eng):
                waited = {}
                prev_dma = [None] * NDMA_SEMS
                dcnt = 0
                for op in self.ops[e]:
                    for d in op.deps:
                        key = id(d.sem)
                        if waited.get(key, 0) >= d.semv:
                            continue
                        eng.wait_ge(d.sem, d.semv)
                        waited[key] = d.semv
                    if op.is_dma:
                        slot = dcnt % NDMA_SEMS
                        dcnt += 1
                        p = prev_dma[slot]
                        if p is not None:
                            key = id(p.sem)
                            if waited.get(key, 0) < p.semv:
                                eng.wait_ge(p.sem, p.semv)
                                waited[key] = p.semv
                        prev_dma[slot] = op
                        ins = op.fn(eng)
                        ins.then_inc(op.sem, 16)
                    else:
                        ins = op.fn(eng)
                        if op.flag:
                            ins.then_inc(op.sem, 1)
                if e == "sp":
                    for o in final_ops:
                        key = id(o.sem)
                        if waited.get(key, 0) < o.semv:
                            eng.wait_ge(o.sem, o.semv)
                            waited[key] = o.semv

            @block.tensor
            def _(eng):
                run_engine("pe", eng)

            @block.scalar
            def _(eng):
                run_engine("act", eng)

            @block.vector
            def _(eng):
                run_engine("dve", eng)

            @block.gpsimd
            def _(eng):
                run_engine("pool", eng)

            @block.sync
            def _(eng):
                run_engine("sp", eng)


class Arena:
    def __init__(self, t, nbytes):
        self.t = t
        self.cap = nbytes
        self.off = 0
        self.peak = 0

    def alloc(self, free_shape, dt):
        n = 1
        for s in free_shape:
            n *= s
        esz = 2 if dt == BF16 else 4
        off = (self.off + 63) // 64 * 64
        nb = n * esz
        assert off + nb <= self.cap, ("arena overflow", off + nb, self.cap)
        self.off = off + nb
        self.peak = max(self.peak, self.off)
        v = self.t[:, off // 2:(off + nb) // 2]
        if dt != BF16:
            v = v.bitcast(dt)
        if len(free_shape) > 1:
            names = ["a%d" % i for i in range(len(free_shape))]
            kw = {names[i]: free_shape[i] for i in range(len(free_shape))}
            v = v.rearrange("p (" + " ".join(names) + ") -> p " + " ".join(names), **kw)
        return v

    def mark(self):
        return self.off

    def release(self, m):
        self.off = m


class Cfg:
    def __init__(self, SEQ=4096, CTX=256, DEPTH=2):
        self.SEQ = SEQ
        self.CTX = CTX
        self.DEPTH = DEPTH
        self.T = SEQ + CTX
        self.NT = self.T // 128
        self.NTL = SEQ // 128
        self.NTC = CTX // 128
        self.ROWS = SEQ // 64
        self.NG = SEQ // 512


def rope_tab(cfg):
    t = np.arange(cfg.SEQ)
    rows = (t // 64).astype(np.float32)
    cols = (t % 64).astype(np.float32)
    out = np.zeros((cfg.T, 192), np.float32)
    out[cfg.SEQ:, 0:64] = 1.0
    out[cfg.SEQ:, 128:160] = 1.0
    off = 0
    for R in (64, 32):
        half = R // 2
        inv = (np.float32(10000.0) ** (-np.arange(0, half, 2, dtype=np.float32) / np.float32(half))).astype(np.float32)
        ar = rows[:, None] * inv
        ac = cols[:, None] * inv
        cr, sr, cc, sc = np.cos(ar), np.sin(ar), np.cos(ac), np.sin(ac)
        C = np.concatenate([cr, cr, cc, cc], axis=1)
        S_ = np.concatenate([-sr, sr, -sc, sc], axis=1)
        out[:cfg.SEQ, off:off + R] = C
        out[:cfg.SEQ, off + R:off + 2 * R] = S_
        off += 2 * R
    return out.astype(np.float32)


def mask_b():
    k = np.arange(128)[:, None]
    q = np.arange(512)[None, :]
    out = np.zeros((6, 128, 512), np.float32)
    for r in range(6):
        rel = r - 1
        ok = np.abs(q - 128 * rel - k) <= 128
        out[r] = np.where(ok, 0.0, NEG)
    return out


def c_tables(cfg):
    rows = cfg.ROWS
    kh = min(8, rows)
    k = np.arange(128)[:, None]
    q = np.arange(512)[None, :]
    k_col = k % 64
    q_col = q % 64
    c_start = np.clip(q_col - 8, 0, 64 - 16)
    col_ok = (k_col >= c_start) & (k_col < c_start + 16)
    DC = (np.clip(k_col - q_col, -15, 15) + 15).astype(np.int64) + np.zeros((128, 512), np.int64)
    DR = np.zeros((8, 128, 512), np.int64)
    for r in range(8):
        rel = r - 2
        d = 2 * rel + k // 64 - q // 64
        DR[r] = np.clip(d, -7, 7) + 7
    masks = []
    case_of_g = []
    valid = []
    for g in range(cfg.NG):
        m = np.full((8, 128, 512), NEG, np.float32)
        v = []
        for r in range(8):
            kt = 4 * g + r - 2
            if kt < 0 or kt >= cfg.NTL:
                continue
            k_row = 2 * kt + k // 64
            q_row = 8 * g + q // 64
            r_start = np.clip(q_row - kh // 2, 0, rows - kh)
            row_ok = (k_row >= r_start) & (k_row < r_start + kh)
            ok = row_ok & col_ok
            if ok.any():
                v.append(r)
            m[r] = np.where(ok, 0.0, NEG)
        found = None
        for ci, mm in enumerate(masks):
            if np.array_equal(mm, m):
                found = ci
                break
        if found is None:
            masks.append(m)
            found = len(masks) - 1
        case_of_g.append(found)
        valid.append(v)
    return np.stack(masks).astype(np.float32), case_of_g, valid, DR, DC


class Prog:
    def __init__(self, cfg):
        self.cfg = cfg
        self.S = Sched()
        self.nc = bass.Bass("TRN2", target_bir_lowering=False)
        self.maskc_np, self.case_of_g, self.valid_c, self.DR, self.DC = c_tables(cfg)
        self.ncase = self.maskc_np.shape[0]

    def MM(self, out, lhsT, rhs, start, stop, r, w):
        return self.S.add("pe", lambda e: e.matmul(out, lhsT=lhsT, rhs=rhs, start=start, stop=stop), r, w)

    def TR(self, out, in_, ident, r, w):
        return self.S.add("pe", lambda e: e.transpose(out=out, in_=in_, identity=ident), r, w)

    def ACT(self, out, in_, func, r, w, scale=1.0, bias=0.0, accum=None):
        if accum is None:
            return self.S.add("act", lambda e: e.activation(out=out, in_=in_, func=func, bias=bias, scale=scale), r, w)
        return self.S.add("act", lambda e: e.activation(out=out, in_=in_, func=func, bias=bias, scale=scale,
                                                        accum_out=accum), r, w)

    def TT(self, eng, out, in0, in1, op, r, w):
        return self.S.add(eng, lambda e: e.tensor_tensor(out=out, in0=in0, in1=in1, op=op), r, w)

    def TS(self, eng, out, in0, s1, s2, op0, op1, r, w):
        if s2 is None:
            return self.S.add(eng, lambda e: e.tensor_scalar(out=out, in0=in0, scalar1=s1, scalar2=None, op0=op0), r, w)
        return self.S.add(eng, lambda e: e.tensor_scalar(out=out, in0=in0, scalar1=s1, scalar2=s2, op0=op0, op1=op1), r, w)

    def STT(self, eng, out, in0, scalar, in1, op0, op1, r, w):
        return self.S.add(eng, lambda e: e.scalar_tensor_tensor(out=out, in0=in0, scalar=scalar, in1=in1,
                                                                 op0=op0, op1=op1), r, w)

    def CP(self, eng, out, in_, r, w):
        if eng == "act":
            return self.S.add("act", lambda e: e.copy(out=out, in_=in_), r, w)
        return self.S.add(eng, lambda e: e.tensor_copy(out=out, in_=in_), r, w)

    def RED(self, out, in_, op, r, w):
        return self.S.add("dve", lambda e: e.tensor_reduce(out=out, in_=in_, axis=AX.X, op=op), r, w)

    def RCP(self, out, in_, r, w):
        return self.S.add("dve", lambda e: e.reciprocal(out=out, in_=in_), r, w)

    def DMA(self, q, out, in_, r, w):
        return self.S.add(q, lambda e: e.dma_start(out=out, in_=in_), r, w, dma=True)

    def MEMSET(self, eng, ap, val, w):
        return self.S.add(eng, lambda e: e.memset(ap, val), (), w)

    def build(self):
        cfg = self.cfg
        nc = self.nc
        L = cfg.DEPTH
        T, NT, NTL = cfg.T, cfg.NT, cfg.NTL

        def din(name, shape):
            return nc.dram_tensor(name, list(shape), F32, kind="ExternalInput").ap()

        self.I = I = {}
        I["x"] = din("x", [cfg.SEQ, D])
        I["c"] = din("c", [1, D])
        I["ctx"] = din("ctx", [cfg.CTX, D])
        I["c_ctx"] = din("c_ctx", [1, D])
        I["w_mod"] = din("w_mod", [L, D, 6 * D])
        I["b_mod"] = din("b_mod", [L, 6 * D])
        I["g_norm_mix"] = din("g_norm_mix", [L, D])
        I["w_in"] = din("w_in", [L, D, IN_COLS])
        I["g_q_a"] = din("g_q_a", [L, 256])
        I["w_q_b"] = din("w_q_b", [L, 256, 384])
        I["g_kv_a"] = din("g_kv_a", [L, 128])
        I["w_kv_b"] = din("w_kv_b", [L, 128, 512])
        I["sink_b"] = din("sink_b", [L, 4])
        I["biasg"] = din("biasg", [L, 4, 8, 128, 512])
        I["g_q_d"] = din("g_q_d", [L, 64])
        I["g_k_d"] = din("g_k_d", [L, 64])
        I["w_gate"] = din("w_gate", [L, 4, D, D])
        I["b_gate"] = din("b_gate", [L, 4 * D])
        I["w_branch"] = din("w_branch", [L, 4, 256, D])
        I["w_out"] = din("w_out", [L, D, D])
        I["g_norm_ffn"] = din("g_norm_ffn", [L, D])
        I["w_group"] = din("w_group", [L, D, 4])
        I["b_group"] = din("b_group", [L, 4])
        I["w_router"] = din("w_router", [L, D, 32])
        I["b_router"] = din("b_router", [L, 32])
        I["w13r"] = din("w13r", [L, NE * 128, 4096])
        I["w2r"] = din("w2r", [L, NE * 128, 2048])
        I["iota32"] = din("iota32", [1, 32])
        I["ltri"] = din("ltri", [128, 128])
        I["pidx"] = din("pidx", [128, 1])
        I["thr"] = din("thr", [1, NBLK_MAX])
        I["g_final"] = din("g_final", [1, D])
        I["ident"] = din("ident", [128, 128])
        I["ropetab"] = din("ropetab", [T, 192])
        I["maskb"] = din("maskb", [6, 128, 512])
        I["maskc"] = din("maskc", [self.ncase, 8, 128, 512])
        self.out = nc.dram_tensor("out", [cfg.SEQ, D], F32, kind="ExternalOutput").ap()

        def dscr(name, shape, dt):
            return nc.dram_tensor(name, list(shape), dt, kind="Internal").ap()

        self.xcur = dscr("xcur", [T, D], F32)
        self.QT = [dscr("QT_A", [96, 4, T], BF16)] + [dscr("QT_%d" % n, [128, 2, T], BF16) for n in (1, 2, 3)]
        self.KT = [dscr("KT_A", [96, 4, T], BF16)] + [dscr("KT_%d" % n, [128, 2, T], BF16) for n in (1, 2, 3)]
        self.VA = [dscr("VA_%d" % n, [128, NT, 4, 128], BF16) for n in range(4)]
        self.OT = dscr("OT", [8, 128, T], BF16)
        self.H2 = dscr("H2", [T, D], BF16)
        self.XS = dscr("XS", [NBLK_MAX * 128, D], BF16)
        self.YS = dscr("YS", [NBLK_MAX * 128, D], BF16)

        with contextlib.ExitStack() as es:
            ARENA_BYTES = 207 * 1024
            at = es.enter_context(nc.sbuf_tensor("arena", [128, ARENA_BYTES // 2], BF16))
            self.A = Arena(at, ARENA_BYTES)
            self.banks = [es.enter_context(nc.psum_tensor("pb%d" % i, [128, 1024], F32)) for i in range(4)]
            self.setup()
            for l in range(L):
                self.layer(l)
            self.S.emit(nc)
        return nc

    def bank(self, i):
        return self.banks[i // 2][:, (i % 2) * 512:(i % 2 + 1) * 512]

    def bank_bf(self, i):
        return self.bank(i).bitcast(BF16)

    def setup(self):
        A, I = self.A, self.I
        self.ident_f = A.alloc([128], F32)
        self.ident_b = A.alloc([128], BF16)
        self.ones_f = A.alloc([128], F32)
        self.ones_b = A.alloc([128], BF16)
        self.DMA("sp", self.ident_f, I["ident"], (), ["ident_f"])
        self.DMA("pool", self.ident_b, I["ident"], (), ["ident_b"])
        self.MEMSET("dve", self.ones_f, 1.0, ["ones_f"])
        self.MEMSET("dve", self.ones_b, 1.0, ["ones_b"])
        self.mod = [A.alloc([6 * D], F32), A.alloc([6 * D], F32)]
        self.rt = A.alloc([self.cfg.NT, 8], F32)
        self.base = A.alloc([32], F32)
        self.iota = A.alloc([32], F32)
        self.ltb = A.alloc([128], BF16)
        self.onesbb = A.alloc([128], BF16)
        self.pidx = A.alloc([1], F32)
        self.thr = A.alloc([NBLK_MAX], F32)
        self.slot_i = A.alloc([self.cfg.NT, 2], I32)
        self.widx = A.alloc([NBLK_MAX], I32)
        self.DMA("sp", self.iota, I["iota32"].partition_broadcast(128), (), ["iota"])
        self.DMA("pool", self.ltb, I["ltri"], (), ["ltb"])
        self.MEMSET("dve", self.onesbb, 1.0, ["onesbb"])
        self.DMA("sp", self.pidx, I["pidx"], (), ["pidx"])
        self.DMA("sp", self.thr, I["thr"].partition_broadcast(128), (), ["thr"])
        self.pers_mark = A.mark()

    def layer(self, l):
        import os
        cfg = self.cfg
        stop = int(os.environ.get("KSTOP", "99"))
        self.with_ctx = l < cfg.DEPTH - 1
        phases = [self.phase_mod, self.phase_p1, self.phase_p2, self.phase_p3, self.phase_p4]
        for pi, ph in enumerate(phases):
            if l * 5 + pi > stop:
                return
            self.S.barrier()
            self.A.release(self.pers_mark)
            ph(l)

    def phase_mod(self, l):
        A, I = self.A, self.I
        cb = [A.alloc([D], F32), A.alloc([D], F32)]
        lh = [A.alloc([8, 128], F32), A.alloc([8, 128], F32)]
        bmod = A.alloc([6 * D], F32)
        gm = A.alloc([D], F32)
        gf = A.alloc([D], F32)
        wm = [A.alloc([8, 512], F32), A.alloc([8, 512], F32)]
        self.DMA("sp", cb[0], I["c"].partition_broadcast(128), (), ["cb0"])
        self.DMA("sp", cb[1], I["c_ctx"].partition_broadcast(128), (), ["cb1"])
        self.DMA("sp", bmod[0:1, :], I["b_mod"][l:l + 1, :], (), ["bmod"])
        self.DMA("sp", gm, I["g_norm_mix"][l:l + 1, :].partition_broadcast(128), (), ["gm"])
        self.DMA("sp", gf, I["g_norm_ffn"][l:l + 1, :].partition_broadcast(128), (), ["gf"])
        for w in range(2):
            self.ACT(cb[w], cb[w], AF.Silu, ["cb%d" % w], ["cb%d" % w])
            for k in range(8):
                bk = self.banks[w][:, k * 128:(k + 1) * 128]
                self.TR(bk, cb[w][:, k * 128:(k + 1) * 128], self.ident_f, ["cb%d" % w, "ident_f"], ["pbk%d" % w])
            self.CP("act", lh[w], self.banks[w][:, :].rearrange("p (k m) -> p k m", k=8), ["pbk%d" % w], ["lh%d" % w])
        for j in range(12):
            wmj = wm[j % 2]
            wk = "wm%d" % (j % 2)
            self.DMA("sp", wmj, I["w_mod"][l, :, j * 512:(j + 1) * 512].rearrange("(k p) n -> p k n", p=128), (), [wk])
            for w in range(2):
                bi = 4 + (j * 2 + w) % 4
                bk = self.bank(bi)
                bkk = "bank%d" % bi
                for k in range(8):
                    self.MM(bk, lh[w][:, k, :], wmj[:, k, :], k == 0, False, ["lh%d" % w, wk], [bkk])
                self.MM(bk, self.ones_f[0:1, :], bmod[0:1, j * 512:(j + 1) * 512], False, True, ["ones_f", "bmod"], [bkk])
                self.CP("dve" if w == 0 else "act", self.mod[w][:, j * 512:(j + 1) * 512], bk, [bkk], ["mod%d" % w])
        for w in range(2):
            m = self.mod[w]
            self.STT("dve", m[:, D:2 * D], m[:, D:2 * D], 1.0, gm, ALU.add, ALU.mult, ["mod%d" % w, "gm"], ["mod%d" % w])
            self.STT("dve", m[:, 4 * D:5 * D], m[:, 4 * D:5 * D], 1.0, gf, ALU.add, ALU.mult, ["mod%d" % w, "gf"], ["mod%d" % w])

    def x_src(self, l, t):
        cfg = self.cfg
        if l == 0:
            if t < cfg.NTL:
                return self.I["x"][t * 128:(t + 1) * 128, :]
            return self.I["ctx"][(t - cfg.NTL) * 128:(t - cfg.NTL + 1) * 128, :]
        return self.xcur[t * 128:(t + 1) * 128, :]

    def rstd_of(self, src, n, junk, ssq, rstd, rk, tag, jk="junk"):
        self.ACT(junk, src, AF.Square, rk, [jk, "ssq" + tag], accum=ssq)
        self.ACT(ssq, ssq, AF.Sqrt, ["ssq" + tag], ["ssq" + tag], scale=1.0 / n, bias=EPS)
        self.RCP(rstd, ssq, ["ssq" + tag], ["rstd" + tag])

    def norm_mod(self, xt, xk, w, goff, soff, junk, tmp, out, outk, ssq, rstd, tag, jk="junk", tk="tmp"):
        self.rstd_of(xt, D, junk, ssq, rstd, [xk], tag, jk=jk)
        m = self.mod[w]
        self.STT("dve", tmp, xt, rstd, m[:, goff:goff + D], ALU.mult, ALU.mult, [xk, "rstd" + tag, "mod%d" % w], [tk])
        self.TT("dve", out, tmp, m[:, soff:soff + D], ALU.add, [tk, "mod%d" % w], [outk])

    def transpose8(self, src, srck, bank_bf, bankk, dst, dstk, nblk=8, ident=None, identk="ident_b"):
        ident = self.ident_b if ident is None else ident
        srcks = [srck] if isinstance(srck, str) else list(srck)
        for k in range(nblk):
            self.TR(bank_bf[:, k * 128:(k + 1) * 128], src[:, k * 128:(k + 1) * 128], ident, srcks + [identk], [bankk])
        self.CP("act", dst, bank_bf[:, 0:nblk * 128].rearrange("p (k m) -> p k m", k=nblk), [bankk], [dstk])

    def rope(self, src, srck, H, R, tab, tabk, coff, soff, t1, t2, outs, tag):
        w = R // 4
        C = tab[:, coff:coff + R]
        Sg = tab[:, soff:soff + R].rearrange("p (a b w) -> p a b w", a=2, b=2)
        t1v = t1[:, 0:H * R].rearrange("p (h r) -> p h r", h=H)
        t2v = t2[:, 0:H * R].rearrange("p (h r) -> p h r", h=H)
        self.TT("dve", t1v, src, C.unsqueeze(1).to_broadcast([128, H, R]), ALU.mult, [srck, tabk], ["t1" + tag])
        s5 = src.rearrange("p h (a b w) -> p h a b w", a=2, b=2)
        t5 = t2v.rearrange("p h (a b w) -> p h a b w", a=2, b=2)
        self.TT("dve", t5[:, :, :, 0, :], s5[:, :, :, 1, :], Sg[:, :, 0, :].unsqueeze(1).to_broadcast([128, H, 2, w]),
                ALU.mult, [srck, tabk], ["t2a" + tag])
        self.TT("dve", t5[:, :, :, 1, :], s5[:, :, :, 0, :], Sg[:, :, 1, :].unsqueeze(1).to_broadcast([128, H, 2, w]),
                ALU.mult, [srck, tabk], ["t2b" + tag])
        for (dst, h0, h1, dk) in outs:
            self.TT("dve", dst, t1v[:, h0:h1, :], t2v[:, h0:h1, :], ALU.add, ["t1" + tag, "t2a" + tag, "t2b" + tag], [dk])

    def phase_p1(self, l):
        cfg, A, I = self.cfg, self.A, self.I
        NT, NTL = cfg.NT, cfg.NTL
        win = A.alloc([8, IN_COLS], BF16)
        for k in range(8):
            self.DMA("pool", win[:, k, :], I["w_in"][l, k * 128:(k + 1) * 128, :], (), ["win"])
        wqb = A.alloc([2, 384], BF16)
        self.DMA("pool", wqb, I["w_q_b"][l].rearrange("(k p) n -> p k n", p=128), (), ["wqb"])
        wkvb = A.alloc([512], BF16)
        self.DMA("pool", wkvb, I["w_kv_b"][l], (), ["wkvb"])
        gqa = A.alloc([256], F32)
        gkv = A.alloc([128], F32)
        gd = A.alloc([6, 64], F32)
        self.DMA("sp", gqa, I["g_q_a"][l:l + 1, :].partition_broadcast(128), (), ["gqa"])
        self.DMA("sp", gkv, I["g_kv_a"][l:l + 1, :].partition_broadcast(128), (), ["gkv"])
        for h in range(6):
            src = I["g_q_d"] if h < 4 else I["g_k_d"]
            self.DMA("sp", gd[:, h, :], src[l:l + 1, :].partition_broadcast(128), (), ["gd"])
        xt2 = [A.alloc([D], F32) for _ in range(2)]
        tab2 = [A.alloc([192], F32) for _ in range(2)]
        junk = A.alloc([D], F32)
        tmp = A.alloc([D], F32)
        hb = A.alloc([D], BF16)
        hT = A.alloc([8, 128], BF16)
        ssq = A.alloc([8], F32)
        rstd = A.alloc([8], F32)
        ssqD = A.alloc([8], F32)
        krs = A.alloc([32], F32)
        qf = A.alloc([384], F32)
        kvf = A.alloc([512], F32)
        bdf = A.alloc([512], F32)
        bdfD = A.alloc([512], F32)
        c2f = A.alloc([256], F32)
        rstdD = A.alloc([8], F32)
        cqkv = A.alloc([384], BF16)
        cT = A.alloc([3, 128], BF16)
        qa = A.alloc([4, 96], BF16)
        ka = A.alloc([4, 96], BF16)
        krr = A.alloc([1, 32], F32)
        t1 = A.alloc([512], F32)
        t2 = A.alloc([512], F32)
        sq = A.alloc([384], F32)
        qn = A.alloc([6, 64], F32)
        qkb = A.alloc([8, 64], BF16)
        qkc = A.alloc([512], BF16)
        stA = [A.alloc([8, 128], BF16) for _ in range(2)]
        stP = [A.alloc([4, 128], BF16) for _ in range(2)]
        va = [[A.alloc([4, 128], BF16) for _ in range(2)] for _ in range(4)]
        for n in range(4):
            for b in range(2):
                self.MEMSET("pool", va[n][b], 1.0, ["va%d_%d" % (n, b)])
        T0, T1 = self.bank_bf(0), self.bank_bf(1)
        pA, pBD, pC1, pC2, pQ, pKV = (self.bank(i) for i in (2, 3, 4, 5, 6, 7))

        def load(t):
            b = t % 2
            self.DMA("sp", xt2[b], self.x_src(l, t), (), ["xt%d" % b])
            self.DMA("sp", tab2[b], I["ropetab"][t * 128:(t + 1) * 128, :], (), ["tab%d" % b])

        import os
        KCUT = int(os.environ.get("KCUT", "99"))
        load(0)
        for t in range(NT):
            if t + 1 < NT:
                load(t + 1)
            b = t % 2
            xt, xk, tab, tabk = xt2[b], "xt%d" % b, tab2[b], "tab%d" % b
            w = 0 if t < NTL else 1
            tok = slice(t * 128, (t + 1) * 128)
            if t == 0:
                self.norm_mod(xt, xk, w, D, 0, junk, tmp, hb, "hb", ssq[:, 0:1], rstd[:, 0:1], "n1")
            self.transpose8(hb, "hb", T0, "T0", hT, "hT")
            if KCUT == 1:
                return
            def proj(dst, dk, c0, c1):
                for k in range(8):
                    self.MM(dst[:, 0:c1 - c0], hT[:, k, :], win[:, k, c0:c1], k == 0, k == 7, ["hT", "win"], [dk])
            proj(pA, "pA", 0, 416)
            proj(pBD, "pBD", 416, 928)
            self.CP("act", bdf, pBD, ["pBD"], ["bdf"])
            proj(pC1, "pC1", 928, 1440)
            proj(pC2, "pC2", 1440, 1696)
            proj(pBD, "pBD", 1696, 2208)
            if t + 1 < NT:
                bn_ = (t + 1) % 2
                self.norm_mod(xt2[bn_], "xt%d" % bn_, 0 if t + 1 < NTL else 1, D, 0, junk, tmp, hb, "hb",
                              ssq[:, 0:1], rstd[:, 0:1], "n1")
            if KCUT == 2:
                return
            self.ACT(junk[:, 0:256], pA[:, 0:256], AF.Square, ["pA"], ["junk", "ssqA0"], accum=ssq[:, 1:2])
            self.ACT(junk[:, 256:384], pA[:, 256:384], AF.Square, ["pA"], ["junk", "ssqA1"], accum=ssq[:, 2:3])
            self.ACT(ssq[:, 1:2], ssq[:, 1:2], AF.Sqrt, ["ssqA0"], ["ssqA0"], scale=1.0 / 256, bias=EPS)
            self.ACT(ssq[:, 2:3], ssq[:, 2:3], AF.Sqrt, ["ssqA1"], ["ssqA1"], scale=1.0 / 128, bias=EPS)
            self.RCP(rstd[:, 1:3], ssq[:, 1:3], ["ssqA0", "ssqA1"], ["rstdA"])
            self.STT("dve", cqkv[:, 0:256], pA[:, 0:256], rstd[:, 1:2], gqa, ALU.mult, ALU.mult, ["pA", "rstdA", "gqa"], ["cqkv0"])
            self.STT("dve", cqkv[:, 256:384], pA[:, 256:384], rstd[:, 2:3], gkv, ALU.mult, ALU.mult, ["pA", "rstdA", "gkv"], ["cqkv1"])
            for k in range(3):
                self.TR(T1[:, k * 128:(k + 1) * 128], cqkv[:, k * 128:(k + 1) * 128], self.ident_b,
                        ["cqkv0", "cqkv1", "ident_b"], ["T1"])
            self.CP("act", cT, T1[:, 0:384].rearrange("p (k m) -> p k m", k=3), ["T1"], ["cT"])
            if KCUT == 3:
                return
            for k in range(2):
                self.MM(pQ[:, 0:384], cT[:, k, :], wqb[:, k, :], k == 0, k == 1, ["cT", "wqb"], ["pQ"])
            self.MM(pKV, cT[:, 2, :], wkvb, True, True, ["cT", "wkvb"], ["pKV"])
            if KCUT == 4:
                return
            self.CP("act", qf, pQ[:, 0:384], ["pQ"], ["qf"])
            self.CP("act", kvf, pKV, ["pKV"], ["kvf"])
            self.CP("act", krs, pA[:, 384:416], ["pA"], ["krs"])
            pQv = qf.rearrange("p (h r) -> p h r", h=4)
            pKVv = kvf.rearrange("p (h r) -> p h r", h=4)
            self.CP("pool", qa[:, :, 0:64], pQv[:, :, 0:64], ["qf"], ["qa0"])
            if KCUT == 41:
                return
            self.rope(pQv[:, :, 64:96], "qf", 4, 32, tab, tabk, 128, 160, t1, t2, [(qa[:, :, 64:96], 0, 4, "qa1")], "R")
            if KCUT == 42:
                return
            self.rope(krs.unsqueeze(1), "krs", 1, 32, tab, tabk, 128, 160, t1, t2, [(krr, 0, 1, "krr")], "R")
            if KCUT == 5:
                return
            self.CP("pool", ka[:, :, 0:64], pKVv[:, :, 0:64], ["kvf"], ["ka0"])
            self.CP("dve", ka[:, :, 64:96], krr.to_broadcast([128, 4, 32]), ["krr"], ["ka1"])
            vA = va[0][b]
            vAp = vA.rearrange("p (j two) d -> p j two d", two=2)
            pKVp = pKVv.rearrange("p (j two) r -> p j two r", two=2)
            self.CP("pool", vAp[:, :, 0, 0:64], pKVp[:, :, 0, 64:128], ["kvf"], ["va0_%d" % b])
            self.CP("pool", vAp[:, :, 1, 64:128], pKVp[:, :, 1, 64:128], ["kvf"], ["va0_%d" % b])
            if KCUT == 6:
                return
            for h in range(4):
                self.TR(T1[0:96, h * 128:(h + 1) * 128], qa[:, h, :], self.ident_b, ["qa0", "qa1", "ident_b"], ["T1"])
                self.TR(T1[0:96, (4 + h) * 128:(5 + h) * 128], ka[:, h, :], self.ident_b, ["ka0", "ka1", "ident_b"], ["T1"])
            sA = stA[b]
            self.CP("act", sA[0:96], T1[0:96, :].rearrange("p (k m) -> p k m", k=8), ["T1"], ["stA%d" % b])
            if KCUT == 7:
                return
            self.DMA("sp", self.QT[0][:, :, tok], sA[0:96, 0:4, :], ["stA%d" % b], [])
            self.DMA("sp", self.KT[0][:, :, tok], sA[0:96, 4:8, :], ["stA%d" % b], [])
            self.DMA("sp", self.VA[0][:, t, :, :], vA, ["va0_%d" % b], [])
            if KCUT == 8:
                return

            def gqa_v(src, srck, n):
                v = va[n][b]
                vp = v.rearrange("p (g two) d -> p g two d", two=2)
                sv = src[:, 384:512].rearrange("p (g r) -> p g r", g=2)
                self.CP("pool", vp[:, :, 0, 0:64], sv, [srck], ["va%d_%d" % (n, b)])
                self.CP("pool", vp[:, :, 1, 64:128], sv, [srck], ["va%d_%d" % (n, b)])
                return v

            def qk_store(src, srcks, n, tbank, tbk):
                for k in range(4):
                    self.TR(tbank[:, k * 128:(k + 1) * 128], src[:, k * 128:(k + 1) * 128], self.ident_b,
                            list(srcks) + ["ident_b"], [tbk])
                sp_ = stP[n % 2]
                spk = "stP%d" % (n % 2)
                self.CP("act", sp_, tbank[:, 0:512].rearrange("p (k m) -> p k m", k=4), [tbk], [spk])
                self.DMA("sp", self.QT[n][:, :, tok], sp_[:, 0:2, :], [spk], [])
                self.DMA("sp", self.KT[n][:, :, tok], sp_[:, 2:4, :], [spk], [])

            pBv = bdf[:, 0:384].rearrange("p (h r) -> p h r", h=6)
            qkb_d = qkb[:, 4:8, :].rearrange("p (g two) r -> p g two r", two=2)
            self.rope(pBv, "bdf", 6, 64, tab, tabk, 0, 64, t1, t2,
                      [(qkb[:, 0:4, :], 0, 4, "qkb0"), (qkb_d[:, :, 0, :], 4, 6, "qkb1"), (qkb_d[:, :, 1, :], 4, 6, "qkb2")], "R")
            vB = gqa_v(bdf, "bdf", 1)
            qk_store(qkb.rearrange("p h r -> p (h r)"), ["qkb0", "qkb1", "qkb2"], 1, T0, "T0")
            self.DMA("sp", self.VA[1][:, t, :, :], vB, ["va1_%d" % b], [])
            if KCUT == 9:
                return
            self.CP("act", qkc, pC1, ["pC1"], ["qkc"])
            vC = va[2][b]
            vCp = vC.rearrange("p (j two) d -> p j two d", two=2)
            self.CP("act", c2f, pC2[:, 0:256], ["pC2"], ["c2f"])
            pC2p = c2f.rearrange("p (j two r) -> p j two r", two=2, r=64)
            self.CP("pool", vCp[:, :, 0, 0:64], pC2p[:, :, 0, :], ["c2f"], ["va2_%d" % b])
            self.CP("pool", vCp[:, :, 1, 64:128], pC2p[:, :, 1, :], ["c2f"], ["va2_%d" % b])
            qk_store(qkc, ["qkc"], 2, T1, "T1")
            self.DMA("sp", self.VA[2][:, t, :, :], vC, ["va2_%d" % b], [])
            if KCUT == 10:
                return
            self.CP("act", bdfD, pBD, ["pBD"], ["bdfD"])
            pDv = bdfD[:, 0:384].rearrange("p (h r) -> p h r", h=6)
            sqv = sq.rearrange("p (h r) -> p h r", h=6)
            self.ACT(sq, pBD[:, 0:384], AF.Square, ["pBD"], ["sq"])
            self.RED(ssqD[:, 0:6], sqv, ALU.add, ["sq"], ["ssqD"])
            self.ACT(ssqD[:, 0:6], ssqD[:, 0:6], AF.Sqrt, ["ssqD"], ["ssqD"], scale=1.0 / 64, bias=EPS)
            self.RCP(rstdD[:, 0:6], ssqD[:, 0:6], ["ssqD"], ["rstdD"])
            self.TT("dve", qn, pDv, rstdD[:, 0:6].unsqueeze(2).to_broadcast([128, 6, 64]), ALU.mult, ["bdfD", "rstdD"], ["qn"])
            self.TT("dve", qn, qn, gd, ALU.mult, ["qn", "gd"], ["qn"])
            self.rope(qn, "qn", 6, 64, tab, tabk, 0, 64, t1, t2,
                      [(qkb[:, 0:4, :], 0, 4, "qkb0"), (qkb_d[:, :, 0, :], 4, 6, "qkb1"), (qkb_d[:, :, 1, :], 4, 6, "qkb2")], "R")
            vD = gqa_v(bdfD, "bdfD", 3)
            qk_store(qkb.rearrange("p h r -> p (h r)"), ["qkb0", "qkb1", "qkb2"], 3, T0, "T0")
            self.DMA("sp", self.VA[3][:, t, :, :], vD, ["va3_%d" % b], [])

    def phase_p2(self, l):
        cfg, A, I = self.cfg, self.A, self.I
        NT, NTL, T = cfg.NT, cfg.NTL, cfg.T
        LOOK = 3
        groups = [(g * 512, 512, g) for g in range(cfg.NG)]
        if self.with_ctx:
            groups.append((cfg.SEQ, cfg.CTX, None))
        ctx_tiles = list(range(NTL, NT))
        base_mark = A.mark()
        pt = [A.alloc([512], BF16) for _ in range(6)]
        tmpf = [A.alloc([512], F32) for _ in range(4)]
        rden = [A.alloc([512], F32) for _ in range(2)]
        rdt = A.alloc([512], F32)
        ost = [A.alloc([512], BF16) for _ in range(2)]
        qt = [A.alloc([512], BF16) for _ in range(2)]
        esink = A.alloc([4], F32)
        self.DMA("sp", esink, I["sink_b"][l:l + 1, :].partition_broadcast(128), (), ["esink"])
        self.ACT(esink, esink, AF.Exp, ["esink"], ["esink"])
        inner_mark = A.mark()
        cnt = {"pt": 0, "s": 0, "tf": 0, "grp": 0}
        for n in range(4):
            A.release(inner_mark)
            self.S.barrier()
            isA = n == 0
            scale = (96 ** -0.5) if isA else 0.125
            LOOK = 2 if n == 2 else 3
            if isA:
                kt = A.alloc([4, T], BF16)
                self.DMA("sp", kt[0:96], self.KT[0], (), ["kt"])
            else:
                kt = A.alloc([4, T], BF16)
                self.MEMSET("pool", kt, 0.0, ["kt"])
                for hh in range(4):
                    hp = (hh % 2) * 64
                    self.DMA("sp", kt[hp:hp + 64, hh, :], self.KT[n][hp:hp + 64, hh // 2, :], (), ["kt"])
            vat = A.alloc([NT, 4, 128], BF16)
            step = max(1, NT // 4)
            for t0 in range(0, NT, step):
                t1_ = min(NT, t0 + step)
                self.DMA("sp", vat[:, t0:t1_], self.VA[n][:, t0:t1_], (), ["vat"])
            mb = mc = bias = comb = None
            if n == 1:
                mb = A.alloc([6, 512], F32)
                self.DMA("sp", mb, I["maskb"].rearrange("r p q -> p r q"), (), ["mb"])
            if n == 2:
                mc = A.alloc([self.ncase * 8, 512], BF16)
                for ci in range(self.ncase):
                    self.DMA("pool", mc[:, ci * 8:(ci + 1) * 8, :], I["maskc"][ci].rearrange("r p q -> p r q"), (), ["mc"])
                bias = A.alloc([8, 512], F32)
                comb1 = A.alloc([8, 512], F32)
                comb = [comb1, comb1]
            G_list = []
            for h in range(4):
                for gi, (q0, nq, g) in enumerate(groups):
                    tiles = []
                    if g is None:
                        tiles = [(t_, None) for t_ in ctx_tiles]
                    elif n in (0, 3):
                        tiles = [(t_, None) for t_ in range(NT)]
                    elif n == 1:
                        for j in range(4 * g - 1, 4 * g + 5):
                            if 0 <= j < NTL:
                                tiles.append((j, ("mb", j - 4 * g + 1)))
                        tiles += [(t_, None) for t_ in ctx_tiles]
                    else:
                        for r in self.valid_c[g]:
                            tiles.append((4 * g + r - 2, ("comb", r)))
                        tiles += [(t_, None) for t_ in ctx_tiles]
                    gid = cnt["grp"]
                    cnt["grp"] += 1
                    G_list.append(dict(h=h, q0=q0, nq=nq, g=g, tiles=tiles, gid=gid, first_of_head=(gi == 0)))
            items = [(G, i) for G in G_list for i in range(len(G["tiles"]))]
            state = {}

            def emit_S(idx):
                G, i = items[idx]
                h, q0, nq, gid = G["h"], G["q0"], G["nq"], G["gid"]
                ph = (h % 2) * 64
                vr = slice(ph, ph + 64)
                qb = gid % 2
                qtb, qk_ = qt[qb], "qt%d" % qb
                if i == 0:
                    if n == 2 and G["first_of_head"]:
                        self.DMA("sp", bias, I["biasg"][l, h].rearrange("r p q -> p r q"), (), ["bias"])
                    if n == 2 and G["first_of_head"]:
                        state["built"] = None
                    if n == 2 and G["g"] is not None and state.get("built") != (self.case_of_g[G["g"]], tuple(self.valid_c[G["g"]])):
                        state["built"] = (self.case_of_g[G["g"]], tuple(self.valid_c[G["g"]]))
                        ci = self.case_of_g[G["g"]]
                        cb_ = comb[gid % 2]
                        for r in self.valid_c[G["g"]]:
                            self.TT("pool", cb_[:, r, :], bias[:, r, :], mc[:, ci * 8 + r, :], ALU.add,
                                    ["bias", "mc"], ["comb_%d" % r])
                    if isA:
                        self.DMA("sp", qtb[0:96, 0:nq], self.QT[0][:, h, q0:q0 + nq], (), [qk_])
                    else:
                        self.DMA("sp", qtb[:, 0:nq], self.QT[n][:, h // 2, q0:q0 + nq], (), [qk_])
                q_ap = qtb[0:96, 0:nq] if isA else qtb[:, 0:nq]
                tk = G["tiles"][i][0]
                sb_ = cnt["s"] % 4
                cnt["s"] += 1
                psS = self.bank(sb_)
                psk = "psS%d" % sb_
                if isA:
                    k_ap = kt[0:96, h, tk * 128:(tk + 1) * 128]
                else:
                    k_ap = kt[:, h, tk * 128:(tk + 1) * 128]
                self.MM(psS[:, 0:nq], k_ap, q_ap, True, True, ["kt", qk_], [psk])
                state[idx] = (psS, psk)

            for j in range(min(LOOK, len(items))):
                emit_S(j)
            for idx, (G, i) in enumerate(items):
                if idx + LOOK < len(items):
                    emit_S(idx + LOOK)
                h, q0, nq, gid = G["h"], G["q0"], G["nq"], G["gid"]
                ph = (h % 2) * 64
                vr = slice(ph, ph + 64)
                dr = slice(64 - ph, 128 - ph)
                psS, psk = state.pop(idx)
                tk, tabinfo = G["tiles"][i]
                ob = gid % 2
                psO = self.bank(6 + ob)
                pok = "psO%d" % ob
                last = len(G["tiles"]) - 1
                pb_ = cnt["pt"] % 6
                cnt["pt"] += 1
                ptb, ptk = pt[pb_], "pt%d" % pb_
                if tabinfo is None:
                    self.ACT(ptb[:, 0:nq], psS[:, 0:nq], AF.Exp, [psk], [ptk], scale=scale)
                else:
                    if tabinfo[0] == "mb":
                        tabv, tabk = mb[:, tabinfo[1], :], "mb"
                    else:
                        tabv, tabk = comb[gid % 2][:, tabinfo[1], :], "comb_%d" % tabinfo[1]
                    fb = cnt["tf"] % 4
                    cnt["tf"] += 1
                    self.STT("dve", tmpf[fb][:, 0:nq], psS[:, 0:nq], scale, tabv[:, 0:nq], ALU.mult, ALU.add,
                             [psk, tabk], ["tmpf%d" % fb])
                    self.ACT(ptb[:, 0:nq], tmpf[fb][:, 0:nq], AF.Exp, ["tmpf%d" % fb], [ptk])
                self.MM(psO[:, 0:nq], vat[:, tk, h, :], ptb[:, 0:nq], i == 0, i == last, ["vat", ptk], [pok])
                if i == last:
                    rb = gid % 2
                    rd, rdk = rden[rb], "rden%d" % rb
                    if n in (1, 2):
                        if n == 1:
                            self.ACT(rdt[dr, 0:nq], psO[dr, 0:nq], AF.Ln, [pok, "esink"], ["rdt"], bias=esink[dr, h:h + 1])
                        else:
                            self.ACT(rdt[dr, 0:nq], psO[dr, 0:nq], AF.Ln, [pok], ["rdt"])
                        self.ACT(rdt[dr, 0:nq], rdt[dr, 0:nq], AF.Exp, ["rdt"], ["rdt"], scale=-1.0)
                        self.CP("dve", rd[vr, 0:nq], rdt[dr, 0:nq], ["rdt"], [rdk])
                    else:
                        self.RCP(rd[vr, 0:nq], psO[dr, 0:nq], [pok], [rdk])
                    osb, osk = ost[rb], "ost%d" % rb
                    self.TT("dve", osb[vr, 0:nq], psO[vr, 0:nq], rd[vr, 0:nq], ALU.mult, [pok, rdk], [osk])
                    self.DMA("sp", self.OT[n * 2 + h // 2][vr, q0:q0 + nq], osb[vr, 0:nq], [osk], [])
        A.release(base_mark)

    def phase_p3(self, l):
        cfg, A, I = self.cfg, self.A, self.I
        NT, NTL = cfg.NT, cfg.NTL
        tiles = list(range(NT)) if self.with_ctx else list(range(NTL))
        wg = A.alloc([32, D], BF16)
        for n in range(4):
            for k in range(8):
                self.DMA("pool", wg[:, n * 8 + k, :], I["w_gate"][l, n, k * 128:(k + 1) * 128, :], (), ["wg"])
        bg = A.alloc([4 * D], BF16)
        self.DMA("pool", bg[0:1, :], I["b_gate"][l:l + 1, :], (), ["bg"])
        wbr = A.alloc([8, D], BF16)
        self.DMA("pool", wbr, I["w_branch"][l].rearrange("n (c p) m -> p (n c) m", p=128), (), ["wbr"])
        wo = A.alloc([8, D], BF16)
        self.DMA("pool", wo, I["w_out"][l].rearrange("(k p) m -> p k m", p=128), (), ["wo"])
        wr = A.alloc([8, 36], F32)
        self.DMA("sp", wr[:, :, 0:4], I["w_group"][l].rearrange("(k p) n -> p k n", p=128), (), ["wr"])
        self.DMA("sp", wr[:, :, 4:36], I["w_router"][l].rearrange("(k p) n -> p k n", p=128), (), ["wr"])
        br = A.alloc([36], F32)
        self.DMA("sp", br[0:1, 0:4], I["b_group"][l:l + 1, :], (), ["br"])
        self.DMA("sp", br[0:1, 4:36], I["b_router"][l:l + 1, :], (), ["br"])
        xt2 = [A.alloc([D], F32) for _ in range(3)]
        ot2 = [A.alloc([8, 128], BF16) for _ in range(2)]
        tmpA = A.alloc([D], F32)
        tmpB = A.alloc([D], F32)
        hb = A.alloc([D], BF16)
        hT = A.alloc([8, 128], BF16)
        gate = A.alloc([D], BF16)
        tmpn = A.alloc([D], BF16)
        ypre = A.alloc([D], F32)
        ypb = A.alloc([D], BF16)
        yT = A.alloc([8, 128], BF16)
        h2Tf = A.alloc([8, 128], F32)
        h2b = A.alloc([D], BF16)
        oh12b = A.alloc([64], BF16)
        csb = A.alloc([64], F32)
        tr0 = A.alloc([32], F32)
        tr1 = A.alloc([32], F32)
        self.MEMSET("dve", self.base, 0.0, ["base"])
        ssq = A.alloc([4], F32)
        rstd = A.alloc([4], F32)
        lg = A.alloc([36], F32)
        sm = A.alloc([16], F32)
        ohg = A.alloc([4], F32)
        pen = A.alloc([4], F32)
        em = A.alloc([32], F32)
        em2 = A.alloc([32], F32)
        oh1 = A.alloc([32], F32)
        oh2 = A.alloc([32], F32)
        c1 = A.alloc([32], F32)
        j4 = A.alloc([4], F32)
        PT = self.bank_bf(0)
        PB, PY = self.banks[2], self.banks[3]
        pR = self.bank(1)
        cntu = [0]

        def load(i):
            t = tiles[i]
            b = i % 2
            self.DMA("sp", xt2[i % 3], self.x_src(l, t), (), ["xt%d" % (i % 3)])
            self.DMA("sp", ot2[b], self.OT[:, :, t * 128:(t + 1) * 128].rearrange("b p t -> p b t"), (), ["ot%d" % b])

        def norm1(i):
            t = tiles[i]
            w = 0 if t < NTL else 1
            self.norm_mod(xt2[i % 3], "xt%d" % (i % 3), w, D, 0, hb, tmpA, hb, "hb", ssq[:, 0:1], rstd[:, 0:1], "n1",
                          jk="hb", tk="tmpA")

        def stageA(i, gen=None):
            t = tiles[i]
            b = i % 2
            xt, xk, ot, otk = xt2[i % 3], "xt%d" % (i % 3), ot2[b], "ot%d" % b
            w = 0 if t < NTL else 1
            m = self.mod[w]
            mk = "mod%d" % w
            tok = slice(t * 128, (t + 1) * 128)
            tmp = tmpA
            self.transpose8(hb, "hb", PT, "PT", hT, "hT")
            for n in range(4):
                for half in range(2):
                    cs = slice(half * 512, (half + 1) * 512)
                    r_ = cntu[0] % 2
                    cntu[0] += 1
                    PGh, pgk = self.bank(2 + r_), "PG%d" % r_
                    PBh, pbk = self.bank(4 + r_), "PB%d" % r_
                    for k in range(8):
                        self.MM(PGh, hT[:, k, :], wg[:, n * 8 + k, cs], k == 0, False, ["hT", "wg"], [pgk])
                    self.MM(PGh, self.ones_b[0:1, :], bg[0:1, n * D + half * 512:n * D + (half + 1) * 512], False, True,
                            ["ones_b", "bg"], [pgk])
                    for c in range(2):
                        self.MM(PBh, ot[:, n * 2 + c, :], wbr[:, n * 2 + c, cs], c == 0, c == 1, [otk, "wbr"], [pbk])
                    gh, ghk = gate[:, r_ * 512:(r_ + 1) * 512], "gate%d" % r_
                    self.ACT(gh, PGh, AF.Sigmoid, [pgk], [ghk])
                    yk = "ypre%d" % half
                    if n == 0:
                        self.TT("dve", ypre[:, cs], gh, PBh, ALU.mult, [ghk, pbk], [yk])
                    else:
                        tn, tnk = tmpn[:, r_ * 512:(r_ + 1) * 512], "tn%d" % r_
                        self.TT("dve", tn, gh, PBh, ALU.mult, [ghk, pbk], [tnk])
                        if n < 3:
                            self.TT("pool", ypre[:, cs], ypre[:, cs], tn, ALU.add, [yk, tnk], [yk])
                        else:
                            self.TT("pool", ypb[:, cs], ypre[:, cs], tn, ALU.add, [yk, tnk], ["ypb%d" % half])
                    u_ = n * 2 + half
                    if u_ in (1, 3, 5, 7) and gen is not None:
                        next(gen, None)
                    if u_ == 3 and i + 1 < len(tiles):
                        norm1(i + 1)
            if i + 2 < len(tiles):
                load(i + 2)
            self.transpose8(ypb, ["ypb0", "ypb1"], PT, "PT", yT, "yT")
            for half in range(2):
                cs = slice(half * 512, (half + 1) * 512)
                for k in range(8):
                    self.MM(PY[:, cs], yT[:, k, :], wo[:, k, cs], k == 0, k == 7, ["yT", "wo"], ["PY"])
            self.TT("dve", tmp, PY[:, :], m[:, 2 * D:3 * D], ALU.mult, ["PY", mk], ["tmpA"])
            self.TT("pool", xt, tmp, xt, ALU.add, ["tmpA", xk], [xk])
            self.DMA("sp", self.xcur[tok, :], xt, [xk], [])
            if gen is not None:
                next(gen, None)

        def stageB(i):
            t = tiles[i]
            xt, xk = xt2[i % 3], "xt%d" % (i % 3)
            w = 0 if t < NTL else 1
            tok = slice(t * 128, (t + 1) * 128)
            tmp = tmpB
            h2 = tmp
            self.norm_mod(xt, xk, w, 4 * D, 3 * D, h2b, tmp, h2, "tmpB", ssq[:, 1:2], rstd[:, 1:2], "n2", jk="h2b", tk="tmpB")
            yield
            for k in range(8):
                self.TR(PB[:, k * 128:(k + 1) * 128], h2[:, k * 128:(k + 1) * 128], self.ident_f, ["tmpB", "ident_f"], ["PB0", "PB1"])
            self.CP("act", h2Tf, PB[:, :].rearrange("p (k m) -> p k m", k=8), ["PB0", "PB1"], ["h2Tf"])
            self.CP("pool", h2b, h2, ["tmpB"], ["h2b"])
            self.DMA("sp", self.H2[tok, :], h2b, ["h2b"], [])
            yield
            for k in range(8):
                self.MM(pR[:, 0:36], h2Tf[:, k, :], wr[:, k, :], k == 0, False, ["h2Tf", "wr"], ["pR"])
            self.MM(pR[:, 0:36], self.ones_f[0:1, :], br[0:1, :], False, True, ["ones_f", "br"], ["pR"])
            self.CP("dve", lg, pR[:, 0:36], ["pR"], ["lg"])
            gl = lg[:, 0:4]
            el = lg[:, 4:36].rearrange("p (g e) -> p g e", g=4)
            gmax, negmax, se, gw, m1, m2, dd, w1, w1g, w2g = (sm[:, j:j + 1] for j in range(10))
            self.RED(gmax, gl, ALU.max, ["lg"], ["gmax"])
            self.TS("dve", ohg, gl, gmax, None, ALU.is_equal, None, ["lg", "gmax"], ["ohg"])
            self.TS("dve", negmax, gmax, -1.0, None, ALU.mult, None, ["gmax"], ["negmax"])
            self.ACT(j4, gl, AF.Exp, ["lg", "negmax"], ["j4", "se"], bias=negmax, accum=se)
            self.RCP(gw, se, ["se"], ["gw"])
            self.TS("dve", pen, ohg, -1.0, 1e9, ALU.add, ALU.mult, ["ohg"], ["pen"])
            emv = em.rearrange("p (g e) -> p g e", g=4)
            self.TT("dve", emv, el, pen.unsqueeze(2).to_broadcast([128, 4, 8]), ALU.add, ["lg", "pen"], ["em"])
            self.RED(m1, em, ALU.max, ["em"], ["m1"])
            self.TS("dve", oh1, em, m1, None, ALU.is_equal, None, ["em", "m1"], ["oh1"])
            self.STT("dve", em2, oh1, -2e9, em, ALU.mult, ALU.add, ["oh1", "em"], ["em2"])
            self.RED(m2, em2, ALU.max, ["em2"], ["m2"])
            self.TS("dve", oh2, em2, m2, None, ALU.is_equal, None, ["em2", "m2"], ["oh2"])
            yield
            self.TT("dve", dd, m2, m1, ALU.subtract, ["m1", "m2"], ["dd"])
            self.ACT(dd, dd, AF.Exp, ["dd"], ["dd"])
            self.TS("dve", dd, dd, 1.0, None, ALU.add, None, ["dd"], ["dd"])
            self.RCP(w1, dd, ["dd"], ["w1"])
            self.TT("dve", w1g, w1, gw, ALU.mult, ["w1", "gw"], ["w1g"])
            self.TT("dve", w2g, gw, w1g, ALU.subtract, ["w1g", "gw"], ["w2g"])
            rt, base, iota = self.rt, self.base, self.iota
            pRK = self.bank(1)[:, 64:192]
            self.CP("pool", oh12b[:, 0:32], oh1, ["oh1"], ["oh12b0"])
            self.CP("pool", oh12b[:, 32:64], oh2, ["oh2"], ["oh12b1"])
            yield
            self.MM(pRK[:, 0:64], self.ltb, oh12b, True, True, ["ltb", "oh12b0", "oh12b1"], ["pRK"])
            self.MM(pRK[:, 64:128], self.onesbb, oh12b, True, True, ["onesbb", "oh12b0", "oh12b1"], ["pRK"])
            self.CP("dve", csb, pRK[:, 64:128], ["pRK"], ["cs"])
            self.TT("dve", tr0, pRK[:, 0:32], base, ALU.add, ["pRK", "base"], ["tr0"])
            self.TT("dve", tr0, tr0, oh1, ALU.mult, ["tr0", "oh1"], ["tr0"])
            self.RED(rt[:, t, 4:5], tr0, ALU.add, ["tr0"], ["rt"])
            self.TT("dve", tr1, pRK[:, 32:64], base, ALU.add, ["pRK", "base"], ["tr1"])
            self.TT("dve", tr1, tr1, csb[:, 0:32], ALU.add, ["tr1", "cs"], ["tr1"])
            self.TT("dve", tr1, tr1, oh2, ALU.mult, ["tr1", "oh2"], ["tr1"])
            self.RED(rt[:, t, 5:6], tr1, ALU.add, ["tr1"], ["rt"])
            self.TT("dve", base, base, csb[:, 0:32], ALU.add, ["base", "cs"], ["base"])
            self.TT("dve", base, base, csb[:, 32:64], ALU.add, ["base", "cs"], ["base"])
            self.TT("dve", tr0, oh1, iota, ALU.mult, ["oh1", "iota", "tr0"], ["tr0"])
            self.RED(rt[:, t, 0:1], tr0, ALU.add, ["tr0"], ["rt"])
            self.TT("dve", tr1, oh2, iota, ALU.mult, ["oh2", "iota", "tr1"], ["tr1"])
            self.RED(rt[:, t, 1:2], tr1, ALU.add, ["tr1"], ["rt"])
            self.CP("dve", rt[:, t, 2:3], w1g, ["w1g"], ["rt"])
            self.CP("dve", rt[:, t, 3:4], w2g, ["w2g"], ["rt"])

        ntl_ = len(tiles)
        load(0)
        if ntl_ > 1:
            load(1)
        norm1(0)
        for i in range(ntl_):
            gen = stageB(i - 1) if i > 0 else None
            stageA(i, gen)
            if gen is not None:
                for _ in gen:
                    pass
        for _ in stageB(ntl_ - 1):
            pass

    def phase_p4(self, l):
        cfg, A, I = self.cfg, self.A, self.I
        NT, NTL = cfg.NT, cfg.NTL
        last = l == cfg.DEPTH - 1
        tiles = list(range(NT)) if self.with_ctx else list(range(NTL))
        ntl = len(tiles)
        NBLK = 2 * ntl + 32
        assert NBLK <= NBLK_MAX
        rt, base, iota = self.rt, self.base, self.iota
        m0 = A.mark()
        nbi = A.alloc([32], I32)
        pcnt = A.alloc([32], F32)
        pa = A.alloc([32], F32)
        pb = A.alloc([32], F32)
        bs = A.alloc([32], F32)
        oh3 = A.alloc([NT, 32], F32)
        sl = A.alloc([NT, 2], F32)
        cmp = A.alloc([NBLK_MAX, 32], F32)
        ble = A.alloc([NBLK_MAX], F32)
        zt = A.alloc([8192], BF16)
        h2t = [A.alloc([D], BF16) for _ in range(2)]
        self.MEMSET("pool", zt, 0.0, ["zt"])
        xsz = self.XS.rearrange("(p b) f -> p (b f)", p=128)
        tot = NBLK_MAX * D
        for c0 in range(0, tot, 8192):
            c1_ = min(tot, c0 + 8192)
            self.DMA("sp", xsz[:, c0:c1_], zt[:, 0:c1_ - c0], ["zt"], ["XS"])
        self.TS("dve", pcnt, base, 1.0 / 128, 0.496, ALU.mult, ALU.add, ["base"], ["pcnt"])
        self.CP("dve", nbi, pcnt, ["pcnt"], ["nbi"])
        self.CP("dve", pcnt, nbi, ["nbi"], ["pcnt"])
        self.TS("dve", pcnt, pcnt, 128.0, None, ALU.mult, None, ["pcnt"], ["pcnt"])
        self.CP("dve", pa, pcnt, ["pcnt"], ["pa"])
        src, dst, sk, dk = pa, pb, "pa", "pb"
        for sft in (1, 2, 4, 8, 16):
            self.CP("dve", dst[:, 0:sft], src[:, 0:sft], [sk], [dk])
            self.TT("dve", dst[:, sft:32], src[:, sft:32], src[:, 0:32 - sft], ALU.add, [sk], [dk])
            src, dst, sk, dk = dst, src, dk, sk
        pe, pek = src, sk
        self.TT("dve", bs, pe, pcnt, ALU.subtract, [pek, "pcnt"], ["bs"])
        for k in range(2):
            self.TT("dve", oh3, iota.unsqueeze(1).to_broadcast([128, NT, 32]),
                    rt[:, :, k:k + 1].to_broadcast([128, NT, 32]), ALU.is_equal, ["iota", "rt"], ["oh3"])
            self.TT("dve", oh3, oh3, bs.unsqueeze(1).to_broadcast([128, NT, 32]), ALU.mult, ["oh3", "bs"], ["oh3"])
            self.RED(sl[:, :, k], oh3, ALU.add, ["oh3"], ["sl%d" % k])
            self.TT("dve", sl[:, :, k], sl[:, :, k], rt[:, :, 4 + k], ALU.add, ["sl%d" % k, "rt"], ["sl%d" % k])
        self.CP("dve", self.slot_i, sl, ["sl0", "sl1"], ["slot_i"])
        self.TT("dve", cmp, self.thr.unsqueeze(2).to_broadcast([128, NBLK_MAX, 32]),
                pe.unsqueeze(1).to_broadcast([128, NBLK_MAX, 32]), ALU.is_ge, ["thr", pek], ["cmp"])
        self.RED(ble, cmp, ALU.add, ["cmp"], ["ble"])
        self.TS("dve", ble, ble, 31.0, 128.0, ALU.min, ALU.mult, ["ble"], ["ble"])
        self.TS("dve", ble, ble, self.pidx[:, 0:1], float(l * NE * 128), ALU.add, ALU.add, ["ble", "pidx"], ["ble"])
        self.CP("dve", self.widx, ble, ["ble"], ["widx"])
        for i, t in enumerate(tiles):
            hb, hk = h2t[i % 2], "h2t%d" % (i % 2)
            self.DMA("sp", hb, self.H2[t * 128:(t + 1) * 128, :], (), [hk])
            for k in range(2):
                self.S.add("pool", (lambda hb=hb, t=t, k=k: (lambda e: e.indirect_dma_start(
                    out=self.XS[:, :], out_offset=bass.IndirectOffsetOnAxis(ap=self.slot_i[:, t, k:k + 1], axis=0),
                    in_=hb, in_offset=None)))(), [hk, "slot_i", "XS"], ["XSs"], dma=True)
        self.S.barrier()
        A.release(m0)
        NWR = 4
        w13 = [A.alloc([8, 512], BF16) for _ in range(NWR)]
        w2 = [A.alloc([2, D], BF16) for _ in range(NWR)]
        xs = [A.alloc([D], BF16) for _ in range(NWR)]
        xsT = [A.alloc([8, 128], BF16) for _ in range(2)]
        sb = [A.alloc([256], F32) for _ in range(2)]
        ab = [A.alloc([256], BF16) for _ in range(2)]
        aT = [A.alloc([2, 128], BF16) for _ in range(2)]
        ys = [A.alloc([D], BF16) for _ in range(2)]
        w13src = I["w13r"].rearrange("l r f -> (l r) f")
        w2src = I["w2r"].rearrange("l r f -> (l r) f")
        def wload(b):
            wr_ = b % NWR
            self.S.add("pool", (lambda b=b, wr_=wr_: (lambda e: e.indirect_dma_start(
                out=w13[wr_].rearrange("p k f -> p (k f)"), out_offset=None, in_=w13src,
                in_offset=bass.IndirectOffsetOnAxis(ap=self.widx[:, b:b + 1], axis=0))))(), ["widx"], ["w13_%d" % wr_], dma=True)
            self.S.add("pool", (lambda b=b, wr_=wr_: (lambda e: e.indirect_dma_start(
                out=w2[wr_].rearrange("p c f -> p (c f)"), out_offset=None, in_=w2src,
                in_offset=bass.IndirectOffsetOnAxis(ap=self.widx[:, b:b + 1], axis=0))))(), ["widx"], ["w2_%d" % wr_], dma=True)
            self.DMA("sp", xs[wr_], self.XS[b * 128:(b + 1) * 128, :], (), ["xsw%d" % wr_])

        for b in range(min(NWR - 1, NBLK)):
            wload(b)
        for b in range(NBLK):
            if b + NWR - 1 < NBLK:
                wload(b + NWR - 1)
            r = b % 2
            wr_ = b % NWR
            PTx = self.bank_bf(r)
            ptxk = "PTx%d" % r
            psH = self.bank(2 + r)
            phk = "psH%d" % r
            PTa = self.bank_bf(4)
            psO = self.banks[3]
            for k in range(8):
                self.TR(PTx[:, k * 128:(k + 1) * 128], xs[wr_][:, k * 128:(k + 1) * 128], self.ident_b, ["xsw%d" % wr_, "ident_b"], [ptxk])
            self.CP("act", xsT[r], PTx[:, :].rearrange("p (k m) -> p k m", k=8), [ptxk], ["xsT%d" % r])
            for k in range(8):
                self.MM(psH, xsT[r][:, k, :], w13[wr_][:, k, :], k == 0, k == 7, ["xsT%d" % r, "w13_%d" % wr_], [phk])
            self.ACT(sb[r], psH[:, 0:256], AF.Silu, [phk], ["sb%d" % r])
            self.TT("dve", ab[r], sb[r], psH[:, 256:512], ALU.mult, ["sb%d" % r, phk], ["ab%d" % r])
            for c in range(2):
                self.TR(PTa[:, c * 128:(c + 1) * 128], ab[r][:, c * 128:(c + 1) * 128], self.ident_b, ["ab%d" % r, "ident_b"], ["PTa"])
            self.CP("act", aT[r], PTa[:, 0:256].rearrange("p (c m) -> p c m", c=2), ["PTa"], ["aT%d" % r])
            for half in range(2):
                cs_ = slice(half * 512, (half + 1) * 512)
                for c in range(2):
                    self.MM(psO[:, cs_], aT[r][:, c, :], w2[wr_][:, c, cs_], c == 0, c == 1, ["aT%d" % r, "w2_%d" % wr_], ["psO"])
            self.CP("act", ys[r], psO[:, :], ["psO"], ["ys%d" % r])
            self.DMA("sp", self.YS[b * 128:(b + 1) * 128, :], ys[r], ["ys%d" % r], [])
        self.S.barrier()
        A.release(m0)
        y0 = [A.alloc([D], BF16) for _ in range(2)]
        y1 = [A.alloc([D], BF16) for _ in range(2)]
        xt2 = [A.alloc([D], F32) for _ in range(2)]
        tmp = A.alloc([D], F32)
        junk = A.alloc([D], BF16)
        ssq = A.alloc([2], F32)
        rstd = A.alloc([2], F32)
        if last:
            self.gfin = A.alloc([D], F32)
            self.DMA("sp", self.gfin, I["g_final"].partition_broadcast(128), (), ["gfin"])
        for i, t in enumerate(tiles):
            b = i % 2
            w = 0 if t < NTL else 1
            tok = slice(t * 128, (t + 1) * 128)
            for k, yb in ((0, y0[b]), (1, y1[b])):
                self.S.add("pool", (lambda yb=yb, t=t, k=k: (lambda e: e.indirect_dma_start(
                    out=yb, out_offset=None, in_=self.YS[:, :],
                    in_offset=bass.IndirectOffsetOnAxis(ap=self.slot_i[:, t, k:k + 1], axis=0))))(), ["slot_i"], ["y%d_%d" % (k, b)], dma=True)
            self.DMA("sp", xt2[b], self.xcur[tok, :], (), ["xt%d" % b])
            self.TS("dve", tmp, y0[b], rt[:, t, 2:3], None, ALU.mult, None, ["y0_%d" % b, "rt"], ["tmp"])
            self.STT("dve", tmp, y1[b], rt[:, t, 3:4], tmp, ALU.mult, ALU.add, ["y1_%d" % b, "rt", "tmp"], ["tmp"])
            self.TT("pool", tmp, tmp, self.mod[w][:, 5 * D:6 * D], ALU.mult, ["tmp", "mod%d" % w], ["tmp"])
            self.TT("pool", xt2[b], tmp, xt2[b], ALU.add, ["tmp", "xt%d" % b], ["xt%d" % b])
            if not last:
                self.DMA("sp", self.xcur[tok, :], xt2[b], ["xt%d" % b], [])
            else:
                self.rstd_of(xt2[b], D, junk, ssq[:, 0:1], rstd[:, 0:1], ["xt%d" % b], "f")
                self.STT("dve", xt2[b], xt2[b], rstd[:, 0:1], self.gfin, ALU.mult, ALU.mult, ["xt%d" % b, "rstdf", "gfin"], ["xt%d" % b])
                o = self.DMA("sp", self.out[tok, :], xt2[b], ["xt%d" % b], [])
                self.S.out_ops.append(o)


_CACHE = {}


def _get_prog(cfg_key):
    if cfg_key not in _CACHE:
        cfg = Cfg(*cfg_key)
        p = Prog(cfg)
        p.build()
        _CACHE[cfg_key] = p
    return _CACHE[cfg_key]


def make_in_maps(prog, inputs, ncores):
    cfg = prog.cfg
    f = lambda a: np.ascontiguousarray(np.asarray(a, dtype=np.float32))
    shared = {}
    for k in ("w_mod", "b_mod", "g_norm_mix", "w_in", "g_q_a", "w_q_b", "g_kv_a", "w_kv_b", "sink_b", "g_q_d", "g_k_d",
              "w_gate", "w_branch", "w_out", "g_norm_ffn", "w_group", "b_group", "w_router", "b_router"):
        shared[k] = f(inputs[k])
    L = cfg.DEPTH
    shared["b_gate"] = f(inputs["b_gate"]).reshape(L, 4 * D)
    shared["g_final"] = f(inputs["g_final"]).reshape(1, D)
    shared["c_ctx"] = f(inputs["c_ctx"]).reshape(1, D)
    rpb = f(inputs["rpb_c"])
    shared["biasg"] = np.ascontiguousarray(rpb[:, :, prog.DR, prog.DC[None]])
    w13 = np.concatenate([f(inputs["w_ff1"]), f(inputs["w_ff3"])], axis=-1)
    w13 = w13.reshape(L, NE, 8, 128, 512).transpose(0, 1, 3, 2, 4)
    shared["w13r"] = np.ascontiguousarray(w13).reshape(L, NE * 128, 4096)
    w2 = f(inputs["w_ff2"]).reshape(L, NE, 2, 128, D).transpose(0, 1, 3, 2, 4)
    shared["w2r"] = np.ascontiguousarray(w2).reshape(L, NE * 128, 2048)
    shared["iota32"] = np.arange(32, dtype=np.float32).reshape(1, 32)
    shared["ltri"] = np.triu(np.ones((128, 128), np.float32), k=1)
    shared["pidx"] = np.arange(128, dtype=np.float32).reshape(128, 1)
    shared["thr"] = (128.0 * np.arange(NBLK_MAX, dtype=np.float32)).reshape(1, NBLK_MAX)
    shared["ident"] = np.eye(128, dtype=np.float32)
    shared["ropetab"] = rope_tab(cfg)
    shared["maskb"] = mask_b()
    shared["maskc"] = prog.maskc_np
    x = f(inputs["x"])
    c = f(inputs["c"])
    ctx = f(inputs["ctx"])
    maps = []
    for b in range(ncores):
        m = dict(shared)
        m["x"] = x[b]
        m["c"] = c[b:b + 1]
        m["ctx"] = ctx[b]
        maps.append(m)
    return maps


def kernel(**inputs):
    x = np.asarray(inputs["x"])
    B, SEQ, _ = x.shape
    CTX = np.asarray(inputs["ctx"]).shape[1]
    DEPTH = np.asarray(inputs["w_mod"]).shape[0]
    prog = _get_prog((SEQ, CTX, DEPTH))
    maps = make_in_maps(prog, inputs, B)
    res = run_bass_kernel_spmd(prog.nc, maps, core_ids=list(range(B)))
    out = np.stack([np.asarray(r["out"], dtype=np.float32) for r in res.results], axis=0)
    return out
```

```python
import contextlib
import numpy as np
import concourse.bass as bass
import concourse.mybir as mybir
from concourse.bass_utils import run_bass_kernel_spmd

F32 = mybir.dt.float32
BF16 = mybir.dt.bfloat16
AF = mybir.ActivationFunctionType
ALU = mybir.AluOpType
AX = mybir.AxisListType
I32 = mybir.dt.int32
NBLK_MAX = 100

SAME_ENGINE_SYNC = True
NDMA_SEMS = 10
import os
MAXOPS = int(os.environ.get('KMAXOPS', '100000000'))
D = 1024
EPS = 1e-6
NEG = -30000.0
IN_COLS = 2208
NE = 32


class Op:
    __slots__ = ("eng", "fn", "deps", "flag", "semv", "sem", "is_dma")

    def __init__(self, eng, fn, is_dma):
        self.eng = eng
        self.fn = fn
        self.deps = []
        self.flag = False
        self.semv = 0
        self.sem = None
        self.is_dma = is_dma


class Sched:
    ENGS = ("pe", "act", "dve", "pool", "sp")

    def __init__(self):
        self.ops = {e: [] for e in self.ENGS}
        self.last_w = {}
        self.readers = {}
        self.pending_barrier = {e: None for e in self.ENGS}
        self.out_ops = []

    def add(self, eng, fn, reads=(), writes=(), dma=False):
        op = Op(eng, fn, dma)
        self.nadd = getattr(self, "nadd", 0) + 1
        if self.nadd > MAXOPS:
            return op
        deps = {}
        raw = set()
        for k in reads:
            w = self.last_w.get(k)
            if w is not None:
                deps[id(w)] = w
                raw.add(id(w))
        for k in writes:
            w = self.last_w.get(k)
            if w is not None:
                deps[id(w)] = w
            for r in self.readers.get(k, ()):
                deps[id(r)] = r
        for k in reads:
            self.readers.setdefault(k, []).append(op)
        for k in writes:
            self.last_w[k] = op
            self.readers[k] = []
        pb = self.pending_barrier[eng]
        if pb is not None:
            for d in pb:
                deps[id(d)] = d
            self.pending_barrier[eng] = None
        for d in deps.values():
            if d is op:
                continue
            if (not d.is_dma) and d.eng == eng and (not dma) and (eng == "pe" or not SAME_ENGINE_SYNC):
                continue
            d.flag = True
            op.deps.append(d)
        self.ops[eng].append(op)
        return op

    def barrier(self):
        lst = []
        for e in self.ENGS:
            ops = self.ops[e]
            if not ops:
                continue
            nd = 0
            got_c = False
            for op in reversed(ops):
                if op.is_dma:
                    if nd < NDMA_SEMS:
                        lst.append(op)
                        nd += 1
                elif not got_c:
                    lst.append(op)
                    got_c = True
                if nd >= NDMA_SEMS and got_c:
                    break
        for e in self.ENGS:
            prev = self.pending_barrier[e]
            self.pending_barrier[e] = lst if prev is None else (prev + lst)
        self.last_w = {}
        self.readers = {}

    def emit(self, nc):
        final_ops = self.out_ops
        for o in final_ops:
            o.flag = True
        with contextlib.ExitStack() as es:
            csem = {}
            for e in ("pe", "act", "dve", "pool"):
                csem[e] = es.enter_context(nc.semaphore("s_" + e))
            dsem = {}
            for e in ("act", "pool", "sp"):
                dsem[e] = [es.enter_context(nc.semaphore("d_%s%d" % (e, i))) for i in range(NDMA_SEMS)]
            for e in self.ENGS:
                cnt = 0
                dcnt = 0
                dvals = [0] * NDMA_SEMS
                for op in self.ops[e]:
                    if op.is_dma:
                        slot = dcnt % NDMA_SEMS
                        dcnt += 1
                        dvals[slot] += 16
                        op.sem = dsem[e][slot]
                        op.semv = dvals[slot]
                    elif op.flag:
                        cnt += 1
                        op.sem = csem[e]
                        op.semv = cnt
            block = es.enter_context(nc.Block())

            def run_engine(e, eng):
                waited = {}
                prev_dma = [None] * NDMA_SEMS
                dcnt = 0
                for op in self.ops[e]:
                    for d in op.deps:
                        key = id(d.sem)
                        if waited.get(key, 0) >= d.semv:
                            continue
                        eng.wait_ge(d.sem, d.semv)
                        waited[key] = d.semv
                    if op.is_dma:
                        slot = dcnt % NDMA_SEMS
                        dcnt += 1
                        p = prev_dma[slot]
                        if p is not None:
                            key = id(p.sem)
                            if waited.get(key, 0) < p.semv:
                                eng.wait_ge(p.sem, p.semv)
                                waited[key] = p.semv
                        prev_dma[slot] = op
                        ins = op.fn(eng)
                        ins.then_inc(op.sem, 16)
                    else:
                        ins = op.fn(eng)
                        if op.flag:
                            ins.then_inc(op.sem, 1)
                if e == "sp":
                    for o in final_ops:
                        key = id(o.sem)
                        if waited.get(key, 0) < o.semv:
                            eng.wait_ge(o.sem, o.semv)
                            waited[key] = o.semv

            @block.tensor
            def _(eng):
                run_engine("pe", eng)

            @block.scalar
            def _(eng):
                run_engine("act", eng)

            @block.vector
            def _(eng):
                run_engine("dve", eng)

            @block.gpsimd
            def _(eng):
                run_engine("pool", eng)

            @block.sync
            def _(eng):
                run_engine("sp", eng)


class Arena:
    def __init__(self, t, nbytes):
        self.t = t
        self.cap = nbytes
        self.off = 0
        self.peak = 0

    def alloc(self, free_shape, dt):
        n = 1
        for s in free_shape:
            n *= s
        esz = 2 if dt == BF16 else 4
        off = (self.off + 63) // 64 * 64
        nb = n * esz
        assert off + nb <= self.cap, ("arena overflow", off + nb, self.cap)
        self.off = off + nb
        self.peak = max(self.peak, self.off)
        v = self.t[:, off // 2:(off + nb) // 2]
        if dt != BF16:
            v = v.bitcast(dt)
        if len(free_shape) > 1:
            names = ["a%d" % i for i in range(len(free_shape))]
            kw = {names[i]: free_shape[i] for i in range(len(free_shape))}
            v = v.rearrange("p (" + " ".join(names) + ") -> p " + " ".join(names), **kw)
        return v

    def mark(self):
        return self.off

    def release(self, m):
        self.off = m


class Cfg:
    def __init__(self, SEQ=4096, CTX=256, DEPTH=2):
        self.SEQ = SEQ
        self.CTX = CTX
        self.DEPTH = DEPTH
        self.T = SEQ + CTX
        self.NT = self.T // 128
        self.NTL = SEQ // 128
        self.NTC = CTX // 128
        self.ROWS = SEQ // 64
        self.NG = SEQ // 512


def rope_tab(cfg):
    t = np.arange(cfg.SEQ)
    rows = (t // 64).astype(np.float32)
    cols = (t % 64).astype(np.float32)
    out = np.zeros((cfg.T, 192), np.float32)
    out[cfg.SEQ:, 0:64] = 1.0
    out[cfg.SEQ:, 128:160] = 1.0
    off = 0
    for R in (64, 32):
        half = R // 2
        inv = (np.float32(10000.0) ** (-np.arange(0, half, 2, dtype=np.float32) / np.float32(half))).astype(np.float32)
        ar = rows[:, None] * inv
        ac = cols[:, None] * inv
        cr, sr, cc, sc = np.cos(ar), np.sin(ar), np.cos(ac), np.sin(ac)
        C = np.concatenate([cr, cr, cc, cc], axis=1)
        S_ = np.concatenate([-sr, sr, -sc, sc], axis=1)
        out[:cfg.SEQ, off:off + R] = C
        out[:cfg.SEQ, off + R:off + 2 * R] = S_
        off += 2 * R
    return out.astype(np.float32)


def mask_b():
    k = np.arange(128)[:, None]
    q = np.arange(512)[None, :]
    out = np.zeros((6, 128, 512), np.float32)
    for r in range(6):
        rel = r - 1
        ok = np.abs(q - 128 * rel - k) <= 128
        out[r] = np.where(ok, 0.0, NEG)
    return out


def c_tables(cfg):
    rows = cfg.ROWS
    kh = min(8, rows)
    k = np.arange(128)[:, None]
    q = np.arange(512)[None, :]
    k_col = k % 64
    q_col = q % 64
    c_start = np.clip(q_col - 8, 0, 64 - 16)
    col_ok = (k_col >= c_start) & (k_col < c_start + 16)
    DC = (np.clip(k_col - q_col, -15, 15) + 15).astype(np.int64) + np.zeros((128, 512), np.int64)
    DR = np.zeros((8, 128, 512), np.int64)
    for r in range(8):
        rel = r - 2
        d = 2 * rel + k // 64 - q // 64
        DR[r] = np.clip(d, -7, 7) + 7
    masks = []
    case_of_g = []
    valid = []
    for g in range(cfg.NG):
        m = np.full((8, 128, 512), NEG, np.float32)
        v = []
        for r in range(8):
            kt = 4 * g + r - 2
            if kt < 0 or kt >= cfg.NTL:
                continue
            k_row = 2 * kt + k // 64
            q_row = 8 * g + q // 64
            r_start = np.clip(q_row - kh // 2, 0, rows - kh)
            row_ok = (k_row >= r_start) & (k_row < r_start + kh)
            ok = row_ok & col_ok
            if ok.any():
                v.append(r)
            m[r] = np.where(ok, 0.0, NEG)
        found = None
        for ci, mm in enumerate(masks):
            if np.array_equal(mm, m):
                found = ci
                break
        if found is None:
            masks.append(m)
            found = len(masks) - 1
        case_of_g.append(found)
        valid.append(v)
    return np.stack(masks).astype(np.float32), case_of_g, valid, DR, DC


class Prog:
    def __init__(self, cfg):
        self.cfg = cfg
        self.S = Sched()
        self.nc = bass.Bass("TRN2", target_bir_lowering=False)
        self.maskc_np, self.case_of_g, self.valid_c, self.DR, self.DC = c_tables(cfg)
        self.ncase = self.maskc_np.shape[0]

    def MM(self, out, lhsT, rhs, start, stop, r, w):
        return self.S.add("pe", lambda e: e.matmul(out, lhsT=lhsT, rhs=rhs, start=start, stop=stop), r, w)

    def TR(self, out, in_, ident, r, w):
        return self.S.add("pe", lambda e: e.transpose(out=out, in_=in_, identity=ident), r, w)

    def ACT(self, out, in_, func, r, w, scale=1.0, bias=0.0, accum=None):
        if accum is None:
            return self.S.add("act", lambda e: e.activation(out=out, in_=in_, func=func, bias=bias, scale=scale), r, w)
        return self.S.add("act", lambda e: e.activation(out=out, in_=in_, func=func, bias=bias, scale=scale,
                                                        accum_out=accum), r, w)

    def TT(self, eng, out, in0, in1, op, r, w):
        return self.S.add(eng, lambda e: e.tensor_tensor(out=out, in0=in0, in1=in1, op=op), r, w)

    def TS(self, eng, out, in0, s1, s2, op0, op1, r, w):
        if s2 is None:
            return self.S.add(eng, lambda e: e.tensor_scalar(out=out, in0=in0, scalar1=s1, scalar2=None, op0=op0), r, w)
        return self.S.add(eng, lambda e: e.tensor_scalar(out=out, in0=in0, scalar1=s1, scalar2=s2, op0=op0, op1=op1), r, w)

    def STT(self, eng, out, in0, scalar, in1, op0, op1, r, w):
        return self.S.add(eng, lambda e: e.scalar_tensor_tensor(out=out, in0=in0, scalar=scalar, in1=in1,
                                                                 op0=op0, op1=op1), r, w)

    def CP(self, eng, out, in_, r, w):
        if eng == "act":
            return self.S.add("act", lambda e: e.copy(out=out, in_=in_), r, w)
        return self.S.add(eng, lambda e: e.tensor_copy(out=out, in_=in_), r, w)

    def RED(self, out, in_, op, r, w):
        return self.S.add("dve", lambda e: e.tensor_reduce(out=out, in_=in_, axis=AX.X, op=op), r, w)

    def RCP(self, out, in_, r, w):
        return self.S.add("dve", lambda e: e.reciprocal(out=out, in_=in_), r, w)

    def DMA(self, q, out, in_, r, w):
        return self.S.add(q, lambda e: e.dma_start(out=out, in_=in_), r, w, dma=True)

    def MEMSET(self, eng, ap, val, w):
        return self.S.add(eng, lambda e: e.memset(ap, val), (), w)

    def build(self):
        cfg = self.cfg
        nc = self.nc
        L = cfg.DEPTH
        T, NT, NTL = cfg.T, cfg.NT, cfg.NTL

        def din(name, shape):
            return nc.dram_tensor(name, list(shape), F32, kind="ExternalInput").ap()

        self.I = I = {}
        I["x"] = din("x", [cfg.SEQ, D])
        I["c"] = din("c", [1, D])
        I["ctx"] = din("ctx", [cfg.CTX, D])
        I["c_ctx"] = din("c_ctx", [1, D])
        I["w_mod"] = din("w_mod", [L, D, 6 * D])
        I["b_mod"] = din("b_mod", [L, 6 * D])
        I["g_norm_mix"] = din("g_norm_mix", [L, D])
        I["w_in"] = din("w_in", [L, D, IN_COLS])
        I["g_q_a"] = din("g_q_a", [L, 256])
        I["w_q_b"] = din("w_q_b", [L, 256, 384])
        I["g_kv_a"] = din("g_kv_a", [L, 128])
        I["w_kv_b"] = din("w_kv_b", [L, 128, 512])
        I["sink_b"] = din("sink_b", [L, 4])
        I["biasg"] = din("biasg", [L, 4, 8, 128, 512])
        I["g_q_d"] = din("g_q_d", [L, 64])
        I["g_k_d"] = din("g_k_d", [L, 64])
        I["w_gate"] = din("w_gate", [L, 4, D, D])
        I["b_gate"] = din("b_gate", [L, 4 * D])
        I["w_branch"] = din("w_branch", [L, 4, 256, D])
        I["w_out"] = din("w_out", [L, D, D])
        I["g_norm_ffn"] = din("g_norm_ffn", [L, D])
        I["w_group"] = din("w_group", [L, D, 4])
        I["b_group"] = din("b_group", [L, 4])
        I["w_router"] = din("w_router", [L, D, 32])
        I["b_router"] = din("b_router", [L, 32])
        I["w13r"] = din("w13r", [L, NE * 128, 4096])
        I["w2r"] = din("w2r", [L, NE * 128, 2048])
        I["iota32"] = din("iota32", [1, 32])
        I["ltri"] = din("ltri", [128, 128])
        I["pidx"] = din("pidx", [128, 1])
        I["thr"] = din("thr", [1, NBLK_MAX])
        I["g_final"] = din("g_final", [1, D])
        I["ident"] = din("ident", [128, 128])
        I["ropetab"] = din("ropetab", [T, 192])
        I["maskb"] = din("maskb", [6, 128, 512])
        I["maskc"] = din("maskc", [self.ncase, 8, 128, 512])
        self.out = nc.dram_tensor("out", [cfg.SEQ, D], F32, kind="ExternalOutput").ap()

        def dscr(name, shape, dt):
            return nc.dram_tensor(name, list(shape), dt, kind="Internal").ap()

        self.xcur = dscr("xcur", [T, D], F32)
        self.QT = [dscr("QT_A", [96, 4, T], BF16)] + [dscr("QT_%d" % n, [128, 2, T], BF16) for n in (1, 2, 3)]
        self.KT = [dscr("KT_A", [96, 4, T], BF16)] + [dscr("KT_%d" % n, [128, 2, T], BF16) for n in (1, 2, 3)]
        self.VA = [dscr("VA_%d" % n, [128, NT, 4, 128], BF16) for n in range(4)]
        self.OT = dscr("OT", [8, 128, T], BF16)
        self.H2 = dscr("H2", [T, D], BF16)
        self.XS = dscr("XS", [NBLK_MAX * 128, D], BF16)
        self.YS = dscr("YS", [NBLK_MAX * 128, D], BF16)

        with contextlib.ExitStack() as es:
            ARENA_BYTES = 207 * 1024
            at = es.enter_context(nc.sbuf_tensor("arena", [128, ARENA_BYTES // 2], BF16))
            self.A = Arena(at, ARENA_BYTES)
            self.banks = [es.enter_context(nc.psum_tensor("pb%d" % i, [128, 1024], F32)) for i in range(4)]
            self.setup()
            for l in range(L):
                self.layer(l)
            self.S.emit(nc)
        return nc

    def bank(self, i):
        return self.banks[i // 2][:, (i % 2) * 512:(i % 2 + 1) * 512]

    def bank_bf(self, i):
        return self.bank(i).bitcast(BF16)

    def setup(self):
        A, I = self.A, self.I
        self.ident_f = A.alloc([128], F32)
        self.ident_b = A.alloc([128], BF16)
        self.ones_f = A.alloc([128], F32)
        self.ones_b = A.alloc([128], BF16)
        self.DMA("sp", self.ident_f, I["ident"], (), ["ident_f"])
        self.DMA("pool", self.ident_b, I["ident"], (), ["ident_b"])
        self.MEMSET("dve", self.ones_f, 1.0, ["ones_f"])
        self.MEMSET("dve", self.ones_b, 1.0, ["ones_b"])
        self.mod = [A.alloc([6 * D], F32), A.alloc([6 * D], F32)]
        self.rt = A.alloc([self.cfg.NT, 8], F32)
        self.base = A.alloc([32], F32)
        self.iota = A.alloc([32], F32)
        self.ltb = A.alloc([128], BF16)
        self.onesbb = A.alloc([128], BF16)
        self.pidx = A.alloc([1], F32)
        self.thr = A.alloc([NBLK_MAX], F32)
        self.slot_i = A.alloc([self.cfg.NT, 2], I32)
        self.widx = A.alloc([NBLK_MAX], I32)
        self.DMA("sp", self.iota, I["iota32"].partition_broadcast(128), (), ["iota"])
        self.DMA("pool", self.ltb, I["ltri"], (), ["ltb"])
        self.MEMSET("dve", self.onesbb, 1.0, ["onesbb"])
        self.DMA("sp", self.pidx, I["pidx"], (), ["pidx"])
        self.DMA("sp", self.thr, I["thr"].partition_broadcast(128), (), ["thr"])
        self.pers_mark = A.mark()

    def layer(self, l):
        import os
        cfg = self.cfg
        stop = int(os.environ.get("KSTOP", "99"))
        self.with_ctx = l < cfg.DEPTH - 1
        phases = [self.phase_mod, self.phase_p1, self.phase_p2, self.phase_p3, self.phase_p4]
        for pi, ph in enumerate(phases):
            if l * 5 + pi > stop:
                return
            self.S.barrier()
            self.A.release(self.pers_mark)
            ph(l)

    def phase_mod(self, l):
        A, I = self.A, self.I
        cb = [A.alloc([D], F32), A.alloc([D], F32)]
        lh = [A.alloc([8, 128], F32), A.alloc([8, 128], F32)]
        bmod = A.alloc([6 * D], F32)
        gm = A.alloc([D], F32)
        gf = A.alloc([D], F32)
        wm = [A.alloc([8, 512], F32), A.alloc([8, 512], F32)]
        self.DMA("sp", cb[0], I["c"].partition_broadcast(128), (), ["cb0"])
        self.DMA("sp", cb[1], I["c_ctx"].partition_broadcast(128), (), ["cb1"])
        self.DMA("sp", bmod[0:1, :], I["b_mod"][l:l + 1, :], (), ["bmod"])
        self.DMA("sp", gm, I["g_norm_mix"][l:l + 1, :].partition_broadcast(128), (), ["gm"])
        self.DMA("sp", gf, I["g_norm_ffn"][l:l + 1, :].partition_broadcast(128), (), ["gf"])
        for w in range(2):
            self.ACT(cb[w], cb[w], AF.Silu, ["cb%d" % w], ["cb%d" % w])
            for k in range(8):
                bk = self.banks[w][:, k * 128:(k + 1) * 128]
                self.TR(bk, cb[w][:, k * 128:(k + 1) * 128], self.ident_f, ["cb%d" % w, "ident_f"], ["pbk%d" % w])
            self.CP("act", lh[w], self.banks[w][:, :].rearrange("p (k m) -> p k m", k=8), ["pbk%d" % w], ["lh%d" % w])
        for j in range(12):
            wmj = wm[j % 2]
            wk = "wm%d" % (j % 2)
            self.DMA("sp", wmj, I["w_mod"][l, :, j * 512:(j + 1) * 512].rearrange("(k p) n -> p k n", p=128), (), [wk])
            for w in range(2):
                bi = 4 + (j * 2 + w) % 4
                bk = self.bank(bi)
                bkk = "bank%d" % bi
                for k in range(8):
                    self.MM(bk, lh[w][:, k, :], wmj[:, k, :], k == 0, False, ["lh%d" % w, wk], [bkk])
                self.MM(bk, self.ones_f[0:1, :], bmod[0:1, j * 512:(j + 1) * 512], False, True, ["ones_f", "bmod"], [bkk])
                self.CP("dve" if w == 0 else "act", self.mod[w][:, j * 512:(j + 1) * 512], bk, [bkk], ["mod%d" % w])
        for w in range(2):
            m = self.mod[w]
            self.STT("dve", m[:, D:2 * D], m[:, D:2 * D], 1.0, gm, ALU.add, ALU.mult, ["mod%d" % w, "gm"], ["mod%d" % w])
            self.STT("dve", m[:, 4 * D:5 * D], m[:, 4 * D:5 * D], 1.0, gf, ALU.add, ALU.mult, ["mod%d" % w, "gf"], ["mod%d" % w])

    def x_src(self, l, t):
        cfg = self.cfg
        if l == 0:
            if t < cfg.NTL:
                return self.I["x"][t * 128:(t + 1) * 128, :]
            return self.I["ctx"][(t - cfg.NTL) * 128:(t - cfg.NTL + 1) * 128, :]
        return self.xcur[t * 128:(t + 1) * 128, :]

    def rstd_of(self, src, n, junk, ssq, rstd, rk, tag, jk="junk"):
        self.ACT(junk, src, AF.Square, rk, [jk, "ssq" + tag], accum=ssq)
        self.ACT(ssq, ssq, AF.Sqrt, ["ssq" + tag], ["ssq" + tag], scale=1.0 / n, bias=EPS)
        self.RCP(rstd, ssq, ["ssq" + tag], ["rstd" + tag])

    def norm_mod(self, xt, xk, w, goff, soff, junk, tmp, out, outk, ssq, rstd, tag, jk="junk", tk="tmp"):
        self.rstd_of(xt, D, junk, ssq, rstd, [xk], tag, jk=jk)
        m = self.mod[w]
        self.STT("dve", tmp, xt, rstd, m[:, goff:goff + D], ALU.mult, ALU.mult, [xk, "rstd" + tag, "mod%d" % w], [tk])
        self.TT("dve", out, tmp, m[:, soff:soff + D], ALU.add, [tk, "mod%d" % w], [outk])

    def transpose8(self, src, srck, bank_bf, bankk, dst, dstk, nblk=8, ident=None, identk="ident_b"):
        ident = self.ident_b if ident is None else ident
        srcks = [srck] if isinstance(srck, str) else list(srck)
        for k in range(nblk):
            self.TR(bank_bf[:, k * 128:(k + 1) * 128], src[:, k * 128:(k + 1) * 128], ident, srcks + [identk], [bankk])
        self.CP("act", dst, bank_bf[:, 0:nblk * 128].rearrange("p (k m) -> p k m", k=nblk), [bankk], [dstk])

    def rope(self, src, srck, H, R, tab, tabk, coff, soff, t1, t2, outs, tag):
        w = R // 4
        C = tab[:, coff:coff + R]
        Sg = tab[:, soff:soff + R].rearrange("p (a b w) -> p a b w", a=2, b=2)
        t1v = t1[:, 0:H * R].rearrange("p (h r) -> p h r", h=H)
        t2v = t2[:, 0:H * R].rearrange("p (h r) -> p h r", h=H)
        self.TT("dve", t1v, src, C.unsqueeze(1).to_broadcast([128, H, R]), ALU.mult, [srck, tabk], ["t1" + tag])
        s5 = src.rearrange("p h (a b w) -> p h a b w", a=2, b=2)
        t5 = t2v.rearrange("p h (a b w) -> p h a b w", a=2, b=2)
        self.TT("dve", t5[:, :, :, 0, :], s5[:, :, :, 1, :], Sg[:, :, 0, :].unsqueeze(1).to_broadcast([128, H, 2, w]),
                ALU.mult, [srck, tabk], ["t2a" + tag])
        self.TT("dve", t5[:, :, :, 1, :], s5[:, :, :, 0, :], Sg[:, :, 1, :].unsqueeze(1).to_broadcast([128, H, 2, w]),
                ALU.mult, [srck, tabk], ["t2b" + tag])
        for (dst, h0, h1, dk) in outs:
            self.TT("dve", dst, t1v[:, h0:h1, :], t2v[:, h0:h1, :], ALU.add, ["t1" + tag, "t2a" + tag, "t2b" + tag], [dk])

    def phase_p1(self, l):
        cfg, A, I = self.cfg, self.A, self.I
        NT, NTL = cfg.NT, cfg.NTL
        win = A.alloc([8, IN_COLS], BF16)
        for k in range(8):
            self.DMA("pool", win[:, k, :], I["w_in"][l, k * 128:(k + 1) * 128, :], (), ["win"])
        wqb = A.alloc([2, 384], BF16)
        self.DMA("pool", wqb, I["w_q_b"][l].rearrange("(k p) n -> p k n", p=128), (), ["wqb"])
        wkvb = A.alloc([512], BF16)
        self.DMA("pool", wkvb, I["w_kv_b"][l], (), ["wkvb"])
        gqa = A.alloc([256], F32)
        gkv = A.alloc([128], F32)
        gd = A.alloc([6, 64], F32)
        self.DMA("sp", gqa, I["g_q_a"][l:l + 1, :].partition_broadcast(128), (), ["gqa"])
        self.DMA("sp", gkv, I["g_kv_a"][l:l + 1, :].partition_broadcast(128), (), ["gkv"])
        for h in range(6):
            src = I["g_q_d"] if h < 4 else I["g_k_d"]
            self.DMA("sp", gd[:, h, :], src[l:l + 1, :].partition_broadcast(128), (), ["gd"])
        xt2 = [A.alloc([D], F32) for _ in range(2)]
        tab2 = [A.alloc([192], F32) for _ in range(2)]
        junk = A.alloc([D], F32)
        tmp = A.alloc([D], F32)
        hb = A.alloc([D], BF16)
        hT = A.alloc([8, 128], BF16)
        ssq = A.alloc([8], F32)
        rstd = A.alloc([8], F32)
        ssqD = A.alloc([8], F32)
        krs = A.alloc([32], F32)
        qf = A.alloc([384], F32)
        kvf = A.alloc([512], F32)
        bdf = A.alloc([512], F32)
        bdfD = A.alloc([512], F32)
        c2f = A.alloc([256], F32)
        rstdD = A.alloc([8], F32)
        cqkv = A.alloc([384], BF16)
        cT = A.alloc([3, 128], BF16)
        qa = A.alloc([4, 96], BF16)
        ka = A.alloc([4, 96], BF16)
        krr = A.alloc([1, 32], F32)
        t1 = A.alloc([512], F32)
        t2 = A.alloc([512], F32)
        sq = A.alloc([384], F32)
        qn = A.alloc([6, 64], F32)
        qkb = A.alloc([8, 64], BF16)
        qkc = A.alloc([512], BF16)
        stA = [A.alloc([8, 128], BF16) for _ in range(2)]
        stP = [A.alloc([4, 128], BF16) for _ in range(2)]
        va = [[A.alloc([4, 128], BF16) for _ in range(2)] for _ in range(4)]
        for n in range(4):
            for b in range(2):
                self.MEMSET("pool", va[n][b], 1.0, ["va%d_%d" % (n, b)])
        T0, T1 = self.bank_bf(0), self.bank_bf(1)
        pA, pBD, pC1, pC2, pQ, pKV = (self.bank(i) for i in (2, 3, 4, 5, 6, 7))

        def load(t):
            b = t % 2
            self.DMA("sp", xt2[b], self.x_src(l, t), (), ["xt%d" % b])
            self.DMA("sp", tab2[b], I["ropetab"][t * 128:(t + 1) * 128, :], (), ["tab%d" % b])

        import os
        KCUT = int(os.environ.get("KCUT", "99"))
        load(0)
        for t in range(NT):
            if t + 1 < NT:
                load(t + 1)
            b = t % 2
            xt, xk, tab, tabk = xt2[b], "xt%d" % b, tab2[b], "tab%d" % b
            w = 0 if t < NTL else 1
            tok = slice(t * 128, (t + 1) * 128)
            if t == 0:
                self.norm_mod(xt, xk, w, D, 0, junk, tmp, hb, "hb", ssq[:, 0:1], rstd[:, 0:1], "n1")
            self.transpose8(hb, "hb", T0, "T0", hT, "hT")
            if KCUT == 1:
                return
            def proj(dst, dk, c0, c1):
                for k in range(8):
                    self.MM(dst[:, 0:c1 - c0], hT[:, k, :], win[:, k, c0:c1], k == 0, k == 7, ["hT", "win"], [dk])
            proj(pA, "pA", 0, 416)
            proj(pBD, "pBD", 416, 928)
            self.CP("act", bdf, pBD, ["pBD"], ["bdf"])
            proj(pC1, "pC1", 928, 1440)
            proj(pC2, "pC2", 1440, 1696)
            proj(pBD, "pBD", 1696, 2208)
            if t + 1 < NT:
                bn_ = (t + 1) % 2
                self.norm_mod(xt2[bn_], "xt%d" % bn_, 0 if t + 1 < NTL else 1, D, 0, junk, tmp, hb, "hb",
                              ssq[:, 0:1], rstd[:, 0:1], "n1")
            if KCUT == 2:
                return
            self.ACT(junk[:, 0:256], pA[:, 0:256], AF.Square, ["pA"], ["junk", "ssqA0"], accum=ssq[:, 1:2])
            self.ACT(junk[:, 256:384], pA[:, 256:384], AF.Square, ["pA"], ["junk", "ssqA1"], accum=ssq[:, 2:3])
            self.ACT(ssq[:, 1:2], ssq[:, 1:2], AF.Sqrt, ["ssqA0"], ["ssqA0"], scale=1.0 / 256, bias=EPS)
            self.ACT(ssq[:, 2:3], ssq[:, 2:3], AF.Sqrt, ["ssqA1"], ["ssqA1"], scale=1.0 / 128, bias=EPS)
            self.RCP(rstd[:, 1:3], ssq[:, 1:3], ["ssqA0", "ssqA1"], ["rstdA"])
            self.STT("dve", cqkv[:, 0:256], pA[:, 0:256], rstd[:, 1:2], gqa, ALU.mult, ALU.mult, ["pA", "rstdA", "gqa"], ["cqkv0"])
            self.STT("dve", cqkv[:, 256:384], pA[:, 256:384], rstd[:, 2:3], gkv, ALU.mult, ALU.mult, ["pA", "rstdA", "gkv"], ["cqkv1"])
            def gqa_v(src, srck, n):
                v = va[n][b]
                vp = v.rearrange("p (g two) d -> p g two d", two=2)
                sv = src[:, 384:512].rearrange("p (g r) -> p g r", g=2)
                self.CP("pool", vp[:, :, 0, 0:64], sv, [srck], ["va%d_%d" % (n, b)])
                self.CP("pool", vp[:, :, 1, 64:128], sv, [srck], ["va%d_%d" % (n, b)])
                return v

            def qk_store(src, srcks, n, tbank, tbk):
                for k in range(4):
                    self.TR(tbank[:, k * 128:(k + 1) * 128], src[:, k * 128:(k + 1) * 128], self.ident_b,
                            list(srcks) + ["ident_b"], [tbk])
                sp_ = stP[n % 2]
                spk = "stP%d" % (n % 2)
                self.CP("act", sp_, tbank[:, 0:512].rearrange("p (k m) -> p k m", k=4), [tbk], [spk])
                self.DMA("sp", self.QT[n][:, :, tok], sp_[:, 0:2, :], [spk], [])
                self.DMA("sp", self.KT[n][:, :, tok], sp_[:, 2:4, :], [spk], [])

            pBv = bdf[:, 0:384].rearrange("p (h r) -> p h r", h=6)
            qkb_d = qkb[:, 4:8, :].rearrange("p (g two) r -> p g two r", two=2)
            self.rope(pBv, "bdf", 6, 64, tab, tabk, 0, 64, t1, t2,
                      [(qkb[:, 0:4, :], 0, 4, "qkb0"), (qkb_d[:, :, 0, :], 4, 6, "qkb1"), (qkb_d[:, :, 1, :], 4, 6, "qkb2")], "R")
            vB = gqa_v(bdf, "bdf", 1)
            self.CP("act", qkc, pC1, ["pC1"], ["qkc"])
            vC = va[2][b]
            vCp = vC.rearrange("p (j two) d -> p j two d", two=2)
            self.CP("act", c2f, pC2[:, 0:256], ["pC2"], ["c2f"])
            pC2p = c2f.rearrange("p (j two r) -> p j two r", two=2, r=64)
            self.CP("pool", vCp[:, :, 0, 0:64], pC2p[:, :, 0, :], ["c2f"], ["va2_%d" % b])
            self.CP("pool", vCp[:, :, 1, 64:128], pC2p[:, :, 1, :], ["c2f"], ["va2_%d" % b])
            for k in range(3):
                self.TR(T1[:, k * 128:(k + 1) * 128], cqkv[:, k * 128:(k + 1) * 128], self.ident_b,
                        ["cqkv0", "cqkv1", "ident_b"], ["T1"])
            self.CP("act", cT, T1[:, 0:384].rearrange("p (k m) -> p k m", k=3), ["T1"], ["cT"])
            if KCUT == 3:
                return
            for k in range(2):
                self.MM(pQ[:, 0:384], cT[:, k, :], wqb[:, k, :], k == 0, k == 1, ["cT", "wqb"], ["pQ"])
            self.MM(pKV, cT[:, 2, :], wkvb, True, True, ["cT", "wkvb"], ["pKV"])
            if KCUT == 4:
                return
            self.CP("act", qf, pQ[:, 0:384], ["pQ"], ["qf"])
            self.CP("act", kvf, pKV, ["pKV"], ["kvf"])
            self.CP("act", krs, pA[:, 384:416], ["pA"], ["krs"])
            pQv = qf.rearrange("p (h r) -> p h r", h=4)
            pKVv = kvf.rearrange("p (h r) -> p h r", h=4)
            self.CP("pool", qa[:, :, 0:64], pQv[:, :, 0:64], ["qf"], ["qa0"])
            if KCUT == 41:
                return
            self.rope(pQv[:, :, 64:96], "qf", 4, 32, tab, tabk, 128, 160, t1, t2, [(qa[:, :, 64:96], 0, 4, "qa1")], "R")
            if KCUT == 42:
                return
            self.rope(krs.unsqueeze(1), "krs", 1, 32, tab, tabk, 128, 160, t1, t2, [(krr, 0, 1, "krr")], "R")
            if KCUT == 5:
                return
            self.CP("pool", ka[:, :, 0:64], pKVv[:, :, 0:64], ["kvf"], ["ka0"])
            self.CP("dve", ka[:, :, 64:96], krr.to_broadcast([128, 4, 32]), ["krr"], ["ka1"])
            vA = va[0][b]
            vAp = vA.rearrange("p (j two) d -> p j two d", two=2)
            pKVp = pKVv.rearrange("p (j two) r -> p j two r", two=2)
            self.CP("pool", vAp[:, :, 0, 0:64], pKVp[:, :, 0, 64:128], ["kvf"], ["va0_%d" % b])
            self.CP("pool", vAp[:, :, 1, 64:128], pKVp[:, :, 1, 64:128], ["kvf"], ["va0_%d" % b])
            if KCUT == 6:
                return
            for h in range(4):
                self.TR(T1[0:96, h * 128:(h + 1) * 128], qa[:, h, :], self.ident_b, ["qa0", "qa1", "ident_b"], ["T1"])
                self.TR(T1[0:96, (4 + h) * 128:(5 + h) * 128], ka[:, h, :], self.ident_b, ["ka0", "ka1", "ident_b"], ["T1"])
            sA = stA[b]
            self.CP("act", sA[0:96], T1[0:96, :].rearrange("p (k m) -> p k m", k=8), ["T1"], ["stA%d" % b])
            if KCUT == 7:
                return
            self.DMA("sp", self.QT[0][:, :, tok], sA[0:96, 0:4, :], ["stA%d" % b], [])
            self.DMA("sp", self.KT[0][:, :, tok], sA[0:96, 4:8, :], ["stA%d" % b], [])
            self.DMA("sp", self.VA[0][:, t, :, :], vA, ["va0_%d" % b], [])
            if KCUT == 8:
                return

            qk_store(qkb.rearrange("p h r -> p (h r)"), ["qkb0", "qkb1", "qkb2"], 1, T0, "T0")
            self.DMA("sp", self.VA[1][:, t, :, :], vB, ["va1_%d" % b], [])
            if KCUT == 9:
                return
            qk_store(qkc, ["qkc"], 2, T1, "T1")
            self.DMA("sp", self.VA[2][:, t, :, :], vC, ["va2_%d" % b], [])
            if KCUT == 10:
                return
            self.CP("act", bdfD, pBD, ["pBD"], ["bdfD"])
            pDv = bdfD[:, 0:384].rearrange("p (h r) -> p h r", h=6)
            sqv = sq.rearrange("p (h r) -> p h r", h=6)
            self.ACT(sq, pBD[:, 0:384], AF.Square, ["pBD"], ["sq"])
            self.RED(ssqD[:, 0:6], sqv, ALU.add, ["sq"], ["ssqD"])
            self.ACT(ssqD[:, 0:6], ssqD[:, 0:6], AF.Sqrt, ["ssqD"], ["ssqD"], scale=1.0 / 64, bias=EPS)
            self.RCP(rstdD[:, 0:6], ssqD[:, 0:6], ["ssqD"], ["rstdD"])
            self.TT("dve", qn, pDv, rstdD[:, 0:6].unsqueeze(2).to_broadcast([128, 6, 64]), ALU.mult, ["bdfD", "rstdD"], ["qn"])
            self.TT("dve", qn, qn, gd, ALU.mult, ["qn", "gd"], ["qn"])
            self.rope(qn, "qn", 6, 64, tab, tabk, 0, 64, t1, t2,
                      [(qkb[:, 0:4, :], 0, 4, "qkb0"), (qkb_d[:, :, 0, :], 4, 6, "qkb1"), (qkb_d[:, :, 1, :], 4, 6, "qkb2")], "R")
            vD = gqa_v(bdfD, "bdfD", 3)
            qk_store(qkb.rearrange("p h r -> p (h r)"), ["qkb0", "qkb1", "qkb2"], 3, T0, "T0")
            self.DMA("sp", self.VA[3][:, t, :, :], vD, ["va3_%d" % b], [])

    def phase_p2(self, l):
        cfg, A, I = self.cfg, self.A, self.I
        NT, NTL, T = cfg.NT, cfg.NTL, cfg.T
        LOOK = 3
        groups = [(g * 512, 512, g) for g in range(cfg.NG)]
        if self.with_ctx:
            groups.append((cfg.SEQ, cfg.CTX, None))
        ctx_tiles = list(range(NTL, NT))
        base_mark = A.mark()
        pt = [A.alloc([512], BF16) for _ in range(6)]
        tmpf = [A.alloc([512], F32) for _ in range(4)]
        rden = [A.alloc([512], F32) for _ in range(2)]
        rdt = A.alloc([512], F32)
        ost = [A.alloc([512], BF16) for _ in range(2)]
        qt = [A.alloc([512], BF16) for _ in range(2)]
        esink = A.alloc([4], F32)
        self.DMA("sp", esink, I["sink_b"][l:l + 1, :].partition_broadcast(128), (), ["esink"])
        self.ACT(esink, esink, AF.Exp, ["esink"], ["esink"])
        inner_mark = A.mark()
        cnt = {"pt": 0, "s": 0, "tf": 0, "grp": 0}
        for n in range(4):
            A.release(inner_mark)
            self.S.barrier()
            isA = n == 0
            scale = (96 ** -0.5) if isA else 0.125
            LOOK = 2 if n == 2 else 3
            if isA:
                kt = A.alloc([4, T], BF16)
                self.DMA("sp", kt[0:96], self.KT[0], (), ["kt"])
            else:
                kt = A.alloc([4, T], BF16)
                self.MEMSET("pool", kt, 0.0, ["kt"])
                for hh in range(4):
                    hp = (hh % 2) * 64
                    self.DMA("sp", kt[hp:hp + 64, hh, :], self.KT[n][hp:hp + 64, hh // 2, :], (), ["kt"])
            vat = A.alloc([NT, 4, 128], BF16)
            step = max(1, NT // 4)
            for t0 in range(0, NT, step):
                t1_ = min(NT, t0 + step)
                self.DMA("sp", vat[:, t0:t1_], self.VA[n][:, t0:t1_], (), ["vat"])
            mb = mc = bias = comb = None
            if n == 1:
                mb = A.alloc([6, 512], F32)
                self.DMA("sp", mb, I["maskb"].rearrange("r p q -> p r q"), (), ["mb"])
            if n == 2:
                mc = A.alloc([self.ncase * 8, 512], BF16)
                for ci in range(self.ncase):
                    self.DMA("pool", mc[:, ci * 8:(ci + 1) * 8, :], I["maskc"][ci].rearrange("r p q -> p r q"), (), ["mc"])
                bias = A.alloc([8, 512], F32)
                comb1 = A.alloc([8, 512], F32)
                comb = [comb1, comb1]
            G_list = []
            for h in range(4):
                for gi, (q0, nq, g) in enumerate(groups):
                    tiles = []
                    if g is None:
                        tiles = [(t_, None) for t_ in ctx_tiles]
                    elif n in (0, 3):
                        tiles = [(t_, None) for t_ in range(NT)]
                    elif n == 1:
                        for j in range(4 * g - 1, 4 * g + 5):
                            if 0 <= j < NTL:
                                tiles.append((j, ("mb", j - 4 * g + 1)))
                        tiles += [(t_, None) for t_ in ctx_tiles]
                    else:
                        for r in self.valid_c[g]:
                            tiles.append((4 * g + r - 2, ("comb", r)))
                        tiles += [(t_, None) for t_ in ctx_tiles]
                    gid = cnt["grp"]
                    cnt["grp"] += 1
                    G_list.append(dict(h=h, q0=q0, nq=nq, g=g, tiles=tiles, gid=gid, first_of_head=(gi == 0)))
            items = [(G, i) for G in G_list for i in range(len(G["tiles"]))]
            state = {}

            def emit_S(idx):
                G, i = items[idx]
                h, q0, nq, gid = G["h"], G["q0"], G["nq"], G["gid"]
                ph = (h % 2) * 64
                vr = slice(ph, ph + 64)
                qb = gid % 2
                qtb, qk_ = qt[qb], "qt%d" % qb
                if i == 0:
                    if n == 2 and G["first_of_head"]:
                        self.DMA("sp", bias, I["biasg"][l, h].rearrange("r p q -> p r q"), (), ["bias"])
                    if n == 2 and G["first_of_head"]:
                        state["built"] = None
                    if n == 2 and G["g"] is not None and state.get("built") != (self.case_of_g[G["g"]], tuple(self.valid_c[G["g"]])):
                        state["built"] = (self.case_of_g[G["g"]], tuple(self.valid_c[G["g"]]))
                        ci = self.case_of_g[G["g"]]
                        cb_ = comb[gid % 2]
                        for r in self.valid_c[G["g"]]:
                            self.TT("pool", cb_[:, r, :], bias[:, r, :], mc[:, ci * 8 + r, :], ALU.add,
                                    ["bias", "mc"], ["comb_%d" % r])
                    if isA:
                        self.DMA("sp", qtb[0:96, 0:nq], self.QT[0][:, h, q0:q0 + nq], (), [qk_])
                    else:
                        self.DMA("sp", qtb[:, 0:nq], self.QT[n][:, h // 2, q0:q0 + nq], (), [qk_])
                q_ap = qtb[0:96, 0:nq] if isA else qtb[:, 0:nq]
                tk = G["tiles"][i][0]
                sb_ = cnt["s"] % 4
                cnt["s"] += 1
                psS = self.bank(sb_)
                psk = "psS%d" % sb_
                if isA:
                    k_ap = kt[0:96, h, tk * 128:(tk + 1) * 128]
                else:
                    k_ap = kt[:, h, tk * 128:(tk + 1) * 128]
                self.MM(psS[:, 0:nq], k_ap, q_ap, True, True, ["kt", qk_], [psk])
                state[idx] = (psS, psk)

            for j in range(min(LOOK, len(items))):
                emit_S(j)
            for idx, (G, i) in enumerate(items):
                if idx + LOOK < len(items):
                    emit_S(idx + LOOK)
                h, q0, nq, gid = G["h"], G["q0"], G["nq"], G["gid"]
                ph = (h % 2) * 64
                vr = slice(ph, ph + 64)
                dr = slice(64 - ph, 128 - ph)
                psS, psk = state.pop(idx)
                tk, tabinfo = G["tiles"][i]
                ob = gid % 2
                psO = self.bank(6 + ob)
                pok = "psO%d" % ob
                last = len(G["tiles"]) - 1
                pb_ = cnt["pt"] % 6
                cnt["pt"] += 1
                ptb, ptk = pt[pb_], "pt%d" % pb_
                if tabinfo is None:
                    self.ACT(ptb[:, 0:nq], psS[:, 0:nq], AF.Exp, [psk], [ptk], scale=scale)
                else:
                    if tabinfo[0] == "mb":
                        tabv, tabk = mb[:, tabinfo[1], :], "mb"
                    else:
                        tabv, tabk = comb[gid % 2][:, tabinfo[1], :], "comb_%d" % tabinfo[1]
                    fb = cnt["tf"] % 4
                    cnt["tf"] += 1
                    self.STT("dve", tmpf[fb][:, 0:nq], psS[:, 0:nq], scale, tabv[:, 0:nq], ALU.mult, ALU.add,
                             [psk, tabk], ["tmpf%d" % fb])
                    self.ACT(ptb[:, 0:nq], tmpf[fb][:, 0:nq], AF.Exp, ["tmpf%d" % fb], [ptk])
                self.MM(psO[:, 0:nq], vat[:, tk, h, :], ptb[:, 0:nq], i == 0, i == last, ["vat", ptk], [pok])
                if i == last:
                    rb = gid % 2
                    rd, rdk = rden[rb], "rden%d" % rb
                    if n in (1, 2):
                        if n == 1:
                            self.ACT(rdt[dr, 0:nq], psO[dr, 0:nq], AF.Ln, [pok, "esink"], ["rdt"], bias=esink[dr, h:h + 1])
                        else:
                            self.ACT(rdt[dr, 0:nq], psO[dr, 0:nq], AF.Ln, [pok], ["rdt"])
                        self.ACT(rdt[dr, 0:nq], rdt[dr, 0:nq], AF.Exp, ["rdt"], ["rdt"], scale=-1.0)
                        self.CP("dve", rd[vr, 0:nq], rdt[dr, 0:nq], ["rdt"], [rdk])
                    else:
                        self.RCP(rd[vr, 0:nq], psO[dr, 0:nq], [pok], [rdk])
                    osb, osk = ost[rb], "ost%d" % rb
                    self.TT("dve", osb[vr, 0:nq], psO[vr, 0:nq], rd[vr, 0:nq], ALU.mult, [pok, rdk], [osk])
                    self.DMA("sp", self.OT[n * 2 + h // 2][vr, q0:q0 + nq], osb[vr, 0:nq], [osk], [])
        A.release(base_mark)

    def phase_p3(self, l):
        cfg, A, I = self.cfg, self.A, self.I
        NT, NTL = cfg.NT, cfg.NTL
        tiles = list(range(NT)) if self.with_ctx else list(range(NTL))
        wg = A.alloc([32, D], BF16)
        for n in range(4):
            for k in range(8):
                self.DMA("pool", wg[:, n * 8 + k, :], I["w_gate"][l, n, k * 128:(k + 1) * 128, :], (), ["wg"])
        bg = A.alloc([4 * D], BF16)
        self.DMA("pool", bg[0:1, :], I["b_gate"][l:l + 1, :], (), ["bg"])
        wbr = A.alloc([8, D], BF16)
        self.DMA("pool", wbr, I["w_branch"][l].rearrange("n (c p) m -> p (n c) m", p=128), (), ["wbr"])
        wo = A.alloc([8, D], BF16)
        self.DMA("pool", wo, I["w_out"][l].rearrange("(k p) m -> p k m", p=128), (), ["wo"])
        wr = A.alloc([8, 36], F32)
        self.DMA("sp", wr[:, :, 0:4], I["w_group"][l].rearrange("(k p) n -> p k n", p=128), (), ["wr"])
        self.DMA("sp", wr[:, :, 4:36], I["w_router"][l].rearrange("(k p) n -> p k n", p=128), (), ["wr"])
        br = A.alloc([36], F32)
        self.DMA("sp", br[0:1, 0:4], I["b_group"][l:l + 1, :], (), ["br"])
        self.DMA("sp", br[0:1, 4:36], I["b_router"][l:l + 1, :], (), ["br"])
        xt2 = [A.alloc([D], F32) for _ in range(3)]
        ot2 = [A.alloc([8, 128], BF16) for _ in range(2)]
        tmpA = A.alloc([D], F32)
        tmpB = A.alloc([D], F32)
        hb = A.alloc([D], BF16)
        hT = A.alloc([8, 128], BF16)
        gate = A.alloc([D], BF16)
        tmpn = A.alloc([D], BF16)
        ypre = A.alloc([D], F32)
        ypb = A.alloc([D], BF16)
        yT = A.alloc([8, 128], BF16)
        h2Tf = A.alloc([8, 128], F32)
        h2b = A.alloc([D], BF16)
        oh12b = A.alloc([64], BF16)
        csb = A.alloc([64], F32)
        tr0 = A.alloc([32], F32)
        tr1 = A.alloc([32], F32)
        self.MEMSET("dve", self.base, 0.0, ["base"])
        ssq = A.alloc([4], F32)
        rstd = A.alloc([4], F32)
        lg = A.alloc([36], F32)
        sm = A.alloc([16], F32)
        ohg = A.alloc([4], F32)
        pen = A.alloc([4], F32)
        em = A.alloc([32], F32)
        em2 = A.alloc([32], F32)
        oh1 = A.alloc([32], F32)
        oh2 = A.alloc([32], F32)
        c1 = A.alloc([32], F32)
        j4 = A.alloc([4], F32)
        PT = self.bank_bf(0)
        PB, PY = self.banks[2], self.banks[3]
        pR = self.bank(1)
        cntu = [0]

        def load(i):
            t = tiles[i]
            b = i % 2
            self.DMA("sp", xt2[i % 3], self.x_src(l, t), (), ["xt%d" % (i % 3)])
            self.DMA("sp", ot2[b], self.OT[:, :, t * 128:(t + 1) * 128].rearrange("b p t -> p b t"), (), ["ot%d" % b])

        def norm1(i):
            t = tiles[i]
            w = 0 if t < NTL else 1
            self.norm_mod(xt2[i % 3], "xt%d" % (i % 3), w, D, 0, hb, tmpA, hb, "hb", ssq[:, 0:1], rstd[:, 0:1], "n1",
                          jk="hb", tk="tmpA")

        def stageA(i, gen=None):
            t = tiles[i]
            b = i % 2
            xt, xk, ot, otk = xt2[i % 3], "xt%d" % (i % 3), ot2[b], "ot%d" % b
            w = 0 if t < NTL else 1
            m = self.mod[w]
            mk = "mod%d" % w
            tok = slice(t * 128, (t + 1) * 128)
            tmp = tmpA
            self.transpose8(hb, "hb", PT, "PT", hT, "hT")
            for n in range(4):
                for half in range(2):
                    cs = slice(half * 512, (half + 1) * 512)
                    r_ = cntu[0] % 2
                    cntu[0] += 1
                    PGh, pgk = self.bank(2 + r_), "PG%d" % r_
                    PBh, pbk = self.bank(4 + r_), "PB%d" % r_
                    for k in range(8):
                        self.MM(PGh, hT[:, k, :], wg[:, n * 8 + k, cs], k == 0, False, ["hT", "wg"], [pgk])
                    self.MM(PGh, self.ones_b[0:1, :], bg[0:1, n * D + half * 512:n * D + (half + 1) * 512], False, True,
                            ["ones_b", "bg"], [pgk])
                    for c in range(2):
                        self.MM(PBh, ot[:, n * 2 + c, :], wbr[:, n * 2 + c, cs], c == 0, c == 1, [otk, "wbr"], [pbk])
                    gh, ghk = gate[:, r_ * 512:(r_ + 1) * 512], "gate%d" % r_
                    self.ACT(gh, PGh, AF.Sigmoid, [pgk], [ghk])
                    yk = "ypre%d" % half
                    if n == 0:
                        self.TT("dve", ypre[:, cs], gh, PBh, ALU.mult, [ghk, pbk], [yk])
                    else:
                        tn, tnk = tmpn[:, r_ * 512:(r_ + 1) * 512], "tn%d" % r_
                        self.TT("dve", tn, gh, PBh, ALU.mult, [ghk, pbk], [tnk])
                        if n < 3:
                            self.TT("pool", ypre[:, cs], ypre[:, cs], tn, ALU.add, [yk, tnk], [yk])
                        else:
                            self.TT("pool", ypb[:, cs], ypre[:, cs], tn, ALU.add, [yk, tnk], ["ypb%d" % half])
                    u_ = n * 2 + half
                    if u_ in (1, 3, 5, 7) and gen is not None:
                        next(gen, None)
                    if u_ == 3 and i + 1 < len(tiles):
                        norm1(i + 1)
            if i + 2 < len(tiles):
                load(i + 2)
            self.transpose8(ypb, ["ypb0", "ypb1"], PT, "PT", yT, "yT")
            for half in range(2):
                cs = slice(half * 512, (half + 1) * 512)
                for k in range(8):
                    self.MM(PY[:, cs], yT[:, k, :], wo[:, k, cs], k == 0, k == 7, ["yT", "wo"], ["PY"])
            self.TT("dve", tmp, PY[:, :], m[:, 2 * D:3 * D], ALU.mult, ["PY", mk], ["tmpA"])
            self.TT("pool", xt, tmp, xt, ALU.add, ["tmpA", xk], [xk])
            self.DMA("sp", self.xcur[tok, :], xt, [xk], [])
            if gen is not None:
                next(gen, None)

        def stageB(i):
            t = tiles[i]
            xt, xk = xt2[i % 3], "xt%d" % (i % 3)
            w = 0 if t < NTL else 1
            tok = slice(t * 128, (t + 1) * 128)
            tmp = tmpB
            h2 = tmp
            self.norm_mod(xt, xk, w, 4 * D, 3 * D, h2b, tmp, h2, "tmpB", ssq[:, 1:2], rstd[:, 1:2], "n2", jk="h2b", tk="tmpB")
            yield
            for k in range(8):
                self.TR(PB[:, k * 128:(k + 1) * 128], h2[:, k * 128:(k + 1) * 128], self.ident_f, ["tmpB", "ident_f"], ["PB0", "PB1"])
            self.CP("act", h2Tf, PB[:, :].rearrange("p (k m) -> p k m", k=8), ["PB0", "PB1"], ["h2Tf"])
            self.CP("pool", h2b, h2, ["tmpB"], ["h2b"])
            self.DMA("sp", self.H2[tok, :], h2b, ["h2b"], [])
            yield
            for k in range(8):
                self.MM(pR[:, 0:36], h2Tf[:, k, :], wr[:, k, :], k == 0, False, ["h2Tf", "wr"], ["pR"])
            self.MM(pR[:, 0:36], self.ones_f[0:1, :], br[0:1, :], False, True, ["ones_f", "br"], ["pR"])
            self.CP("dve", lg, pR[:, 0:36], ["pR"], ["lg"])
            gl = lg[:, 0:4]
            el = lg[:, 4:36].rearrange("p (g e) -> p g e", g=4)
            gmax, negmax, se, gw, m1, m2, dd, w1, w1g, w2g = (sm[:, j:j + 1] for j in range(10))
            self.RED(gmax, gl, ALU.max, ["lg"], ["gmax"])
            self.TS("dve", ohg, gl, gmax, None, ALU.is_equal, None, ["lg", "gmax"], ["ohg"])
            self.TS("dve", negmax, gmax, -1.0, None, ALU.mult, None, ["gmax"], ["negmax"])
            self.ACT(j4, gl, AF.Exp, ["lg", "negmax"], ["j4", "se"], bias=negmax, accum=se)
            self.RCP(gw, se, ["se"], ["gw"])
            self.TS("dve", pen, ohg, -1.0, 1e9, ALU.add, ALU.mult, ["ohg"], ["pen"])
            emv = em.rearrange("p (g e) -> p g e", g=4)
            self.TT("dve", emv, el, pen.unsqueeze(2).to_broadcast([128, 4, 8]), ALU.add, ["lg", "pen"], ["em"])
            self.RED(m1, em, ALU.max, ["em"], ["m1"])
            self.TS("dve", oh1, em, m1, None, ALU.is_equal, None, ["em", "m1"], ["oh1"])
            self.STT("dve", em2, oh1, -2e9, em, ALU.mult, ALU.add, ["oh1", "em"], ["em2"])
            self.RED(m2, em2, ALU.max, ["em2"], ["m2"])
            self.TS("dve", oh2, em2, m2, None, ALU.is_equal, None, ["em2", "m2"], ["oh2"])
            yield
            self.TT("dve", dd, m2, m1, ALU.subtract, ["m1", "m2"], ["dd"])
            self.ACT(dd, dd, AF.Exp, ["dd"], ["dd"])
            self.TS("dve", dd, dd, 1.0, None, ALU.add, None, ["dd"], ["dd"])
            self.RCP(w1, dd, ["dd"], ["w1"])
            self.TT("dve", w1g, w1, gw, ALU.mult, ["w1", "gw"], ["w1g"])
            self.TT("dve", w2g, gw, w1g, ALU.subtract, ["w1g", "gw"], ["w2g"])
            rt, base, iota = self.rt, self.base, self.iota
            pRK = self.bank(1)[:, 64:192]
            self.CP("pool", oh12b[:, 0:32], oh1, ["oh1"], ["oh12b0"])
            self.CP("pool", oh12b[:, 32:64], oh2, ["oh2"], ["oh12b1"])
            yield
            self.MM(pRK[:, 0:64], self.ltb, oh12b, True, True, ["ltb", "oh12b0", "oh12b1"], ["pRK"])
            self.MM(pRK[:, 64:128], self.onesbb, oh12b, True, True, ["onesbb", "oh12b0", "oh12b1"], ["pRK"])
            self.CP("dve", csb, pRK[:, 64:128], ["pRK"], ["cs"])
            self.TT("dve", tr0, pRK[:, 0:32], base, ALU.add, ["pRK", "base"], ["tr0"])
            self.TT("dve", tr0, tr0, oh1, ALU.mult, ["tr0", "oh1"], ["tr0"])
            self.RED(rt[:, t, 4:5], tr0, ALU.add, ["tr0"], ["rt"])
            self.TT("dve", tr1, pRK[:, 32:64], base, ALU.add, ["pRK", "base"], ["tr1"])
            self.TT("dve", tr1, tr1, csb[:, 0:32], ALU.add, ["tr1", "cs"], ["tr1"])
            self.TT("dve", tr1, tr1, oh2, ALU.mult, ["tr1", "oh2"], ["tr1"])
            self.RED(rt[:, t, 5:6], tr1, ALU.add, ["tr1"], ["rt"])
            self.TT("dve", base, base, csb[:, 0:32], ALU.add, ["base", "cs"], ["base"])
            self.TT("dve", base, base, csb[:, 32:64], ALU.add, ["base", "cs"], ["base"])
            self.TT("dve", tr0, oh1, iota, ALU.mult, ["oh1", "iota", "tr0"], ["tr0"])
            self.RED(rt[:, t, 0:1], tr0, ALU.add, ["tr0"], ["rt"])
            self.TT("dve", tr1, oh2, iota, ALU.mult, ["oh2", "iota", "tr1"], ["tr1"])
            self.RED(rt[:, t, 1:2], tr1, ALU.add, ["tr1"], ["rt"])
            self.CP("dve", rt[:, t, 2:3], w1g, ["w1g"], ["rt"])
            self.CP("dve", rt[:, t, 3:4], w2g, ["w2g"], ["rt"])

        ntl_ = len(tiles)
        load(0)
        if ntl_ > 1:
            load(1)
        norm1(0)
        for i in range(ntl_):
            gen = stageB(i - 1) if i > 0 else None
            stageA(i, gen)
            if gen is not None:
                for _ in gen:
                    pass
        for _ in stageB(ntl_ - 1):
            pass

    def phase_p4(self, l):
        cfg, A, I = self.cfg, self.A, self.I
        NT, NTL = cfg.NT, cfg.NTL
        last = l == cfg.DEPTH - 1
        tiles = list(range(NT)) if self.with_ctx else list(range(NTL))
        ntl = len(tiles)
        NBLK = 2 * ntl + 32
        assert NBLK <= NBLK_MAX
        rt, base, iota = self.rt, self.base, self.iota
        m0 = A.mark()
        nbi = A.alloc([32], I32)
        pcnt = A.alloc([32], F32)
        pa = A.alloc([32], F32)
        pb = A.alloc([32], F32)
        bs = A.alloc([32], F32)
        oh3 = A.alloc([NT, 32], F32)
        sl = A.alloc([NT, 2], F32)
        cmp = A.alloc([NBLK_MAX, 32], F32)
        ble = A.alloc([NBLK_MAX], F32)
        zt = A.alloc([8192], BF16)
        h2t = [A.alloc([D], BF16) for _ in range(2)]
        self.MEMSET("pool", zt, 0.0, ["zt"])
        xsz = self.XS.rearrange("(p b) f -> p (b f)", p=128)
        tot = NBLK_MAX * D
        for c0 in range(0, tot, 8192):
            c1_ = min(tot, c0 + 8192)
            self.DMA("sp", xsz[:, c0:c1_], zt[:, 0:c1_ - c0], ["zt"], ["XS"])
        self.TS("dve", pcnt, base, 1.0 / 128, 0.496, ALU.mult, ALU.add, ["base"], ["pcnt"])
        self.CP("dve", nbi, pcnt, ["pcnt"], ["nbi"])
        self.CP("dve", pcnt, nbi, ["nbi"], ["pcnt"])
        self.TS("dve", pcnt, pcnt, 128.0, None, ALU.mult, None, ["pcnt"], ["pcnt"])
        self.CP("dve", pa, pcnt, ["pcnt"], ["pa"])
        src, dst, sk, dk = pa, pb, "pa", "pb"
        for sft in (1, 2, 4, 8, 16):
            self.CP("dve", dst[:, 0:sft], src[:, 0:sft], [sk], [dk])
            self.TT("dve", dst[:, sft:32], src[:, sft:32], src[:, 0:32 - sft], ALU.add, [sk], [dk])
            src, dst, sk, dk = dst, src, dk, sk
        pe, pek = src, sk
        self.TT("dve", bs, pe, pcnt, ALU.subtract, [pek, "pcnt"], ["bs"])
        for k in range(2):
            self.TT("dve", oh3, iota.unsqueeze(1).to_broadcast([128, NT, 32]),
                    rt[:, :, k:k + 1].to_broadcast([128, NT, 32]), ALU.is_equal, ["iota", "rt"], ["oh3"])
            self.TT("dve", oh3, oh3, bs.unsqueeze(1).to_broadcast([128, NT, 32]), ALU.mult, ["oh3", "bs"], ["oh3"])
            self.RED(sl[:, :, k], oh3, ALU.add, ["oh3"], ["sl%d" % k])
            self.TT("dve", sl[:, :, k], sl[:, :, k], rt[:, :, 4 + k], ALU.add, ["sl%d" % k, "rt"], ["sl%d" % k])
        self.CP("dve", self.slot_i, sl, ["sl0", "sl1"], ["slot_i"])
        self.TT("dve", cmp, self.thr.unsqueeze(2).to_broadcast([128, NBLK_MAX, 32]),
                pe.unsqueeze(1).to_broadcast([128, NBLK_MAX, 32]), ALU.is_ge, ["thr", pek], ["cmp"])
        self.RED(ble, cmp, ALU.add, ["cmp"], ["ble"])
        self.TS("dve", ble, ble, 31.0, 128.0, ALU.min, ALU.mult, ["ble"], ["ble"])
        self.TS("dve", ble, ble, self.pidx[:, 0:1], float(l * NE * 128), ALU.add, ALU.add, ["ble", "pidx"], ["ble"])
        self.CP("dve", self.widx, ble, ["ble"], ["widx"])
        for i, t in enumerate(tiles):
            hb, hk = h2t[i % 2], "h2t%d" % (i % 2)
            self.DMA("sp", hb, self.H2[t * 128:(t + 1) * 128, :], (), [hk])
            for k in range(2):
                self.S.add("pool", (lambda hb=hb, t=t, k=k: (lambda e: e.indirect_dma_start(
                    out=self.XS[:, :], out_offset=bass.IndirectOffsetOnAxis(ap=self.slot_i[:, t, k:k + 1], axis=0),
                    in_=hb, in_offset=None)))(), [hk, "slot_i", "XS"], ["XSs"], dma=True)
        self.S.barrier()
        A.release(m0)
        NWR = 4
        w13 = [A.alloc([8, 512], BF16) for _ in range(NWR)]
        w2 = [A.alloc([2, D], BF16) for _ in range(NWR)]
        xs = [A.alloc([D], BF16) for _ in range(NWR)]
        xsT = [A.alloc([8, 128], BF16) for _ in range(2)]
        sb = [A.alloc([256], F32) for _ in range(2)]
        ab = [A.alloc([256], BF16) for _ in range(2)]
        aT = [A.alloc([2, 128], BF16) for _ in range(2)]
        ys = [A.alloc([D], BF16) for _ in range(2)]
        w13src = I["w13r"].rearrange("l r f -> (l r) f")
        w2src = I["w2r"].rearrange("l r f -> (l r) f")
        def wload(b):
            wr_ = b % NWR
            self.S.add("pool", (lambda b=b, wr_=wr_: (lambda e: e.indirect_dma_start(
                out=w13[wr_].rearrange("p k f -> p (k f)"), out_offset=None, in_=w13src,
                in_offset=bass.IndirectOffsetOnAxis(ap=self.widx[:, b:b + 1], axis=0))))(), ["widx"], ["w13_%d" % wr_], dma=True)
            self.S.add("pool", (lambda b=b, wr_=wr_: (lambda e: e.indirect_dma_start(
                out=w2[wr_].rearrange("p c f -> p (c f)"), out_offset=None, in_=w2src,
                in_offset=bass.IndirectOffsetOnAxis(ap=self.widx[:, b:b + 1], axis=0))))(), ["widx"], ["w2_%d" % wr_], dma=True)
            self.DMA("sp", xs[wr_], self.XS[b * 128:(b + 1) * 128, :], (), ["xsw%d" % wr_])

        for b in range(min(NWR - 1, NBLK)):
            wload(b)
        for b in range(NBLK):
            if b + NWR - 1 < NBLK:
                wload(b + NWR - 1)
            r = b % 2
            wr_ = b % NWR
            PTx = self.bank_bf(r)
            ptxk = "PTx%d" % r
            psH = self.bank(2 + r)
            phk = "psH%d" % r
            PTa = self.bank_bf(4)
            psO = self.banks[3]
            for k in range(8):
                self.TR(PTx[:, k * 128:(k + 1) * 128], xs[wr_][:, k * 128:(k + 1) * 128], self.ident_b, ["xsw%d" % wr_, "ident_b"], [ptxk])
            self.CP("act", xsT[r], PTx[:, :].rearrange("p (k m) -> p k m", k=8), [ptxk], ["xsT%d" % r])
            for k in range(8):
                self.MM(psH, xsT[r][:, k, :], w13[wr_][:, k, :], k == 0, k == 7, ["xsT%d" % r, "w13_%d" % wr_], [phk])
            self.ACT(sb[r], psH[:, 0:256], AF.Silu, [phk], ["sb%d" % r])
            self.TT("dve", ab[r], sb[r], psH[:, 256:512], ALU.mult, ["sb%d" % r, phk], ["ab%d" % r])
            for c in range(2):
                self.TR(PTa[:, c * 128:(c + 1) * 128], ab[r][:, c * 128:(c + 1) * 128], self.ident_b, ["ab%d" % r, "ident_b"], ["PTa"])
            self.CP("act", aT[r], PTa[:, 0:256].rearrange("p (c m) -> p c m", c=2), ["PTa"], ["aT%d" % r])
            for half in range(2):
                cs_ = slice(half * 512, (half + 1) * 512)
                for c in range(2):
                    self.MM(psO[:, cs_], aT[r][:, c, :], w2[wr_][:, c, cs_], c == 0, c == 1, ["aT%d" % r, "w2_%d" % wr_], ["psO"])
            self.CP("act", ys[r], psO[:, :], ["psO"], ["ys%d" % r])
            self.DMA("sp", self.YS[b * 128:(b + 1) * 128, :], ys[r], ["ys%d" % r], [])
        self.S.barrier()
        A.release(m0)
        y0 = [A.alloc([D], BF16) for _ in range(2)]
        y1 = [A.alloc([D], BF16) for _ in range(2)]
        xt2 = [A.alloc([D], F32) for _ in range(2)]
        tmp = A.alloc([D], F32)
        junk = A.alloc([D], BF16)
        ssq = A.alloc([2], F32)
        rstd = A.alloc([2], F32)
        if last:
            self.gfin = A.alloc([D], F32)
            self.DMA("sp", self.gfin, I["g_final"].partition_broadcast(128), (), ["gfin"])
        for i, t in enumerate(tiles):
            b = i % 2
            w = 0 if t < NTL else 1
            tok = slice(t * 128, (t + 1) * 128)
            for k, yb in ((0, y0[b]), (1, y1[b])):
                self.S.add("pool", (lambda yb=yb, t=t, k=k: (lambda e: e.indirect_dma_start(
                    out=yb, out_offset=None, in_=self.YS[:, :],
                    in_offset=bass.IndirectOffsetOnAxis(ap=self.slot_i[:, t, k:k + 1], axis=0))))(), ["slot_i"], ["y%d_%d" % (k, b)], dma=True)
            self.DMA("sp", xt2[b], self.xcur[tok, :], (), ["xt%d" % b])
            self.TS("dve", tmp, y0[b], rt[:, t, 2:3], None, ALU.mult, None, ["y0_%d" % b, "rt"], ["tmp"])
            self.STT("dve", tmp, y1[b], rt[:, t, 3:4], tmp, ALU.mult, ALU.add, ["y1_%d" % b, "rt", "tmp"], ["tmp"])
            self.TT("pool", tmp, tmp, self.mod[w][:, 5 * D:6 * D], ALU.mult, ["tmp", "mod%d" % w], ["tmp"])
            self.TT("pool", xt2[b], tmp, xt2[b], ALU.add, ["tmp", "xt%d" % b], ["xt%d" % b])
            if not last:
                self.DMA("sp", self.xcur[tok, :], xt2[b], ["xt%d" % b], [])
            else:
                self.rstd_of(xt2[b], D, junk, ssq[:, 0:1], rstd[:, 0:1], ["xt%d" % b], "f")
                self.STT("dve", xt2[b], xt2[b], rstd[:, 0:1], self.gfin, ALU.mult, ALU.mult, ["xt%d" % b, "rstdf", "gfin"], ["xt%d" % b])
                o = self.DMA("sp", self.out[tok, :], xt2[b], ["xt%d" % b], [])
                self.S.out_ops.append(o)


_CACHE = {}


def _get_prog(cfg_key):
    if cfg_key not in _CACHE:
        cfg = Cfg(*cfg_key)
        p = Prog(cfg)
        p.build()
        _CACHE[cfg_key] = p
    return _CACHE[cfg_key]


def make_in_maps(prog, inputs, ncores):
    cfg = prog.cfg
    f = lambda a: np.ascontiguousarray(np.asarray(a, dtype=np.float32))
    shared = {}
    for k in ("w_mod", "b_mod", "g_norm_mix", "w_in", "g_q_a", "w_q_b", "g_kv_a", "w_kv_b", "sink_b", "g_q_d", "g_k_d",
              "w_gate", "w_branch", "w_out", "g_norm_ffn", "w_group", "b_group", "w_router", "b_router"):
        shared[k] = f(inputs[k])
    L = cfg.DEPTH
    shared["b_gate"] = f(inputs["b_gate"]).reshape(L, 4 * D)
    shared["g_final"] = f(inputs["g_final"]).reshape(1, D)
    shared["c_ctx"] = f(inputs["c_ctx"]).reshape(1, D)
    rpb = f(inputs["rpb_c"])
    shared["biasg"] = np.ascontiguousarray(rpb[:, :, prog.DR, prog.DC[None]])
    w13 = np.concatenate([f(inputs["w_ff1"]), f(inputs["w_ff3"])], axis=-1)
    w13 = w13.reshape(L, NE, 8, 128, 512).transpose(0, 1, 3, 2, 4)
    shared["w13r"] = np.ascontiguousarray(w13).reshape(L, NE * 128, 4096)
    w2 = f(inputs["w_ff2"]).reshape(L, NE, 2, 128, D).transpose(0, 1, 3, 2, 4)
    shared["w2r"] = np.ascontiguousarray(w2).reshape(L, NE * 128, 2048)
    shared["iota32"] = np.arange(32, dtype=np.float32).reshape(1, 32)
    shared["ltri"] = np.triu(np.ones((128, 128), np.float32), k=1)
    shared["pidx"] = np.arange(128, dtype=np.float32).reshape(128, 1)
    shared["thr"] = (128.0 * np.arange(NBLK_MAX, dtype=np.float32)).reshape(1, NBLK_MAX)
    shared["ident"] = np.eye(128, dtype=np.float32)
    shared["ropetab"] = rope_tab(cfg)
    shared["maskb"] = mask_b()
    shared["maskc"] = prog.maskc_np
    x = f(inputs["x"])
    c = f(inputs["c"])
    ctx = f(inputs["ctx"])
    maps = []
    for b in range(ncores):
        m = dict(shared)
        m["x"] = x[b]
        m["c"] = c[b:b + 1]
        m["ctx"] = ctx[b]
        maps.append(m)
    return maps


def kernel(**inputs):
    x = np.asarray(inputs["x"])
    B, SEQ, _ = x.shape
    CTX = np.asarray(inputs["ctx"]).shape[1]
    DEPTH = np.asarray(inputs["w_mod"]).shape[0]
    prog = _get_prog((SEQ, CTX, DEPTH))
    maps = make_in_maps(prog, inputs, B)
    res = run_bass_kernel_spmd(prog.nc, maps, core_ids=list(range(B)))
    out = np.stack([np.asarray(r["out"], dtype=np.float32) for r in res.results], axis=0)
    return out
```

```python
import contextlib
import numpy as np
import concourse.bass as bass
import concourse.mybir as mybir
from concourse.bass_utils import run_bass_kernel_spmd

F32 = mybir.dt.float32
BF16 = mybir.dt.bfloat16
AF = mybir.ActivationFunctionType
ALU = mybir.AluOpType
AX = mybir.AxisListType
I32 = mybir.dt.int32
NBLK_MAX = 100

SAME_ENGINE_SYNC = True
NDMA_SEMS = 10
import os
MAXOPS = int(os.environ.get('KMAXOPS', '100000000'))
D = 1024
EPS = 1e-6
NEG = -30000.0
IN_COLS = 2208
NE = 32


class Op:
    __slots__ = ("eng", "fn", "deps", "flag", "semv", "sem", "is_dma")

    def __init__(self, eng, fn, is_dma):
        self.eng = eng
        self.fn = fn
        self.deps = []
        self.flag = False
        self.semv = 0
        self.sem = None
        self.is_dma = is_dma


class Sched:
    ENGS = ("pe", "act", "dve", "pool", "sp")

    def __init__(self):
        self.ops = {e: [] for e in self.ENGS}
        self.last_w = {}
        self.readers = {}
        self.pending_barrier = {e: None for e in self.ENGS}
        self.out_ops = []

    def add(self, eng, fn, reads=(), writes=(), dma=False):
        op = Op(eng, fn, dma)
        self.nadd = getattr(self, "nadd", 0) + 1
        if self.nadd > MAXOPS:
            return op
        deps = {}
        raw = set()
        for k in reads:
            w = self.last_w.get(k)
            if w is not None:
                deps[id(w)] = w
                raw.add(id(w))
        for k in writes:
            w = self.last_w.get(k)
            if w is not None:
                deps[id(w)] = w
            for r in self.readers.get(k, ()):
                deps[id(r)] = r
        for k in reads:
            self.readers.setdefault(k, []).append(op)
        for k in writes:
            self.last_w[k] = op
            self.readers[k] = []
        pb = self.pending_barrier[eng]
        if pb is not None:
            for d in pb:
                deps[id(d)] = d
            self.pending_barrier[eng] = None
        for d in deps.values():
            if d is op:
                continue
            if (not d.is_dma) and d.eng == eng and (not dma) and (eng == "pe" or not SAME_ENGINE_SYNC):
                continue
            d.flag = True
            op.deps.append(d)
        self.ops[eng].append(op)
        return op

    def barrier(self):
        lst = []
        for e in self.ENGS:
            ops = self.ops[e]
            if not ops:
                continue
            nd = 0
            got_c = False
            for op in reversed(ops):
                if op.is_dma:
                    if nd < NDMA_SEMS:
                        lst.append(op)
                        nd += 1
                elif not got_c:
                    lst.append(op)
                    got_c = True
                if nd >= NDMA_SEMS and got_c:
                    break
        for e in self.ENGS:
            prev = self.pending_barrier[e]
            self.pending_barrier[e] = lst if prev is None else (prev + lst)
        self.last_w = {}
        self.readers = {}

    def emit(self, nc):
        final_ops = self.out_ops
        for o in final_ops:
            o.flag = True
        with contextlib.ExitStack() as es:
            csem = {}
            for e in ("pe", "act", "dve", "pool"):
                csem[e] = es.enter_context(nc.semaphore("s_" + e))
            dsem = {}
            for e in ("act", "pool", "sp"):
                dsem[e] = [es.enter_context(nc.semaphore("d_%s%d" % (e, i))) for i in range(NDMA_SEMS)]
            for e in self.ENGS:
                cnt = 0
                dcnt = 0
                dvals = [0] * NDMA_SEMS
                for op in self.ops[e]:
                    if op.is_dma:
                        slot = dcnt % NDMA_SEMS
                        dcnt += 1
                        dvals[slot] += 16
                        op.sem = dsem[e][slot]
                        op.semv = dvals[slot]
                    elif op.flag:
                        cnt += 1
                        op.sem = csem[e]
                        op.semv = cnt
            block = es.enter_context(nc.Block())

            def run_engine(e, eng):
                waited = {}
                prev_dma = [None] * NDMA_SEMS
                dcnt = 0
                for op in self.ops[e]:
                    for d in op.deps:
                        key = id(d.sem)
                        if waited.get(key, 0) >= d.semv:
                            continue
                        eng.wait_ge(d.sem, d.semv)
                        waited[key] = d.semv
                    if op.is_dma:
                        slot = dcnt % NDMA_SEMS
                        dcnt += 1
                        p = prev_dma[slot]
                        if p is not None:
                            key = id(p.sem)
                            if waited.get(key, 0) < p.semv:
                                eng.wait_ge(p.sem, p.semv)
                                waited[key] = p.semv
                        prev_dma[slot] = op
                        ins = op.fn(eng)
                        ins.then_inc(op.sem, 16)
                    else:
                        ins = op.fn(eng)
                        if op.flag:
                            ins.then_inc(op.sem, 1)
                if e == "sp":
                    for o in final_ops:
                        key = id(o.sem)
                        if waited.get(key, 0) < o.semv:
                            eng.wait_ge(o.sem, o.semv)
                            waited[key] = o.semv

            @block.tensor
            def _(eng):
                run_engine("pe", eng)

            @block.scalar
            def _(eng):
                run_engine("act", eng)

            @block.vector
            def _(eng):
                run_engine("dve", eng)

            @block.gpsimd
            def _(eng):
                run_engine("pool", eng)

            @block.sync
            def _(eng):
                run_engine("sp", eng)


class Arena:
    def __init__(self, t, nbytes):
        self.t = t
        self.cap = nbytes
        self.off = 0
        self.peak = 0

    def alloc(self, free_shape, dt):
        n = 1
        for s in free_shape:
            n *= s
        esz = 2 if dt == BF16 else 4
        off = (self.off + 63) // 64 * 64
        nb = n * esz
        assert off + nb <= self.cap, ("arena overflow", off + nb, self.cap)
        self.off = off + nb
        self.peak = max(self.peak, self.off)
        v = self.t[:, off // 2:(off + nb) // 2]
        if dt != BF16:
            v = v.bitcast(dt)
        if len(free_shape) > 1:
            names = ["a%d" % i for i in range(len(free_shape))]
            kw = {names[i]: free_shape[i] for i in range(len(free_shape))}
            v = v.rearrange("p (" + " ".join(names) + ") -> p " + " ".join(names), **kw)
        return v

    def mark(self):
        return self.off

    def release(self, m):
        self.off = m


class Cfg:
    def __init__(self, SEQ=4096, CTX=256, DEPTH=2):
        self.SEQ = SEQ
        self.CTX = CTX
        self.DEPTH = DEPTH
        self.T = SEQ + CTX
        self.NT = self.T // 128
        self.NTL = SEQ // 128
        self.NTC = CTX // 128
        self.ROWS = SEQ // 64
        self.NG = SEQ // 512


def rope_tab(cfg):
    t = np.arange(cfg.SEQ)
    rows = (t // 64).astype(np.float32)
    cols = (t % 64).astype(np.float32)
    out = np.zeros((cfg.T, 192), np.float32)
    out[cfg.SEQ:, 0:64] = 1.0
    out[cfg.SEQ:, 128:160] = 1.0
    off = 0
    for R in (64, 32):
        half = R // 2
        inv = (np.float32(10000.0) ** (-np.arange(0, half, 2, dtype=np.float32) / np.float32(half))).astype(np.float32)
        ar = rows[:, None] * inv
        ac = cols[:, None] * inv
        cr, sr, cc, sc = np.cos(ar), np.sin(ar), np.cos(ac), np.sin(ac)
        C = np.concatenate([cr, cr, cc, cc], axis=1)
        S_ = np.concatenate([-sr, sr, -sc, sc], axis=1)
        out[:cfg.SEQ, off:off + R] = C
        out[:cfg.SEQ, off + R:off + 2 * R] = S_
        off += 2 * R
    return out.astype(np.float32)


def mask_b():
    k = np.arange(128)[:, None]
    q = np.arange(512)[None, :]
    out = np.zeros((6, 128, 512), np.float32)
    for r in range(6):
        rel = r - 1
        ok = np.abs(q - 128 * rel - k) <= 128
        out[r] = np.where(ok, 0.0, NEG)
    return out


def c_tables(cfg):
    rows = cfg.ROWS
    kh = min(8, rows)
    k = np.arange(128)[:, None]
    q = np.arange(512)[None, :]
    k_col = k % 64
    q_col = q % 64
    c_start = np.clip(q_col - 8, 0, 64 - 16)
    col_ok = (k_col >= c_start) & (k_col < c_start + 16)
    DC = (np.clip(k_col - q_col, -15, 15) + 15).astype(np.int64) + np.zeros((128, 512), np.int64)
    DR = np.zeros((8, 128, 512), np.int64)
    for r in range(8):
        rel = r - 2
        d = 2 * rel + k // 64 - q // 64
        DR[r] = np.clip(d, -7, 7) + 7
    masks = []
    case_of_g = []
    valid = []
    for g in range(cfg.NG):
        m = np.full((8, 128, 512), NEG, np.float32)
        v = []
        for r in range(8):
            kt = 4 * g + r - 2
            if kt < 0 or kt >= cfg.NTL:
                continue
            k_row = 2 * kt + k // 64
            q_row = 8 * g + q // 64
            r_start = np.clip(q_row - kh // 2, 0, rows - kh)
            row_ok = (k_row >= r_start) & (k_row < r_start + kh)
            ok = row_ok & col_ok
            if ok.any():
                v.append(r)
            m[r] = np.where(ok, 0.0, NEG)
        found = None
        for ci, mm in enumerate(masks):
            if np.array_equal(mm, m):
                found = ci
                break
        if found is None:
            masks.append(m)
            found = len(masks) - 1
        case_of_g.append(found)
        valid.append(v)
    return np.stack(masks).astype(np.float32), case_of_g, valid, DR, DC


class Prog:
    def __init__(self, cfg):
        self.cfg = cfg
        self.S = Sched()
        self.nc = bass.Bass("TRN2", target_bir_lowering=False)
        self.maskc_np, self.case_of_g, self.valid_c, self.DR, self.DC = c_tables(cfg)
        self.ncase = self.maskc_np.shape[0]

    def MM(self, out, lhsT, rhs, start, stop, r, w):
        return self.S.add("pe", lambda e: e.matmul(out, lhsT=lhsT, rhs=rhs, start=start, stop=stop), r, w)

    def TR(self, out, in_, ident, r, w):
        return self.S.add("pe", lambda e: e.transpose(out=out, in_=in_, identity=ident), r, w)

    def ACT(self, out, in_, func, r, w, scale=1.0, bias=0.0, accum=None):
        if accum is None:
            return self.S.add("act", lambda e: e.activation(out=out, in_=in_, func=func, bias=bias, scale=scale), r, w)
        return self.S.add("act", lambda e: e.activation(out=out, in_=in_, func=func, bias=bias, scale=scale,
                                                        accum_out=accum), r, w)

    def TT(self, eng, out, in0, in1, op, r, w):
        return self.S.add(eng, lambda e: e.tensor_tensor(out=out, in0=in0, in1=in1, op=op), r, w)

    def TS(self, eng, out, in0, s1, s2, op0, op1, r, w):
        if s2 is None:
            return self.S.add(eng, lambda e: e.tensor_scalar(out=out, in0=in0, scalar1=s1, scalar2=None, op0=op0), r, w)
        return self.S.add(eng, lambda e: e.tensor_scalar(out=out, in0=in0, scalar1=s1, scalar2=s2, op0=op0, op1=op1), r, w)

    def STT(self, eng, out, in0, scalar, in1, op0, op1, r, w):
        return self.S.add(eng, lambda e: e.scalar_tensor_tensor(out=out, in0=in0, scalar=scalar, in1=in1,
                                                                 op0=op0, op1=op1), r, w)

    def CP(self, eng, out, in_, r, w):
        if eng == "act":
            return self.S.add("act", lambda e: e.copy(out=out, in_=in_), r, w)
        return self.S.add(eng, lambda e: e.tensor_copy(out=out, in_=in_), r, w)

    def RED(self, out, in_, op, r, w):
        return self.S.add("dve", lambda e: e.tensor_reduce(out=out, in_=in_, axis=AX.X, op=op), r, w)

    def RCP(self, out, in_, r, w):
        return self.S.add("dve", lambda e: e.reciprocal(out=out, in_=in_), r, w)

    def DMA(self, q, out, in_, r, w):
        return self.S.add(q, lambda e: e.dma_start(out=out, in_=in_), r, w, dma=True)

    def MEMSET(self, eng, ap, val, w):
        return self.S.add(eng, lambda e: e.memset(ap, val), (), w)

    def build(self):
        cfg = self.cfg
        nc = self.nc
        L = cfg.DEPTH
        T, NT, NTL = cfg.T, cfg.NT, cfg.NTL

        def din(name, shape):
            return nc.dram_tensor(name, list(shape), F32, kind="ExternalInput").ap()

        self.I = I = {}
        I["x"] = din("x", [cfg.SEQ, D])
        I["c"] = din("c", [1, D])
        I["ctx"] = din("ctx", [cfg.CTX, D])
        I["c_ctx"] = din("c_ctx", [1, D])
        I["w_mod"] = din("w_mod", [L, D, 6 * D])
        I["b_mod"] = din("b_mod", [L, 6 * D])
        I["g_norm_mix"] = din("g_norm_mix", [L, D])
        I["w_in"] = din("w_in", [L, D, IN_COLS])
        I["g_q_a"] = din("g_q_a", [L, 256])
        I["w_q_b"] = din("w_q_b", [L, 256, 384])
        I["g_kv_a"] = din("g_kv_a", [L, 128])
        I["w_kv_b"] = din("w_kv_b", [L, 128, 512])
        I["sink_b"] = din("sink_b", [L, 4])
        I["biasg"] = din("biasg", [L, 4, 8, 128, 512])
        I["g_q_d"] = din("g_q_d", [L, 64])
        I["g_k_d"] = din("g_k_d", [L, 64])
        I["w_gate"] = din("w_gate", [L, 4, D, D])
        I["b_gate"] = din("b_gate", [L, 4 * D])
        I["w_branch"] = din("w_branch", [L, 4, 256, D])
        I["w_out"] = din("w_out", [L, D, D])
        I["g_norm_ffn"] = din("g_norm_ffn", [L, D])
        I["w_group"] = din("w_group", [L, D, 4])
        I["b_group"] = din("b_group", [L, 4])
        I["w_router"] = din("w_router", [L, D, 32])
        I["b_router"] = din("b_router", [L, 32])
        I["w13r"] = din("w13r", [L, NE * 128, 4096])
        I["w2r"] = din("w2r", [L, NE * 128, 2048])
        I["iota32"] = din("iota32", [1, 32])
        I["ltri"] = din("ltri", [128, 128])
        I["pidx"] = din("pidx", [128, 1])
        I["thr"] = din("thr", [1, NBLK_MAX])
        I["g_final"] = din("g_final", [1, D])
        I["ident"] = din("ident", [128, 128])
        I["ropetab"] = din("ropetab", [T, 192])
        I["maskb"] = din("maskb", [6, 128, 512])
        I["maskc"] = din("maskc", [self.ncase, 8, 128, 512])
        self.out = nc.dram_tensor("out", [cfg.SEQ, D], F32, kind="ExternalOutput").ap()

        def dscr(name, shape, dt):
            return nc.dram_tensor(name, list(shape), dt, kind="Internal").ap()

        self.xcur = dscr("xcur", [T, D], F32)
        self.QT = [dscr("QT_A", [96, 4, T], BF16)] + [dscr("QT_%d" % n, [128, 2, T], BF16) for n in (1, 2, 3)]
        self.KT = [dscr("KT_A", [96, 4, T], BF16)] + [dscr("KT_%d" % n, [128, 2, T], BF16) for n in (1, 2, 3)]
        self.VA = [dscr("VA_%d" % n, [128, NT, 4, 128], BF16) for n in range(4)]
        self.OT = dscr("OT", [8, 128, T], BF16)
        self.H2 = dscr("H2", [T, D], BF16)
        self.XS = dscr("XS", [NBLK_MAX * 128, D], BF16)
        self.YS = dscr("YS", [NBLK_MAX * 128, D], BF16)

        with contextlib.ExitStack() as es:
            ARENA_BYTES = 207 * 1024
            at = es.enter_context(nc.sbuf_tensor("arena", [128, ARENA_BYTES // 2], BF16))
            self.A = Arena(at, ARENA_BYTES)
            self.banks = [es.enter_context(nc.psum_tensor("pb%d" % i, [128, 1024], F32)) for i in range(4)]
            self.setup()
            for l in range(L):
                self.layer(l)
            self.S.emit(nc)
        return nc

    def bank(self, i):
        return self.banks[i // 2][:, (i % 2) * 512:(i % 2 + 1) * 512]

    def bank_bf(self, i):
        return self.bank(i).bitcast(BF16)

    def setup(self):
        A, I = self.A, self.I
        self.ident_f = A.alloc([128], F32)
        self.ident_b = A.alloc([128], BF16)
        self.ones_f = A.alloc([128], F32)
        self.ones_b = A.alloc([128], BF16)
        self.DMA("sp", self.ident_f, I["ident"], (), ["ident_f"])
        self.DMA("pool", self.ident_b, I["ident"], (), ["ident_b"])
        self.MEMSET("dve", self.ones_f, 1.0, ["ones_f"])
        self.MEMSET("dve", self.ones_b, 1.0, ["ones_b"])
        self.mod = [A.alloc([6 * D], F32), A.alloc([6 * D], F32)]
        self.rt = A.alloc([self.cfg.NT, 8], F32)
        self.base = A.alloc([32], F32)
        self.iota = A.alloc([32], F32)
        self.ltb = A.alloc([128], BF16)
        self.onesbb = A.alloc([128], BF16)
        self.pidx = A.alloc([1], F32)
        self.thr = A.alloc([NBLK_MAX], F32)
        self.slot_i = A.alloc([self.cfg.NT, 2], I32)
        self.widx = A.alloc([NBLK_MAX], I32)
        self.DMA("sp", self.iota, I["iota32"].partition_broadcast(128), (), ["iota"])
        self.DMA("pool", self.ltb, I["ltri"], (), ["ltb"])
        self.MEMSET("dve", self.onesbb, 1.0, ["onesbb"])
        self.DMA("sp", self.pidx, I["pidx"], (), ["pidx"])
        self.DMA("sp", self.thr, I["thr"].partition_broadcast(128), (), ["thr"])
        self.pers_mark = A.mark()

    def layer(self, l):
        import os
        cfg = self.cfg
        stop = int(os.environ.get("KSTOP", "99"))
        self.with_ctx = l < cfg.DEPTH - 1
        phases = [self.phase_mod, self.phase_p1, self.phase_p2, self.phase_p3, self.phase_p4]
        for pi, ph in enumerate(phases):
            if l * 5 + pi > stop:
                return
            self.S.barrier()
            self.A.release(self.pers_mark)
            ph(l)

    def phase_mod(self, l):
        A, I = self.A, self.I
        cb = [A.alloc([D], F32), A.alloc([D], F32)]
        lh = [A.alloc([8, 128], F32), A.alloc([8, 128], F32)]
        bmod = A.alloc([6 * D], F32)
        gm = A.alloc([D], F32)
        gf = A.alloc([D], F32)
        wm = [A.alloc([8, 512], F32), A.alloc([8, 512], F32)]
        self.DMA("sp", cb[0], I["c"].partition_broadcast(128), (), ["cb0"])
        self.DMA("sp", cb[1], I["c_ctx"].partition_broadcast(128), (), ["cb1"])
        self.DMA("sp", bmod[0:1, :], I["b_mod"][l:l + 1, :], (), ["bmod"])
        self.DMA("sp", gm, I["g_norm_mix"][l:l + 1, :].partition_broadcast(128), (), ["gm"])
        self.DMA("sp", gf, I["g_norm_ffn"][l:l + 1, :].partition_broadcast(128), (), ["gf"])
        for w in range(2):
            self.ACT(cb[w], cb[w], AF.Silu, ["cb%d" % w], ["cb%d" % w])
            for k in range(8):
                bk = self.banks[w][:, k * 128:(k + 1) * 128]
                self.TR(bk, cb[w][:, k * 128:(k + 1) * 128], self.ident_f, ["cb%d" % w, "ident_f"], ["pbk%d" % w])
            self.CP("act", lh[w], self.banks[w][:, :].rearrange("p (k m) -> p k m", k=8), ["pbk%d" % w], ["lh%d" % w])
        for j in range(12):
            wmj = wm[j % 2]
            wk = "wm%d" % (j % 2)
            self.DMA("sp", wmj, I["w_mod"][l, :, j * 512:(j + 1) * 512].rearrange("(k p) n -> p k n", p=128), (), [wk])
            for w in range(2):
                bi = 4 + (j * 2 + w) % 4
                bk = self.bank(bi)
                bkk = "bank%d" % bi
                for k in range(8):
                    self.MM(bk, lh[w][:, k, :], wmj[:, k, :], k == 0, False, ["lh%d" % w, wk], [bkk])
                self.MM(bk, self.ones_f[0:1, :], bmod[0:1, j * 512:(j + 1) * 512], False, True, ["ones_f", "bmod"], [bkk])
                self.CP("dve" if w == 0 else "act", self.mod[w][:, j * 512:(j + 1) * 512], bk, [bkk], ["mod%d" % w])
        for w in range(2):
            m = self.mod[w]
            self.STT("dve", m[:, D:2 * D], m[:, D:2 * D], 1.0, gm, ALU.add, ALU.mult, ["mod%d" % w, "gm"], ["mod%d" % w])
            self.STT("dve", m[:, 4 * D:5 * D], m[:, 4 * D:5 * D], 1.0, gf, ALU.add, ALU.mult, ["mod%d" % w, "gf"], ["mod%d" % w])

    def x_src(self, l, t):
        cfg = self.cfg
        if l == 0:
            if t < cfg.NTL:
                return self.I["x"][t * 128:(t + 1) * 128, :]
            return self.I["ctx"][(t - cfg.NTL) * 128:(t - cfg.NTL + 1) * 128, :]
        return self.xcur[t * 128:(t + 1) * 128, :]

    def rstd_of(self, src, n, junk, ssq, rstd, rk, tag, jk="junk"):
        self.ACT(junk, src, AF.Square, rk, [jk, "ssq" + tag], accum=ssq)
        self.ACT(ssq, ssq, AF.Sqrt, ["ssq" + tag], ["ssq" + tag], scale=1.0 / n, bias=EPS)
        self.RCP(rstd, ssq, ["ssq" + tag], ["rstd" + tag])

    def norm_mod(self, xt, xk, w, goff, soff, junk, tmp, out, outk, ssq, rstd, tag, jk="junk", tk="tmp"):
        self.rstd_of(xt, D, junk, ssq, rstd, [xk], tag, jk=jk)
        m = self.mod[w]
        self.STT("dve", tmp, xt, rstd, m[:, goff:goff + D], ALU.mult, ALU.mult, [xk, "rstd" + tag, "mod%d" % w], [tk])
        self.TT("dve", out, tmp, m[:, soff:soff + D], ALU.add, [tk, "mod%d" % w], [outk])

    def transpose8(self, src, srck, bank_bf, bankk, dst, dstk, nblk=8, ident=None, identk="ident_b"):
        ident = self.ident_b if ident is None else ident
        srcks = [srck] if isinstance(srck, str) else list(srck)
        for k in range(nblk):
            self.TR(bank_bf[:, k * 128:(k + 1) * 128], src[:, k * 128:(k + 1) * 128], ident, srcks + [identk], [bankk])
        self.CP("act", dst, bank_bf[:, 0:nblk * 128].rearrange("p (k m) -> p k m", k=nblk), [bankk], [dstk])

    def rope(self, src, srck, H, R, tab, tabk, coff, soff, t1, t2, outs, tag):
        w = R // 4
        C = tab[:, coff:coff + R]
        Sg = tab[:, soff:soff + R].rearrange("p (a b w) -> p a b w", a=2, b=2)
        t1v = t1[:, 0:H * R].rearrange("p (h r) -> p h r", h=H)
        t2v = t2[:, 0:H * R].rearrange("p (h r) -> p h r", h=H)
        self.TT("dve", t1v, src, C.unsqueeze(1).to_broadcast([128, H, R]), ALU.mult, [srck, tabk], ["t1" + tag])
        s5 = src.rearrange("p h (a b w) -> p h a b w", a=2, b=2)
        t5 = t2v.rearrange("p h (a b w) -> p h a b w", a=2, b=2)
        self.TT("dve", t5[:, :, :, 0, :], s5[:, :, :, 1, :], Sg[:, :, 0, :].unsqueeze(1).to_broadcast([128, H, 2, w]),
                ALU.mult, [srck, tabk], ["t2a" + tag])
        self.TT("dve", t5[:, :, :, 1, :], s5[:, :, :, 0, :], Sg[:, :, 1, :].unsqueeze(1).to_broadcast([128, H, 2, w]),
                ALU.mult, [srck, tabk], ["t2b" + tag])
        for (dst, h0, h1, dk) in outs:
            self.TT("dve", dst, t1v[:, h0:h1, :], t2v[:, h0:h1, :], ALU.add, ["t1" + tag, "t2a" + tag, "t2b" + tag], [dk])

    def phase_p1(self, l):
        cfg, A, I = self.cfg, self.A, self.I
        NT, NTL = cfg.NT, cfg.NTL
        win = A.alloc([8, IN_COLS], BF16)
        for k in range(8):
            self.DMA("pool", win[:, k, :], I["w_in"][l, k * 128:(k + 1) * 128, :], (), ["win"])
        wqb = A.alloc([2, 384], BF16)
        self.DMA("pool", wqb, I["w_q_b"][l].rearrange("(k p) n -> p k n", p=128), (), ["wqb"])
        wkvb = A.alloc([512], BF16)
        self.DMA("pool", wkvb, I["w_kv_b"][l], (), ["wkvb"])
        gqa = A.alloc([256], F32)
        gkv = A.alloc([128], F32)
        gd = A.alloc([6, 64], F32)
        self.DMA("sp", gqa, I["g_q_a"][l:l + 1, :].partition_broadcast(128), (), ["gqa"])
        self.DMA("sp", gkv, I["g_kv_a"][l:l + 1, :].partition_broadcast(128), (), ["gkv"])
        for h in range(6):
            src = I["g_q_d"] if h < 4 else I["g_k_d"]
            self.DMA("sp", gd[:, h, :], src[l:l + 1, :].partition_broadcast(128), (), ["gd"])
        xt2 = [A.alloc([D], F32) for _ in range(2)]
        tab2 = [A.alloc([192], F32) for _ in range(2)]
        junk = A.alloc([D], F32)
        tmp = A.alloc([D], F32)
        hb = A.alloc([D], BF16)
        hT = A.alloc([8, 128], BF16)
        ssq = A.alloc([8], F32)
        rstd = A.alloc([8], F32)
        ssqD = A.alloc([8], F32)
        krs = A.alloc([32], F32)
        qf = A.alloc([384], F32)
        kvf = A.alloc([512], F32)
        bdf = A.alloc([512], F32)
        bdfD = A.alloc([512], F32)
        c2f = A.alloc([256], F32)
        rstdD = A.alloc([8], F32)
        cqkv = A.alloc([384], BF16)
        cT = A.alloc([3, 128], BF16)
        qa = A.alloc([4, 96], BF16)
        ka = A.alloc([4, 96], BF16)
        krr = A.alloc([1, 32], F32)
        t1 = A.alloc([512], F32)
        t2 = A.alloc([512], F32)
        sq = A.alloc([384], F32)
        qn = A.alloc([6, 64], F32)
        qkb = A.alloc([8, 64], BF16)
        qkbD = A.alloc([8, 64], BF16)
        qkc = A.alloc([512], BF16)
        stA = [A.alloc([8, 128], BF16) for _ in range(2)]
        stP = [A.alloc([4, 128], BF16) for _ in range(2)]
        va = [[A.alloc([4, 128], BF16) for _ in range(2)] for _ in range(4)]
        for n in range(4):
            for b in range(2):
                self.MEMSET("pool", va[n][b], 1.0, ["va%d_%d" % (n, b)])
        T0, T1 = self.bank_bf(0), self.bank_bf(1)
        pA, pBD, pC1, pC2, pQ, pKV = (self.bank(i) for i in (2, 3, 4, 5, 6, 7))

        def load(t):
            b = t % 2
            self.DMA("sp", xt2[b], self.x_src(l, t), (), ["xt%d" % b])
            self.DMA("sp", tab2[b], I["ropetab"][t * 128:(t + 1) * 128, :], (), ["tab%d" % b])

        import os
        KCUT = int(os.environ.get("KCUT", "99"))
        load(0)
        for t in range(NT):
            if t + 1 < NT:
                load(t + 1)
            b = t % 2
            xt, xk, tab, tabk = xt2[b], "xt%d" % b, tab2[b], "tab%d" % b
            w = 0 if t < NTL else 1
            tok = slice(t * 128, (t + 1) * 128)
            if t == 0:
                self.norm_mod(xt, xk, w, D, 0, junk, tmp, hb, "hb", ssq[:, 0:1], rstd[:, 0:1], "n1")
            self.transpose8(hb, "hb", T0, "T0", hT, "hT")
            if KCUT == 1:
                return
            def proj(dst, dk, c0, c1):
                for k in range(8):
                    self.MM(dst[:, 0:c1 - c0], hT[:, k, :], win[:, k, c0:c1], k == 0, k == 7, ["hT", "win"], [dk])
            proj(pA, "pA", 0, 416)
            proj(pBD, "pBD", 416, 928)
            self.CP("act", bdf, pBD, ["pBD"], ["bdf"])
            proj(pC1, "pC1", 928, 1440)
            proj(pC2, "pC2", 1440, 1696)
            proj(pBD, "pBD", 1696, 2208)
            if t + 1 < NT:
                bn_ = (t + 1) % 2
                self.norm_mod(xt2[bn_], "xt%d" % bn_, 0 if t + 1 < NTL else 1, D, 0, junk, tmp, hb, "hb",
                              ssq[:, 0:1], rstd[:, 0:1], "n1")
            if KCUT == 2:
                return
            self.ACT(junk[:, 0:256], pA[:, 0:256], AF.Square, ["pA"], ["junk", "ssqA0"], accum=ssq[:, 1:2])
            self.ACT(junk[:, 256:384], pA[:, 256:384], AF.Square, ["pA"], ["junk", "ssqA1"], accum=ssq[:, 2:3])
            self.ACT(ssq[:, 1:2], ssq[:, 1:2], AF.Sqrt, ["ssqA0"], ["ssqA0"], scale=1.0 / 256, bias=EPS)
            self.ACT(ssq[:, 2:3], ssq[:, 2:3], AF.Sqrt, ["ssqA1"], ["ssqA1"], scale=1.0 / 128, bias=EPS)
            self.RCP(rstd[:, 1:3], ssq[:, 1:3], ["ssqA0", "ssqA1"], ["rstdA"])
            self.STT("dve", cqkv[:, 0:256], pA[:, 0:256], rstd[:, 1:2], gqa, ALU.mult, ALU.mult, ["pA", "rstdA", "gqa"], ["cqkv0"])
            self.STT("dve", cqkv[:, 256:384], pA[:, 256:384], rstd[:, 2:3], gkv, ALU.mult, ALU.mult, ["pA", "rstdA", "gkv"], ["cqkv1"])
            def gqa_v(src, srck, n):
                v = va[n][b]
                vp = v.rearrange("p (g two) d -> p g two d", two=2)
                sv = src[:, 384:512].rearrange("p (g r) -> p g r", g=2)
                self.CP("pool", vp[:, :, 0, 0:64], sv, [srck], ["va%d_%d" % (n, b)])
                self.CP("pool", vp[:, :, 1, 64:128], sv, [srck], ["va%d_%d" % (n, b)])
                return v

            def qk_store(src, srcks, n, tbank, tbk):
                for k in range(4):
                    self.TR(tbank[:, k * 128:(k + 1) * 128], src[:, k * 128:(k + 1) * 128], self.ident_b,
                            list(srcks) + ["ident_b"], [tbk])
                sp_ = stP[n % 2]
                spk = "stP%d" % (n % 2)
                self.CP("act", sp_, tbank[:, 0:512].rearrange("p (k m) -> p k m", k=4), [tbk], [spk])
                self.DMA("sp", self.QT[n][:, :, tok], sp_[:, 0:2, :], [spk], [])
                self.DMA("sp", self.KT[n][:, :, tok], sp_[:, 2:4, :], [spk], [])

            pBv = bdf[:, 0:384].rearrange("p (h r) -> p h r", h=6)
            qkb_d = qkb[:, 4:8, :].rearrange("p (g two) r -> p g two r", two=2)
            self.rope(pBv, "bdf", 6, 64, tab, tabk, 0, 64, t1, t2,
                      [(qkb[:, 0:4, :], 0, 4, "qkb0"), (qkb_d[:, :, 0, :], 4, 6, "qkb1"), (qkb_d[:, :, 1, :], 4, 6, "qkb2")], "R")
            vB = gqa_v(bdf, "bdf", 1)
            self.CP("act", qkc, pC1, ["pC1"], ["qkc"])
            vC = va[2][b]
            vCp = vC.rearrange("p (j two) d -> p j two d", two=2)
            self.CP("act", c2f, pC2[:, 0:256], ["pC2"], ["c2f"])
            pC2p = c2f.rearrange("p (j two r) -> p j two r", two=2, r=64)
            self.CP("pool", vCp[:, :, 0, 0:64], pC2p[:, :, 0, :], ["c2f"], ["va2_%d" % b])
            self.CP("pool", vCp[:, :, 1, 64:128], pC2p[:, :, 1, :], ["c2f"], ["va2_%d" % b])
            qkbD_d = qkbD[:, 4:8, :].rearrange("p (g two) r -> p g two r", two=2)
            self.CP("act", bdfD, pBD, ["pBD"], ["bdfD"])
            pDv = bdfD[:, 0:384].rearrange("p (h r) -> p h r", h=6)
            sqv = sq.rearrange("p (h r) -> p h r", h=6)
            self.ACT(sq, pBD[:, 0:384], AF.Square, ["pBD"], ["sq"])
            self.RED(ssqD[:, 0:6], sqv, ALU.add, ["sq"], ["ssqD"])
            self.ACT(ssqD[:, 0:6], ssqD[:, 0:6], AF.Sqrt, ["ssqD"], ["ssqD"], scale=1.0 / 64, bias=EPS)
            self.RCP(rstdD[:, 0:6], ssqD[:, 0:6], ["ssqD"], ["rstdD"])
            self.TT("dve", qn, pDv, rstdD[:, 0:6].unsqueeze(2).to_broadcast([128, 6, 64]), ALU.mult, ["bdfD", "rstdD"], ["qn"])
            self.TT("dve", qn, qn, gd, ALU.mult, ["qn", "gd"], ["qn"])
            self.rope(qn, "qn", 6, 64, tab, tabk, 0, 64, t1, t2,
                      [(qkbD[:, 0:4, :], 0, 4, "qkbD0"), (qkbD_d[:, :, 0, :], 4, 6, "qkbD1"), (qkbD_d[:, :, 1, :], 4, 6, "qkbD2")], "R")
            vD = gqa_v(bdfD, "bdfD", 3)
            for k in range(3):
                self.TR(T1[:, k * 128:(k + 1) * 128], cqkv[:, k * 128:(k + 1) * 128], self.ident_b,
                        ["cqkv0", "cqkv1", "ident_b"], ["T1"])
            self.CP("act", cT, T1[:, 0:384].rearrange("p (k m) -> p k m", k=3), ["T1"], ["cT"])
            if KCUT == 3:
                return
            for k in range(2):
                self.MM(pQ[:, 0:384], cT[:, k, :], wqb[:, k, :], k == 0, k == 1, ["cT", "wqb"], ["pQ"])
            self.MM(pKV, cT[:, 2, :], wkvb, True, True, ["cT", "wkvb"], ["pKV"])
            if KCUT == 4:
                return
            self.CP("act", qf, pQ[:, 0:384], ["pQ"], ["qf"])
            self.CP("act", kvf, pKV, ["pKV"], ["kvf"])
            self.CP("act", krs, pA[:, 384:416], ["pA"], ["krs"])
            pQv = qf.rearrange("p (h r) -> p h r", h=4)
            pKVv = kvf.rearrange("p (h r) -> p h r", h=4)
            self.CP("pool", qa[:, :, 0:64], pQv[:, :, 0:64], ["qf"], ["qa0"])
            if KCUT == 41:
                return
            self.rope(pQv[:, :, 64:96], "qf", 4, 32, tab, tabk, 128, 160, t1, t2, [(qa[:, :, 64:96], 0, 4, "qa1")], "R")
            if KCUT == 42:
                return
            self.rope(krs.unsqueeze(1), "krs", 1, 32, tab, tabk, 128, 160, t1, t2, [(krr, 0, 1, "krr")], "R")
            if KCUT == 5:
                return
            self.CP("pool", ka[:, :, 0:64], pKVv[:, :, 0:64], ["kvf"], ["ka0"])
            self.CP("dve", ka[:, :, 64:96], krr.to_broadcast([128, 4, 32]), ["krr"], ["ka1"])
            vA = va[0][b]
            vAp = vA.rearrange("p (j two) d -> p j two d", two=2)
            pKVp = pKVv.rearrange("p (j two) r -> p j two r", two=2)
            self.CP("pool", vAp[:, :, 0, 0:64], pKVp[:, :, 0, 64:128], ["kvf"], ["va0_%d" % b])
            self.CP("pool", vAp[:, :, 1, 64:128], pKVp[:, :, 1, 64:128], ["kvf"], ["va0_%d" % b])
            if KCUT == 6:
                return
            for h in range(4):
                self.TR(T1[0:96, h * 128:(h + 1) * 128], qa[:, h, :], self.ident_b, ["qa0", "qa1", "ident_b"], ["T1"])
                self.TR(T1[0:96, (4 + h) * 128:(5 + h) * 128], ka[:, h, :], self.ident_b, ["ka0", "ka1", "ident_b"], ["T1"])
            sA = stA[b]
            self.CP("act", sA[0:96], T1[0:96, :].rearrange("p (k m) -> p k m", k=8), ["T1"], ["stA%d" % b])
            if KCUT == 7:
                return
            self.DMA("sp", self.QT[0][:, :, tok], sA[0:96, 0:4, :], ["stA%d" % b], [])
            self.DMA("sp", self.KT[0][:, :, tok], sA[0:96, 4:8, :], ["stA%d" % b], [])
            self.DMA("sp", self.VA[0][:, t, :, :], vA, ["va0_%d" % b], [])
            if KCUT == 8:
                return

            qk_store(qkb.rearrange("p h r -> p (h r)"), ["qkb0", "qkb1", "qkb2"], 1, T0, "T0")
            self.DMA("sp", self.VA[1][:, t, :, :], vB, ["va1_%d" % b], [])
            if KCUT == 9:
                return
            qk_store(qkc, ["qkc"], 2, T1, "T1")
            self.DMA("sp", self.VA[2][:, t, :, :], vC, ["va2_%d" % b], [])
            if KCUT == 10:
                return
            qk_store(qkbD.rearrange("p h r -> p (h r)"), ["qkbD0", "qkbD1", "qkbD2"], 3, T0, "T0")
            self.DMA("sp", self.VA[3][:, t, :, :], vD, ["va3_%d" % b], [])

    def phase_p2(self, l):
        cfg, A, I = self.cfg, self.A, self.I
        NT, NTL, T = cfg.NT, cfg.NTL, cfg.T
        LOOK = 3
        groups = [(g * 512, 512, g) for g in range(cfg.NG)]
        if self.with_ctx:
            groups.append((cfg.SEQ, cfg.CTX, None))
        ctx_tiles = list(range(NTL, NT))
        base_mark = A.mark()
        pt = [A.alloc([512], BF16) for _ in range(6)]
        tmpf = [A.alloc([512], F32) for _ in range(4)]
        rden = [A.alloc([512], F32) for _ in range(2)]
        rdt = A.alloc([512], F32)
        ost = [A.alloc([512], BF16) for _ in range(2)]
        qt = [A.alloc([512], BF16) for _ in range(2)]
        esink = A.alloc([4], F32)
        self.DMA("sp", esink, I["sink_b"][l:l + 1, :].partition_broadcast(128), (), ["esink"])
        self.ACT(esink, esink, AF.Exp, ["esink"], ["esink"])
        inner_mark = A.mark()
        cnt = {"pt": 0, "s": 0, "tf": 0, "grp": 0}
        for n in range(4):
            A.release(inner_mark)
            self.S.barrier()
            isA = n == 0
            scale = (96 ** -0.5) if isA else 0.125
            LOOK = 2 if n == 2 else 3
            if isA:
                kt = A.alloc([4, T], BF16)
                self.DMA("sp", kt[0:96], self.KT[0], (), ["kt"])
            else:
                kt = A.alloc([4, T], BF16)
                self.MEMSET("pool", kt, 0.0, ["kt"])
                for hh in range(4):
                    hp = (hh % 2) * 64
                    self.DMA("sp", kt[hp:hp + 64, hh, :], self.KT[n][hp:hp + 64, hh // 2, :], (), ["kt"])
            vat = A.alloc([NT, 4, 128], BF16)
            step = max(1, NT // 4)
            for t0 in range(0, NT, step):
                t1_ = min(NT, t0 + step)
                self.DMA("sp", vat[:, t0:t1_], self.VA[n][:, t0:t1_], (), ["vat"])
            mb = mc = bias = comb = None
            if n == 1:
                mb = A.alloc([6, 512], F32)
                self.DMA("sp", mb, I["maskb"].rearrange("r p q -> p r q"), (), ["mb"])
            if n == 2:
                mc = A.alloc([self.ncase * 8, 512], BF16)
                for ci in range(self.ncase):
                    self.DMA("pool", mc[:, ci * 8:(ci + 1) * 8, :], I["maskc"][ci].rearrange("r p q -> p r q"), (), ["mc"])
                bias = A.alloc([8, 512], F32)
                comb1 = A.alloc([8, 512], F32)
                comb = [comb1, comb1]
            G_list = []
            for h in range(4):
                for gi, (q0, nq, g) in enumerate(groups):
                    tiles = []
                    if g is None:
                        tiles = [(t_, None) for t_ in ctx_tiles]
                    elif n in (0, 3):
                        tiles = [(t_, None) for t_ in range(NT)]
                    elif n == 1:
                        for j in range(4 * g - 1, 4 * g + 5):
                            if 0 <= j < NTL:
                                tiles.append((j, ("mb", j - 4 * g + 1)))
                        tiles += [(t_, None) for t_ in ctx_tiles]
                    else:
                        for r in self.valid_c[g]:
                            tiles.append((4 * g + r - 2, ("comb", r)))
                        tiles += [(t_, None) for t_ in ctx_tiles]
                    gid = cnt["grp"]
                    cnt["grp"] += 1
                    G_list.append(dict(h=h, q0=q0, nq=nq, g=g, tiles=tiles, gid=gid, first_of_head=(gi == 0)))
            items = [(G, i) for G in G_list for i in range(len(G["tiles"]))]
            state = {}

            def emit_S(idx):
                G, i = items[idx]
                h, q0, nq, gid = G["h"], G["q0"], G["nq"], G["gid"]
                ph = (h % 2) * 64
                vr = slice(ph, ph + 64)
                qb = gid % 2
                qtb, qk_ = qt[qb], "qt%d" % qb
                if i == 0:
                    if n == 2 and G["first_of_head"]:
                        self.DMA("sp", bias, I["biasg"][l, h].rearrange("r p q -> p r q"), (), ["bias"])
                    if n == 2 and G["first_of_head"]:
                        state["built"] = None
                    if n == 2 and G["g"] is not None and state.get("built") != (self.case_of_g[G["g"]], tuple(self.valid_c[G["g"]])):
                        state["built"] = (self.case_of_g[G["g"]], tuple(self.valid_c[G["g"]]))
                        ci = self.case_of_g[G["g"]]
                        cb_ = comb[gid % 2]
                        for r in self.valid_c[G["g"]]:
                            self.TT("pool", cb_[:, r, :], bias[:, r, :], mc[:, ci * 8 + r, :], ALU.add,
                                    ["bias", "mc"], ["comb_%d" % r])
                    if isA:
                        self.DMA("sp", qtb[0:96, 0:nq], self.QT[0][:, h, q0:q0 + nq], (), [qk_])
                    else:
                        self.DMA("sp", qtb[:, 0:nq], self.QT[n][:, h // 2, q0:q0 + nq], (), [qk_])
                q_ap = qtb[0:96, 0:nq] if isA else qtb[:, 0:nq]
                tk = G["tiles"][i][0]
                sb_ = cnt["s"] % 4
                cnt["s"] += 1
                psS = self.bank(sb_)
                psk = "psS%d" % sb_
                if isA:
                    k_ap = kt[0:96, h, tk * 128:(tk + 1) * 128]
                else:
                    k_ap = kt[:, h, tk * 128:(tk + 1) * 128]
                self.MM(psS[:, 0:nq], k_ap, q_ap, True, True, ["kt", qk_], [psk])
                state[idx] = (psS, psk)

            for j in range(min(LOOK, len(items))):
                emit_S(j)
            for idx, (G, i) in enumerate(items):
                if idx + LOOK < len(items):
                    emit_S(idx + LOOK)
                h, q0, nq, gid = G["h"], G["q0"], G["nq"], G["gid"]
                ph = (h % 2) * 64
                vr = slice(ph, ph + 64)
                dr = slice(64 - ph, 128 - ph)
                psS, psk = state.pop(idx)
                tk, tabinfo = G["tiles"][i]
                ob = gid % 2
                psO = self.bank(6 + ob)
                pok = "psO%d" % ob
                last = len(G["tiles"]) - 1
                pb_ = cnt["pt"] % 6
                cnt["pt"] += 1
                ptb, ptk = pt[pb_], "pt%d" % pb_
                if tabinfo is None:
                    self.ACT(ptb[:, 0:nq], psS[:, 0:nq], AF.Exp, [psk], [ptk], scale=scale)
                else:
                    if tabinfo[0] == "mb":
                        tabv, tabk = mb[:, tabinfo[1], :], "mb"
                    else:
                        tabv, tabk = comb[gid % 2][:, tabinfo[1], :], "comb_%d" % tabinfo[1]
                    fb = cnt["tf"] % 4
                    cnt["tf"] += 1
                    self.STT("dve", tmpf[fb][:, 0:nq], psS[:, 0:nq], scale, tabv[:, 0:nq], ALU.mult, ALU.add,
                             [psk, tabk], ["tmpf%d" % fb])
                    self.ACT(ptb[:, 0:nq], tmpf[fb][:, 0:nq], AF.Exp, ["tmpf%d" % fb], [ptk])
                self.MM(psO[:, 0:nq], vat[:, tk, h, :], ptb[:, 0:nq], i == 0, i == last, ["vat", ptk], [pok])
                if i == last:
                    rb = gid % 2
                    rd, rdk = rden[rb], "rden%d" % rb
                    if n in (1, 2):
                        if n == 1:
                            self.ACT(rdt[dr, 0:nq], psO[dr, 0:nq], AF.Ln, [pok, "esink"], ["rdt"], bias=esink[dr, h:h + 1])
                        else:
                            self.ACT(rdt[dr, 0:nq], psO[dr, 0:nq], AF.Ln, [pok], ["rdt"])
                        self.ACT(rdt[dr, 0:nq], rdt[dr, 0:nq], AF.Exp, ["rdt"], ["rdt"], scale=-1.0)
                        self.CP("dve", rd[vr, 0:nq], rdt[dr, 0:nq], ["rdt"], [rdk])
                    else:
                        self.RCP(rd[vr, 0:nq], psO[dr, 0:nq], [pok], [rdk])
                    osb, osk = ost[rb], "ost%d" % rb
                    self.TT("dve", osb[vr, 0:nq], psO[vr, 0:nq], rd[vr, 0:nq], ALU.mult, [pok, rdk], [osk])
                    self.DMA("sp", self.OT[n * 2 + h // 2][vr, q0:q0 + nq], osb[vr, 0:nq], [osk], [])
        A.release(base_mark)

    def phase_p3(self, l):
        cfg, A, I = self.cfg, self.A, self.I
        NT, NTL = cfg.NT, cfg.NTL
        tiles = list(range(NT)) if self.with_ctx else list(range(NTL))
        wg = A.alloc([32, D], BF16)
        for n in range(4):
            for k in range(8):
                self.DMA("pool", wg[:, n * 8 + k, :], I["w_gate"][l, n, k * 128:(k + 1) * 128, :], (), ["wg"])
        bg = A.alloc([4 * D], BF16)
        self.DMA("pool", bg[0:1, :], I["b_gate"][l:l + 1, :], (), ["bg"])
        wbr = A.alloc([8, D], BF16)
        self.DMA("pool", wbr, I["w_branch"][l].rearrange("n (c p) m -> p (n c) m", p=128), (), ["wbr"])
        wo = A.alloc([8, D], BF16)
        self.DMA("pool", wo, I["w_out"][l].rearrange("(k p) m -> p k m", p=128), (), ["wo"])
        wr = A.alloc([8, 36], F32)
        self.DMA("sp", wr[:, :, 0:4], I["w_group"][l].rearrange("(k p) n -> p k n", p=128), (), ["wr"])
        self.DMA("sp", wr[:, :, 4:36], I["w_router"][l].rearrange("(k p) n -> p k n", p=128), (), ["wr"])
        br = A.alloc([36], F32)
        self.DMA("sp", br[0:1, 0:4], I["b_group"][l:l + 1, :], (), ["br"])
        self.DMA("sp", br[0:1, 4:36], I["b_router"][l:l + 1, :], (), ["br"])
        xt2 = [A.alloc([D], F32) for _ in range(3)]
        ot2 = [A.alloc([8, 128], BF16) for _ in range(2)]
        tmpA = A.alloc([D], F32)
        tmpB = A.alloc([D], F32)
        hb = A.alloc([D], BF16)
        hT = A.alloc([8, 128], BF16)
        gate = A.alloc([D], BF16)
        tmpn = A.alloc([D], BF16)
        ypre = A.alloc([D], F32)
        ypb = A.alloc([D], BF16)
        yT = A.alloc([8, 128], BF16)
        h2Tf = A.alloc([8, 128], F32)
        h2b = A.alloc([D], BF16)
        oh12b = A.alloc([64], BF16)
        csb = A.alloc([64], F32)
        tr0 = A.alloc([32], F32)
        tr1 = A.alloc([32], F32)
        self.MEMSET("dve", self.base, 0.0, ["base"])
        ssq = A.alloc([4], F32)
        rstd = A.alloc([4], F32)
        lg = A.alloc([36], F32)
        sm = A.alloc([16], F32)
        ohg = A.alloc([4], F32)
        pen = A.alloc([4], F32)
        em = A.alloc([32], F32)
        em2 = A.alloc([32], F32)
        oh1 = A.alloc([32], F32)
        oh2 = A.alloc([32], F32)
        c1 = A.alloc([32], F32)
        j4 = A.alloc([4], F32)
        PT = self.bank_bf(0)
        PB, PY = self.banks[2], self.banks[3]
        pR = self.bank(1)
        cntu = [0]

        def load(i):
            t = tiles[i]
            b = i % 2
            self.DMA("sp", xt2[i % 3], self.x_src(l, t), (), ["xt%d" % (i % 3)])
            self.DMA("sp", ot2[b], self.OT[:, :, t * 128:(t + 1) * 128].rearrange("b p t -> p b t"), (), ["ot%d" % b])

        def norm1(i):
            t = tiles[i]
            w = 0 if t < NTL else 1
            self.norm_mod(xt2[i % 3], "xt%d" % (i % 3), w, D, 0, hb, tmpA, hb, "hb", ssq[:, 0:1], rstd[:, 0:1], "n1",
                          jk="hb", tk="tmpA")

        def stageA(i, gen=None):
            t = tiles[i]
            b = i % 2
            xt, xk, ot, otk = xt2[i % 3], "xt%d" % (i % 3), ot2[b], "ot%d" % b
            w = 0 if t < NTL else 1
            m = self.mod[w]
            mk = "mod%d" % w
            tok = slice(t * 128, (t + 1) * 128)
            tmp = tmpA
            self.transpose8(hb, "hb", PT, "PT", hT, "hT")
            for n in range(4):
                for half in range(2):
                    cs = slice(half * 512, (half + 1) * 512)
                    r_ = cntu[0] % 2
                    cntu[0] += 1
                    PGh, pgk = self.bank(2 + r_), "PG%d" % r_
                    PBh, pbk = self.bank(4 + r_), "PB%d" % r_
                    for k in range(8):
                        self.MM(PGh, hT[:, k, :], wg[:, n * 8 + k, cs], k == 0, False, ["hT", "wg"], [pgk])
                    self.MM(PGh, self.ones_b[0:1, :], bg[0:1, n * D + half * 512:n * D + (half + 1) * 512], False, True,
                            ["ones_b", "bg"], [pgk])
                    for c in range(2):
                        self.MM(PBh, ot[:, n * 2 + c, :], wbr[:, n * 2 + c, cs], c == 0, c == 1, [otk, "wbr"], [pbk])
                    gh, ghk = gate[:, r_ * 512:(r_ + 1) * 512], "gate%d" % r_
                    self.ACT(gh, PGh, AF.Sigmoid, [pgk], [ghk])
                    yk = "ypre%d" % half
                    if n == 0:
                        self.TT("dve", ypre[:, cs], gh, PBh, ALU.mult, [ghk, pbk], [yk])
                    else:
                        tn, tnk = tmpn[:, r_ * 512:(r_ + 1) * 512], "tn%d" % r_
                        self.TT("dve", tn, gh, PBh, ALU.mult, [ghk, pbk], [tnk])
                        if n < 3:
                            self.TT("pool", ypre[:, cs], ypre[:, cs], tn, ALU.add, [yk, tnk], [yk])
                        else:
                            self.TT("pool", ypb[:, cs], ypre[:, cs], tn, ALU.add, [yk, tnk], ["ypb%d" % half])
                    u_ = n * 2 + half
                    if u_ in (1, 3, 5, 7) and gen is not None:
                        next(gen, None)
                    if u_ == 3 and i + 1 < len(tiles):
                        norm1(i + 1)
            if i + 2 < len(tiles):
                load(i + 2)
            self.transpose8(ypb, ["ypb0", "ypb1"], PT, "PT", yT, "yT")
            for half in range(2):
                cs = slice(half * 512, (half + 1) * 512)
                for k in range(8):
                    self.MM(PY[:, cs], yT[:, k, :], wo[:, k, cs], k == 0, k == 7, ["yT", "wo"], ["PY"])
            self.TT("dve", tmp, PY[:, :], m[:, 2 * D:3 * D], ALU.mult, ["PY", mk], ["tmpA"])
            self.TT("pool", xt, tmp, xt, ALU.add, ["tmpA", xk], [xk])
            self.DMA("sp", self.xcur[tok, :], xt, [xk], [])
            if gen is not None:
                next(gen, None)

        def stageB(i):
            t = tiles[i]
            xt, xk = xt2[i % 3], "xt%d" % (i % 3)
            w = 0 if t < NTL else 1
            tok = slice(t * 128, (t + 1) * 128)
            tmp = tmpB
            h2 = tmp
            self.norm_mod(xt, xk, w, 4 * D, 3 * D, h2b, tmp, h2, "tmpB", ssq[:, 1:2], rstd[:, 1:2], "n2", jk="h2b", tk="tmpB")
            yield
            for k in range(8):
                self.TR(PB[:, k * 128:(k + 1) * 128], h2[:, k * 128:(k + 1) * 128], self.ident_f, ["tmpB", "ident_f"], ["PB0", "PB1"])
            self.CP("act", h2Tf, PB[:, :].rearrange("p (k m) -> p k m", k=8), ["PB0", "PB1"], ["h2Tf"])
            self.CP("pool", h2b, h2, ["tmpB"], ["h2b"])
            self.DMA("sp", self.H2[tok, :], h2b, ["h2b"], [])
            yield
            for k in range(8):
                self.MM(pR[:, 0:36], h2Tf[:, k, :], wr[:, k, :], k == 0, False, ["h2Tf", "wr"], ["pR"])
            self.MM(pR[:, 0:36], self.ones_f[0:1, :], br[0:1, :], False, True, ["ones_f", "br"], ["pR"])
            self.CP("dve", lg, pR[:, 0:36], ["pR"], ["lg"])
            gl = lg[:, 0:4]
            el = lg[:, 4:36].rearrange("p (g e) -> p g e", g=4)
            gmax, negmax, se, gw, m1, m2, dd, w1, w1g, w2g = (sm[:, j:j + 1] for j in range(10))
            self.RED(gmax, gl, ALU.max, ["lg"], ["gmax"])
            self.TS("dve", ohg, gl, gmax, None, ALU.is_equal, None, ["lg", "gmax"], ["ohg"])
            self.TS("dve", negmax, gmax, -1.0, None, ALU.mult, None, ["gmax"], ["negmax"])
            self.ACT(j4, gl, AF.Exp, ["lg", "negmax"], ["j4", "se"], bias=negmax, accum=se)
            self.RCP(gw, se, ["se"], ["gw"])
            self.TS("dve", pen, ohg, -1.0, 1e9, ALU.add, ALU.mult, ["ohg"], ["pen"])
            emv = em.rearrange("p (g e) -> p g e", g=4)
            self.TT("dve", emv, el, pen.unsqueeze(2).to_broadcast([128, 4, 8]), ALU.add, ["lg", "pen"], ["em"])
            self.RED(m1, em, ALU.max, ["em"], ["m1"])
            self.TS("dve", oh1, em, m1, None, ALU.is_equal, None, ["em", "m1"], ["oh1"])
            self.STT("dve", em2, oh1, -2e9, em, ALU.mult, ALU.add, ["oh1", "em"], ["em2"])
            self.RED(m2, em2, ALU.max, ["em2"], ["m2"])
            self.TS("dve", oh2, em2, m2, None, ALU.is_equal, None, ["em2", "m2"], ["oh2"])
            yield
            self.TT("dve", dd, m2, m1, ALU.subtract, ["m1", "m2"], ["dd"])
            self.ACT(dd, dd, AF.Exp, ["dd"], ["dd"])
            self.TS("dve", dd, dd, 1.0, None, ALU.add, None, ["dd"], ["dd"])
            self.RCP(w1, dd, ["dd"], ["w1"])
            self.TT("dve", w1g, w1, gw, ALU.mult, ["w1", "gw"], ["w1g"])
            self.TT("dve", w2g, gw, w1g, ALU.subtract, ["w1g", "gw"], ["w2g"])
            rt, base, iota = self.rt, self.base, self.iota
            pRK = self.bank(1)[:, 64:192]
            self.CP("pool", oh12b[:, 0:32], oh1, ["oh1"], ["oh12b0"])
            self.CP("pool", oh12b[:, 32:64], oh2, ["oh2"], ["oh12b1"])
            yield
            self.MM(pRK[:, 0:64], self.ltb, oh12b, True, True, ["ltb", "oh12b0", "oh12b1"], ["pRK"])
            self.MM(pRK[:, 64:128], self.onesbb, oh12b, True, True, ["onesbb", "oh12b0", "oh12b1"], ["pRK"])
            self.CP("dve", csb, pRK[:, 64:128], ["pRK"], ["cs"])
            self.TT("dve", tr0, pRK[:, 0:32], base, ALU.add, ["pRK", "base"], ["tr0"])
            self.TT("dve", tr0, tr0, oh1, ALU.mult, ["tr0", "oh1"], ["tr0"])
            self.RED(rt[:, t, 4:5], tr0, ALU.add, ["tr0"], ["rt"])
            self.TT("dve", tr1, pRK[:, 32:64], base, ALU.add, ["pRK", "base"], ["tr1"])
            self.TT("dve", tr1, tr1, csb[:, 0:32], ALU.add, ["tr1", "cs"], ["tr1"])
            self.TT("dve", tr1, tr1, oh2, ALU.mult, ["tr1", "oh2"], ["tr1"])
            self.RED(rt[:, t, 5:6], tr1, ALU.add, ["tr1"], ["rt"])
            self.TT("dve", base, base, csb[:, 0:32], ALU.add, ["base", "cs"], ["base"])
            self.TT("dve", base, base, csb[:, 32:64], ALU.add, ["base", "cs"], ["base"])
            self.TT("dve", tr0, oh1, iota, ALU.mult, ["oh1", "iota", "tr0"], ["tr0"])
            self.RED(rt[:, t, 0:1], tr0, ALU.add, ["tr0"], ["rt"])
            self.TT("dve", tr1, oh2, iota, ALU.mult, ["oh2", "iota", "tr1"], ["tr1"])
            self.RED(rt[:, t, 1:2], tr1, ALU.add, ["tr1"], ["rt"])
            self.CP("dve", rt[:, t, 2:3], w1g, ["w1g"], ["rt"])
            self.CP("dve", rt[:, t, 3:4], w2g, ["w2g"], ["rt"])

        ntl_ = len(tiles)
        load(0)
        if ntl_ > 1:
            load(1)
        norm1(0)
        for i in range(ntl_):
            gen = stageB(i - 1) if i > 0 else None
            stageA(i, gen)
            if gen is not None:
                for _ in gen:
                    pass
        for _ in stageB(ntl_ - 1):
            pass

    def phase_p4(self, l):
        cfg, A, I = self.cfg, self.A, self.I
        NT, NTL = cfg.NT, cfg.NTL
        last = l == cfg.DEPTH - 1
        tiles = list(range(NT)) if self.with_ctx else list(range(NTL))
        ntl = len(tiles)
        NBLK = 2 * ntl + 32
        assert NBLK <= NBLK_MAX
        rt, base, iota = self.rt, self.base, self.iota
        m0 = A.mark()
        nbi = A.alloc([32], I32)
        pcnt = A.alloc([32], F32)
        pa = A.alloc([32], F32)
        pb = A.alloc([32], F32)
        bs = A.alloc([32], F32)
        oh3 = A.alloc([NT, 32], F32)
        sl = A.alloc([NT, 2], F32)
        cmp = A.alloc([NBLK_MAX, 32], F32)
        ble = A.alloc([NBLK_MAX], F32)
        zt = A.alloc([8192], BF16)
        h2t = [A.alloc([D], BF16) for _ in range(2)]
        self.MEMSET("pool", zt, 0.0, ["zt"])
        xsz = self.XS.rearrange("(p b) f -> p (b f)", p=128)
        tot = NBLK_MAX * D
        for c0 in range(0, tot, 8192):
            c1_ = min(tot, c0 + 8192)
            self.DMA("sp", xsz[:, c0:c1_], zt[:, 0:c1_ - c0], ["zt"], ["XS"])
        self.TS("dve", pcnt, base, 1.0 / 128, 0.496, ALU.mult, ALU.add, ["base"], ["pcnt"])
        self.CP("dve", nbi, pcnt, ["pcnt"], ["nbi"])
        self.CP("dve", pcnt, nbi, ["nbi"], ["pcnt"])
        self.TS("dve", pcnt, pcnt, 128.0, None, ALU.mult, None, ["pcnt"], ["pcnt"])
        self.CP("dve", pa, pcnt, ["pcnt"], ["pa"])
        src, dst, sk, dk = pa, pb, "pa", "pb"
        for sft in (1, 2, 4, 8, 16):
            self.CP("dve", dst[:, 0:sft], src[:, 0:sft], [sk], [dk])
            self.TT("dve", dst[:, sft:32], src[:, sft:32], src[:, 0:32 - sft], ALU.add, [sk], [dk])
            src, dst, sk, dk = dst, src, dk, sk
        pe, pek = src, sk
        self.TT("dve", bs, pe, pcnt, ALU.subtract, [pek, "pcnt"], ["bs"])
        for k in range(2):
            self.TT("dve", oh3, iota.unsqueeze(1).to_broadcast([128, NT, 32]),
                    rt[:, :, k:k + 1].to_broadcast([128, NT, 32]), ALU.is_equal, ["iota", "rt"], ["oh3"])
            self.TT("dve", oh3, oh3, bs.unsqueeze(1).to_broadcast([128, NT, 32]), ALU.mult, ["oh3", "bs"], ["oh3"])
            self.RED(sl[:, :, k], oh3, ALU.add, ["oh3"], ["sl%d" % k])
            self.TT("dve", sl[:, :, k], sl[:, :, k], rt[:, :, 4 + k], ALU.add, ["sl%d" % k, "rt"], ["sl%d" % k])
        self.CP("dve", self.slot_i, sl, ["sl0", "sl1"], ["slot_i"])
        self.TT("dve", cmp, self.thr.unsqueeze(2).to_broadcast([128, NBLK_MAX, 32]),
                pe.unsqueeze(1).to_broadcast([128, NBLK_MAX, 32]), ALU.is_ge, ["thr", pek], ["cmp"])
        self.RED(ble, cmp, ALU.add, ["cmp"], ["ble"])
        self.TS("dve", ble, ble, 31.0, 128.0, ALU.min, ALU.mult, ["ble"], ["ble"])
        self.TS("dve", ble, ble, self.pidx[:, 0:1], float(l * NE * 128), ALU.add, ALU.add, ["ble", "pidx"], ["ble"])
        self.CP("dve", self.widx, ble, ["ble"], ["widx"])
        for i, t in enumerate(tiles):
            hb, hk = h2t[i % 2], "h2t%d" % (i % 2)
            self.DMA("sp", hb, self.H2[t * 128:(t + 1) * 128, :], (), [hk])
            for k in range(2):
                self.S.add("pool", (lambda hb=hb, t=t, k=k: (lambda e: e.indirect_dma_start(
                    out=self.XS[:, :], out_offset=bass.IndirectOffsetOnAxis(ap=self.slot_i[:, t, k:k + 1], axis=0),
                    in_=hb, in_offset=None)))(), [hk, "slot_i", "XS"], ["XSs"], dma=True)
        self.S.barrier()
        A.release(m0)
        NWR = 4
        w13 = [A.alloc([8, 512], BF16) for _ in range(NWR)]
        w2 = [A.alloc([2, D], BF16) for _ in range(NWR)]
        xs = [A.alloc([D], BF16) for _ in range(NWR)]
        xsT = [A.alloc([8, 128], BF16) for _ in range(2)]
        sb = [A.alloc([256], F32) for _ in range(2)]
        ab = [A.alloc([256], BF16) for _ in range(2)]
        aT = [A.alloc([2, 128], BF16) for _ in range(2)]
        ys = [A.alloc([D], BF16) for _ in range(2)]
        w13src = I["w13r"].rearrange("l r f -> (l r) f")
        w2src = I["w2r"].rearrange("l r f -> (l r) f")
        def wload(b):
            wr_ = b % NWR
            self.S.add("pool", (lambda b=b, wr_=wr_: (lambda e: e.indirect_dma_start(
                out=w13[wr_].rearrange("p k f -> p (k f)"), out_offset=None, in_=w13src,
                in_offset=bass.IndirectOffsetOnAxis(ap=self.widx[:, b:b + 1], axis=0))))(), ["widx"], ["w13_%d" % wr_], dma=True)
            self.S.add("pool", (lambda b=b, wr_=wr_: (lambda e: e.indirect_dma_start(
                out=w2[wr_].rearrange("p c f -> p (c f)"), out_offset=None, in_=w2src,
                in_offset=bass.IndirectOffsetOnAxis(ap=self.widx[:, b:b + 1], axis=0))))(), ["widx"], ["w2_%d" % wr_], dma=True)
            self.DMA("sp", xs[wr_], self.XS[b * 128:(b + 1) * 128, :], (), ["xsw%d" % wr_])

        for b in range(min(NWR - 1, NBLK)):
            wload(b)
        for b in range(NBLK):
            if b + NWR - 1 < NBLK:
                wload(b + NWR - 1)
            r = b % 2
            wr_ = b % NWR
            PTx = self.bank_bf(r)
            ptxk = "PTx%d" % r
            psH = self.bank(2 + r)
            phk = "psH%d" % r
            PTa = self.bank_bf(4)
            psO = self.banks[3]
            for k in range(8):
                self.TR(PTx[:, k * 128:(k + 1) * 128], xs[wr_][:, k * 128:(k + 1) * 128], self.ident_b, ["xsw%d" % wr_, "ident_b"], [ptxk])
            self.CP("act", xsT[r], PTx[:, :].rearrange("p (k m) -> p k m", k=8), [ptxk], ["xsT%d" % r])
            for k in range(8):
                self.MM(psH, xsT[r][:, k, :], w13[wr_][:, k, :], k == 0, k == 7, ["xsT%d" % r, "w13_%d" % wr_], [phk])
            self.ACT(sb[r], psH[:, 0:256], AF.Silu, [phk], ["sb%d" % r])
            self.TT("dve", ab[r], sb[r], psH[:, 256:512], ALU.mult, ["sb%d" % r, phk], ["ab%d" % r])
            for c in range(2):
                self.TR(PTa[:, c * 128:(c + 1) * 128], ab[r][:, c * 128:(c + 1) * 128], self.ident_b, ["ab%d" % r, "ident_b"], ["PTa"])
            self.CP("act", aT[r], PTa[:, 0:256].rearrange("p (c m) -> p c m", c=2), ["PTa"], ["aT%d" % r])
            for half in range(2):
                cs_ = slice(half * 512, (half + 1) * 512)
                for c in range(2):
                    self.MM(psO[:, cs_], aT[r][:, c, :], w2[wr_][:, c, cs_], c == 0, c == 1, ["aT%d" % r, "w2_%d" % wr_], ["psO"])
            self.CP("act", ys[r], psO[:, :], ["psO"], ["ys%d" % r])
            self.DMA("sp", self.YS[b * 128:(b + 1) * 128, :], ys[r], ["ys%d" % r], [])
        self.S.barrier()
        A.release(m0)
        y0 = [A.alloc([D], BF16) for _ in range(2)]
        y1 = [A.alloc([D], BF16) for _ in range(2)]
        xt2 = [A.alloc([D], F32) for _ in range(2)]
        tmp = A.alloc([D], F32)
        junk = A.alloc([D], BF16)
        ssq = A.alloc([2], F32)
        rstd = A.alloc([2], F32)
        if last:
            self.gfin = A.alloc([D], F32)
            self.DMA("sp", self.gfin, I["g_final"].partition_broadcast(128), (), ["gfin"])
        for i, t in enumerate(tiles):
            b = i % 2
            w = 0 if t < NTL else 1
            tok = slice(t * 128, (t + 1) * 128)
            for k, yb in ((0, y0[b]), (1, y1[b])):
                self.S.add("pool", (lambda yb=yb, t=t, k=k: (lambda e: e.indirect_dma_start(
                    out=yb, out_offset=None, in_=self.YS[:, :],
                    in_offset=bass.IndirectOffsetOnAxis(ap=self.slot_i[:, t, k:k + 1], axis=0))))(), ["slot_i"], ["y%d_%d" % (k, b)], dma=True)
            self.DMA("sp", xt2[b], self.xcur[tok, :], (), ["xt%d" % b])
            self.TS("dve", tmp, y0[b], rt[:, t, 2:3], None, ALU.mult, None, ["y0_%d" % b, "rt"], ["tmp"])
            self.STT("dve", tmp, y1[b], rt[:, t, 3:4], tmp, ALU.mult, ALU.add, ["y1_%d" % b, "rt", "tmp"], ["tmp"])
            self.TT("pool", tmp, tmp, self.mod[w][:, 5 * D:6 * D], ALU.mult, ["tmp", "mod%d" % w], ["tmp"])
            self.TT("pool", xt2[b], tmp, xt2[b], ALU.add, ["tmp", "xt%d" % b], ["xt%d" % b])
            if not last:
                self.DMA("sp", self.xcur[tok, :], xt2[b], ["xt%d" % b], [])
            else:
                self.rstd_of(xt2[b], D, junk, ssq[:, 0:1], rstd[:, 0:1], ["xt%d" % b], "f")
                self.STT("dve", xt2[b], xt2[b], rstd[:, 0:1], self.gfin, ALU.mult, ALU.mult, ["xt%d" % b, "rstdf", "gfin"], ["xt%d" % b])
                o = self.DMA("sp", self.out[tok, :], xt2[b], ["xt%d" % b], [])
                self.S.out_ops.append(o)


_CACHE = {}


def _get_prog(cfg_key):
    if cfg_key not in _CACHE:
        cfg = Cfg(*cfg_key)
        p = Prog(cfg)
        p.build()
        _CACHE[cfg_key] = p
    return _CACHE[cfg_key]


def make_in_maps(prog, inputs, ncores):
    cfg = prog.cfg
    f = lambda a: np.ascontiguousarray(np.asarray(a, dtype=np.float32))
    shared = {}
    for k in ("w_mod", "b_mod", "g_norm_mix", "w_in", "g_q_a", "w_q_b", "g_kv_a", "w_kv_b", "sink_b", "g_q_d", "g_k_d",
              "w_gate", "w_branch", "w_out", "g_norm_ffn", "w_group", "b_group", "w_router", "b_router"):
        shared[k] = f(inputs[k])
    L = cfg.DEPTH
    shared["b_gate"] = f(inputs["b_gate"]).reshape(L, 4 * D)
    shared["g_final"] = f(inputs["g_final"]).reshape(1, D)
    shared["c_ctx"] = f(inputs["c_ctx"]).reshape(1, D)
    rpb = f(inputs["rpb_c"])
    shared["biasg"] = np.ascontiguousarray(rpb[:, :, prog.DR, prog.DC[None]])
    w13 = np.concatenate([f(inputs["w_ff1"]), f(inputs["w_ff3"])], axis=-1)
    w13 = w13.reshape(L, NE, 8, 128, 512).transpose(0, 1, 3, 2, 4)
    shared["w13r"] = np.ascontiguousarray(w13).reshape(L, NE * 128, 4096)
    w2 = f(inputs["w_ff2"]).reshape(L, NE, 2, 128, D).transpose(0, 1, 3, 2, 4)
    shared["w2r"] = np.ascontiguousarray(w2).reshape(L, NE * 128, 2048)
    shared["iota32"] = np.arange(32, dtype=np.float32).reshape(1, 32)
    shared["ltri"] = np.triu(np.ones((128, 128), np.float32), k=1)
    shared["pidx"] = np.arange(128, dtype=np.float32).reshape(128, 1)
    shared["thr"] = (128.0 * np.arange(NBLK_MAX, dtype=np.float32)).reshape(1, NBLK_MAX)
    shared["ident"] = np.eye(128, dtype=np.float32)
    shared["ropetab"] = rope_tab(cfg)
    shared["maskb"] = mask_b()
    shared["maskc"] = prog.maskc_np
    x = f(inputs["x"])
    c = f(inputs["c"])
    ctx = f(inputs["ctx"])
    maps = []
    for b in range(ncores):
        m = dict(shared)
        m["x"] = x[b]
        m["c"] = c[b:b + 1]
        m["ctx"] = ctx[b]
        maps.append(m)
    return maps


def kernel(**inputs):
    x = np.asarray(inputs["x"])
    B, SEQ, _ = x.shape
    CTX = np.asarray(inputs["ctx"]).shape[1]
    DEPTH = np.asarray(inputs["w_mod"]).shape[0]
    prog = _get_prog((SEQ, CTX, DEPTH))
    maps = make_in_maps(prog, inputs, B)
    res = run_bass_kernel_spmd(prog.nc, maps, core_ids=list(range(B)))
    out = np.stack([np.asarray(r["out"], dtype=np.float32) for r in res.results], axis=0)
    return out
```
